# Optimizing a Trainium2 kernel written in Bass

```python
import math
import jax
import jax.numpy as jnp
from jax import lax
import numpy as np

D_MODEL = 1024
BATCH = 1
SEQ = 16384
DEPTH = 1

N_META = 16
BLOCK = 128
WINDOW = 128
ATTN_PAD = BLOCK - N_META
ATTN_WIDTH = D_MODEL // 2
HEAD_DIM = 64
N_Q_HEADS = ATTN_WIDTH // HEAD_DIM
N_KV_HEADS = 2
GQA = N_Q_HEADS // N_KV_HEADS
KV_WIDTH = N_KV_HEADS * HEAD_DIM
SSM_WIDTH = D_MODEL - ATTN_WIDTH
SSM_CH = 16
SSM_GROUPS = SSM_WIDTH // SSM_CH
SSM_STATE = 64
DT_MIN = 0.001
DT_MAX = 0.1
IN_WIDTH = ATTN_WIDTH + 2 * KV_WIDTH + SSM_WIDTH
PEER_HEADS = 8
PEER_N_KEYS = 128
PEER_EXPERTS = PEER_N_KEYS * PEER_N_KEYS
PEER_TOPK = 16
PEER_DK = 256
PEER_HALF = PEER_DK // 2
PEER_CHUNK = 256

NORM_EPS = 1e-6
MASK_VALUE = -1e30

kernel_name = "hymba_swa_s5_peer_block"


def rms_norm(x, g):
    xf = x.astype(jnp.float32)
    y = xf * lax.rsqrt(jnp.mean(xf * xf, axis=-1, keepdims=True) + NORM_EPS)
    return (y * g.astype(jnp.float32)).astype(x.dtype)


def sliding_window_attention(q, k, v, q_norm_g, k_norm_g, sinks):
    B, L, _ = q.shape
    q = rms_norm(q.reshape(B, L, N_Q_HEADS, HEAD_DIM), q_norm_g).astype(jnp.float32)
    k = rms_norm(k.reshape(B, L, N_KV_HEADS, HEAD_DIM), k_norm_g).astype(jnp.float32)
    v = v.reshape(B, L, N_KV_HEADS, HEAD_DIM).astype(jnp.float32)
    k_meta, v_meta = k[:, :N_META], v[:, :N_META]
    pad = ((0, 0), (ATTN_PAD, 0), (0, 0), (0, 0))
    nb = (L + ATTN_PAD) // BLOCK
    qb = jnp.pad(q, pad).reshape(B, nb, BLOCK, N_KV_HEADS, GQA, HEAD_DIM)
    kb = jnp.pad(k, pad).reshape(B, nb, BLOCK, N_KV_HEADS, HEAD_DIM)
    vb = jnp.pad(v, pad).reshape(B, nb, BLOCK, N_KV_HEADS, HEAD_DIM)

    def with_prev(t):
        prev = jnp.concatenate([jnp.zeros_like(t[:, :1]), t[:, :-1]], axis=1)
        return jnp.concatenate([prev, t], axis=2)

    kb2, vb2 = with_prev(kb), with_prev(vb)
    scale = HEAD_DIM ** -0.5
    s_band = jnp.einsum('bnqhgd,bnkhd->bnhgqk', qb, kb2) * scale
    s_meta = jnp.einsum('bnqhgd,bmhd->bnhgqm', qb, k_meta) * scale

    qpos = jnp.arange(nb)[:, None] * BLOCK + jnp.arange(BLOCK)[None, :]
    kpos = jnp.arange(nb)[:, None] * BLOCK - BLOCK + jnp.arange(2 * BLOCK)[None]
    diff = qpos[:, :, None] - kpos[:, None, :]
    band_ok = (diff >= 0) & (diff < WINDOW) & (kpos[:, None, :] >= BLOCK)
    meta_ok = (ATTN_PAD + jnp.arange(N_META))[None, None, :] <= qpos[:, :, None]
    s_band = jnp.where(band_ok[None, :, None, None], s_band, MASK_VALUE)
    s_meta = jnp.where(meta_ok[None, :, None, None], s_meta, MASK_VALUE)
    sink = jnp.broadcast_to(sinks.astype(jnp.float32).reshape(1, 1, N_KV_HEADS, GQA, 1, 1),
                            s_band.shape[:-1] + (1,))
    p = jax.nn.softmax(jnp.concatenate([s_band, s_meta, sink], axis=-1), axis=-1)
    out = (jnp.einsum('bnhgqk,bnkhd->bnqhgd', p[..., :2 * BLOCK], vb2)
           + jnp.einsum('bnhgqm,bmhd->bnqhgd', p[..., 2 * BLOCK:2 * BLOCK + N_META], v_meta))
    out = out.reshape(B, nb * BLOCK, ATTN_WIDTH)[:, ATTN_PAD:]
    return out.astype(q.dtype)


def _complex_linear_combine(e1, e2):
    a1r, a1i, b1r, b1i = e1
    a2r, a2i, b2r, b2i = e2
    return (a2r * a1r - a2i * a1i,
            a2r * a1i + a2i * a1r,
            a2r * b1r - a2i * b1i + b2r,
            a2r * b1i + a2i * b1r + b2i)


def s5_ssm(u, a_re, a_im, log_dt, b_re, b_im, c_re, c_im, d, glu_w, glu_b):
    B, L, _ = u.shape
    uf = u.astype(jnp.float32).reshape(B, L, SSM_GROUPS, SSM_CH)
    dt = jnp.exp(log_dt.astype(jnp.float32))[:, None]
    lr, li = a_re.astype(jnp.float32), a_im.astype(jnp.float32)
    mag = jnp.exp(lr * dt)
    abar_r, abar_i = mag * jnp.cos(li * dt), mag * jnp.sin(li * dt)
    den = lr * lr + li * li
    nr, ni = abar_r - 1.0, abar_i
    coef_r = (nr * lr + ni * li) / den
    coef_i = (ni * lr - nr * li) / den
    br, bi = b_re.astype(jnp.float32), b_im.astype(jnp.float32)
    bbar_r = coef_r[..., None] * br - coef_i[..., None] * bi
    bbar_i = coef_r[..., None] * bi + coef_i[..., None] * br
    bu_r = jnp.einsum('blgp,gnp->blgn', uf, bbar_r)
    bu_i = jnp.einsum('blgp,gnp->blgn', uf, bbar_i)
    a_r = jnp.broadcast_to(abar_r[None, None], bu_r.shape)
    a_i = jnp.broadcast_to(abar_i[None, None], bu_i.shape)
    _, _, s_r, s_i = lax.associative_scan(_complex_linear_combine, (a_r, a_i, bu_r, bu_i), axis=1)
    y = (jnp.einsum('blgn,gpn->blgp', s_r, c_re.astype(jnp.float32))
         - jnp.einsum('blgn,gpn->blgp', s_i, c_im.astype(jnp.float32))
         + d.astype(jnp.float32).reshape(SSM_GROUPS, SSM_CH) * uf)
    z = jax.nn.gelu(y.reshape(B, L, SSM_WIDTH), approximate=False)
    z = z * jax.nn.sigmoid(z @ glu_w.astype(jnp.float32) + glu_b.astype(jnp.float32))
    return z.astype(u.dtype)


def peer_ffn(h, w_query, sub_keys, peer_u, peer_v):
    B, L, D = h.shape
    T = B * L
    n_chunks = -(-T // PEER_CHUNK)
    tok = jnp.pad(h.reshape(T, D), ((0, n_chunks * PEER_CHUNK - T), (0, 0)))
    tok = tok.reshape(n_chunks, PEER_CHUNK, D)
    keys1 = sub_keys[:, 0].astype(jnp.float32)
    keys2 = sub_keys[:, 1].astype(jnp.float32)

    def chunk_fn(hc):
        q = (hc @ w_query).astype(jnp.float32).reshape(PEER_CHUNK, PEER_HEADS, PEER_DK)
        s1 = jnp.einsum('chd,hkd->chk', q[..., :PEER_HALF], keys1)
        s2 = jnp.einsum('chd,hkd->chk', q[..., PEER_HALF:], keys2)
        v1, i1 = lax.top_k(s1, PEER_TOPK)
        v2, i2 = lax.top_k(s2, PEER_TOPK)
        cand = (v1[..., :, None] + v2[..., None, :]).reshape(PEER_CHUNK, PEER_HEADS, PEER_TOPK * PEER_TOPK)
        sc, ci = lax.top_k(cand, PEER_TOPK)
        idx = (jnp.take_along_axis(i1, ci // PEER_TOPK, axis=-1) * PEER_N_KEYS
               + jnp.take_along_axis(i2, ci % PEER_TOPK, axis=-1))
        g = jax.nn.softmax(sc, axis=-1)
        u_sel = peer_u[idx]
        act = jax.nn.gelu(jnp.einsum('chkd,cd->chk', u_sel, hc).astype(jnp.float32),
                          approximate=False) * g
        v_sel = peer_v[idx]
        return jnp.einsum('chk,chkd->cd', act.astype(hc.dtype), v_sel)

    out = lax.map(chunk_fn, tok).reshape(n_chunks * PEER_CHUNK, D)[:T]
    return out.reshape(B, L, D).astype(h.dtype)


def setup_inputs(seed: int = 0) -> dict:
    key = jax.random.key(seed)
    ks = jax.random.split(key, 26)
    nrm = jax.random.normal
    f32 = jnp.float32
    n_idx = jnp.arange(SSM_STATE, dtype=f32)
    return {
        "x": nrm(ks[0], (BATCH, SEQ, D_MODEL), f32),
        "meta_tokens": nrm(ks[1], (N_META, D_MODEL), f32),
        "norm1_g": 1.0 + 0.02 * nrm(ks[2], (DEPTH, D_MODEL), f32),
        "w_in": nrm(ks[3], (DEPTH, D_MODEL, IN_WIDTH), f32) * D_MODEL ** -0.5,
        "q_norm_g": 1.0 + 0.02 * nrm(ks[4], (DEPTH, HEAD_DIM), f32),
        "k_norm_g": 1.0 + 0.02 * nrm(ks[5], (DEPTH, HEAD_DIM), f32),
        "attn_sinks": 0.5 * nrm(ks[6], (DEPTH, N_Q_HEADS), f32),
        "ssm_a_re": -0.5 + 0.01 * nrm(ks[7], (DEPTH, SSM_GROUPS, SSM_STATE), f32),
        "ssm_a_im": math.pi * n_idx + 0.01 * nrm(ks[8], (DEPTH, SSM_GROUPS, SSM_STATE), f32),
        "ssm_log_dt": jax.random.uniform(ks[9], (DEPTH, SSM_GROUPS), f32,
                                         minval=math.log(DT_MIN), maxval=math.log(DT_MAX)),
        "ssm_b_re": nrm(ks[10], (DEPTH, SSM_GROUPS, SSM_STATE, SSM_CH), f32) * (2 * SSM_CH) ** -0.5,
        "ssm_b_im": nrm(ks[11], (DEPTH, SSM_GROUPS, SSM_STATE, SSM_CH), f32) * (2 * SSM_CH) ** -0.5,
        "ssm_c_re": nrm(ks[12], (DEPTH, SSM_GROUPS, SSM_CH, SSM_STATE), f32) * SSM_STATE ** -0.5,
        "ssm_c_im": nrm(ks[13], (DEPTH, SSM_GROUPS, SSM_CH, SSM_STATE), f32) * SSM_STATE ** -0.5,
        "ssm_d": nrm(ks[14], (DEPTH, SSM_WIDTH), f32),
        "ssm_glu_w": nrm(ks[15], (DEPTH, SSM_WIDTH, SSM_WIDTH), f32) * SSM_WIDTH ** -0.5,
        "ssm_glu_b": 0.02 * nrm(ks[16], (DEPTH, SSM_WIDTH), f32),
        "attn_out_g": 1.0 + 0.02 * nrm(ks[17], (DEPTH, ATTN_WIDTH), f32),
        "ssm_out_g": 1.0 + 0.02 * nrm(ks[18], (DEPTH, SSM_WIDTH), f32),
        "w_out": nrm(ks[19], (DEPTH, D_MODEL, D_MODEL), f32) * D_MODEL ** -0.5,
        "norm2_g": 1.0 + 0.02 * nrm(ks[20], (DEPTH, D_MODEL), f32),
        "peer_w_query": nrm(ks[21], (DEPTH, D_MODEL, PEER_HEADS * PEER_DK), f32) * D_MODEL ** -0.5,
        "peer_sub_keys": nrm(ks[22], (DEPTH, PEER_HEADS, 2, PEER_N_KEYS, PEER_HALF), f32) * PEER_HALF ** -0.5,
        "peer_u": nrm(ks[23], (DEPTH, PEER_EXPERTS, D_MODEL), f32) * D_MODEL ** -0.5,
        "peer_v": nrm(ks[24], (DEPTH, PEER_EXPERTS, D_MODEL), f32) * PEER_HEADS ** -0.5,
    }


def reference(x, meta_tokens, norm1_g, w_in, q_norm_g, k_norm_g, attn_sinks, ssm_a_re, ssm_a_im,
              ssm_log_dt, ssm_b_re, ssm_b_im, ssm_c_re, ssm_c_im, ssm_d, ssm_glu_w, ssm_glu_b,
              attn_out_g, ssm_out_g, w_out, norm2_g, peer_w_query, peer_sub_keys, peer_u, peer_v):
    B = x.shape[0]
    meta = jnp.broadcast_to(meta_tokens.astype(x.dtype)[None], (B, N_META, D_MODEL))
    h = jnp.concatenate([meta, x], axis=1)
    o_k = ATTN_WIDTH
    o_v = ATTN_WIDTH + KV_WIDTH
    o_u = ATTN_WIDTH + 2 * KV_WIDTH
    for l in range(DEPTH):
        proj = rms_norm(h, norm1_g[l]) @ w_in[l]
        attn = sliding_window_attention(proj[..., :o_k], proj[..., o_k:o_v], proj[..., o_v:o_u],
                                        q_norm_g[l], k_norm_g[l], attn_sinks[l])
        ssm = s5_ssm(proj[..., o_u:], ssm_a_re[l], ssm_a_im[l], ssm_log_dt[l], ssm_b_re[l], ssm_b_im[l],
                     ssm_c_re[l], ssm_c_im[l], ssm_d[l], ssm_glu_w[l], ssm_glu_b[l])
        mixed = jnp.concatenate([rms_norm(attn, attn_out_g[l]), rms_norm(ssm, ssm_out_g[l])], axis=-1)
        h = h + mixed @ w_out[l]
        h = h + peer_ffn(rms_norm(h, norm2_g[l]), peer_w_query[l], peer_sub_keys[l], peer_u[l], peer_v[l])
    return h[:, N_META:]
```

```python
import contextlib
import types
import numpy as np
import concourse.bass as bass
import concourse.mybir as mybir
from concourse.bass_utils import run_bass_kernel_spmd

F32 = mybir.dt.float32
BF16 = mybir.dt.bfloat16
U32 = mybir.dt.uint32
AF = mybir.ActivationFunctionType
ALU = mybir.AluOpType
AX = mybir.AxisListType

NCORES = 8
TOK = 2048
NT = 16
NPRE = 113
EPS = 1e-6
ENGS = ("pe", "act", "dve", "pool", "sp")


class StopBuild(Exception):
    pass


def _freeze(fn):
    if fn.__closure__ is None:
        return fn
    cells = []
    for c in fn.__closure__:
        try:
            cells.append(types.CellType(c.cell_contents))
        except ValueError:
            cells.append(c)
    return types.FunctionType(fn.__code__, fn.__globals__, fn.__name__, fn.__defaults__, tuple(cells))


class Sched:
    def __init__(self, nc, n_dma_streams=8):
        self.nc = nc
        self.ops = {e: [] for e in ENGS}
        self.count = {}
        self.last_w = {}
        self.readers = {}
        self.seen = {e: {} for e in ENGS}
        self.n_dma = n_dma_streams
        self.dma_rr = {e: 0 for e in ENGS}
        self.pending = {e: {} for e in ENGS}

    def barrier(self):
        for e in ENGS:
            for k, v in self.count.items():
                if self.pending[e].get(k, 0) < v:
                    self.pending[e][k] = v

    def _deps(self, eng, reads, writes):
        need = dict(self.pending[eng])
        self.pending[eng] = {}

        def add(ev):
            if ev is not None and need.get(ev[0], 0) < ev[1]:
                need[ev[0]] = ev[1]
        for b in reads:
            add(self.last_w.get(b))
        for b in writes:
            add(self.last_w.get(b))
            for ev in self.readers.get(b, ()):
                add(ev)
        waits = []
        for k, v in need.items():
            if eng == "pe" and k == ("e", "pe"):
                continue
            if self.seen[eng].get(k, 0) < v:
                self.seen[eng][k] = v
                waits.append((k, v))
        return waits

    def _record(self, ev, reads, writes):
        for b in reads:
            self.readers.setdefault(b, []).append(ev)
        for b in writes:
            self.last_w[b] = ev
            self.readers[b] = []

    def op(self, eng, fn, reads=(), writes=()):
        fn = _freeze(fn)
        waits = self._deps(eng, reads, writes)
        k = ("e", eng)
        v = self.count.get(k, 0) + 1
        self.count[k] = v
        self.ops[eng].append((waits, fn, (k, 1)))
        self._record((k, v), reads, writes)

    def dma(self, fn, reads=(), writes=(), eng="sp"):
        fn = _freeze(fn)
        waits = self._deps(eng, reads, writes)
        k = ("d", eng, self.dma_rr[eng] % self.n_dma)
        self.dma_rr[eng] += 1
        v = self.count.get(k, 0) + 16
        self.count[k] = v
        self.ops[eng].append((waits, fn, (k, 16)))
        self._record((k, v), reads, writes)

    def emit(self):
        nc = self.nc
        keys = sorted(self.count.keys(), key=str)
        with contextlib.ExitStack() as st:
            sems = {k: st.enter_context(nc.semaphore("s_" + "_".join(map(str, k)))) for k in keys}
            block = st.enter_context(nc.Block())
            final = [(k, self.count[k]) for k in keys]

            def run(engobj, lst, is_final):
                for waits, fn, (k, inc) in lst:
                    for wk, wv in waits:
                        engobj.wait_ge(sems[wk], wv)
                    fn(engobj).then_inc(sems[k], inc)
                if is_final:
                    for wk, wv in final:
                        engobj.wait_ge(sems[wk], wv)
            names = {"pe": "tensor", "act": "scalar", "dve": "vector", "pool": "gpsimd", "sp": "sync"}
            for e in ENGS:
                lst = self.ops[e]
                getattr(block, names[e])(lambda engobj, lst=lst, e=e: run(engobj, lst, e == "sp"))


def build(stage="full", off=()):
    nc = bass.Bass("TRN2", target_bir_lowering=False)
    S = Sched(nc)
    D = {}

    def din(name, shape, dt=F32):
        D[name] = nc.dram_tensor(name, list(shape), dt, kind="ExternalInput").ap()
        return D[name]
    xown = din("xown", [TOK, 1024]); xhalo = din("xhalo", [128, 1024]); xpre = din("xpre", [NPRE * 128, 1024])
    xmeta = din("xmeta", [128, 1024])
    din("ident", [128, 128]); din("mask_cur", [128, 128]); din("mask_prev", [128, 128]); din("mask_halo", [128, 128])
    din("w_in", [128, 8, 1280]); din("g1", [128, 8]); din("gq2", [128, 1]); din("gk2", [128, 1]); din("sinks", [64, 8])
    for nm in ("a_re", "a_im", "ldt"):
        din(nm + "_s", [128, 16]); din(nm + "_r", [128, 2048])
    din("bT_re", [128, 16, 128]); din("bT_im", [128, 16, 128]); din("bs_re", [128, 16, 32]); din("bs_im", [128, 16, 32])
    din("cb_re", [128, 16, 128]); din("cb_im", [128, 16, 128]); din("ssm_d", [128, 4]); din("glu_w", [128, 4, 512])
    din("glu_b", [128, 4]); din("ga", [64, 8]); din("gs", [128, 4]); din("woa", [64, 8, 1024]); din("wos", [128, 4, 1024])
    din("g2", [128, 8]); din("is0", [128, 1])
    if stage in ("peerfront", "full"):
        din("wq", [128, 8, 2048]); din("keysT", [128, 16, 128])
    if stage == "full":
        din("puT", [128, 128, 1024]); din("pv", [128, 128, 1024]); din("iota_i", [128, 128]); din("blkmask", [128, 8])
        UTs = nc.dram_tensor("UTs", [128, 128, 1024], BF16).ap(); Vs = nc.dram_tensor("Vs", [128, 128, 1024], BF16).ap()
    out = nc.dram_tensor("out", [TOK, 1024], F32, kind="ExternalOutput").ap()

    with contextlib.ExitStack() as top:
        def SB(st, name, shape, dt=F32):
            return st.enter_context(nc.sbuf_tensor("sb_" + name, list(shape), dt))

        def PS(st, name, shape, dt=F32):
            return st.enter_context(nc.psum_tensor("ps_" + name, list(shape), dt))

        def V(fn, r, w): S.op("dve", fn, r, w)
        def A(fn, r, w): S.op("act", fn, r, w)
        def G(fn, r, w): S.op("pool", fn, r, w)
        def M(fn, r, w): S.op("pe", fn, r, w)

        @contextlib.contextmanager
        def scope():
            with contextlib.ExitStack() as st_:
                yield st_
                S.barrier()

        def load(dst, src, key):
            S.dma(lambda e: e.dma_start(out=dst, in_=src), writes=[key])

        idf = SB(top, "idf", [128, 128]); idb = SB(top, "idb", [128, 128], BF16)
        load(idf[:], D["ident"], "idf")
        V(lambda e: e.tensor_copy(idb[:], idf[:]), ["idf"], ["idb"])
        ones = SB(top, "ones", [128, 64], BF16)
        V(lambda e: e.memset(ones[:], 1.0), [], ["ones"])
        h2T = SB(top, "h2T", [128, 8, TOK if "smallh2T" not in off else 128], BF16)

        mix = top.enter_context(contextlib.ExitStack())
        ptr = PS(mix, "ptr", [128, 1024], BF16)
        psA = PS(mix, "psA", [128, 1024])
        pb = [PS(mix, f"pb{i}", [128, 512]) for i in range(5)]

        win = SB(mix, "win", [128, 8, 1280], BF16); woa = SB(mix, "woa", [64, 8, 1024], BF16)
        wos = SB(mix, "wos", [128, 4, 1024], BF16); glu = SB(mix, "glu", [128, 4, 512], BF16)
        small = {}
        for nm, shp in (("g1", [128, 8]), ("gq2", [128, 1]), ("gk2", [128, 1]), ("sinks", [64, 8]), ("ssm_d", [128, 4]),
                        ("glu_b", [128, 4]), ("ga", [64, 8]), ("gs", [128, 4]), ("g2", [128, 8]), ("is0", [128, 1])):
            small[nm] = SB(mix, "c_" + nm, shp)
            load(small[nm][:], D[nm], "c_" + nm)
        with scope() as stg:
            s1 = SB(stg, "stg1", [128, 8, 1280])
            load(s1[:], D["w_in"], "stg1")
            for k in range(8):
                V(lambda e, k=k: e.tensor_scalar(win[:, k, :], s1[:, k, :], small["g1"][:, k:k + 1], None, ALU.mult),
                  ["stg1", "c_g1"], ["win"])
        with scope() as stg:
            s2 = SB(stg, "stg2", [64, 8, 1024]); s3 = SB(stg, "stg3", [128, 4, 1024]); s4 = SB(stg, "stg4", [128, 4, 512])
            load(s2[:], D["woa"], "stg2"); load(s3[:], D["wos"], "stg3"); load(s4[:], D["glu_w"], "stg4")
            for h in range(8):
                V(lambda e, h=h: e.tensor_scalar(woa[:, h, :], s2[:, h, :], small["ga"][:, h:h + 1], None, ALU.mult),
                  ["stg2", "c_ga"], ["woa"])
            for c in range(4):
                V(lambda e, c=c: e.tensor_scalar(wos[:, c, :], s3[:, c, :], small["gs"][:, c:c + 1], None, ALU.mult),
                  ["stg3", "c_gs"], ["wos"])
            V(lambda e: e.tensor_copy(glu[:], s4[:]), ["stg4"], ["glu"])
        cqk = SB(mix, "cqk", [128, 1]); esink = SB(mix, "esink", [64, 8])
        V(lambda e: e.tensor_tensor(cqk[:], small["gq2"][:], small["gk2"][:], ALU.mult), ["c_gq2", "c_gk2"], ["cqk"])
        V(lambda e: e.tensor_scalar(cqk[:], cqk[:], 0.125, None, ALU.mult), ["cqk"], ["cqk"])
        A(lambda e: e.activation(out=esink[:], in_=small["sinks"][:], func=AF.Exp), ["c_sinks"], ["esink"])
        mcur = SB(mix, "mcur", [128, 128], BF16); mprev = SB(mix, "mprev", [128, 128], BF16); mhalo = SB(mix, "mhalo", [128, 128], BF16)
        with scope() as stg:
            m1 = SB(stg, "m1", [128, 128]); m2 = SB(stg, "m2", [128, 128]); m3 = SB(stg, "m3", [128, 128])
            load(m1[:], D["mask_cur"], "m1"); load(m2[:], D["mask_prev"], "m2"); load(m3[:], D["mask_halo"], "m3")
            V(lambda e: e.tensor_copy(mcur[:], m1[:]), ["m1"], ["mcur"])
            V(lambda e: e.tensor_copy(mprev[:], m2[:]), ["m2"], ["mprev"])
            V(lambda e: e.tensor_copy(mhalo[:], m3[:]), ["m3"], ["mhalo"])

        TWO_PI = 2.0 * np.pi

        def ssm_scalars(st, tag, are, aim, ldt, shape, t=None):
            if t is None:
                t = {n: SB(st, f"{tag}_{n}", shape) for n in ("dt", "rho", "th", "ph", "cos", "sin", "abr", "abi", "den", "nr", "t1", "t2", "cfr", "cfi")}
                t["ri"] = SB(st, f"{tag}_ri", shape, mybir.dt.int32)
            k = lambda n: f"{tag}_{n}"
            A(lambda e: e.activation(out=t["dt"][:], in_=ldt[:], func=AF.Exp), [k("ldt")], [k("dt")])
            V(lambda e: e.tensor_tensor(t["rho"][:], are[:], t["dt"][:], ALU.mult), [k("are"), k("dt")], [k("rho")])
            A(lambda e: e.activation(out=t["rho"][:], in_=t["rho"][:], func=AF.Exp), [k("rho")], [k("rho")])
            V(lambda e: e.tensor_tensor(t["th"][:], aim[:], t["dt"][:], ALU.mult), [k("aim"), k("dt")], [k("th")])
            for nm, off in (("sin", 0.0), ("cos", np.pi / 2)):
                V(lambda e, off=off: e.tensor_scalar(t["t2"][:], t["th"][:], float(off), None, ALU.add), [k("th")], [k("t2")])
                V(lambda e: e.tensor_scalar(t["t1"][:], t["t2"][:], float(1.0 / TWO_PI), None, ALU.mult), [k("t2")], [k("t1")])
                V(lambda e: e.tensor_copy(t["ri"][:], t["t1"][:]), [k("t1")], [k("ri")])
                V(lambda e: e.tensor_copy(t["t1"][:], t["ri"][:]), [k("ri")], [k("t1")])
                V(lambda e: e.scalar_tensor_tensor(t["ph"][:], t["t1"][:], float(-TWO_PI), t["t2"][:], ALU.mult, ALU.add), [k("t1"), k("t2")], [k("ph")])
                V(lambda e: e.tensor_scalar(t["t1"][:], t["ph"][:], float(np.pi), float(-TWO_PI), ALU.is_gt, ALU.mult), [k("ph")], [k("t1")])
                V(lambda e: e.tensor_tensor(t["ph"][:], t["ph"][:], t["t1"][:], ALU.add), [k("ph"), k("t1")], [k("ph")])
                A(lambda e, nm=nm: e.activation(out=t[nm][:], in_=t["ph"][:], func=AF.Sin), [k("ph")], [k(nm)])
            V(lambda e: e.tensor_tensor(t["abr"][:], t["rho"][:], t["cos"][:], ALU.mult), [k("rho"), k("cos")], [k("abr")])
            V(lambda e: e.tensor_tensor(t["abi"][:], t["rho"][:], t["sin"][:], ALU.mult), [k("rho"), k("sin")], [k("abi")])
            V(lambda e: e.tensor_tensor(t["den"][:], are[:], are[:], ALU.mult), [k("are")], [k("den")])
            V(lambda e: e.tensor_tensor(t["t1"][:], aim[:], aim[:], ALU.mult), [k("aim")], [k("t1")])
            V(lambda e: e.tensor_tensor(t["den"][:], t["den"][:], t["t1"][:], ALU.add), [k("den"), k("t1")], [k("den")])
            V(lambda e: e.reciprocal(t["den"][:], t["den"][:]), [k("den")], [k("den")])
            V(lambda e: e.tensor_scalar(t["nr"][:], t["abr"][:], -1.0, None, ALU.add), [k("abr")], [k("nr")])
            V(lambda e: e.tensor_tensor(t["t1"][:], t["nr"][:], are[:], ALU.mult), [k("nr"), k("are")], [k("t1")])
            V(lambda e: e.tensor_tensor(t["t2"][:], t["abi"][:], aim[:], ALU.mult), [k("abi"), k("aim")], [k("t2")])
            V(lambda e: e.tensor_tensor(t["t1"][:], t["t1"][:], t["t2"][:], ALU.add), [k("t1"), k("t2")], [k("t1")])
            V(lambda e: e.tensor_tensor(t["cfr"][:], t["t1"][:], t["den"][:], ALU.mult), [k("t1"), k("den")], [k("cfr")])
            V(lambda e: e.tensor_tensor(t["t1"][:], t["abi"][:], are[:], ALU.mult), [k("abi"), k("are")], [k("t1")])
            V(lambda e: e.tensor_tensor(t["t2"][:], t["nr"][:], aim[:], ALU.mult), [k("nr"), k("aim")], [k("t2")])
            V(lambda e: e.tensor_tensor(t["t1"][:], t["t1"][:], t["t2"][:], ALU.subtract), [k("t1"), k("t2")], [k("t1")])
            V(lambda e: e.tensor_tensor(t["cfi"][:], t["t1"][:], t["den"][:], ALU.mult), [k("t1"), k("den")], [k("cfi")])
            return t

        def cmul(dst_r, dst_i, ar, ai, br, bi, t1, t2, rk, wk):
            V(lambda e: e.tensor_tensor(t1, ar, br, ALU.mult), rk, wk)
            V(lambda e: e.tensor_tensor(t2, ai, bi, ALU.mult), rk, wk)
            V(lambda e: e.tensor_tensor(dst_r, t1, t2, ALU.subtract), rk + wk, wk)
            V(lambda e: e.tensor_tensor(t1, ar, bi, ALU.mult), rk + wk, wk)
            V(lambda e: e.tensor_tensor(t2, ai, br, ALU.mult), rk + wk, wk)
            V(lambda e: e.tensor_tensor(dst_i, t1, t2, ALU.add), rk + wk, wk)

        ss_are = SB(mix, "ss_are", [128, 16]); ss_aim = SB(mix, "ss_aim", [128, 16]); ss_ldt = SB(mix, "ss_ldt", [128, 16])
        load(ss_are[:], D["a_re_s"], "ss_are"); load(ss_aim[:], D["a_im_s"], "ss_aim"); load(ss_ldt[:], D["ldt_s"], "ss_ldt")
        sc = ssm_scalars(mix, "ss", ss_are, ss_aim, ss_ldt, [128, 16])
        Er = SB(mix, "Er", [128, 16, 128]); Ei = SB(mix, "Ei", [128, 16, 128]); Rho0 = SB(mix, "Rho0", [128, 16, 128])
        WTr = SB(mix, "WTr", [128, 16, 128], BF16); WTi = SB(mix, "WTi", [128, 16, 128], BF16)
        A128r = SB(mix, "A128r", [128, 16]); A128i = SB(mix, "A128i", [128, 16])
        E127r = SB(mix, "E127r", [128, 16]); E127i = SB(mix, "E127i", [128, 16])
        bsr = SB(mix, "bsr", [128, 16, 32]); bsi = SB(mix, "bsi", [128, 16, 32])
        BTr = SB(mix, "BTr", [128, 16, 128], BF16); BTi = SB(mix, "BTi", [128, 16, 128], BF16)
        Cbr = SB(mix, "Cbr", [128, 16, 128], BF16); Cbi = SB(mix, "Cbi", [128, 16, 128], BF16)

        def bc3(ap2, n):
            return ap2.rearrange("p (a o) -> p a o", o=1).to_broadcast([128, 16, n])

        with scope() as stg:
            Hr = SB(stg, "Hr", [128, 16, 128]); Hi = SB(stg, "Hi", [128, 16, 128])
            T1 = SB(stg, "T1", [128, 16, 128]); T2 = SB(stg, "T2", [128, 16, 128])
            q1 = SB(stg, "q1", [128, 16]); q2 = SB(stg, "q2", [128, 16]); ir = SB(stg, "ir", [128, 16]); ii_ = SB(stg, "ii", [128, 16])
            pr = SB(stg, "pr", [128, 16]); pi_ = SB(stg, "pi", [128, 16])
            V(lambda e: e.memset(Er[:, :, 0:1], 1.0), [], ["Er"]); V(lambda e: e.memset(Ei[:, :, 0:1], 0.0), [], ["Ei"])
            V(lambda e: e.tensor_copy(Er[:, :, 1], sc["cos"][:]), ["ss_cos"], ["Er"])
            V(lambda e: e.tensor_copy(Ei[:, :, 1], sc["sin"][:]), ["ss_sin"], ["Ei"])
            V(lambda e: e.tensor_tensor(q1[:], sc["rho"][:], sc["rho"][:], ALU.mult), ["ss_rho"], ["q1"])
            V(lambda e: e.reciprocal(q1[:], q1[:]), ["q1"], ["q1"])
            V(lambda e: e.tensor_tensor(ir[:], sc["abr"][:], q1[:], ALU.mult), ["ss_abr", "q1"], ["ir"])
            V(lambda e: e.tensor_tensor(ii_[:], sc["abi"][:], q1[:], ALU.mult), ["ss_abi", "q1"], ["ii"])
            V(lambda e: e.tensor_scalar(ii_[:], ii_[:], -1.0, None, ALU.mult), ["ii"], ["ii"])
            V(lambda e: e.memset(Hr[:, :, 0:1], 1.0), [], ["Hr"]); V(lambda e: e.memset(Hi[:, :, 0:1], 0.0), [], ["Hi"])
            V(lambda e: e.tensor_copy(Hr[:, :, 1], ir[:]), ["ir"], ["Hr"]); V(lambda e: e.tensor_copy(Hi[:, :, 1], ii_[:]), ["ii"], ["Hi"])
            for (Xr, Xi, kr, ki) in ((Er, Ei, "Er", "Ei"), (Hr, Hi, "Hr", "Hi")):
                m = 2
                while m < 128:
                    h = m // 2
                    cmul(Xr[:, :, m], Xi[:, :, m], Xr[:, :, h], Xi[:, :, h], Xr[:, :, h], Xi[:, :, h], q1[:], q2[:], [kr, ki, "q1", "q2"], [kr, ki, "q1", "q2"])
                    cmul(Xr[:, :, m + 1:2 * m], Xi[:, :, m + 1:2 * m], Xr[:, :, 1:m], Xi[:, :, 1:m],
                         bc3(Xr[:, :, m], m - 1), bc3(Xi[:, :, m], m - 1), T1[:, :, 1:m], T2[:, :, 1:m], [kr, ki, "T1", "T2"], [kr, ki, "T1", "T2"])
                    m *= 2
            V(lambda e: e.tensor_copy(A128r[:], sc["abr"][:]), ["ss_abr"], ["A128"]); V(lambda e: e.tensor_copy(A128i[:], sc["abi"][:]), ["ss_abi"], ["A128"])
            for _ in range(7):
                cmul(pr[:], pi_[:], A128r[:], A128i[:], A128r[:], A128i[:], q1[:], q2[:], ["A128", "q1", "q2", "pp"], ["pp", "q1", "q2"])
                V(lambda e: e.tensor_copy(A128r[:], pr[:]), ["pp"], ["A128"]); V(lambda e: e.tensor_copy(A128i[:], pi_[:]), ["pp"], ["A128"])
            V(lambda e: e.tensor_copy(E127r[:], Er[:, :, 127]), ["Er"], ["E127"]); V(lambda e: e.tensor_copy(E127i[:], Ei[:, :, 127]), ["Ei"], ["E127"])
            cmul(pr[:], pi_[:], A128r[:], A128i[:], ir[:], ii_[:], q1[:], q2[:], ["A128", "ir", "ii", "q1", "q2", "pp"], ["pp", "q1", "q2"])
            cmul(T1[:], T2[:], Hr[:], Hi[:], bc3(pr[:], 128), bc3(pi_[:], 128), Er[:] if False else Rho0[:], SB(stg, "T3", [128, 16, 128])[:],
                 ["Hr", "Hi", "pp", "Rho0", "T3", "T1", "T2"], ["T1", "T2", "Rho0", "T3"])
            for (Ts, WT, kk) in ((T1, WTr, "WTr"), (T2, WTi, "WTi")):
                for g4 in range(4):
                    for j in range(4):
                        i = 4 * g4 + j
                        M(lambda e, i=i, j=j, Ts=Ts: e.transpose(pb[0][:, j * 128:(j + 1) * 128], Ts[:, i, :], idf[:]), ["T1", "T2", "idf"], ["pb0"])
                    V(lambda e, g4=g4, WT=WT: e.tensor_copy(WT[:, 4 * g4:4 * g4 + 4, :], pb[0][:].rearrange("p (a b) -> p a b", a=4)), ["pb0"], [kk])
            V(lambda e: e.tensor_copy(Rho0[:], bc3(sc["rho"][:], 128)), ["ss_rho", "T1", "T2"], ["Rho0"])
            V(lambda e: e.memset(Rho0[:, :, 0:1], 0.0), ["Rho0"], ["Rho0"])
            b0r = SB(stg, "b0r", [128, 16, 32]); b0i = SB(stg, "b0i", [128, 16, 32]); u1 = SB(stg, "u1", [128, 16, 32]); u2 = SB(stg, "u2", [128, 16, 32])
            load(b0r[:], D["bs_re"], "b0r"); load(b0i[:], D["bs_im"], "b0i")
            cmul(bsr[:], bsi[:], bc3(sc["cfr"][:], 32), bc3(sc["cfi"][:], 32), b0r[:], b0i[:], u1[:], u2[:], ["ss_cfr", "ss_cfi", "b0r", "b0i", "u1", "u2"], ["bs", "u1", "u2"])
        with scope() as stg:
            CW = 512
            r_are = SB(stg, "rr_are", [128, CW]); r_aim = SB(stg, "rr_aim", [128, CW]); r_ldt = SB(stg, "rr_ldt", [128, CW])
            rc = None
            BTr2 = BTr[:].rearrange("p a b -> p (a b)"); BTi2 = BTi[:].rearrange("p a b -> p (a b)")
            Cbr2 = Cbr[:].rearrange("p a b -> p (a b)"); Cbi2 = Cbi[:].rearrange("p a b -> p (a b)")
            for cc in range(2048 // CW):
                cs = slice(cc * CW, (cc + 1) * CW)
                load(r_are[:], D["a_re_r"][:, cs], "rr_are"); load(r_aim[:], D["a_im_r"][:, cs], "rr_aim"); load(r_ldt[:], D["ldt_r"][:, cs], "rr_ldt")
                rc = ssm_scalars(stg, "rr", r_are, r_aim, r_ldt, [128, CW], t=rc)
                b1r = rc["dt"]; b1i = rc["th"]
                load(b1r[:], D["bT_re"].rearrange("p a b -> p (a b)")[:, cs], "rr_dt"); load(b1i[:], D["bT_im"].rearrange("p a b -> p (a b)")[:, cs], "rr_th")
                cmul(rc["cos"][:], rc["sin"][:], rc["cfr"][:], rc["cfi"][:], b1r[:], b1i[:], rc["t1"][:], rc["t2"][:],
                     ["rr_cfr", "rr_cfi", "rr_dt", "rr_th", "rr_t1", "rr_t2", "rr_cos", "rr_sin"], ["rr_cos", "rr_sin", "rr_t1", "rr_t2"])
                V(lambda e, cs=cs: e.tensor_copy(BTr2[:, cs], rc["cos"][:]), ["rr_cos"], ["BT"])
                V(lambda e, cs=cs: e.tensor_copy(BTi2[:, cs], rc["sin"][:]), ["rr_sin"], ["BT"])
                load(rc["t1"][:], D["cb_re"].rearrange("p a b -> p (a b)")[:, cs], "rr_t1"); load(rc["t2"][:], D["cb_im"].rearrange("p a b -> p (a b)")[:, cs], "rr_t2")
                V(lambda e, cs=cs: e.tensor_copy(Cbr2[:, cs], rc["t1"][:]), ["rr_t1"], ["Cb"])
                V(lambda e, cs=cs: e.tensor_scalar(Cbi2[:, cs], rc["t2"][:], -1.0, None, ALU.mult), ["rr_t2"], ["Cb"])

        xt = [SB(mix, f"xt{i}", [128, 1024]) for i in range(2)]
        junk = SB(mix, "junk", [128, 1024], BF16); xnb = SB(mix, "xnb", [128, 1024], BF16); nT = SB(mix, "nT", [128, 8, 128], BF16)
        st1 = SB(mix, "st1", [128, 4])
        Sr = SB(mix, "Sr", [128, 16]); Si = SB(mix, "Si", [128, 16])
        V(lambda e: e.memset(Sr[:], 0.0), [], ["S"]); V(lambda e: e.memset(Si[:], 0.0), [], ["S"])
        c1 = SB(mix, "c1", [128, 16]); c2 = SB(mix, "c2", [128, 16]); c3 = SB(mix, "c3", [128, 16]); c4 = SB(mix, "c4", [128, 16])
        xi = [0]

        def front(src):
            sl = xi[0] % 2; xi[0] += 1
            kx = f"xt{sl}"
            S.dma(lambda e: e.dma_start(out=xt[sl][:], in_=src), writes=[kx])
            A(lambda e: e.activation(out=junk[:], in_=xt[sl][:], func=AF.Square, accum_out=st1[:, 0:1]), [kx], ["junk", "st1"])
            V(lambda e: e.tensor_scalar(st1[:, 1:2], st1[:, 0:1], 1.0 / 1024, EPS, ALU.mult, ALU.add), ["st1"], ["st1b"])
            A(lambda e: e.activation(out=st1[:, 3:4], in_=st1[:, 1:2], func=AF.Sqrt), ["st1b"], ["st1d"])
            V(lambda e: e.reciprocal(st1[:, 2:3], st1[:, 3:4]), ["st1d"], ["st1c"])
            V(lambda e: e.tensor_scalar(xnb[:], xt[sl][:], st1[:, 2:3], None, ALU.mult), [kx, "st1c"], ["xnb"])
            for k in range(8):
                M(lambda e, k=k: e.transpose(ptr[:, k * 128:(k + 1) * 128], xnb[:, k * 128:(k + 1) * 128], idb[:]), ["xnb", "idb"], ["ptr"])
            A(lambda e: e.copy(nT[:].rearrange("p a b -> p (a b)"), ptr[:]), ["ptr"], ["nT"])
            return sl

        ub = SB(mix, "ub", [128, 512], BF16)
        fr = SB(mix, "fr", [128, 16]); fi = SB(mix, "fi", [128, 16])
        w1 = SB(mix, "w1", [128, 16, 32]); w2 = SB(mix, "w2", [128, 16, 32])

        def state_step():
            cmul(c3[:], c4[:], A128r[:], A128i[:], Sr[:], Si[:], c1[:], c2[:], ["A128", "S", "c"], ["c"])
            V(lambda e: e.tensor_tensor(Sr[:], c3[:], fr[:], ALU.add), ["c", "f"], ["S"])
            V(lambda e: e.tensor_tensor(Si[:], c4[:], fi[:], ALU.add), ["c", "f"], ["S"])

        def prefix_tile(src):
            front(src)
            for k in range(8):
                M(lambda e, k=k: e.matmul(pb[0][:], lhsT=nT[:, k, :], rhs=win[:, k, 768:1280], start=(k == 0), stop=(k == 7)), ["nT", "win"], ["pb0"])
            A(lambda e: e.copy(ub[:], pb[0][:]), ["pb0"], ["ub"])
            for i in range(16):
                M(lambda e, i=i: e.matmul(pb[1][:, i * 32:(i + 1) * 32], lhsT=WTr[:, i, :], rhs=ub[:, i * 32:(i + 1) * 32], start=True, stop=True), ["ub", "WTr"], ["pb1"])
                M(lambda e, i=i: e.matmul(pb[2][:, i * 32:(i + 1) * 32], lhsT=WTi[:, i, :], rhs=ub[:, i * 32:(i + 1) * 32], start=True, stop=True), ["ub", "WTi"], ["pb2"])
            mr = pb[1][:].rearrange("p (a b) -> p a b", a=16); mi = pb[2][:].rearrange("p (a b) -> p a b", a=16)
            V(lambda e: e.tensor_tensor(w1[:], bsr[:], mr, ALU.mult), ["bs", "pb1"], ["w1"])
            V(lambda e: e.tensor_tensor(w2[:], bsi[:], mi, ALU.mult), ["bs", "pb2"], ["w2"])
            V(lambda e: e.tensor_tensor(w1[:], w1[:], w2[:], ALU.subtract), ["w1", "w2"], ["w1"])
            V(lambda e: e.tensor_reduce(fr[:], w1[:], AX.X, ALU.add), ["w1"], ["f"])
            V(lambda e: e.tensor_tensor(w1[:], bsr[:], mi, ALU.mult), ["bs", "pb2", "f"], ["w1"])
            V(lambda e: e.tensor_tensor(w2[:], bsi[:], mr, ALU.mult), ["bs", "pb1"], ["w2"])
            V(lambda e: e.tensor_tensor(w1[:], w1[:], w2[:], ALU.add), ["w1", "w2"], ["w1"])
            V(lambda e: e.tensor_reduce(fi[:], w1[:], AX.X, ALU.add), ["w1"], ["f"])
            state_step()

        for t in range(NPRE if "prefix" not in off else 0):
            prefix_tile(xpre[t * 128:(t + 1) * 128, :])

        if stage == "prefix":
            dbg = SB(mix, "dbg", [128, 1024])
            V(lambda e: e.memset(dbg[:], 0.0), [], ["dbg"])
            V(lambda e: e.tensor_copy(dbg[:, 0:16], Sr[:]), ["S"], ["dbg"]); V(lambda e: e.tensor_copy(dbg[:, 16:32], Si[:]), ["S"], ["dbg"])
            for j, (ap, key) in enumerate([(sc["cos"][:], "ss_cos"), (sc["sin"][:], "ss_sin"), (sc["abr"][:], "ss_abr"), (sc["abi"][:], "ss_abi"),
                                           (sc["cfr"][:], "ss_cfr"), (sc["cfi"][:], "ss_cfi"), (Er[:, :, 5], "Er"), (Ei[:, :, 5], "Ei"),
                                           (A128r[:], "A128"), (A128i[:], "A128"), (fr[:], "f"), (fi[:], "f"), (sc["rho"][:], "ss_rho"), (sc["th"][:], "ss_th")]):
                V(lambda e, j=j, ap=ap: e.tensor_copy(dbg[:, 32 + 16 * j:48 + 16 * j], ap), [key], ["dbg"])
            V(lambda e: e.tensor_copy(dbg[:, 512:1024], ub[:]), ["ub"], ["dbg"])
            S.dma(lambda e: e.dma_start(out=out[0:128, :], in_=dbg[:]), reads=["dbg"], writes=["out"])
            S.emit()
            return nc

        qkv_sb = SB(mix, "qkv_sb", [128, 768]); sqt = SB(mix, "sqt", [128, 640]); s10 = SB(mix, "s10", [128, 10]); s10b = SB(mix, "s10b", [128, 10])
        qkn = SB(mix, "qkn", [128, 640], BF16); qT = SB(mix, "qT", [128, 4, 128], BF16)
        kTs = [SB(mix, f"kT{i}", [128, 128], BF16) for i in range(3)]; vss = [SB(mix, f"vs{i}", [128, 128], BF16) for i in range(3)]
        kTm = SB(mix, "kTm", [128, 128], BF16); vm = SB(mix, "vm", [128, 128], BF16)
        pex = SB(mix, "pex", [128, 512], BF16); PT = [SB(mix, f"PT{i}", [128, 4, 128], BF16) for i in range(2)]; PTm = SB(mix, "PTm", [16, 512], BF16)
        den_sb = SB(mix, "den_sb", [64, 4, 128]); oT = SB(mix, "oT", [64, 8, 128], BF16); osq = SB(mix, "osq", [64, 4, 128], BF16)
        uTb = SB(mix, "uTb", [128, 4, 128], BF16)
        t1 = SB(mix, "t1", [128, 512]); t2 = SB(mix, "t2", [128, 512]); btr = SB(mix, "btr", [128, 512]); bti = SB(mix, "bti", [128, 512])
        str_ = SB(mix, "str", [128, 512]); sti = SB(mix, "sti", [128, 512]); t3 = SB(mix, "t3", [128, 512]); t4 = SB(mix, "t4", [128, 512])
        sre = SB(mix, "sre", [128, 512], BF16); sim = SB(mix, "sim", [128, 512], BF16)
        injr = SB(mix, "injr", [128, 16]); inji = SB(mix, "inji", [128, 16]); stlr = SB(mix, "stlr", [128, 16]); stli = SB(mix, "stli", [128, 16])
        ysb = SB(mix, "ysb", [128, 4, 128]); zb = SB(mix, "zb", [128, 4, 128], BF16); sg = SB(mix, "sg", [128, 4, 128], BF16)
        zz = SB(mix, "zz", [128, 4, 128], BF16); zsq = SB(mix, "zsq", [128, 4, 128], BF16)
        rs = SB(mix, "rs", [128, 4]); hm = SB(mix, "hm", [128, 1024]); h2b = SB(mix, "h2b", [128, 1024], BF16); st2 = SB(mix, "st2", [128, 4])

        def bcg(ap2, a, n):
            return ap2.rearrange("p (a o) -> p a o", o=1).to_broadcast([ap2.shape[0], a, n])

        def kv_part(kslot, vslot, with_q):
            c0 = 0 if with_q else 512
            g0 = c0 // 64
            if with_q:
                for k in range(8):
                    M(lambda e, k=k: e.matmul(psA[:, 0:512], lhsT=nT[:, k, :], rhs=win[:, k, 0:512], start=(k == 0), stop=(k == 7)), ["nT", "win"], ["psAq"])
                A(lambda e: e.copy(qkv_sb[:, 0:512], psA[:, 0:512]), ["psAq"], ["qkv_q"])
                if "q1" in off:
                    raise StopBuild()
            for k in range(8):
                M(lambda e, k=k: e.matmul(psA[:, 512:768], lhsT=nT[:, k, :], rhs=win[:, k, 512:768], start=(k == 0), stop=(k == 7)), ["nT", "win"], ["psAk"])
            A(lambda e: e.copy(qkv_sb[:, 512:768], psA[:, 512:768]), ["psAk"], ["qkv_k"])
            rk = ["qkv_q", "qkv_k"] if with_q else ["qkv_k"]
            V(lambda e: e.tensor_tensor(sqt[:, c0:640], qkv_sb[:, c0:640], qkv_sb[:, c0:640], ALU.mult), rk, ["sqt"])
            V(lambda e: e.tensor_reduce(s10[:, g0:10], sqt[:, c0:640].rearrange("p (a b) -> p a b", b=64), AX.X, ALU.add), ["sqt"], ["s10"])
            V(lambda e: e.tensor_scalar(s10[:, g0:10], s10[:, g0:10], 1.0 / 64, EPS, ALU.mult, ALU.add), ["s10"], ["s10"])
            A(lambda e: e.activation(out=s10b[:, g0:10], in_=s10[:, g0:10], func=AF.Sqrt), ["s10"], ["s10b"])
            V(lambda e: e.reciprocal(s10b[:, g0:10], s10b[:, g0:10]), ["s10b"], ["s10b"])
            V(lambda e: e.tensor_tensor(qkn[:, c0:640].rearrange("p (a b) -> p a b", b=64), qkv_sb[:, c0:640].rearrange("p (a b) -> p a b", b=64),
                                        bcg(s10b[:, g0:10], 10 - g0, 64), ALU.mult), rk + ["s10b"], ["qkn"])
            if with_q and "q2" in off:
                raise StopBuild()
            M(lambda e: e.transpose(ptr[:, 512:640], qkn[:, 512:640], idb[:]), ["qkn", "idb"], ["ptr"])
            A(lambda e: e.copy(kslot[0][:], ptr[:, 512:640]), ["ptr"], [kslot[1]])
            A(lambda e: e.copy(vslot[0][:], qkv_sb[:, 640:768]), ["qkv_k"], [vslot[1]])
            if with_q:
                for m in range(4):
                    M(lambda e, m=m: e.transpose(ptr[:, m * 128:(m + 1) * 128], qkn[:, m * 128:(m + 1) * 128], idb[:]), ["qkn", "idb"], ["ptr"])
                if "q3" in off:
                    raise StopBuild()
                V(lambda e: e.tensor_scalar(qT[:].rearrange("p a b -> p (a b)"), ptr[:, 0:512], cqk[:, 0:1], None, ALU.mult), ["ptr", "cqk"], ["qT"])

        def attention(prev, cur, pmask):
            first = [True]
            for g in range(2):
                ps_ = slice(64 * g, 64 * g + 64)
                for b, (kk, vv, msk, mkey) in enumerate(((prev[0], prev[1], pmask[0], pmask[1]), (cur[0], cur[1], mcur, "mcur"))):
                    M(lambda e, b=b, kk=kk: e.matmul(pb[b][:], lhsT=kk[0][ps_, :], rhs=qT[ps_, :, :].rearrange("p a b -> p (a b)"), start=True, stop=True), [kk[1], "qT"], [f"pb{b}"])
                    A(lambda e, b=b: e.activation(out=pex[:], in_=pb[b][:], func=AF.Exp), [f"pb{b}"], ["pex"])
                    V(lambda e, b=b, msk=msk: e.tensor_tensor(PT[b][:], pex[:].rearrange("p (a b) -> p a b", a=4),
                                                            msk[:].rearrange("p (o q) -> p o q", o=1).to_broadcast([128, 4, 128]), ALU.mult), ["pex", mkey], [f"PT{b}"])
                M(lambda e: e.matmul(pb[2][0:16, :], lhsT=kTm[ps_, 0:16], rhs=qT[ps_, :, :].rearrange("p a b -> p (a b)"), start=True, stop=True), ["kTm", "qT"], ["pb2"])
                A(lambda e: e.activation(out=PTm[:], in_=pb[2][0:16, :], func=AF.Exp), ["pb2"], ["PTm"])
                cs_ = slice(64 * g, 64 * g + 64)
                PT0 = PT[0][:].rearrange("p a b -> p (a b)"); PT1 = PT[1][:].rearrange("p a b -> p (a b)")
                M(lambda e: e.matmul(pb[3][0:64, :], lhsT=prev[1][0][:, cs_], rhs=PT0, start=True, stop=False), [prev[1][1], "PT0"], ["pb3"])
                M(lambda e: e.matmul(pb[3][0:64, :], lhsT=cur[1][0][:, cs_], rhs=PT1, start=False, stop=False), [cur[1][1], "PT1"], ["pb3"])
                M(lambda e: e.matmul(pb[3][0:64, :], lhsT=vm[0:16, cs_], rhs=PTm[:], start=False, stop=True), ["vm", "PTm"], ["pb3"])
                M(lambda e: e.matmul(pb[4][0:64, :], lhsT=ones[:, 0:64], rhs=PT0, start=True, stop=False), ["ones", "PT0"], ["pb4"])
                M(lambda e: e.matmul(pb[4][0:64, :], lhsT=ones[:, 0:64], rhs=PT1, start=False, stop=False), ["ones", "PT1"], ["pb4"])
                M(lambda e: e.matmul(pb[4][0:64, :], lhsT=ones[0:16, 0:64], rhs=PTm[:], start=False, stop=True), ["ones", "PTm"], ["pb4"])
                for j in range(4):
                    h = 4 * g + j
                    A(lambda e, j=j, h=h: e.activation(out=den_sb[:, j, :], in_=pb[4][0:64, j * 128:(j + 1) * 128], func=AF.Identity, bias=esink[:, h:h + 1]), ["pb4", "esink"], ["den"])
                V(lambda e: e.reciprocal(den_sb[:], den_sb[:]), ["den"], ["den"])
                V(lambda e, g=g: e.tensor_tensor(oT[:, 4 * g:4 * g + 4, :], pb[3][0:64, :].rearrange("p (a b) -> p a b", a=4), den_sb[:], ALU.mult), ["pb3", "den"], ["oT"])
                V(lambda e, g=g: e.tensor_tensor(osq[:], oT[:, 4 * g:4 * g + 4, :], oT[:, 4 * g:4 * g + 4, :], ALU.mult), ["oT"], ["osq"])
                for j in range(4):
                    M(lambda e, j=j, g=g: e.matmul(psA[:, 768:769], lhsT=osq[:, j, :], rhs=ones[0:64, 0:1], start=(g == 0 and j == 0), stop=(g == 1 and j == 3)), ["osq", "ones"], ["pssa"])

        def ssm_tile():
            for c in range(4):
                for k in range(8):
                    M(lambda e, c=c, k=k: e.matmul(pb[0][:, c * 128:(c + 1) * 128], lhsT=win[:, k, 768 + 128 * c:768 + 128 * (c + 1)], rhs=nT[:, k, :], start=(k == 0), stop=(k == 7)), ["nT", "win"], ["pb0"])
            A(lambda e: e.copy(uTb[:].rearrange("p a b -> p (a b)"), pb[0][:]), ["pb0"], ["uTb"])
            cmul(injr[:], inji[:], sc["abr"][:], sc["abi"][:], Sr[:], Si[:], c1[:], c2[:], ["ss_abr", "ss_abi", "S", "c"], ["inj", "c"])
            for c in range(4):
                isl = slice(4 * c, 4 * c + 4)
                Erc = Er[:, isl, :].rearrange("p a b -> p (a b)"); Eic = Ei[:, isl, :].rearrange("p a b -> p (a b)")
                Rc = Rho0[:, isl, :].rearrange("p a b -> p (a b)")
                for ii in range(4):
                    i = 4 * c + ii
                    M(lambda e, i=i, ii=ii, c=c: e.matmul(pb[1][:, ii * 128:(ii + 1) * 128], lhsT=BTr[:, i, :], rhs=uTb[:, c, :], start=True, stop=True), ["BT", "uTb"], ["pb1"])
                    M(lambda e, i=i, ii=ii, c=c: e.matmul(pb[2][:, ii * 128:(ii + 1) * 128], lhsT=BTi[:, i, :], rhs=uTb[:, c, :], start=True, stop=True), ["BT", "uTb"], ["pb2"])
                V(lambda e: e.tensor_tensor(t1[:], pb[1][:], Erc, ALU.mult), ["pb1", "Er"], ["t1"])
                V(lambda e: e.tensor_tensor(t2[:], pb[2][:], Eic, ALU.mult), ["pb2", "Ei"], ["t2"])
                V(lambda e: e.tensor_tensor(btr[:], t1[:], t2[:], ALU.add), ["t1", "t2"], ["btr"])
                V(lambda e: e.tensor_tensor(t1[:], pb[2][:], Erc, ALU.mult), ["pb2", "Er", "btr"], ["t1"])
                V(lambda e: e.tensor_tensor(t2[:], pb[1][:], Eic, ALU.mult), ["pb1", "Ei", "btr"], ["t2"])
                V(lambda e: e.tensor_tensor(bti[:], t1[:], t2[:], ALU.subtract), ["t1", "t2"], ["bti"])
                b3r = btr[:].rearrange("p (a b) -> p a b", a=4); b3i = bti[:].rearrange("p (a b) -> p a b", a=4)
                V(lambda e, isl=isl: e.tensor_tensor(b3r[:, :, 0], b3r[:, :, 0], injr[:, isl], ALU.add), ["btr", "inj"], ["btr"])
                V(lambda e, isl=isl: e.tensor_tensor(b3i[:, :, 0], b3i[:, :, 0], inji[:, isl], ALU.add), ["bti", "inj"], ["bti"])
                V(lambda e: e.tensor_tensor_scan(str_[:], Rc, btr[:], 0.0, ALU.mult, ALU.add), ["Rho0", "btr"], ["str"])
                V(lambda e: e.tensor_tensor_scan(sti[:], Rc, bti[:], 0.0, ALU.mult, ALU.add), ["Rho0", "bti"], ["sti"])
                s3r = str_[:].rearrange("p (a b) -> p a b", a=4); s3i = sti[:].rearrange("p (a b) -> p a b", a=4)
                V(lambda e, isl=isl: e.tensor_copy(stlr[:, isl], s3r[:, :, 127]), ["str"], ["stl"])
                V(lambda e, isl=isl: e.tensor_copy(stli[:, isl], s3i[:, :, 127]), ["sti"], ["stl"])
                G(lambda e: e.tensor_tensor(t3[:], str_[:], Erc, ALU.mult), ["str", "Er"], ["t3"])
                G(lambda e: e.tensor_tensor(t4[:], sti[:], Eic, ALU.mult), ["sti", "Ei"], ["t4"])
                G(lambda e: e.tensor_tensor(sre[:], t3[:], t4[:], ALU.subtract), ["t3", "t4"], ["sre"])
                G(lambda e: e.tensor_tensor(t3[:], str_[:], Eic, ALU.mult), ["str", "Ei", "sre"], ["t3"])
                G(lambda e: e.tensor_tensor(t4[:], sti[:], Erc, ALU.mult), ["sti", "Er", "sre"], ["t4"])
                G(lambda e: e.tensor_tensor(sim[:], t3[:], t4[:], ALU.add), ["t3", "t4"], ["sim"])
                for ii in range(4):
                    i = 4 * c + ii
                    M(lambda e, i=i, ii=ii, c=c: e.matmul(pb[3][:, c * 128:(c + 1) * 128], lhsT=Cbr[:, i, :], rhs=sre[:, ii * 128:(ii + 1) * 128], start=(ii == 0), stop=False), ["Cb", "sre"], ["pb3"])
                    M(lambda e, i=i, ii=ii, c=c: e.matmul(pb[3][:, c * 128:(c + 1) * 128], lhsT=Cbi[:, i, :], rhs=sim[:, ii * 128:(ii + 1) * 128], start=False, stop=(ii == 3)), ["Cb", "sim"], ["pb3"])
            cmul(Sr[:], Si[:], stlr[:], stli[:], E127r[:], E127i[:], c1[:], c2[:], ["stl", "E127", "c", "inj"], ["S", "c"])
            for c in range(4):
                V(lambda e, c=c: e.scalar_tensor_tensor(ysb[:, c, :], uTb[:, c, :], small["ssm_d"][:, c:c + 1], pb[3][:, c * 128:(c + 1) * 128], ALU.mult, ALU.add), ["uTb", "c_ssm_d", "pb3"], ["ysb"])
            A(lambda e: e.activation(out=zb[:], in_=ysb[:], func=AF.Gelu), ["ysb"], ["zb"])
            for cp in range(4):
                for c in range(4):
                    M(lambda e, c=c, cp=cp: e.matmul(pb[4][:, cp * 128:(cp + 1) * 128], lhsT=glu[:, c, cp * 128:(cp + 1) * 128], rhs=zb[:, c, :], start=(c == 0), stop=(c == 3)), ["glu", "zb"], ["pb4"])
            for cp in range(4):
                A(lambda e, cp=cp: e.activation(out=sg[:, cp, :], in_=pb[4][:, cp * 128:(cp + 1) * 128], func=AF.Sigmoid, bias=small["glu_b"][:, cp:cp + 1]), ["pb4", "c_glu_b"], ["sg"])
            V(lambda e: e.tensor_tensor(zz[:], zb[:], sg[:], ALU.mult), ["zb", "sg"], ["zz"])
            V(lambda e: e.tensor_tensor(zsq[:], zz[:], zz[:], ALU.mult), ["zz"], ["zsq"])
            for c in range(4):
                M(lambda e, c=c: e.matmul(psA[:, 769:770], lhsT=zsq[:, c, :], rhs=ones[:, 0:1], start=(c == 0), stop=(c == 3)), ["zsq", "ones"], ["psss"])

        def out_proj(sl, t):
            for half in range(2):
                hs = slice(half * 512, (half + 1) * 512)
                for h in range(8):
                    M(lambda e, h=h, half=half, hs=hs: e.matmul(pb[half][:], lhsT=oT[:, h, :], rhs=woa[:, h, hs], start=(h == 0), stop=(h == 7)), ["oT", "woa"], [f"pb{half}"])
                for c in range(4):
                    M(lambda e, c=c, half=half, hs=hs: e.matmul(pb[2 + half][:], lhsT=zz[:, c, :], rhs=wos[:, c, hs], start=(c == 0), stop=(c == 3)), ["zz", "wos"], [f"pb{2 + half}"])
            V(lambda e: e.tensor_scalar(rs[:, 0:2], psA[:, 768:770], 1.0 / 512, EPS, ALU.mult, ALU.add), ["pssa", "psss"], ["rs"])
            A(lambda e: e.activation(out=rs[:, 2:4], in_=rs[:, 0:2], func=AF.Sqrt), ["rs"], ["rsb"])
            V(lambda e: e.reciprocal(rs[:, 0:2], rs[:, 2:4]), ["rsb", "rs"], ["rs"])
            kx = f"xt{sl}"
            for half in range(2):
                hs = slice(half * 512, (half + 1) * 512)
                V(lambda e, half=half, hs=hs: e.scalar_tensor_tensor(hm[:, hs], pb[half][:], rs[:, 0:1], xt[sl][:, hs], ALU.mult, ALU.add), [f"pb{half}", "rs", kx], ["hm"])
                V(lambda e, half=half, hs=hs: e.scalar_tensor_tensor(hm[:, hs], pb[2 + half][:], rs[:, 1:2], hm[:, hs], ALU.mult, ALU.add), [f"pb{2 + half}", "rs", "hm"], ["hm"])
            S.dma(lambda e: e.dma_start(out=out[t * 128:(t + 1) * 128, :], in_=hm[:]), reads=["hm"], writes=[f"out{t}"])
            A(lambda e: e.activation(out=junk[:], in_=hm[:], func=AF.Square, accum_out=st2[:, 0:1]), ["hm"], ["junk", "st2"])
            V(lambda e: e.tensor_scalar(st2[:, 1:2], st2[:, 0:1], 1.0 / 1024, EPS, ALU.mult, ALU.add), ["st2"], ["st2b"])
            A(lambda e: e.activation(out=st2[:, 3:4], in_=st2[:, 1:2], func=AF.Sqrt), ["st2b"], ["st2d"])
            V(lambda e: e.reciprocal(st2[:, 2:3], st2[:, 3:4]), ["st2d"], ["st2c"])
            V(lambda e: e.tensor_scalar(h2b[:], hm[:], st2[:, 2:3], None, ALU.mult), ["hm", "st2c"], ["h2b"])
            for k in range(8):
                M(lambda e, k=k: e.transpose(ptr[:, k * 128:(k + 1) * 128], h2b[:, k * 128:(k + 1) * 128], idb[:]), ["h2b", "idb"], ["ptr"])
            V(lambda e: e.tensor_tensor(h2T[:, :, t * 128:(t + 1) * 128], ptr[:].rearrange("p (a b) -> p a b", a=8), bcg(small["g2"][:], 8, 128), ALU.mult), ["ptr", "c_g2"], ["h2T"])

        if "nomain" in off:
            S.dma(lambda e: e.dma_start(out=xt[0][:], in_=xown[0:128, :]), writes=["xt0"])
            S.dma(lambda e: e.dma_start(out=out[0:128, :], in_=xt[0][:]), reads=["xt0"], writes=["out0"])
            S.emit()
            return nc
        front(xmeta)
        kv_part((kTm, "kTm"), (vm, "vm"), False)
        if "stopmeta" in off:
            S.dma(lambda e: e.dma_start(out=out[0:128, :], in_=xt[0][:]), reads=["xt0", "kTm", "vm"], writes=["out0"])
            S.emit()
            return nc
        front(xhalo)
        kv_part((kTs[2], "kT2"), (vss[2], "vs2"), False)
        if "stophalo" in off:
            S.dma(lambda e: e.dma_start(out=out[0:128, :], in_=xt[1][:]), reads=["xt1", "kT2", "vs2"], writes=["out0"])
            S.emit()
            return nc
        n_own = 1 if (stage == "mix1" or "one" in off) else NT
        for t in range(n_own):
            sl = front(xown[t * 128:(t + 1) * 128, :])
            pslot, cslot = (t + 2) % 3, t % 3
            try:
                kv_part((kTs[cslot], f"kT{cslot}"), (vss[cslot], f"vs{cslot}"), True)
            except StopBuild:
                S.dma(lambda e: e.dma_start(out=out[0:128, :], in_=xt[sl][:]), reads=[f"xt{sl}", "qkv_q", "qkv_k", "qkn", "ptr"], writes=["out0"])
                S.emit()
                return nc
            if "attn" not in off:
                attention(((kTs[pslot], f"kT{pslot}"), (vss[pslot], f"vs{pslot}")), ((kTs[cslot], f"kT{cslot}"), (vss[cslot], f"vs{cslot}")),
                          (mhalo, "mhalo") if t == 0 else (mprev, "mprev"))
            if "dumpqk" in off:
                V(lambda e: e.tensor_copy(hm[:, 0:512], qT[:].rearrange("p a b -> p (a b)")), ["qT"], ["hm"])
                V(lambda e: e.tensor_copy(hm[:, 512:640], kTs[cslot][:]), [f"kT{cslot}"], ["hm"])
                V(lambda e: e.tensor_copy(hm[:, 640:768], vss[cslot][:]), [f"vs{cslot}"], ["hm"])
                V(lambda e: e.tensor_copy(hm[:, 768:896], kTm[:]), ["kTm"], ["hm"])
                V(lambda e: e.tensor_copy(hm[:, 896:1024], vm[:]), ["vm"], ["hm"])
                S.dma(lambda e: e.dma_start(out=out[0:128, :], in_=hm[:]), reads=["hm"], writes=["out0"])
                S.emit()
                return nc
            if "dumpv" in off:
                V(lambda e: e.tensor_copy(hm[0:64, 0:512], pb[3][0:64, :]), ["pb3"], ["hm"])
                V(lambda e: e.tensor_copy(hm[0:64, 512:1024], pb[4][0:64, :]), ["pb4", "hm"], ["hm"])
                S.dma(lambda e: e.dma_start(out=out[0:64, :], in_=hm[0:64, :]), reads=["hm"], writes=["out0"])
                S.emit()
                return nc
            if "dumpp" in off:
                V(lambda e: e.tensor_copy(hm[:, 0:512], PT[1][:].rearrange("p a b -> p (a b)")), ["PT1"], ["hm"])
                V(lambda e: e.tensor_copy(hm[0:64, 512:1024], den_sb[:].rearrange("p a b -> p (a b)")), ["den"], ["hm"])
                S.dma(lambda e: e.dma_start(out=out[0:128, :], in_=hm[:]), reads=["hm"], writes=["out0"])
                S.emit()
                return nc
            if "dumpo" in off:
                V(lambda e: e.tensor_copy(hm[0:64, :], oT[:].rearrange("p a b -> p (a b)")), ["oT"], ["hm"])
                S.dma(lambda e: e.dma_start(out=out[0:64, :], in_=hm[0:64, :]), reads=["hm"], writes=["out0"])
                S.emit()
                return nc
            if "ssm" not in off:
                ssm_tile()
            if "outp" not in off:
                out_proj(sl, t)
            else:
                S.dma(lambda e, t=t, sl=sl: e.dma_start(out=out[t * 128:(t + 1) * 128, :], in_=xt[sl][:]), reads=[f"xt{sl}"], writes=[f"out{t}"])

        if "dumph2" in off:
            V(lambda e: e.tensor_copy(hm[:], h2T[:, :, 0:128]), ["h2T", "hm"], ["hm"])
            S.dma(lambda e: e.dma_start(out=out[128:256, :], in_=hm[:]), reads=["hm"], writes=["out1"])
        if stage in ("mix", "mix1"):
            S.emit()
            return nc

        S.barrier()
        mix.close()
        pe_ = top.enter_context(contextlib.ExitStack())
        pA = PS(pe_, "pA", [128, 2048]); pQ = PS(pe_, "pQ", [128, 512])
        wq = SB(pe_, "wq", [128, 8, 2048], BF16); keysT = SB(pe_, "keysT", [128, 16, 128], BF16)
        with scope() as stg:
            wst = SB(stg, "wst", [128, 2, 2048])
            for kk4 in range(4):
                load(wst[:], D["wq"][:, 2 * kk4:2 * kk4 + 2, :], "wst")
                V(lambda e, kk4=kk4: e.tensor_copy(wq[:, 2 * kk4:2 * kk4 + 2, :], wst[:]), ["wst"], ["wq"])
            kst = SB(stg, "kst", [128, 16, 128])
            load(kst[:], D["keysT"], "kst")
            V(lambda e: e.tensor_copy(keysT[:], kst[:]), ["kst"], ["keysT"])
        qTp = SB(pe_, "qTp", [128, 16, 128], BF16); scs = SB(pe_, "scs", [128, 16, 128]); sc2 = SB(pe_, "sc2", [128, 16, 128])
        v16 = SB(pe_, "v16", [128, 16, 16]); cand = SB(pe_, "cand", [128, 8, 256]); cand2 = SB(pe_, "cand2", [128, 8, 256]); s16 = SB(pe_, "s16", [128, 8, 16])
        dbgp = SB(pe_, "dbgp", [128, 1024])

        def peer_front(t):
            ts_ = slice(t * 128, (t + 1) * 128)
            for g4 in range(4):
                for j in range(4):
                    cb = 4 * g4 + j
                    for k in range(8):
                        M(lambda e, cb=cb, j=j, k=k: e.matmul(pQ[:, j * 128:(j + 1) * 128], lhsT=wq[:, k, cb * 128:(cb + 1) * 128], rhs=h2T[:, k, ts_], start=(k == 0), stop=(k == 7)), ["wq", "h2T"], ["pQ"])
                A(lambda e, g4=g4: e.copy(qTp[:, 4 * g4:4 * g4 + 4, :].rearrange("p a b -> p (a b)"), pQ[:]), ["pQ"], ["qTp"])
            for idx in range(16):
                M(lambda e, idx=idx: e.matmul(pA[:, idx * 128:(idx + 1) * 128], lhsT=qTp[:, idx, :], rhs=keysT[:, idx, :], start=True, stop=True), ["qTp", "keysT"], ["pA"])
            for q4 in range(4):
                A(lambda e, q4=q4: e.copy(scs[:, 4 * q4:4 * q4 + 4, :].rearrange("p a b -> p (a b)"), pA[:, q4 * 512:(q4 + 1) * 512]), ["pA"], ["scs"])
            for idx in range(16):
                V(lambda e, idx=idx: e.max(out=v16[:, idx, 0:8], in_=scs[:, idx, :]), ["scs"], ["v16"])
                V(lambda e, idx=idx: e.match_replace(out=sc2[:, idx, :], in_to_replace=v16[:, idx, 0:8], in_values=scs[:, idx, :], imm_value=-1e30), ["scs", "v16"], ["sc2"])
                V(lambda e, idx=idx: e.max(out=v16[:, idx, 8:16], in_=sc2[:, idx, :]), ["sc2"], ["v16"])
            v4 = v16[:].rearrange("p (h two) a -> p h two a", two=2)
            V(lambda e: e.tensor_tensor(cand[:].rearrange("p h (a b) -> p h a b", a=16),
                                        v4[:, :, 0, :].rearrange("p h (a o) -> p h a o", o=1).to_broadcast([128, 8, 16, 16]),
                                        v4[:, :, 1, :].rearrange("p h (o b) -> p h o b", o=1).to_broadcast([128, 8, 16, 16]), ALU.add), ["v16"], ["cand"])
            for h in range(8):
                V(lambda e, h=h: e.max(out=s16[:, h, 0:8], in_=cand[:, h, :]), ["cand"], ["s16"])
                V(lambda e, h=h: e.match_replace(out=cand2[:, h, :], in_to_replace=s16[:, h, 0:8], in_values=cand[:, h, :], imm_value=-1e30), ["cand", "s16"], ["cand2"])
                V(lambda e, h=h: e.max(out=s16[:, h, 8:16], in_=cand2[:, h, :]), ["cand2"], ["s16"])

        if stage == "peerfront":
            peer_front(0)
            V(lambda e: e.memset(dbgp[:], 0.0), [], ["dbgp"])
            V(lambda e: e.tensor_copy(dbgp[:, 0:256], v16[:].rearrange("p a b -> p (a b)")), ["v16"], ["dbgp"])
            V(lambda e: e.tensor_copy(dbgp[:, 256:384], s16[:].rearrange("p a b -> p (a b)")), ["s16"], ["dbgp"])
            V(lambda e: e.tensor_copy(dbgp[:, 512:1024], scs[:, 0:4, :].rearrange("p a b -> p (a b)")), ["scs"], ["dbgp"])
            S.dma(lambda e: e.dma_start(out=out[256:384, :], in_=dbgp[:]), reads=["dbgp"], writes=["out2"])
            S.emit()
            return nc

        n_i = 128
        with scope() as stg:
            NS = 4
            cst = [SB(stg, f"cst{i}", [128, 1024]) for i in range(NS)]; cbf = [SB(stg, f"cbf{i}", [128, 1024], BF16) for i in range(NS)]
            cnt = 0
            for (src, dst, nm) in ((D["puT"], UTs, "UTs"), (D["pv"], Vs, "Vs")):
                for i in range(n_i):
                    sl = cnt % NS
                    S.dma(lambda e, i=i, sl=sl, src=src: e.dma_start(out=cst[sl][:], in_=src[i]), writes=[f"cst{sl}"])
                    if cnt % 3 == 0:
                        A(lambda e, sl=sl: e.copy(cbf[sl][:], cst[sl][:]), [f"cst{sl}"], [f"cbf{sl}"])
                    elif cnt % 3 == 1:
                        V(lambda e, sl=sl: e.tensor_copy(cbf[sl][:], cst[sl][:]), [f"cst{sl}"], [f"cbf{sl}"])
                    else:
                        G(lambda e, sl=sl: e.tensor_copy(cbf[sl][:], cst[sl][:]), [f"cst{sl}"], [f"cbf{sl}"])
                    S.dma(lambda e, i=i, sl=sl, dst=dst: e.dma_start(out=dst[i], in_=cbf[sl][:]), reads=[f"cbf{sl}"], writes=[f"{nm}{i}"], eng="act" if False else "sp")
                    cnt += 1

        iota_i = SB(pe_, "iota_i", [128, 128]); bm = SB(pe_, "bm", [128, 8])
        load(iota_i[:], D["iota_i"], "iota_i"); load(bm[:], D["blkmask"], "bm")
        pG2 = PS(pe_, "pG2", [128, 512]); pZ = PS(pe_, "pZ", [128, 256])
        ixu = SB(pe_, "ixu", [128, 16, 16], U32); ixf = SB(pe_, "ixf", [128, 16, 16])
        i1c = SB(pe_, "i1c", [128, 128]); i2c = SB(pe_, "i2c", [128, 128]); i1T = SB(pe_, "i1T", [128, 128]); i2T = SB(pe_, "i2T", [128, 128])
        zs = SB(pe_, "zs", [128, 8]); wT = SB(pe_, "wT", [128, 128, 16], BF16)
        NB = 16
        P1b = SB(pe_, "P1b", [128, NB, 128], BF16); P2b = SB(pe_, "P2b", [128, NB, 128], BF16); Wb = SB(pe_, "Wb", [128, NB, 128], BF16)
        Tsb = SB(pe_, "Tsb", [128, 4, 128], BF16); Gt = SB(pe_, "Gt", [128, 128, 128], BF16)
        ND = 4
        ubuf = [SB(pe_, f"ubuf{i}", [128, 1024], BF16) for i in range(ND)]; vbuf = [SB(pe_, f"vbuf{i}", [128, 1024], BF16) for i in range(ND)]
        zg = [SB(pe_, f"zg{i}", [128, 128], BF16) for i in range(2)]; AT = [SB(pe_, f"AT{i}", [128, 128], BF16) for i in range(2)]
        hmt = SB(pe_, "hmt", [128, 1024]); res = SB(pe_, "res", [128, 1024])

        def b3(ap2, a, n):
            return ap2.rearrange("p (a o) -> p a o", o=1).to_broadcast([128, a, n])

        def gate_build(t):
            for idx in range(16):
                V(lambda e, idx=idx: e.max_index(out=ixu[:, idx, 0:8], in_max=v16[:, idx, 0:8], in_values=scs[:, idx, :]), ["v16", "scs"], ["ixu"])
                V(lambda e, idx=idx: e.max_index(out=ixu[:, idx, 8:16], in_max=v16[:, idx, 8:16], in_values=sc2[:, idx, :]), ["v16", "sc2"], ["ixu"])
            V(lambda e: e.tensor_copy(ixf[:], ixu[:]), ["ixu"], ["ixf"])
            ix4 = ixf[:].rearrange("p (h two) a -> p h two a", two=2)
            V(lambda e: e.tensor_copy(i1c[:].rearrange("p (h a) -> p h a", h=8), ix4[:, :, 0, :]), ["ixf"], ["i1c"])
            V(lambda e: e.tensor_copy(i2c[:].rearrange("p (h a) -> p h a", h=8), ix4[:, :, 1, :]), ["ixf"], ["i2c"])
            wx = scs[:].rearrange("p a b -> p (a b)").rearrange("p (h c) -> p h c", h=8)
            mk = sc2[:].rearrange("p a b -> p (a b)").rearrange("p (h c) -> p h c", h=8)
            V(lambda e: e.tensor_tensor(wx, cand[:], b3(s16[:, :, 0], 8, 256), ALU.subtract), ["cand", "s16", "ixu"], ["scs"])
            A(lambda e: e.activation(out=wx, in_=wx, func=AF.Exp), ["scs"], ["scs"])
            V(lambda e: e.tensor_tensor(mk, cand[:], b3(s16[:, :, 15], 8, 256), ALU.is_ge), ["cand", "s16", "ixu"], ["sc2"])
            V(lambda e: e.tensor_tensor(wx, wx, mk, ALU.mult), ["scs", "sc2"], ["scs"])
            V(lambda e: e.tensor_reduce(zs[:], wx, AX.X, ALU.add), ["scs"], ["zs"])
            V(lambda e: e.reciprocal(zs[:], zs[:]), ["zs"], ["zs"])
            V(lambda e: e.tensor_tensor(wx, wx, b3(zs[:], 8, 256), ALU.mult), ["scs", "zs"], ["scs"])
            wperm = sc2[:].rearrange("p a b -> p (a b)").rearrange("p (b h a) -> p b h a", b=16, h=8)
            V(lambda e: e.tensor_copy(wperm, scs[:].rearrange("p a b -> p (a b)").rearrange("p (h a b) -> p b h a", h=8, a=16)), ["scs", "sc2"], ["sc2"])
            w2d = sc2[:].rearrange("p a b -> p (a b)")
            for b in range(16):
                M(lambda e, b=b: e.transpose(pA[:, b * 128:(b + 1) * 128], w2d[:, b * 128:(b + 1) * 128], idf[:]), ["sc2", "idf"], ["pA"])
            V(lambda e: e.tensor_copy(wT[:].rearrange("p t b -> p b t"), pA[:].rearrange("p (b t) -> p b t", b=16)), ["pA"], ["wT"])
            M(lambda e: e.transpose(pQ[:, 0:128], i1c[:], idf[:]), ["i1c", "idf"], ["pQ"])
            M(lambda e: e.transpose(pQ[:, 128:256], i2c[:], idf[:]), ["i2c", "idf"], ["pQ"])
            V(lambda e: e.tensor_copy(i1T[:], pQ[:, 0:128]), ["pQ"], ["i1T"])
            V(lambda e: e.tensor_copy(i2T[:], pQ[:, 128:256]), ["pQ"], ["i2T"])
            for nb in range(128 // NB):
                tsl = slice(nb * NB, (nb + 1) * NB)
                io3 = iota_i[:].rearrange("p (o i) -> p o i", o=1).to_broadcast([128, NB, 128])
                V(lambda e, tsl=tsl: e.tensor_tensor(P1b[:], io3, b3(i1T[:, tsl], NB, 128), ALU.is_equal), ["iota_i", "i1T", "P1b"], ["P1b"])
                V(lambda e, tsl=tsl: e.tensor_tensor(P2b[:], io3, b3(i2T[:, tsl], NB, 128), ALU.is_equal), ["iota_i", "i2T", "P2b"], ["P2b"])
                V(lambda e, tsl=tsl: e.tensor_tensor(Wb[:].rearrange("p t (h b) -> p t h b", h=8),
                                                      wT[:, tsl, :].rearrange("p t (o b) -> p t o b", o=1).to_broadcast([128, NB, 8, 16]),
                                                      bm[:].rearrange("p (o h q) -> p o h q", o=1, q=1).to_broadcast([128, NB, 8, 16]), ALU.mult), ["wT", "bm", "Wb"], ["Wb"])
                for q4 in range(NB // 4):
                    for j in range(4):
                        tk = q4 * 4 + j
                        M(lambda e, tk=tk, j=j: e.matmul(pQ[:, j * 128:(j + 1) * 128], lhsT=Wb[:, tk, :], rhs=P1b[:, tk, :], start=True, stop=True), ["Wb", "P1b"], ["pQ"])
                    A(lambda e: e.copy(Tsb[:].rearrange("p a b -> p (a b)"), pQ[:]), ["pQ"], ["Tsb"])
                    for j in range(4):
                        tk = q4 * 4 + j
                        M(lambda e, tk=tk, j=j: e.matmul(pG2[:, j * 128:(j + 1) * 128], lhsT=P2b[:, tk, :], rhs=Tsb[:, j, :], start=True, stop=True), ["P2b", "Tsb"], ["pG2"])
                    t0 = nb * NB + q4 * 4
                    V(lambda e, t0=t0: e.tensor_copy(Gt[:, t0:t0 + 4, :].rearrange("p a b -> p (a b)"), pG2[:]), ["pG2"], ["Gt"])

        def dense_block(t):
            ts_ = slice(t * 128, (t + 1) * 128)
            for i in range(n_i):
                sl = i % 2; ds_ = i % ND
                S.dma(lambda e, i=i, ds_=ds_: e.dma_start(out=ubuf[ds_][:], in_=UTs[i]), reads=[f"UTs{i}"], writes=[f"ubuf{ds_}"])
                S.dma(lambda e, i=i, ds_=ds_: e.dma_start(out=vbuf[ds_][:], in_=Vs[i]), reads=[f"Vs{i}"], writes=[f"vbuf{ds_}"], eng="pool")
                for k in range(8):
                    M(lambda e, k=k, sl=sl, ds_=ds_: e.matmul(pZ[:, sl * 128:(sl + 1) * 128], lhsT=ubuf[ds_][:, k * 128:(k + 1) * 128], rhs=h2T[:, k, ts_], start=(k == 0), stop=(k == 7)), [f"ubuf{ds_}", "h2T"], [f"pZ{sl}"])
                A(lambda e, sl=sl: e.activation(out=zg[sl][:], in_=pZ[:, sl * 128:(sl + 1) * 128], func=AF.Gelu), [f"pZ{sl}"], [f"zg{sl}"])
                V(lambda e, sl=sl, i=i: e.tensor_tensor(AT[sl][:], zg[sl][:], Gt[:, :, i], ALU.mult), [f"zg{sl}", "Gt"], [f"AT{sl}"])
                for half in range(2):
                    M(lambda e, half=half, sl=sl, ds_=ds_, i=i: e.matmul(pA[:, half * 512:(half + 1) * 512], lhsT=AT[sl][:], rhs=vbuf[ds_][:, half * 512:(half + 1) * 512], start=(i == 0), stop=(i == n_i - 1)), [f"AT{sl}", f"vbuf{ds_}"], ["pA"])
            S.dma(lambda e: e.dma_start(out=hmt[:], in_=out[t * 128:(t + 1) * 128, :]), reads=[f"out{t}"], writes=["hmt"])
            for half in range(2):
                hs = slice(half * 512, (half + 1) * 512)
                V(lambda e, hs=hs: e.tensor_tensor(res[:, hs], hmt[:, hs], pA[:, hs], ALU.add), ["hmt", "pA"], ["res"])
            S.dma(lambda e: e.dma_start(out=out[t * 128:(t + 1) * 128, :], in_=res[:]), reads=["res"], writes=[f"out{t}"])

        if "keephm" in off:
            S.dma(lambda e: e.dma_start(out=hmt[:], in_=out[0:128, :]), reads=["out0"], writes=["hmt"])
            S.dma(lambda e: e.dma_start(out=out[128:256, :], in_=hmt[:]), reads=["hmt"], writes=["out1"])
        for t in range(n_own):
            peer_front(t)
            gate_build(t)
            dense_block(t)
        S.emit()
        return nc


def _host_inputs(inp):
    f = np.float32
    x = np.ascontiguousarray(inp["x"][0]); meta = inp["meta_tokens"]
    common = {}
    common["ident"] = np.eye(128, dtype=f)
    kq = np.arange(128)
    common["mask_cur"] = (kq[:, None] <= kq[None, :]).astype(f)
    common["xmeta"] = np.concatenate([meta, np.zeros((112, 1024), f)], 0)
    w_in = inp["w_in"][0]
    qperm = np.concatenate([np.arange(64) + 64 * h for h in (0, 4, 1, 5, 2, 6, 3, 7)])
    w_in = np.concatenate([w_in[:, :512][:, qperm], w_in[:, 512:]], 1)
    common["w_in"] = np.ascontiguousarray(w_in.reshape(8, 128, 1280).transpose(1, 0, 2))
    common["mask_prev"] = (kq[:, None] > kq[None, :]).astype(f)
    common["g1"] = np.ascontiguousarray(inp["norm1_g"][0].reshape(8, 128).T)
    common["gq2"] = np.tile(inp["q_norm_g"][0], 2).reshape(128, 1).astype(f)
    common["gk2"] = np.tile(inp["k_norm_g"][0], 2).reshape(128, 1).astype(f)
    common["sinks"] = np.ascontiguousarray(np.broadcast_to(inp["attn_sinks"][0][None, :], (64, 8))).astype(f)

    def state_layout(a):
        return np.ascontiguousarray(a.reshape(16, 2, 64).transpose(1, 2, 0).reshape(128, 16))

    def row_layout(a):
        return np.ascontiguousarray(np.broadcast_to(a.reshape(1, 2048), (128, 2048)))
    ldt64 = np.ascontiguousarray(np.broadcast_to(inp["ssm_log_dt"][0][:, None], (32, 64)))
    for nm, a in (("a_re", inp["ssm_a_re"][0]), ("a_im", inp["ssm_a_im"][0]), ("ldt", ldt64)):
        common[nm + "_s"] = state_layout(a); common[nm + "_r"] = row_layout(a)
    for nm, B in (("re", inp["ssm_b_re"][0]), ("im", inp["ssm_b_im"][0])):
        bT = np.zeros((128, 16, 128), f); bs = np.zeros((128, 16, 32), f)
        for g in range(32):
            i, hh = g // 2, g % 2
            bT[(g % 8) * 16:(g % 8) * 16 + 16, i, hh * 64:hh * 64 + 64] = B[g].T
            bs[hh * 64:hh * 64 + 64, i, hh * 16:hh * 16 + 16] = B[g]
        common["bT_" + nm] = bT; common["bs_" + nm] = bs
    for nm, C in (("re", inp["ssm_c_re"][0]), ("im", inp["ssm_c_im"][0])):
        cb = np.zeros((128, 16, 128), f)
        for g in range(32):
            i, hh = g // 2, g % 2
            cb[hh * 64:hh * 64 + 64, i, (g % 8) * 16:(g % 8) * 16 + 16] = C[g].T
        common["cb_" + nm] = cb
    common["ssm_d"] = np.ascontiguousarray(inp["ssm_d"][0].reshape(4, 128).T)
    common["glu_w"] = np.ascontiguousarray(inp["ssm_glu_w"][0].reshape(4, 128, 512).transpose(1, 0, 2))
    common["glu_b"] = np.ascontiguousarray(inp["ssm_glu_b"][0].reshape(4, 128).T)
    common["ga"] = np.ascontiguousarray(inp["attn_out_g"][0].reshape(8, 64).T)
    common["gs"] = np.ascontiguousarray(inp["ssm_out_g"][0].reshape(4, 128).T)
    common["woa"] = np.ascontiguousarray(inp["w_out"][0][:512].reshape(8, 64, 1024).transpose(1, 0, 2))
    common["wos"] = np.ascontiguousarray(inp["w_out"][0][512:].reshape(4, 128, 1024).transpose(1, 0, 2))
    common["g2"] = np.ascontiguousarray(inp["norm2_g"][0].reshape(8, 128).T)
    if inp.get("peer_w_query") is not None:
        common["wq"] = np.ascontiguousarray(inp["peer_w_query"][0].reshape(8, 128, 2048).transpose(1, 0, 2))
        sk = inp["peer_sub_keys"][0]
        common["keysT"] = np.ascontiguousarray(sk.reshape(16, 128, 128).transpose(2, 0, 1))
    if inp.get("peer_u") is not None:
        common["puT"] = np.ascontiguousarray(inp["peer_u"][0].reshape(128, 128, 8, 128).transpose(0, 3, 2, 1)).reshape(128, 128, 1024)
        common["pv"] = np.ascontiguousarray(inp["peer_v"][0].reshape(128, 128, 1024))
        common["iota_i"] = np.ascontiguousarray(np.broadcast_to(np.arange(128, dtype=f)[None, :], (128, 128)))
        common["blkmask"] = (np.arange(128)[:, None] // 16 == np.arange(8)[None, :]).astype(f)
    maps = []
    for r in range(NCORES):
        m = dict(common)
        m["xown"] = x[r * TOK:(r + 1) * TOK]
        m["xhalo"] = x[r * TOK - 128:r * TOK] if r > 0 else np.zeros((128, 1024), f)
        m["mask_halo"] = common["mask_prev"] if r > 0 else np.zeros((128, 128), f)
        pre = np.zeros((NPRE * 128, 1024), f)
        n = r * TOK
        pre[NPRE * 128 - n - 16:NPRE * 128 - n] = meta
        if n:
            pre[NPRE * 128 - n:] = x[:n]
        m["xpre"] = pre
        m["is0"] = np.full((128, 1), 1.0 if r == 0 else 0.0, f)
        maps.append(m)
    return maps


def kernel(**inputs):
    nc = build("full")
    maps = _host_inputs({k: np.asarray(v) for k, v in inputs.items()})
    res = run_bass_kernel_spmd(nc, maps, core_ids=list(range(NCORES)))
    return np.concatenate([r["out"] for r in res.results], 0)[None].astype(np.float32)
```

```python
import contextlib
import types
import numpy as np
import concourse.bass as bass
import concourse.mybir as mybir
from concourse.bass_utils import run_bass_kernel_spmd

F32 = mybir.dt.float32
BF16 = mybir.dt.bfloat16
U32 = mybir.dt.uint32
AF = mybir.ActivationFunctionType
ALU = mybir.AluOpType
AX = mybir.AxisListType

NCORES = 8
TOK = 2048
NT = 16
NPRE = 113
EPS = 1e-6
ENGS = ("pe", "act", "dve", "pool", "sp")


class StopBuild(Exception):
    pass


def _freeze(fn):
    if fn.__closure__ is None:
        return fn
    cells = []
    for c in fn.__closure__:
        try:
            cells.append(types.CellType(c.cell_contents))
        except ValueError:
            cells.append(c)
    return types.FunctionType(fn.__code__, fn.__globals__, fn.__name__, fn.__defaults__, tuple(cells))


class Sched:
    def __init__(self, nc, n_dma_streams=8):
        self.nc = nc
        self.ops = {e: [] for e in ENGS}
        self.count = {}
        self.last_w = {}
        self.readers = {}
        self.seen = {e: {} for e in ENGS}
        self.n_dma = n_dma_streams
        self.dma_rr = {e: 0 for e in ENGS}
        self.pending = {e: {} for e in ENGS}
        self.pe_run = []

    def barrier(self):
        self._flush_pe()
        for e in ENGS:
            for k, v in self.count.items():
                if self.pending[e].get(k, 0) < v:
                    self.pending[e][k] = v

    def _deps(self, eng, reads, writes):
        need = dict(self.pending[eng])
        self.pending[eng] = {}

        def add(ev):
            if ev is not None and need.get(ev[0], 0) < ev[1]:
                need[ev[0]] = ev[1]
        for b in reads:
            add(self.last_w.get(b))
        for b in writes:
            add(self.last_w.get(b))
            for ev in self.readers.get(b, ()):
                add(ev)
        waits = []
        for k, v in need.items():
            if eng == "pe" and k == ("e", "pe"):
                continue
            if self.seen[eng].get(k, 0) < v:
                self.seen[eng][k] = v
                waits.append((k, v))
        return waits

    def _record(self, ev, reads, writes):
        for b in reads:
            self.readers.setdefault(b, []).append(ev)
        for b in writes:
            self.last_w[b] = ev
            self.readers[b] = []

    def _flush_pe(self):
        if self.pe_run:
            k = ("e", "pe")
            self.count[k] = self.count.get(k, 0) + 1
            self.pe_run[-1][2] = (k, 1)
            self.pe_run = []

    def op(self, eng, fn, reads=(), writes=()):
        fn = _freeze(fn)
        if eng != "pe":
            self._flush_pe()
        waits = self._deps(eng, reads, writes)
        k = ("e", eng)
        if eng == "pe":
            v = self.count.get(k, 0) + 1
            entry = [waits, fn, None]
            self.ops[eng].append(entry)
            self.pe_run.append(entry)
            self._record((k, v), reads, writes)
            return
        v = self.count.get(k, 0) + 1
        self.count[k] = v
        self.ops[eng].append([waits, fn, (k, 1)])
        self._record((k, v), reads, writes)

    def dma(self, fn, reads=(), writes=(), eng="sp"):
        fn = _freeze(fn)
        self._flush_pe()
        waits = self._deps(eng, reads, writes)
        k = ("d", eng, self.dma_rr[eng] % self.n_dma)
        self.dma_rr[eng] += 1
        v = self.count.get(k, 0) + 16
        self.count[k] = v
        self.ops[eng].append([waits, fn, (k, 16)])
        self._record((k, v), reads, writes)

    def emit(self):
        self._flush_pe()
        nc = self.nc
        keys = sorted(self.count.keys(), key=str)
        with contextlib.ExitStack() as st:
            sems = {k: st.enter_context(nc.semaphore("s_" + "_".join(map(str, k)))) for k in keys}
            block = st.enter_context(nc.Block())
            final = [(k, self.count[k]) for k in keys]

            def run(engobj, lst, is_final):
                for waits, fn, incr in lst:
                    for wk, wv in waits:
                        engobj.wait_ge(sems[wk], wv)
                    ins_ = fn(engobj)
                    if incr is not None:
                        ins_.then_inc(sems[incr[0]], incr[1])
                if is_final:
                    for wk, wv in final:
                        engobj.wait_ge(sems[wk], wv)
            names = {"pe": "tensor", "act": "scalar", "dve": "vector", "pool": "gpsimd", "sp": "sync"}
            for e in ENGS:
                lst = self.ops[e]
                getattr(block, names[e])(lambda engobj, lst=lst, e=e: run(engobj, lst, e == "sp"))


def build(stage="full", off=()):
    nc = bass.Bass("TRN2", target_bir_lowering=False)
    S = Sched(nc)
    D = {}

    def din(name, shape, dt=F32):
        D[name] = nc.dram_tensor(name, list(shape), dt, kind="ExternalInput").ap()
        return D[name]
    xown = din("xown", [TOK, 1024]); xhalo = din("xhalo", [128, 1024]); xpre = din("xpre", [NPRE * 128, 1024])
    xmeta = din("xmeta", [128, 1024])
    din("ident", [128, 128]); din("mask_cur", [128, 128]); din("mask_prev", [128, 128]); din("mask_halo", [128, 128])
    din("w_in", [128, 8, 1280]); din("g1", [128, 8]); din("gq2", [128, 1]); din("gk2", [128, 1]); din("sinks", [64, 8])
    for nm in ("a_re", "a_im", "ldt"):
        din(nm + "_s", [128, 16]); din(nm + "_r", [128, 2048])
    din("bT_re", [128, 16, 128]); din("bT_im", [128, 16, 128]); din("bs_re", [128, 16, 32]); din("bs_im", [128, 16, 32])
    din("cb_re", [128, 16, 128]); din("cb_im", [128, 16, 128]); din("ssm_d", [128, 4]); din("glu_w", [128, 4, 512])
    din("glu_b", [128, 4]); din("ga", [64, 8]); din("gs", [128, 4]); din("woa", [64, 8, 1024]); din("wos", [128, 4, 1024])
    din("g2", [128, 8]); din("is0", [128, 1])
    if stage in ("peerfront", "full"):
        din("wq", [128, 8, 2048]); din("keysT", [128, 16, 128])
    if stage == "full":
        din("puT", [128, 128, 1024]); din("pv", [128, 128, 1024]); din("iota_i", [128, 128]); din("blkmask", [128, 8])
        UTs = nc.dram_tensor("UTs", [128, 128, 1024], BF16).ap(); Vs = nc.dram_tensor("Vs", [128, 128, 1024], BF16).ap()
    out = nc.dram_tensor("out", [TOK, 1024], F32, kind="ExternalOutput").ap()

    with contextlib.ExitStack() as top:
        def SB(st, name, shape, dt=F32):
            return st.enter_context(nc.sbuf_tensor("sb_" + name, list(shape), dt))

        def PS(st, name, shape, dt=F32):
            return st.enter_context(nc.psum_tensor("ps_" + name, list(shape), dt))

        def V(fn, r, w): S.op("dve", fn, r, w)
        def A(fn, r, w): S.op("act", fn, r, w)
        def G(fn, r, w): S.op("pool", fn, r, w)
        def M(fn, r, w): S.op("pe", fn, r, w)

        @contextlib.contextmanager
        def scope():
            with contextlib.ExitStack() as st_:
                yield st_
                S.barrier()

        def load(dst, src, key):
            S.dma(lambda e: e.dma_start(out=dst, in_=src), writes=[key])

        idf = SB(top, "idf", [128, 128]); idb = SB(top, "idb", [128, 128], BF16)
        load(idf[:], D["ident"], "idf")
        V(lambda e: e.tensor_copy(idb[:], idf[:]), ["idf"], ["idb"])
        ones = SB(top, "ones", [128, 64], BF16)
        V(lambda e: e.memset(ones[:], 1.0), [], ["ones"])
        h2T = SB(top, "h2T", [128, 8, TOK if "smallh2T" not in off else 128], BF16)

        mix = top.enter_context(contextlib.ExitStack())
        ptr = PS(mix, "ptr", [128, 1024], BF16)
        psA = PS(mix, "psA", [128, 1024])
        pb = [PS(mix, f"pb{i}", [128, 512]) for i in range(5)]

        win = SB(mix, "win", [128, 8, 1280], BF16); woa = SB(mix, "woa", [64, 8, 1024], BF16)
        wos = SB(mix, "wos", [128, 4, 1024], BF16); glu = SB(mix, "glu", [128, 4, 512], BF16)
        small = {}
        for nm, shp in (("g1", [128, 8]), ("gq2", [128, 1]), ("gk2", [128, 1]), ("sinks", [64, 8]), ("ssm_d", [128, 4]),
                        ("glu_b", [128, 4]), ("ga", [64, 8]), ("gs", [128, 4]), ("g2", [128, 8]), ("is0", [128, 1])):
            small[nm] = SB(mix, "c_" + nm, shp)
            load(small[nm][:], D[nm], "c_" + nm)
        with scope() as stg:
            s1 = SB(stg, "stg1", [128, 8, 1280])
            load(s1[:], D["w_in"], "stg1")
            for k in range(8):
                V(lambda e, k=k: e.tensor_scalar(win[:, k, :], s1[:, k, :], small["g1"][:, k:k + 1], None, ALU.mult),
                  ["stg1", "c_g1"], ["win"])
        with scope() as stg:
            s2 = SB(stg, "stg2", [64, 8, 1024]); s3 = SB(stg, "stg3", [128, 4, 1024]); s4 = SB(stg, "stg4", [128, 4, 512])
            load(s2[:], D["woa"], "stg2"); load(s3[:], D["wos"], "stg3"); load(s4[:], D["glu_w"], "stg4")
            for h in range(8):
                V(lambda e, h=h: e.tensor_scalar(woa[:, h, :], s2[:, h, :], small["ga"][:, h:h + 1], None, ALU.mult),
                  ["stg2", "c_ga"], ["woa"])
            for c in range(4):
                V(lambda e, c=c: e.tensor_scalar(wos[:, c, :], s3[:, c, :], small["gs"][:, c:c + 1], None, ALU.mult),
                  ["stg3", "c_gs"], ["wos"])
            V(lambda e: e.tensor_copy(glu[:], s4[:]), ["stg4"], ["glu"])
        cqk = SB(mix, "cqk", [128, 1]); esink = SB(mix, "esink", [64, 8])
        V(lambda e: e.tensor_tensor(cqk[:], small["gq2"][:], small["gk2"][:], ALU.mult), ["c_gq2", "c_gk2"], ["cqk"])
        V(lambda e: e.tensor_scalar(cqk[:], cqk[:], 0.125, None, ALU.mult), ["cqk"], ["cqk"])
        A(lambda e: e.activation(out=esink[:], in_=small["sinks"][:], func=AF.Exp), ["c_sinks"], ["esink"])
        mcur = SB(mix, "mcur", [128, 128], BF16); mprev = SB(mix, "mprev", [128, 128], BF16); mhalo = SB(mix, "mhalo", [128, 128], BF16)
        with scope() as stg:
            m1 = SB(stg, "m1", [128, 128]); m2 = SB(stg, "m2", [128, 128]); m3 = SB(stg, "m3", [128, 128])
            load(m1[:], D["mask_cur"], "m1"); load(m2[:], D["mask_prev"], "m2"); load(m3[:], D["mask_halo"], "m3")
            V(lambda e: e.tensor_copy(mcur[:], m1[:]), ["m1"], ["mcur"])
            V(lambda e: e.tensor_copy(mprev[:], m2[:]), ["m2"], ["mprev"])
            V(lambda e: e.tensor_copy(mhalo[:], m3[:]), ["m3"], ["mhalo"])

        TWO_PI = 2.0 * np.pi

        def ssm_scalars(st, tag, are, aim, ldt, shape, t=None):
            if t is None:
                t = {n: SB(st, f"{tag}_{n}", shape) for n in ("dt", "rho", "th", "ph", "cos", "sin", "abr", "abi", "den", "nr", "t1", "t2", "cfr", "cfi")}
                t["ri"] = SB(st, f"{tag}_ri", shape, mybir.dt.int32)
            k = lambda n: f"{tag}_{n}"
            A(lambda e: e.activation(out=t["dt"][:], in_=ldt[:], func=AF.Exp), [k("ldt")], [k("dt")])
            V(lambda e: e.tensor_tensor(t["rho"][:], are[:], t["dt"][:], ALU.mult), [k("are"), k("dt")], [k("rho")])
            A(lambda e: e.activation(out=t["rho"][:], in_=t["rho"][:], func=AF.Exp), [k("rho")], [k("rho")])
            V(lambda e: e.tensor_tensor(t["th"][:], aim[:], t["dt"][:], ALU.mult), [k("aim"), k("dt")], [k("th")])
            for nm, off in (("sin", 0.0), ("cos", np.pi / 2)):
                V(lambda e, off=off: e.tensor_scalar(t["t2"][:], t["th"][:], float(off), None, ALU.add), [k("th")], [k("t2")])
                V(lambda e: e.tensor_scalar(t["t1"][:], t["t2"][:], float(1.0 / TWO_PI), None, ALU.mult), [k("t2")], [k("t1")])
                V(lambda e: e.tensor_copy(t["ri"][:], t["t1"][:]), [k("t1")], [k("ri")])
                V(lambda e: e.tensor_copy(t["t1"][:], t["ri"][:]), [k("ri")], [k("t1")])
                V(lambda e: e.scalar_tensor_tensor(t["ph"][:], t["t1"][:], float(-TWO_PI), t["t2"][:], ALU.mult, ALU.add), [k("t1"), k("t2")], [k("ph")])
                V(lambda e: e.tensor_scalar(t["t1"][:], t["ph"][:], float(np.pi), float(-TWO_PI), ALU.is_gt, ALU.mult), [k("ph")], [k("t1")])
                V(lambda e: e.tensor_tensor(t["ph"][:], t["ph"][:], t["t1"][:], ALU.add), [k("ph"), k("t1")], [k("ph")])
                A(lambda e, nm=nm: e.activation(out=t[nm][:], in_=t["ph"][:], func=AF.Sin), [k("ph")], [k(nm)])
            V(lambda e: e.tensor_tensor(t["abr"][:], t["rho"][:], t["cos"][:], ALU.mult), [k("rho"), k("cos")], [k("abr")])
            V(lambda e: e.tensor_tensor(t["abi"][:], t["rho"][:], t["sin"][:], ALU.mult), [k("rho"), k("sin")], [k("abi")])
            V(lambda e: e.tensor_tensor(t["den"][:], are[:], are[:], ALU.mult), [k("are")], [k("den")])
            V(lambda e: e.tensor_tensor(t["t1"][:], aim[:], aim[:], ALU.mult), [k("aim")], [k("t1")])
            V(lambda e: e.tensor_tensor(t["den"][:], t["den"][:], t["t1"][:], ALU.add), [k("den"), k("t1")], [k("den")])
            V(lambda e: e.reciprocal(t["den"][:], t["den"][:]), [k("den")], [k("den")])
            V(lambda e: e.tensor_scalar(t["nr"][:], t["abr"][:], -1.0, None, ALU.add), [k("abr")], [k("nr")])
            V(lambda e: e.tensor_tensor(t["t1"][:], t["nr"][:], are[:], ALU.mult), [k("nr"), k("are")], [k("t1")])
            V(lambda e: e.tensor_tensor(t["t2"][:], t["abi"][:], aim[:], ALU.mult), [k("abi"), k("aim")], [k("t2")])
            V(lambda e: e.tensor_tensor(t["t1"][:], t["t1"][:], t["t2"][:], ALU.add), [k("t1"), k("t2")], [k("t1")])
            V(lambda e: e.tensor_tensor(t["cfr"][:], t["t1"][:], t["den"][:], ALU.mult), [k("t1"), k("den")], [k("cfr")])
            V(lambda e: e.tensor_tensor(t["t1"][:], t["abi"][:], are[:], ALU.mult), [k("abi"), k("are")], [k("t1")])
            V(lambda e: e.tensor_tensor(t["t2"][:], t["nr"][:], aim[:], ALU.mult), [k("nr"), k("aim")], [k("t2")])
            V(lambda e: e.tensor_tensor(t["t1"][:], t["t1"][:], t["t2"][:], ALU.subtract), [k("t1"), k("t2")], [k("t1")])
            V(lambda e: e.tensor_tensor(t["cfi"][:], t["t1"][:], t["den"][:], ALU.mult), [k("t1"), k("den")], [k("cfi")])
            return t

        def cmul(dst_r, dst_i, ar, ai, br, bi, t1, t2, rk, wk):
            V(lambda e: e.tensor_tensor(t1, ar, br, ALU.mult), rk, wk)
            V(lambda e: e.tensor_tensor(t2, ai, bi, ALU.mult), rk, wk)
            V(lambda e: e.tensor_tensor(dst_r, t1, t2, ALU.subtract), rk + wk, wk)
            V(lambda e: e.tensor_tensor(t1, ar, bi, ALU.mult), rk + wk, wk)
            V(lambda e: e.tensor_tensor(t2, ai, br, ALU.mult), rk + wk, wk)
            V(lambda e: e.tensor_tensor(dst_i, t1, t2, ALU.add), rk + wk, wk)

        ss_are = SB(mix, "ss_are", [128, 16]); ss_aim = SB(mix, "ss_aim", [128, 16]); ss_ldt = SB(mix, "ss_ldt", [128, 16])
        load(ss_are[:], D["a_re_s"], "ss_are"); load(ss_aim[:], D["a_im_s"], "ss_aim"); load(ss_ldt[:], D["ldt_s"], "ss_ldt")
        sc = ssm_scalars(mix, "ss", ss_are, ss_aim, ss_ldt, [128, 16])
        Er = SB(mix, "Er", [128, 16, 128]); Ei = SB(mix, "Ei", [128, 16, 128]); Rho0 = SB(mix, "Rho0", [128, 16, 128])
        WTr = SB(mix, "WTr", [128, 16, 128], BF16); WTi = SB(mix, "WTi", [128, 16, 128], BF16)
        A128r = SB(mix, "A128r", [128, 16]); A128i = SB(mix, "A128i", [128, 16])
        E127r = SB(mix, "E127r", [128, 16]); E127i = SB(mix, "E127i", [128, 16])
        bsr = SB(mix, "bsr", [128, 16, 32]); bsi = SB(mix, "bsi", [128, 16, 32])
        BTr = SB(mix, "BTr", [128, 16, 128], BF16); BTi = SB(mix, "BTi", [128, 16, 128], BF16)
        Cbr = SB(mix, "Cbr", [128, 16, 128], BF16); Cbi = SB(mix, "Cbi", [128, 16, 128], BF16)

        def bc3(ap2, n):
            return ap2.rearrange("p (a o) -> p a o", o=1).to_broadcast([128, 16, n])

        with scope() as stg:
            Hr = SB(stg, "Hr", [128, 16, 128]); Hi = SB(stg, "Hi", [128, 16, 128])
            T1 = SB(stg, "T1", [128, 16, 128]); T2 = SB(stg, "T2", [128, 16, 128])
            q1 = SB(stg, "q1", [128, 16]); q2 = SB(stg, "q2", [128, 16]); ir = SB(stg, "ir", [128, 16]); ii_ = SB(stg, "ii", [128, 16])
            pr = SB(stg, "pr", [128, 16]); pi_ = SB(stg, "pi", [128, 16])
            V(lambda e: e.memset(Er[:, :, 0:1], 1.0), [], ["Er"]); V(lambda e: e.memset(Ei[:, :, 0:1], 0.0), [], ["Ei"])
            V(lambda e: e.tensor_copy(Er[:, :, 1], sc["cos"][:]), ["ss_cos"], ["Er"])
            V(lambda e: e.tensor_copy(Ei[:, :, 1], sc["sin"][:]), ["ss_sin"], ["Ei"])
            V(lambda e: e.tensor_tensor(q1[:], sc["rho"][:], sc["rho"][:], ALU.mult), ["ss_rho"], ["q1"])
            V(lambda e: e.reciprocal(q1[:], q1[:]), ["q1"], ["q1"])
            V(lambda e: e.tensor_tensor(ir[:], sc["abr"][:], q1[:], ALU.mult), ["ss_abr", "q1"], ["ir"])
            V(lambda e: e.tensor_tensor(ii_[:], sc["abi"][:], q1[:], ALU.mult), ["ss_abi", "q1"], ["ii"])
            V(lambda e: e.tensor_scalar(ii_[:], ii_[:], -1.0, None, ALU.mult), ["ii"], ["ii"])
            V(lambda e: e.memset(Hr[:, :, 0:1], 1.0), [], ["Hr"]); V(lambda e: e.memset(Hi[:, :, 0:1], 0.0), [], ["Hi"])
            V(lambda e: e.tensor_copy(Hr[:, :, 1], ir[:]), ["ir"], ["Hr"]); V(lambda e: e.tensor_copy(Hi[:, :, 1], ii_[:]), ["ii"], ["Hi"])
            for (Xr, Xi, kr, ki) in ((Er, Ei, "Er", "Ei"), (Hr, Hi, "Hr", "Hi")):
                m = 2
                while m < 128:
                    h = m // 2
                    cmul(Xr[:, :, m], Xi[:, :, m], Xr[:, :, h], Xi[:, :, h], Xr[:, :, h], Xi[:, :, h], q1[:], q2[:], [kr, ki, "q1", "q2"], [kr, ki, "q1", "q2"])
                    cmul(Xr[:, :, m + 1:2 * m], Xi[:, :, m + 1:2 * m], Xr[:, :, 1:m], Xi[:, :, 1:m],
                         bc3(Xr[:, :, m], m - 1), bc3(Xi[:, :, m], m - 1), T1[:, :, 1:m], T2[:, :, 1:m], [kr, ki, "T1", "T2"], [kr, ki, "T1", "T2"])
                    m *= 2
            V(lambda e: e.tensor_copy(A128r[:], sc["abr"][:]), ["ss_abr"], ["A128"]); V(lambda e: e.tensor_copy(A128i[:], sc["abi"][:]), ["ss_abi"], ["A128"])
            for _ in range(7):
                cmul(pr[:], pi_[:], A128r[:], A128i[:], A128r[:], A128i[:], q1[:], q2[:], ["A128", "q1", "q2", "pp"], ["pp", "q1", "q2"])
                V(lambda e: e.tensor_copy(A128r[:], pr[:]), ["pp"], ["A128"]); V(lambda e: e.tensor_copy(A128i[:], pi_[:]), ["pp"], ["A128"])
            V(lambda e: e.tensor_copy(E127r[:], Er[:, :, 127]), ["Er"], ["E127"]); V(lambda e: e.tensor_copy(E127i[:], Ei[:, :, 127]), ["Ei"], ["E127"])
            cmul(pr[:], pi_[:], A128r[:], A128i[:], ir[:], ii_[:], q1[:], q2[:], ["A128", "ir", "ii", "q1", "q2", "pp"], ["pp", "q1", "q2"])
            cmul(T1[:], T2[:], Hr[:], Hi[:], bc3(pr[:], 128), bc3(pi_[:], 128), Er[:] if False else Rho0[:], SB(stg, "T3", [128, 16, 128])[:],
                 ["Hr", "Hi", "pp", "Rho0", "T3", "T1", "T2"], ["T1", "T2", "Rho0", "T3"])
            for (Ts, WT, kk) in ((T1, WTr, "WTr"), (T2, WTi, "WTi")):
                for g4 in range(4):
                    for j in range(4):
                        i = 4 * g4 + j
                        M(lambda e, i=i, j=j, Ts=Ts: e.transpose(pb[0][:, j * 128:(j + 1) * 128], Ts[:, i, :], idf[:]), ["T1", "T2", "idf"], ["pb0"])
                    V(lambda e, g4=g4, WT=WT: e.tensor_copy(WT[:, 4 * g4:4 * g4 + 4, :], pb[0][:].rearrange("p (a b) -> p a b", a=4)), ["pb0"], [kk])
            V(lambda e: e.tensor_copy(Rho0[:], bc3(sc["rho"][:], 128)), ["ss_rho", "T1", "T2"], ["Rho0"])
            V(lambda e: e.memset(Rho0[:, :, 0:1], 0.0), ["Rho0"], ["Rho0"])
            b0r = SB(stg, "b0r", [128, 16, 32]); b0i = SB(stg, "b0i", [128, 16, 32]); u1 = SB(stg, "u1", [128, 16, 32]); u2 = SB(stg, "u2", [128, 16, 32])
            load(b0r[:], D["bs_re"], "b0r"); load(b0i[:], D["bs_im"], "b0i")
            cmul(bsr[:], bsi[:], bc3(sc["cfr"][:], 32), bc3(sc["cfi"][:], 32), b0r[:], b0i[:], u1[:], u2[:], ["ss_cfr", "ss_cfi", "b0r", "b0i", "u1", "u2"], ["bs", "u1", "u2"])
        with scope() as stg:
            CW = 512
            r_are = SB(stg, "rr_are", [128, CW]); r_aim = SB(stg, "rr_aim", [128, CW]); r_ldt = SB(stg, "rr_ldt", [128, CW])
            rc = None
            BTr2 = BTr[:].rearrange("p a b -> p (a b)"); BTi2 = BTi[:].rearrange("p a b -> p (a b)")
            Cbr2 = Cbr[:].rearrange("p a b -> p (a b)"); Cbi2 = Cbi[:].rearrange("p a b -> p (a b)")
            for cc in range(2048 // CW):
                cs = slice(cc * CW, (cc + 1) * CW)
                load(r_are[:], D["a_re_r"][:, cs], "rr_are"); load(r_aim[:], D["a_im_r"][:, cs], "rr_aim"); load(r_ldt[:], D["ldt_r"][:, cs], "rr_ldt")
                rc = ssm_scalars(stg, "rr", r_are, r_aim, r_ldt, [128, CW], t=rc)
                b1r = rc["dt"]; b1i = rc["th"]
                load(b1r[:], D["bT_re"].rearrange("p a b -> p (a b)")[:, cs], "rr_dt"); load(b1i[:], D["bT_im"].rearrange("p a b -> p (a b)")[:, cs], "rr_th")
                cmul(rc["cos"][:], rc["sin"][:], rc["cfr"][:], rc["cfi"][:], b1r[:], b1i[:], rc["t1"][:], rc["t2"][:],
                     ["rr_cfr", "rr_cfi", "rr_dt", "rr_th", "rr_t1", "rr_t2", "rr_cos", "rr_sin"], ["rr_cos", "rr_sin", "rr_t1", "rr_t2"])
                V(lambda e, cs=cs: e.tensor_copy(BTr2[:, cs], rc["cos"][:]), ["rr_cos"], ["BT"])
                V(lambda e, cs=cs: e.tensor_copy(BTi2[:, cs], rc["sin"][:]), ["rr_sin"], ["BT"])
                load(rc["t1"][:], D["cb_re"].rearrange("p a b -> p (a b)")[:, cs], "rr_t1"); load(rc["t2"][:], D["cb_im"].rearrange("p a b -> p (a b)")[:, cs], "rr_t2")
                V(lambda e, cs=cs: e.tensor_copy(Cbr2[:, cs], rc["t1"][:]), ["rr_t1"], ["Cb"])
                V(lambda e, cs=cs: e.tensor_scalar(Cbi2[:, cs], rc["t2"][:], -1.0, None, ALU.mult), ["rr_t2"], ["Cb"])

        xt = [SB(mix, f"xt{i}", [128, 1024]) for i in range(2)]
        junk = SB(mix, "junk", [128, 1024], BF16); xnb = SB(mix, "xnb", [128, 1024], BF16); nT = SB(mix, "nT", [128, 8, 128], BF16)
        st1 = SB(mix, "st1", [128, 4])
        Sr = SB(mix, "Sr", [128, 16]); Si = SB(mix, "Si", [128, 16])
        V(lambda e: e.memset(Sr[:], 0.0), [], ["S"]); V(lambda e: e.memset(Si[:], 0.0), [], ["S"])
        c1 = SB(mix, "c1", [128, 16]); c2 = SB(mix, "c2", [128, 16]); c3 = SB(mix, "c3", [128, 16]); c4 = SB(mix, "c4", [128, 16])
        xi = [0]

        def front(src):
            sl = xi[0] % 2; xi[0] += 1
            kx = f"xt{sl}"
            S.dma(lambda e: e.dma_start(out=xt[sl][:], in_=src), writes=[kx])
            A(lambda e: e.activation(out=junk[:], in_=xt[sl][:], func=AF.Square, accum_out=st1[:, 0:1]), [kx], ["junk", "st1"])
            V(lambda e: e.tensor_scalar(st1[:, 1:2], st1[:, 0:1], 1.0 / 1024, EPS, ALU.mult, ALU.add), ["st1"], ["st1b"])
            A(lambda e: e.activation(out=st1[:, 3:4], in_=st1[:, 1:2], func=AF.Sqrt), ["st1b"], ["st1d"])
            V(lambda e: e.reciprocal(st1[:, 2:3], st1[:, 3:4]), ["st1d"], ["st1c"])
            V(lambda e: e.tensor_scalar(xnb[:], xt[sl][:], st1[:, 2:3], None, ALU.mult), [kx, "st1c"], ["xnb"])
            for k in range(8):
                M(lambda e, k=k: e.transpose(ptr[:, k * 128:(k + 1) * 128], xnb[:, k * 128:(k + 1) * 128], idb[:]), ["xnb", "idb"], ["ptr"])
            A(lambda e: e.copy(nT[:].rearrange("p a b -> p (a b)"), ptr[:]), ["ptr"], ["nT"])
            return sl

        ub = SB(mix, "ub", [128, 512], BF16)
        fr = SB(mix, "fr", [128, 16]); fi = SB(mix, "fi", [128, 16])
        w1 = SB(mix, "w1", [128, 16, 32]); w2 = SB(mix, "w2", [128, 16, 32])

        def state_step():
            cmul(c3[:], c4[:], A128r[:], A128i[:], Sr[:], Si[:], c1[:], c2[:], ["A128", "S", "c"], ["c"])
            V(lambda e: e.tensor_tensor(Sr[:], c3[:], fr[:], ALU.add), ["c", "f"], ["S"])
            V(lambda e: e.tensor_tensor(Si[:], c4[:], fi[:], ALU.add), ["c", "f"], ["S"])

        def prefix_tile(src):
            front(src)
            for k in range(8):
                M(lambda e, k=k: e.matmul(pb[0][:], lhsT=nT[:, k, :], rhs=win[:, k, 768:1280], start=(k == 0), stop=(k == 7)), ["nT", "win"], ["pb0"])
            A(lambda e: e.copy(ub[:], pb[0][:]), ["pb0"], ["ub"])
            for i in range(16):
                M(lambda e, i=i: e.matmul(pb[1][:, i * 32:(i + 1) * 32], lhsT=WTr[:, i, :], rhs=ub[:, i * 32:(i + 1) * 32], start=True, stop=True), ["ub", "WTr"], ["pb1"])
                M(lambda e, i=i: e.matmul(pb[2][:, i * 32:(i + 1) * 32], lhsT=WTi[:, i, :], rhs=ub[:, i * 32:(i + 1) * 32], start=True, stop=True), ["ub", "WTi"], ["pb2"])
            mr = pb[1][:].rearrange("p (a b) -> p a b", a=16); mi = pb[2][:].rearrange("p (a b) -> p a b", a=16)
            V(lambda e: e.tensor_tensor(w1[:], bsr[:], mr, ALU.mult), ["bs", "pb1"], ["w1"])
            V(lambda e: e.tensor_tensor(w2[:], bsi[:], mi, ALU.mult), ["bs", "pb2"], ["w2"])
            V(lambda e: e.tensor_tensor(w1[:], w1[:], w2[:], ALU.subtract), ["w1", "w2"], ["w1"])
            V(lambda e: e.tensor_reduce(fr[:], w1[:], AX.X, ALU.add), ["w1"], ["f"])
            V(lambda e: e.tensor_tensor(w1[:], bsr[:], mi, ALU.mult), ["bs", "pb2", "f"], ["w1"])
            V(lambda e: e.tensor_tensor(w2[:], bsi[:], mr, ALU.mult), ["bs", "pb1"], ["w2"])
            V(lambda e: e.tensor_tensor(w1[:], w1[:], w2[:], ALU.add), ["w1", "w2"], ["w1"])
            V(lambda e: e.tensor_reduce(fi[:], w1[:], AX.X, ALU.add), ["w1"], ["f"])
            state_step()

        for t in range(NPRE if "prefix" not in off else 0):
            prefix_tile(xpre[t * 128:(t + 1) * 128, :])

        if stage == "prefix":
            dbg = SB(mix, "dbg", [128, 1024])
            V(lambda e: e.memset(dbg[:], 0.0), [], ["dbg"])
            V(lambda e: e.tensor_copy(dbg[:, 0:16], Sr[:]), ["S"], ["dbg"]); V(lambda e: e.tensor_copy(dbg[:, 16:32], Si[:]), ["S"], ["dbg"])
            for j, (ap, key) in enumerate([(sc["cos"][:], "ss_cos"), (sc["sin"][:], "ss_sin"), (sc["abr"][:], "ss_abr"), (sc["abi"][:], "ss_abi"),
                                           (sc["cfr"][:], "ss_cfr"), (sc["cfi"][:], "ss_cfi"), (Er[:, :, 5], "Er"), (Ei[:, :, 5], "Ei"),
                                           (A128r[:], "A128"), (A128i[:], "A128"), (fr[:], "f"), (fi[:], "f"), (sc["rho"][:], "ss_rho"), (sc["th"][:], "ss_th")]):
                V(lambda e, j=j, ap=ap: e.tensor_copy(dbg[:, 32 + 16 * j:48 + 16 * j], ap), [key], ["dbg"])
            V(lambda e: e.tensor_copy(dbg[:, 512:1024], ub[:]), ["ub"], ["dbg"])
            S.dma(lambda e: e.dma_start(out=out[0:128, :], in_=dbg[:]), reads=["dbg"], writes=["out"])
            S.emit()
            return nc

        qkv_sb = SB(mix, "qkv_sb", [128, 768]); sqt = SB(mix, "sqt", [128, 640]); s10 = SB(mix, "s10", [128, 10]); s10b = SB(mix, "s10b", [128, 10])
        qkn = SB(mix, "qkn", [128, 640], BF16); qT = SB(mix, "qT", [128, 4, 128], BF16)
        kTs = [SB(mix, f"kT{i}", [128, 128], BF16) for i in range(3)]; vss = [SB(mix, f"vs{i}", [128, 128], BF16) for i in range(3)]
        kTm = SB(mix, "kTm", [128, 128], BF16); vm = SB(mix, "vm", [128, 128], BF16)
        pex = SB(mix, "pex", [128, 512], BF16); PT = [SB(mix, f"PT{i}", [128, 4, 128], BF16) for i in range(2)]; PTm = SB(mix, "PTm", [16, 512], BF16)
        den_sb = SB(mix, "den_sb", [64, 4, 128]); oT = SB(mix, "oT", [64, 8, 128], BF16); osq = SB(mix, "osq", [64, 4, 128], BF16)
        uTb = SB(mix, "uTb", [128, 4, 128], BF16)
        t1 = SB(mix, "t1", [128, 512]); t2 = SB(mix, "t2", [128, 512]); btr = SB(mix, "btr", [128, 512]); bti = SB(mix, "bti", [128, 512])
        str_ = SB(mix, "str", [128, 512]); sti = SB(mix, "sti", [128, 512]); t3 = SB(mix, "t3", [128, 512]); t4 = SB(mix, "t4", [128, 512])
        sre = SB(mix, "sre", [128, 512], BF16); sim = SB(mix, "sim", [128, 512], BF16)
        injr = SB(mix, "injr", [128, 16]); inji = SB(mix, "inji", [128, 16]); stlr = SB(mix, "stlr", [128, 16]); stli = SB(mix, "stli", [128, 16])
        ysb = SB(mix, "ysb", [128, 4, 128]); zb = SB(mix, "zb", [128, 4, 128], BF16); sg = SB(mix, "sg", [128, 4, 128], BF16)
        zz = SB(mix, "zz", [128, 4, 128], BF16); zsq = SB(mix, "zsq", [128, 4, 128], BF16)
        rs = SB(mix, "rs", [128, 4]); hm = SB(mix, "hm", [128, 1024]); h2b = SB(mix, "h2b", [128, 1024], BF16); st2 = SB(mix, "st2", [128, 4])

        def bcg(ap2, a, n):
            return ap2.rearrange("p (a o) -> p a o", o=1).to_broadcast([ap2.shape[0], a, n])

        def kv_part(kslot, vslot, with_q):
            c0 = 0 if with_q else 512
            g0 = c0 // 64
            if with_q:
                for k in range(8):
                    M(lambda e, k=k: e.matmul(psA[:, 0:512], lhsT=nT[:, k, :], rhs=win[:, k, 0:512], start=(k == 0), stop=(k == 7)), ["nT", "win"], ["psAq"])
                A(lambda e: e.copy(qkv_sb[:, 0:512], psA[:, 0:512]), ["psAq"], ["qkv_q"])
                if "q1" in off:
                    raise StopBuild()
            for k in range(8):
                M(lambda e, k=k: e.matmul(psA[:, 512:768], lhsT=nT[:, k, :], rhs=win[:, k, 512:768], start=(k == 0), stop=(k == 7)), ["nT", "win"], ["psAk"])
            A(lambda e: e.copy(qkv_sb[:, 512:768], psA[:, 512:768]), ["psAk"], ["qkv_k"])
            rk = ["qkv_q", "qkv_k"] if with_q else ["qkv_k"]
            V(lambda e: e.tensor_tensor(sqt[:, c0:640], qkv_sb[:, c0:640], qkv_sb[:, c0:640], ALU.mult), rk, ["sqt"])
            V(lambda e: e.tensor_reduce(s10[:, g0:10], sqt[:, c0:640].rearrange("p (a b) -> p a b", b=64), AX.X, ALU.add), ["sqt"], ["s10"])
            V(lambda e: e.tensor_scalar(s10[:, g0:10], s10[:, g0:10], 1.0 / 64, EPS, ALU.mult, ALU.add), ["s10"], ["s10"])
            A(lambda e: e.activation(out=s10b[:, g0:10], in_=s10[:, g0:10], func=AF.Sqrt), ["s10"], ["s10b"])
            V(lambda e: e.reciprocal(s10b[:, g0:10], s10b[:, g0:10]), ["s10b"], ["s10b"])
            V(lambda e: e.tensor_tensor(qkn[:, c0:640].rearrange("p (a b) -> p a b", b=64), qkv_sb[:, c0:640].rearrange("p (a b) -> p a b", b=64),
                                        bcg(s10b[:, g0:10], 10 - g0, 64), ALU.mult), rk + ["s10b"], ["qkn"])
            if with_q and "q2" in off:
                raise StopBuild()
            M(lambda e: e.transpose(ptr[:, 512:640], qkn[:, 512:640], idb[:]), ["qkn", "idb"], ["ptr"])
            A(lambda e: e.copy(kslot[0][:], ptr[:, 512:640]), ["ptr"], [kslot[1]])
            A(lambda e: e.copy(vslot[0][:], qkv_sb[:, 640:768]), ["qkv_k"], [vslot[1]])
            if with_q:
                for m in range(4):
                    M(lambda e, m=m: e.transpose(ptr[:, m * 128:(m + 1) * 128], qkn[:, m * 128:(m + 1) * 128], idb[:]), ["qkn", "idb"], ["ptr"])
                if "q3" in off:
                    raise StopBuild()
                V(lambda e: e.tensor_scalar(qT[:].rearrange("p a b -> p (a b)"), ptr[:, 0:512], cqk[:, 0:1], None, ALU.mult), ["ptr", "cqk"], ["qT"])

        def attention(prev, cur, pmask):
            first = [True]
            for g in range(2):
                ps_ = slice(64 * g, 64 * g + 64)
                for b, (kk, vv, msk, mkey) in enumerate(((prev[0], prev[1], pmask[0], pmask[1]), (cur[0], cur[1], mcur, "mcur"))):
                    M(lambda e, b=b, kk=kk: e.matmul(pb[b][:], lhsT=kk[0][ps_, :], rhs=qT[ps_, :, :].rearrange("p a b -> p (a b)"), start=True, stop=True), [kk[1], "qT"], [f"pb{b}"])
                    A(lambda e, b=b: e.activation(out=pex[:], in_=pb[b][:], func=AF.Exp), [f"pb{b}"], ["pex"])
                    V(lambda e, b=b, msk=msk: e.tensor_tensor(PT[b][:], pex[:].rearrange("p (a b) -> p a b", a=4),
                                                            msk[:].rearrange("p (o q) -> p o q", o=1).to_broadcast([128, 4, 128]), ALU.mult), ["pex", mkey], [f"PT{b}"])
                M(lambda e: e.matmul(pb[2][0:16, :], lhsT=kTm[ps_, 0:16], rhs=qT[ps_, :, :].rearrange("p a b -> p (a b)"), start=True, stop=True), ["kTm", "qT"], ["pb2"])
                A(lambda e: e.activation(out=PTm[:], in_=pb[2][0:16, :], func=AF.Exp), ["pb2"], ["PTm"])
                cs_ = slice(64 * g, 64 * g + 64)
                PT0 = PT[0][:].rearrange("p a b -> p (a b)"); PT1 = PT[1][:].rearrange("p a b -> p (a b)")
                M(lambda e: e.matmul(pb[3][0:64, :], lhsT=prev[1][0][:, cs_], rhs=PT0, start=True, stop=False), [prev[1][1], "PT0"], ["pb3"])
                M(lambda e: e.matmul(pb[3][0:64, :], lhsT=cur[1][0][:, cs_], rhs=PT1, start=False, stop=False), [cur[1][1], "PT1"], ["pb3"])
                M(lambda e: e.matmul(pb[3][0:64, :], lhsT=vm[0:16, cs_], rhs=PTm[:], start=False, stop=True), ["vm", "PTm"], ["pb3"])
                M(lambda e: e.matmul(pb[4][0:64, :], lhsT=ones[:, 0:64], rhs=PT0, start=True, stop=False), ["ones", "PT0"], ["pb4"])
                M(lambda e: e.matmul(pb[4][0:64, :], lhsT=ones[:, 0:64], rhs=PT1, start=False, stop=False), ["ones", "PT1"], ["pb4"])
                M(lambda e: e.matmul(pb[4][0:64, :], lhsT=ones[0:16, 0:64], rhs=PTm[:], start=False, stop=True), ["ones", "PTm"], ["pb4"])
                for j in range(4):
                    h = 4 * g + j
                    A(lambda e, j=j, h=h: e.activation(out=den_sb[:, j, :], in_=pb[4][0:64, j * 128:(j + 1) * 128], func=AF.Identity, bias=esink[:, h:h + 1]), ["pb4", "esink"], ["den"])
                V(lambda e: e.reciprocal(den_sb[:], den_sb[:]), ["den"], ["den"])
                V(lambda e, g=g: e.tensor_tensor(oT[:, 4 * g:4 * g + 4, :], pb[3][0:64, :].rearrange("p (a b) -> p a b", a=4), den_sb[:], ALU.mult), ["pb3", "den"], ["oT"])
                V(lambda e, g=g: e.tensor_tensor(osq[:], oT[:, 4 * g:4 * g + 4, :], oT[:, 4 * g:4 * g + 4, :], ALU.mult), ["oT"], ["osq"])
                for j in range(4):
                    M(lambda e, j=j, g=g: e.matmul(psA[:, 768:769], lhsT=osq[:, j, :], rhs=ones[0:64, 0:1], start=(g == 0 and j == 0), stop=(g == 1 and j == 3)), ["osq", "ones"], ["pssa"])

        def ssm_tile():
            for c in range(4):
                for k in range(8):
                    M(lambda e, c=c, k=k: e.matmul(pb[0][:, c * 128:(c + 1) * 128], lhsT=win[:, k, 768 + 128 * c:768 + 128 * (c + 1)], rhs=nT[:, k, :], start=(k == 0), stop=(k == 7)), ["nT", "win"], ["pb0"])
            A(lambda e: e.copy(uTb[:].rearrange("p a b -> p (a b)"), pb[0][:]), ["pb0"], ["uTb"])
            cmul(injr[:], inji[:], sc["abr"][:], sc["abi"][:], Sr[:], Si[:], c1[:], c2[:], ["ss_abr", "ss_abi", "S", "c"], ["inj", "c"])
            for c in range(4):
                isl = slice(4 * c, 4 * c + 4)
                Erc = Er[:, isl, :].rearrange("p a b -> p (a b)"); Eic = Ei[:, isl, :].rearrange("p a b -> p (a b)")
                Rc = Rho0[:, isl, :].rearrange("p a b -> p (a b)")
                for ii in range(4):
                    i = 4 * c + ii
                    M(lambda e, i=i, ii=ii, c=c: e.matmul(pb[1][:, ii * 128:(ii + 1) * 128], lhsT=BTr[:, i, :], rhs=uTb[:, c, :], start=True, stop=True), ["BT", "uTb"], ["pb1"])
                    M(lambda e, i=i, ii=ii, c=c: e.matmul(pb[2][:, ii * 128:(ii + 1) * 128], lhsT=BTi[:, i, :], rhs=uTb[:, c, :], start=True, stop=True), ["BT", "uTb"], ["pb2"])
                V(lambda e: e.tensor_tensor(t1[:], pb[1][:], Erc, ALU.mult), ["pb1", "Er"], ["t1"])
                V(lambda e: e.tensor_tensor(t2[:], pb[2][:], Eic, ALU.mult), ["pb2", "Ei"], ["t2"])
                V(lambda e: e.tensor_tensor(btr[:], t1[:], t2[:], ALU.add), ["t1", "t2"], ["btr"])
                V(lambda e: e.tensor_tensor(t1[:], pb[2][:], Erc, ALU.mult), ["pb2", "Er", "btr"], ["t1"])
                V(lambda e: e.tensor_tensor(t2[:], pb[1][:], Eic, ALU.mult), ["pb1", "Ei", "btr"], ["t2"])
                V(lambda e: e.tensor_tensor(bti[:], t1[:], t2[:], ALU.subtract), ["t1", "t2"], ["bti"])
                b3r = btr[:].rearrange("p (a b) -> p a b", a=4); b3i = bti[:].rearrange("p (a b) -> p a b", a=4)
                V(lambda e, isl=isl: e.tensor_tensor(b3r[:, :, 0], b3r[:, :, 0], injr[:, isl], ALU.add), ["btr", "inj"], ["btr"])
                V(lambda e, isl=isl: e.tensor_tensor(b3i[:, :, 0], b3i[:, :, 0], inji[:, isl], ALU.add), ["bti", "inj"], ["bti"])
                V(lambda e: e.tensor_tensor_scan(str_[:], Rc, btr[:], 0.0, ALU.mult, ALU.add), ["Rho0", "btr"], ["str"])
                V(lambda e: e.tensor_tensor_scan(sti[:], Rc, bti[:], 0.0, ALU.mult, ALU.add), ["Rho0", "bti"], ["sti"])
                s3r = str_[:].rearrange("p (a b) -> p a b", a=4); s3i = sti[:].rearrange("p (a b) -> p a b", a=4)
                V(lambda e, isl=isl: e.tensor_copy(stlr[:, isl], s3r[:, :, 127]), ["str"], ["stl"])
                V(lambda e, isl=isl: e.tensor_copy(stli[:, isl], s3i[:, :, 127]), ["sti"], ["stl"])
                G(lambda e: e.tensor_tensor(t3[:], str_[:], Erc, ALU.mult), ["str", "Er"], ["t3"])
                G(lambda e: e.tensor_tensor(t4[:], sti[:], Eic, ALU.mult), ["sti", "Ei"], ["t4"])
                G(lambda e: e.tensor_tensor(sre[:], t3[:], t4[:], ALU.subtract), ["t3", "t4"], ["sre"])
                G(lambda e: e.tensor_tensor(t3[:], str_[:], Eic, ALU.mult), ["str", "Ei", "sre"], ["t3"])
                G(lambda e: e.tensor_tensor(t4[:], sti[:], Erc, ALU.mult), ["sti", "Er", "sre"], ["t4"])
                G(lambda e: e.tensor_tensor(sim[:], t3[:], t4[:], ALU.add), ["t3", "t4"], ["sim"])
                for ii in range(4):
                    i = 4 * c + ii
                    M(lambda e, i=i, ii=ii, c=c: e.matmul(pb[3][:, c * 128:(c + 1) * 128], lhsT=Cbr[:, i, :], rhs=sre[:, ii * 128:(ii + 1) * 128], start=(ii == 0), stop=False), ["Cb", "sre"], ["pb3"])
                    M(lambda e, i=i, ii=ii, c=c: e.matmul(pb[3][:, c * 128:(c + 1) * 128], lhsT=Cbi[:, i, :], rhs=sim[:, ii * 128:(ii + 1) * 128], start=False, stop=(ii == 3)), ["Cb", "sim"], ["pb3"])
            cmul(Sr[:], Si[:], stlr[:], stli[:], E127r[:], E127i[:], c1[:], c2[:], ["stl", "E127", "c", "inj"], ["S", "c"])
            for c in range(4):
                V(lambda e, c=c: e.scalar_tensor_tensor(ysb[:, c, :], uTb[:, c, :], small["ssm_d"][:, c:c + 1], pb[3][:, c * 128:(c + 1) * 128], ALU.mult, ALU.add), ["uTb", "c_ssm_d", "pb3"], ["ysb"])
            A(lambda e: e.activation(out=zb[:], in_=ysb[:], func=AF.Gelu), ["ysb"], ["zb"])
            for cp in range(4):
                for c in range(4):
                    M(lambda e, c=c, cp=cp: e.matmul(pb[4][:, cp * 128:(cp + 1) * 128], lhsT=glu[:, c, cp * 128:(cp + 1) * 128], rhs=zb[:, c, :], start=(c == 0), stop=(c == 3)), ["glu", "zb"], ["pb4"])
            for cp in range(4):
                A(lambda e, cp=cp: e.activation(out=sg[:, cp, :], in_=pb[4][:, cp * 128:(cp + 1) * 128], func=AF.Sigmoid, bias=small["glu_b"][:, cp:cp + 1]), ["pb4", "c_glu_b"], ["sg"])
            V(lambda e: e.tensor_tensor(zz[:], zb[:], sg[:], ALU.mult), ["zb", "sg"], ["zz"])
            V(lambda e: e.tensor_tensor(zsq[:], zz[:], zz[:], ALU.mult), ["zz"], ["zsq"])
            for c in range(4):
                M(lambda e, c=c: e.matmul(psA[:, 769:770], lhsT=zsq[:, c, :], rhs=ones[:, 0:1], start=(c == 0), stop=(c == 3)), ["zsq", "ones"], ["psss"])

        def out_proj(sl, t):
            for half in range(2):
                hs = slice(half * 512, (half + 1) * 512)
                for h in range(8):
                    M(lambda e, h=h, half=half, hs=hs: e.matmul(pb[half][:], lhsT=oT[:, h, :], rhs=woa[:, h, hs], start=(h == 0), stop=(h == 7)), ["oT", "woa"], [f"pb{half}"])
                for c in range(4):
                    M(lambda e, c=c, half=half, hs=hs: e.matmul(pb[2 + half][:], lhsT=zz[:, c, :], rhs=wos[:, c, hs], start=(c == 0), stop=(c == 3)), ["zz", "wos"], [f"pb{2 + half}"])
            V(lambda e: e.tensor_scalar(rs[:, 0:2], psA[:, 768:770], 1.0 / 512, EPS, ALU.mult, ALU.add), ["pssa", "psss"], ["rs"])
            A(lambda e: e.activation(out=rs[:, 2:4], in_=rs[:, 0:2], func=AF.Sqrt), ["rs"], ["rsb"])
            V(lambda e: e.reciprocal(rs[:, 0:2], rs[:, 2:4]), ["rsb", "rs"], ["rs"])
            kx = f"xt{sl}"
            for half in range(2):
                hs = slice(half * 512, (half + 1) * 512)
                V(lambda e, half=half, hs=hs: e.scalar_tensor_tensor(hm[:, hs], pb[half][:], rs[:, 0:1], xt[sl][:, hs], ALU.mult, ALU.add), [f"pb{half}", "rs", kx], ["hm"])
                V(lambda e, half=half, hs=hs: e.scalar_tensor_tensor(hm[:, hs], pb[2 + half][:], rs[:, 1:2], hm[:, hs], ALU.mult, ALU.add), [f"pb{2 + half}", "rs", "hm"], ["hm"])
            S.dma(lambda e: e.dma_start(out=out[t * 128:(t + 1) * 128, :], in_=hm[:]), reads=["hm"], writes=[f"out{t}"])
            A(lambda e: e.activation(out=junk[:], in_=hm[:], func=AF.Square, accum_out=st2[:, 0:1]), ["hm"], ["junk", "st2"])
            V(lambda e: e.tensor_scalar(st2[:, 1:2], st2[:, 0:1], 1.0 / 1024, EPS, ALU.mult, ALU.add), ["st2"], ["st2b"])
            A(lambda e: e.activation(out=st2[:, 3:4], in_=st2[:, 1:2], func=AF.Sqrt), ["st2b"], ["st2d"])
            V(lambda e: e.reciprocal(st2[:, 2:3], st2[:, 3:4]), ["st2d"], ["st2c"])
            V(lambda e: e.tensor_scalar(h2b[:], hm[:], st2[:, 2:3], None, ALU.mult), ["hm", "st2c"], ["h2b"])
            for k in range(8):
                M(lambda e, k=k: e.transpose(ptr[:, k * 128:(k + 1) * 128], h2b[:, k * 128:(k + 1) * 128], idb[:]), ["h2b", "idb"], ["ptr"])
            V(lambda e: e.tensor_tensor(h2T[:, :, t * 128:(t + 1) * 128], ptr[:].rearrange("p (a b) -> p a b", a=8), bcg(small["g2"][:], 8, 128), ALU.mult), ["ptr", "c_g2"], ["h2T"])

        if "nomain" in off:
            S.dma(lambda e: e.dma_start(out=xt[0][:], in_=xown[0:128, :]), writes=["xt0"])
            S.dma(lambda e: e.dma_start(out=out[0:128, :], in_=xt[0][:]), reads=["xt0"], writes=["out0"])
            S.emit()
            return nc
        front(xmeta)
        kv_part((kTm, "kTm"), (vm, "vm"), False)
        if "stopmeta" in off:
            S.dma(lambda e: e.dma_start(out=out[0:128, :], in_=xt[0][:]), reads=["xt0", "kTm", "vm"], writes=["out0"])
            S.emit()
            return nc
        front(xhalo)
        kv_part((kTs[2], "kT2"), (vss[2], "vs2"), False)
        if "stophalo" in off:
            S.dma(lambda e: e.dma_start(out=out[0:128, :], in_=xt[1][:]), reads=["xt1", "kT2", "vs2"], writes=["out0"])
            S.emit()
            return nc
        n_own = 1 if (stage == "mix1" or "one" in off) else NT
        for t in range(n_own):
            sl = front(xown[t * 128:(t + 1) * 128, :])
            pslot, cslot = (t + 2) % 3, t % 3
            try:
                kv_part((kTs[cslot], f"kT{cslot}"), (vss[cslot], f"vs{cslot}"), True)
            except StopBuild:
                S.dma(lambda e: e.dma_start(out=out[0:128, :], in_=xt[sl][:]), reads=[f"xt{sl}", "qkv_q", "qkv_k", "qkn", "ptr"], writes=["out0"])
                S.emit()
                return nc
            if "attn" not in off:
                attention(((kTs[pslot], f"kT{pslot}"), (vss[pslot], f"vs{pslot}")), ((kTs[cslot], f"kT{cslot}"), (vss[cslot], f"vs{cslot}")),
                          (mhalo, "mhalo") if t == 0 else (mprev, "mprev"))
            if "dumpqk" in off:
                V(lambda e: e.tensor_copy(hm[:, 0:512], qT[:].rearrange("p a b -> p (a b)")), ["qT"], ["hm"])
                V(lambda e: e.tensor_copy(hm[:, 512:640], kTs[cslot][:]), [f"kT{cslot}"], ["hm"])
                V(lambda e: e.tensor_copy(hm[:, 640:768], vss[cslot][:]), [f"vs{cslot}"], ["hm"])
                V(lambda e: e.tensor_copy(hm[:, 768:896], kTm[:]), ["kTm"], ["hm"])
                V(lambda e: e.tensor_copy(hm[:, 896:1024], vm[:]), ["vm"], ["hm"])
                S.dma(lambda e: e.dma_start(out=out[0:128, :], in_=hm[:]), reads=["hm"], writes=["out0"])
                S.emit()
                return nc
            if "dumpv" in off:
                V(lambda e: e.tensor_copy(hm[0:64, 0:512], pb[3][0:64, :]), ["pb3"], ["hm"])
                V(lambda e: e.tensor_copy(hm[0:64, 512:1024], pb[4][0:64, :]), ["pb4", "hm"], ["hm"])
                S.dma(lambda e: e.dma_start(out=out[0:64, :], in_=hm[0:64, :]), reads=["hm"], writes=["out0"])
                S.emit()
                return nc
            if "dumpp" in off:
                V(lambda e: e.tensor_copy(hm[:, 0:512], PT[1][:].rearrange("p a b -> p (a b)")), ["PT1"], ["hm"])
                V(lambda e: e.tensor_copy(hm[0:64, 512:1024], den_sb[:].rearrange("p a b -> p (a b)")), ["den"], ["hm"])
                S.dma(lambda e: e.dma_start(out=out[0:128, :], in_=hm[:]), reads=["hm"], writes=["out0"])
                S.emit()
                return nc
            if "dumpo" in off:
                V(lambda e: e.tensor_copy(hm[0:64, :], oT[:].rearrange("p a b -> p (a b)")), ["oT"], ["hm"])
                S.dma(lambda e: e.dma_start(out=out[0:64, :], in_=hm[0:64, :]), reads=["hm"], writes=["out0"])
                S.emit()
                return nc
            if "ssm" not in off:
                ssm_tile()
            if "outp" not in off:
                out_proj(sl, t)
            else:
                S.dma(lambda e, t=t, sl=sl: e.dma_start(out=out[t * 128:(t + 1) * 128, :], in_=xt[sl][:]), reads=[f"xt{sl}"], writes=[f"out{t}"])

        if "dumph2" in off:
            V(lambda e: e.tensor_copy(hm[:], h2T[:, :, 0:128]), ["h2T", "hm"], ["hm"])
            S.dma(lambda e: e.dma_start(out=out[128:256, :], in_=hm[:]), reads=["hm"], writes=["out1"])
        if stage in ("mix", "mix1"):
            S.emit()
            return nc

        S.barrier()
        mix.close()
        pe_ = top.enter_context(contextlib.ExitStack())
        pA = PS(pe_, "pA", [128, 2048]); pQ = PS(pe_, "pQ", [128, 512])
        wq = SB(pe_, "wq", [128, 8, 2048], BF16); keysT = SB(pe_, "keysT", [128, 16, 128], BF16)
        with scope() as stg:
            wst = SB(stg, "wst", [128, 2, 2048])
            for kk4 in range(4):
                load(wst[:], D["wq"][:, 2 * kk4:2 * kk4 + 2, :], "wst")
                V(lambda e, kk4=kk4: e.tensor_copy(wq[:, 2 * kk4:2 * kk4 + 2, :], wst[:]), ["wst"], ["wq"])
            kst = SB(stg, "kst", [128, 16, 128])
            load(kst[:], D["keysT"], "kst")
            V(lambda e: e.tensor_copy(keysT[:], kst[:]), ["kst"], ["keysT"])
        qTp = SB(pe_, "qTp", [128, 16, 128], BF16); scs = SB(pe_, "scs", [128, 16, 128]); sc2 = SB(pe_, "sc2", [128, 16, 128])
        v16 = SB(pe_, "v16", [128, 16, 16]); cand = SB(pe_, "cand", [128, 8, 256]); cand2 = SB(pe_, "cand2", [128, 8, 256]); s16 = SB(pe_, "s16", [128, 8, 16])
        dbgp = SB(pe_, "dbgp", [128, 1024])

        def peer_front(t):
            ts_ = slice(t * 128, (t + 1) * 128)
            for g4 in range(4):
                for j in range(4):
                    cb = 4 * g4 + j
                    for k in range(8):
                        M(lambda e, cb=cb, j=j, k=k: e.matmul(pQ[:, j * 128:(j + 1) * 128], lhsT=wq[:, k, cb * 128:(cb + 1) * 128], rhs=h2T[:, k, ts_], start=(k == 0), stop=(k == 7)), ["wq", "h2T"], ["pQ"])
                A(lambda e, g4=g4: e.copy(qTp[:, 4 * g4:4 * g4 + 4, :].rearrange("p a b -> p (a b)"), pQ[:]), ["pQ"], ["qTp"])
            for idx in range(16):
                M(lambda e, idx=idx: e.matmul(pA[:, idx * 128:(idx + 1) * 128], lhsT=qTp[:, idx, :], rhs=keysT[:, idx, :], start=True, stop=True), ["qTp", "keysT"], ["pA"])
            for q4 in range(4):
                A(lambda e, q4=q4: e.copy(scs[:, 4 * q4:4 * q4 + 4, :].rearrange("p a b -> p (a b)"), pA[:, q4 * 512:(q4 + 1) * 512]), ["pA"], ["scs"])
            for idx in range(16):
                V(lambda e, idx=idx: e.max(out=v16[:, idx, 0:8], in_=scs[:, idx, :]), ["scs"], ["v16"])
                V(lambda e, idx=idx: e.match_replace(out=sc2[:, idx, :], in_to_replace=v16[:, idx, 0:8], in_values=scs[:, idx, :], imm_value=-1e30), ["scs", "v16"], ["sc2"])
                V(lambda e, idx=idx: e.max(out=v16[:, idx, 8:16], in_=sc2[:, idx, :]), ["sc2"], ["v16"])
            v4 = v16[:].rearrange("p (h two) a -> p h two a", two=2)
            V(lambda e: e.tensor_tensor(cand[:].rearrange("p h (a b) -> p h a b", a=16),
                                        v4[:, :, 0, :].rearrange("p h (a o) -> p h a o", o=1).to_broadcast([128, 8, 16, 16]),
                                        v4[:, :, 1, :].rearrange("p h (o b) -> p h o b", o=1).to_broadcast([128, 8, 16, 16]), ALU.add), ["v16"], ["cand"])
            for h in range(8):
                V(lambda e, h=h: e.max(out=s16[:, h, 0:8], in_=cand[:, h, :]), ["cand"], ["s16"])
                V(lambda e, h=h: e.match_replace(out=cand2[:, h, :], in_to_replace=s16[:, h, 0:8], in_values=cand[:, h, :], imm_value=-1e30), ["cand", "s16"], ["cand2"])
                V(lambda e, h=h: e.max(out=s16[:, h, 8:16], in_=cand2[:, h, :]), ["cand2"], ["s16"])

        if stage == "peerfront":
            peer_front(0)
            V(lambda e: e.memset(dbgp[:], 0.0), [], ["dbgp"])
            V(lambda e: e.tensor_copy(dbgp[:, 0:256], v16[:].rearrange("p a b -> p (a b)")), ["v16"], ["dbgp"])
            V(lambda e: e.tensor_copy(dbgp[:, 256:384], s16[:].rearrange("p a b -> p (a b)")), ["s16"], ["dbgp"])
            V(lambda e: e.tensor_copy(dbgp[:, 512:1024], scs[:, 0:4, :].rearrange("p a b -> p (a b)")), ["scs"], ["dbgp"])
            S.dma(lambda e: e.dma_start(out=out[256:384, :], in_=dbgp[:]), reads=["dbgp"], writes=["out2"])
            S.emit()
            return nc

        n_i = 128
        with scope() as stg:
            NS = 4
            cst = [SB(stg, f"cst{i}", [128, 1024]) for i in range(NS)]; cbf = [SB(stg, f"cbf{i}", [128, 1024], BF16) for i in range(NS)]
            cnt = 0
            for (src, dst, nm) in ((D["puT"], UTs, "UTs"), (D["pv"], Vs, "Vs")):
                for i in range(n_i):
                    sl = cnt % NS
                    S.dma(lambda e, i=i, sl=sl, src=src: e.dma_start(out=cst[sl][:], in_=src[i]), writes=[f"cst{sl}"])
                    if cnt % 3 == 0:
                        A(lambda e, sl=sl: e.copy(cbf[sl][:], cst[sl][:]), [f"cst{sl}"], [f"cbf{sl}"])
                    elif cnt % 3 == 1:
                        V(lambda e, sl=sl: e.tensor_copy(cbf[sl][:], cst[sl][:]), [f"cst{sl}"], [f"cbf{sl}"])
                    else:
                        G(lambda e, sl=sl: e.tensor_copy(cbf[sl][:], cst[sl][:]), [f"cst{sl}"], [f"cbf{sl}"])
                    S.dma(lambda e, i=i, sl=sl, dst=dst: e.dma_start(out=dst[i], in_=cbf[sl][:]), reads=[f"cbf{sl}"], writes=[f"{nm}{i}"], eng="act" if False else "sp")
                    cnt += 1

        iota_i = SB(pe_, "iota_i", [128, 128]); bm = SB(pe_, "bm", [128, 8])
        load(iota_i[:], D["iota_i"], "iota_i"); load(bm[:], D["blkmask"], "bm")
        pG2 = PS(pe_, "pG2", [128, 512]); pZs = [PS(pe_, f"pZ{i}", [128, 512]) for i in range(2)]
        ixu = SB(pe_, "ixu", [128, 16, 16], U32); ixf = SB(pe_, "ixf", [128, 16, 16])
        i1c = SB(pe_, "i1c", [128, 128]); i2c = SB(pe_, "i2c", [128, 128]); i1T = SB(pe_, "i1T", [128, 128]); i2T = SB(pe_, "i2T", [128, 128])
        zs = SB(pe_, "zs", [128, 8]); wT = SB(pe_, "wT", [128, 128, 16], BF16)
        NB = 16
        P1b = SB(pe_, "P1b", [128, NB, 128], BF16); P2b = SB(pe_, "P2b", [128, NB, 128], BF16); Wb = SB(pe_, "Wb", [128, NB, 128], BF16)
        Tsb = SB(pe_, "Tsb", [128, 4, 128], BF16); Gt = SB(pe_, "Gt", [128, 128, 128], BF16)
        ND = 4
        ubuf = [SB(pe_, f"ubuf{i}", [128, 1024], BF16) for i in range(ND)]; vbuf = [SB(pe_, f"vbuf{i}", [128, 1024], BF16) for i in range(ND)]
        zg = [SB(pe_, f"zg{i}", [128, 128], BF16) for i in range(2)]; AT = [SB(pe_, f"AT{i}", [128, 128], BF16) for i in range(2)]
        hmt = SB(pe_, "hmt", [128, 1024]); res = SB(pe_, "res", [128, 1024])

        def b3(ap2, a, n):
            return ap2.rearrange("p (a o) -> p a o", o=1).to_broadcast([128, a, n])

        def gate_build(t):
            for idx in range(16):
                V(lambda e, idx=idx: e.max_index(out=ixu[:, idx, 0:8], in_max=v16[:, idx, 0:8], in_values=scs[:, idx, :]), ["v16", "scs"], ["ixu"])
                V(lambda e, idx=idx: e.max_index(out=ixu[:, idx, 8:16], in_max=v16[:, idx, 8:16], in_values=sc2[:, idx, :]), ["v16", "sc2"], ["ixu"])
            V(lambda e: e.tensor_copy(ixf[:], ixu[:]), ["ixu"], ["ixf"])
            ix4 = ixf[:].rearrange("p (h two) a -> p h two a", two=2)
            V(lambda e: e.tensor_copy(i1c[:].rearrange("p (h a) -> p h a", h=8), ix4[:, :, 0, :]), ["ixf"], ["i1c"])
            V(lambda e: e.tensor_copy(i2c[:].rearrange("p (h a) -> p h a", h=8), ix4[:, :, 1, :]), ["ixf"], ["i2c"])
            wx = scs[:].rearrange("p a b -> p (a b)").rearrange("p (h c) -> p h c", h=8)
            mk = sc2[:].rearrange("p a b -> p (a b)").rearrange("p (h c) -> p h c", h=8)
            V(lambda e: e.tensor_tensor(wx, cand[:], b3(s16[:, :, 0], 8, 256), ALU.subtract), ["cand", "s16", "ixu"], ["scs"])
            A(lambda e: e.activation(out=wx, in_=wx, func=AF.Exp), ["scs"], ["scs"])
            V(lambda e: e.tensor_tensor(mk, cand[:], b3(s16[:, :, 15], 8, 256), ALU.is_ge), ["cand", "s16", "ixu"], ["sc2"])
            V(lambda e: e.tensor_tensor(wx, wx, mk, ALU.mult), ["scs", "sc2"], ["scs"])
            V(lambda e: e.tensor_reduce(zs[:], wx, AX.X, ALU.add), ["scs"], ["zs"])
            V(lambda e: e.reciprocal(zs[:], zs[:]), ["zs"], ["zs"])
            V(lambda e: e.tensor_tensor(wx, wx, b3(zs[:], 8, 256), ALU.mult), ["scs", "zs"], ["scs"])
            wperm = sc2[:].rearrange("p a b -> p (a b)").rearrange("p (b h a) -> p b h a", b=16, h=8)
            V(lambda e: e.tensor_copy(wperm, scs[:].rearrange("p a b -> p (a b)").rearrange("p (h a b) -> p b h a", h=8, a=16)), ["scs", "sc2"], ["sc2"])
            w2d = sc2[:].rearrange("p a b -> p (a b)")
            for b in range(16):
                M(lambda e, b=b: e.transpose(pA[:, b * 128:(b + 1) * 128], w2d[:, b * 128:(b + 1) * 128], idf[:]), ["sc2", "idf"], ["pA"])
            V(lambda e: e.tensor_copy(wT[:].rearrange("p t b -> p b t"), pA[:].rearrange("p (b t) -> p b t", b=16)), ["pA"], ["wT"])
            M(lambda e: e.transpose(pQ[:, 0:128], i1c[:], idf[:]), ["i1c", "idf"], ["pQ"])
            M(lambda e: e.transpose(pQ[:, 128:256], i2c[:], idf[:]), ["i2c", "idf"], ["pQ"])
            V(lambda e: e.tensor_copy(i1T[:], pQ[:, 0:128]), ["pQ"], ["i1T"])
            V(lambda e: e.tensor_copy(i2T[:], pQ[:, 128:256]), ["pQ"], ["i2T"])
            for nb in range(128 // NB):
                tsl = slice(nb * NB, (nb + 1) * NB)
                io3 = iota_i[:].rearrange("p (o i) -> p o i", o=1).to_broadcast([128, NB, 128])
                V(lambda e, tsl=tsl: e.tensor_tensor(P1b[:], io3, b3(i1T[:, tsl], NB, 128), ALU.is_equal), ["iota_i", "i1T", "P1b"], ["P1b"])
                V(lambda e, tsl=tsl: e.tensor_tensor(P2b[:], io3, b3(i2T[:, tsl], NB, 128), ALU.is_equal), ["iota_i", "i2T", "P2b"], ["P2b"])
                V(lambda e, tsl=tsl: e.tensor_tensor(Wb[:].rearrange("p t (h b) -> p t h b", h=8),
                                                      wT[:, tsl, :].rearrange("p t (o b) -> p t o b", o=1).to_broadcast([128, NB, 8, 16]),
                                                      bm[:].rearrange("p (o h q) -> p o h q", o=1, q=1).to_broadcast([128, NB, 8, 16]), ALU.mult), ["wT", "bm", "Wb"], ["Wb"])
                for q4 in range(NB // 4):
                    for j in range(4):
                        tk = q4 * 4 + j
                        M(lambda e, tk=tk, j=j: e.matmul(pQ[:, j * 128:(j + 1) * 128], lhsT=Wb[:, tk, :], rhs=P1b[:, tk, :], start=True, stop=True), ["Wb", "P1b"], ["pQ"])
                    A(lambda e: e.copy(Tsb[:].rearrange("p a b -> p (a b)"), pQ[:]), ["pQ"], ["Tsb"])
                    for j in range(4):
                        tk = q4 * 4 + j
                        M(lambda e, tk=tk, j=j: e.matmul(pG2[:, j * 128:(j + 1) * 128], lhsT=P2b[:, tk, :], rhs=Tsb[:, j, :], start=True, stop=True), ["P2b", "Tsb"], ["pG2"])
                    t0 = nb * NB + q4 * 4
                    V(lambda e, t0=t0: e.tensor_copy(Gt[:, t0:t0 + 4, :].rearrange("p a b -> p (a b)"), pG2[:]), ["pG2"], ["Gt"])

        def dense_block(t):
            ts_ = slice(t * 128, (t + 1) * 128)
            def zstage(i):
                sl = i % 2; ds_ = i % ND
                S.dma(lambda e, i=i, ds_=ds_: e.dma_start(out=ubuf[ds_][:], in_=UTs[i]), reads=[f"UTs{i}"], writes=[f"ubuf{ds_}"])
                S.dma(lambda e, i=i, ds_=ds_: e.dma_start(out=vbuf[ds_][:], in_=Vs[i]), reads=[f"Vs{i}"], writes=[f"vbuf{ds_}"], eng="pool")
                for k in range(8):
                    M(lambda e, k=k, sl=sl, ds_=ds_: e.matmul(pZs[sl][:, 0:128], lhsT=ubuf[ds_][:, k * 128:(k + 1) * 128], rhs=h2T[:, k, ts_], start=(k == 0), stop=(k == 7)), [f"ubuf{ds_}", "h2T"], [f"pZ{sl}"])
                A(lambda e, sl=sl: e.activation(out=zg[sl][:], in_=pZs[sl][:, 0:128], func=AF.Gelu), [f"pZ{sl}"], [f"zg{sl}"])
                V(lambda e, sl=sl, i=i: e.tensor_tensor(AT[sl][:], zg[sl][:], Gt[:, :, i], ALU.mult), [f"zg{sl}", "Gt"], [f"AT{sl}"])

            def ostage(i):
                sl = i % 2; ds_ = i % ND
                for half in range(2):
                    M(lambda e, half=half, sl=sl, ds_=ds_, i=i: e.matmul(pA[:, half * 512:(half + 1) * 512], lhsT=AT[sl][:], rhs=vbuf[ds_][:, half * 512:(half + 1) * 512], start=(i == 0), stop=(i == n_i - 1)), [f"AT{sl}", f"vbuf{ds_}"], ["pA"])

            zstage(0)
            for i in range(n_i):
                if i + 1 < n_i:
                    zstage(i + 1)
                ostage(i)
            S.dma(lambda e: e.dma_start(out=hmt[:], in_=out[t * 128:(t + 1) * 128, :]), reads=[f"out{t}"], writes=["hmt"])
            for half in range(2):
                hs = slice(half * 512, (half + 1) * 512)
                V(lambda e, hs=hs: e.tensor_tensor(res[:, hs], hmt[:, hs], pA[:, hs], ALU.add), ["hmt", "pA"], ["res"])
            S.dma(lambda e: e.dma_start(out=out[t * 128:(t + 1) * 128, :], in_=res[:]), reads=["res"], writes=[f"out{t}"])

        if "keephm" in off:
            S.dma(lambda e: e.dma_start(out=hmt[:], in_=out[0:128, :]), reads=["out0"], writes=["hmt"])
            S.dma(lambda e: e.dma_start(out=out[128:256, :], in_=hmt[:]), reads=["hmt"], writes=["out1"])
        for t in range(n_own):
            peer_front(t)
            gate_build(t)
            dense_block(t)
        S.emit()
        return nc


def _host_inputs(inp):
    f = np.float32
    x = np.ascontiguousarray(inp["x"][0]); meta = inp["meta_tokens"]
    common = {}
    common["ident"] = np.eye(128, dtype=f)
    kq = np.arange(128)
    common["mask_cur"] = (kq[:, None] <= kq[None, :]).astype(f)
    common["xmeta"] = np.concatenate([meta, np.zeros((112, 1024), f)], 0)
    w_in = inp["w_in"][0]
    qperm = np.concatenate([np.arange(64) + 64 * h for h in (0, 4, 1, 5, 2, 6, 3, 7)])
    w_in = np.concatenate([w_in[:, :512][:, qperm], w_in[:, 512:]], 1)
    common["w_in"] = np.ascontiguousarray(w_in.reshape(8, 128, 1280).transpose(1, 0, 2))
    common["mask_prev"] = (kq[:, None] > kq[None, :]).astype(f)
    common["g1"] = np.ascontiguousarray(inp["norm1_g"][0].reshape(8, 128).T)
    common["gq2"] = np.tile(inp["q_norm_g"][0], 2).reshape(128, 1).astype(f)
    common["gk2"] = np.tile(inp["k_norm_g"][0], 2).reshape(128, 1).astype(f)
    common["sinks"] = np.ascontiguousarray(np.broadcast_to(inp["attn_sinks"][0][None, :], (64, 8))).astype(f)

    def state_layout(a):
        return np.ascontiguousarray(a.reshape(16, 2, 64).transpose(1, 2, 0).reshape(128, 16))

    def row_layout(a):
        return np.ascontiguousarray(np.broadcast_to(a.reshape(1, 2048), (128, 2048)))
    ldt64 = np.ascontiguousarray(np.broadcast_to(inp["ssm_log_dt"][0][:, None], (32, 64)))
    for nm, a in (("a_re", inp["ssm_a_re"][0]), ("a_im", inp["ssm_a_im"][0]), ("ldt", ldt64)):
        common[nm + "_s"] = state_layout(a); common[nm + "_r"] = row_layout(a)
    for nm, B in (("re", inp["ssm_b_re"][0]), ("im", inp["ssm_b_im"][0])):
        bT = np.zeros((128, 16, 128), f); bs = np.zeros((128, 16, 32), f)
        for g in range(32):
            i, hh = g // 2, g % 2
            bT[(g % 8) * 16:(g % 8) * 16 + 16, i, hh * 64:hh * 64 + 64] = B[g].T
            bs[hh * 64:hh * 64 + 64, i, hh * 16:hh * 16 + 16] = B[g]
        common["bT_" + nm] = bT; common["bs_" + nm] = bs
    for nm, C in (("re", inp["ssm_c_re"][0]), ("im", inp["ssm_c_im"][0])):
        cb = np.zeros((128, 16, 128), f)
        for g in range(32):
            i, hh = g // 2, g % 2
            cb[hh * 64:hh * 64 + 64, i, (g % 8) * 16:(g % 8) * 16 + 16] = C[g].T
        common["cb_" + nm] = cb
    common["ssm_d"] = np.ascontiguousarray(inp["ssm_d"][0].reshape(4, 128).T)
    common["glu_w"] = np.ascontiguousarray(inp["ssm_glu_w"][0].reshape(4, 128, 512).transpose(1, 0, 2))
    common["glu_b"] = np.ascontiguousarray(inp["ssm_glu_b"][0].reshape(4, 128).T)
    common["ga"] = np.ascontiguousarray(inp["attn_out_g"][0].reshape(8, 64).T)
    common["gs"] = np.ascontiguousarray(inp["ssm_out_g"][0].reshape(4, 128).T)
    common["woa"] = np.ascontiguousarray(inp["w_out"][0][:512].reshape(8, 64, 1024).transpose(1, 0, 2))
    common["wos"] = np.ascontiguousarray(inp["w_out"][0][512:].reshape(4, 128, 1024).transpose(1, 0, 2))
    common["g2"] = np.ascontiguousarray(inp["norm2_g"][0].reshape(8, 128).T)
    if inp.get("peer_w_query") is not None:
        common["wq"] = np.ascontiguousarray(inp["peer_w_query"][0].reshape(8, 128, 2048).transpose(1, 0, 2))
        sk = inp["peer_sub_keys"][0]
        common["keysT"] = np.ascontiguousarray(sk.reshape(16, 128, 128).transpose(2, 0, 1))
    if inp.get("peer_u") is not None:
        common["puT"] = np.ascontiguousarray(inp["peer_u"][0].reshape(128, 128, 8, 128).transpose(0, 3, 2, 1)).reshape(128, 128, 1024)
        common["pv"] = np.ascontiguousarray(inp["peer_v"][0].reshape(128, 128, 1024))
        common["iota_i"] = np.ascontiguousarray(np.broadcast_to(np.arange(128, dtype=f)[None, :], (128, 128)))
        common["blkmask"] = (np.arange(128)[:, None] // 16 == np.arange(8)[None, :]).astype(f)
    maps = []
    for r in range(NCORES):
        m = dict(common)
        m["xown"] = x[r * TOK:(r + 1) * TOK]
        m["xhalo"] = x[r * TOK - 128:r * TOK] if r > 0 else np.zeros((128, 1024), f)
        m["mask_halo"] = common["mask_prev"] if r > 0 else np.zeros((128, 128), f)
        pre = np.zeros((NPRE * 128, 1024), f)
        n = r * TOK
        pre[NPRE * 128 - n - 16:NPRE * 128 - n] = meta
        if n:
            pre[NPRE * 128 - n:] = x[:n]
        m["xpre"] = pre
        m["is0"] = np.full((128, 1), 1.0 if r == 0 else 0.0, f)
        maps.append(m)
    return maps


def kernel(**inputs):
    nc = build("full")
    maps = _host_inputs({k: np.asarray(v) for k, v in inputs.items()})
    res = run_bass_kernel_spmd(nc, maps, core_ids=list(range(NCORES)))
    return np.concatenate([r["out"] for r in res.results], 0)[None].astype(np.float32)
```

```python
import contextlib
import types
import numpy as np
import concourse.bass as bass
import concourse.mybir as mybir
from concourse.bass_utils import run_bass_kernel_spmd

F32 = mybir.dt.float32
BF16 = mybir.dt.bfloat16
U32 = mybir.dt.uint32
AF = mybir.ActivationFunctionType
ALU = mybir.AluOpType
AX = mybir.AxisListType

NCORES = 8
TOK = 2048
NT = 16
NPRE = 113
EPS = 1e-6
ENGS = ("pe", "act", "dve", "pool", "sp")


class StopBuild(Exception):
    pass


def _freeze(fn):
    if fn.__closure__ is None:
        return fn
    cells = []
    for c in fn.__closure__:
        try:
            cells.append(types.CellType(c.cell_contents))
        except ValueError:
            cells.append(c)
    return types.FunctionType(fn.__code__, fn.__globals__, fn.__name__, fn.__defaults__, tuple(cells))


class Sched:
    def __init__(self, nc, n_dma_streams=8):
        self.nc = nc
        self.ops = {e: [] for e in ENGS}
        self.count = {}
        self.last_w = {}
        self.readers = {}
        self.seen = {e: {} for e in ENGS}
        self.n_dma = n_dma_streams
        self.dma_rr = {e: 0 for e in ENGS}
        self.pending = {e: {} for e in ENGS}
        self.pe_run = []

    def barrier(self):
        self._flush_pe()
        for e in ENGS:
            for k, v in self.count.items():
                if self.pending[e].get(k, 0) < v:
                    self.pending[e][k] = v

    def _deps(self, eng, reads, writes):
        need = dict(self.pending[eng])
        self.pending[eng] = {}

        def add(ev):
            if ev is not None and need.get(ev[0], 0) < ev[1]:
                need[ev[0]] = ev[1]
        for b in reads:
            add(self.last_w.get(b))
        for b in writes:
            add(self.last_w.get(b))
            for ev in self.readers.get(b, ()):
                add(ev)
        waits = []
        for k, v in need.items():
            if eng == "pe" and k == ("e", "pe"):
                continue
            if self.seen[eng].get(k, 0) < v:
                self.seen[eng][k] = v
                waits.append((k, v))
        return waits

    def _record(self, ev, reads, writes):
        for b in reads:
            self.readers.setdefault(b, []).append(ev)
        for b in writes:
            self.last_w[b] = ev
            self.readers[b] = []

    def _flush_pe(self):
        if self.pe_run:
            k = ("e", "pe")
            self.count[k] = self.count.get(k, 0) + 1
            self.pe_run[-1][2] = (k, 1)
            self.pe_run = []

    def op(self, eng, fn, reads=(), writes=()):
        fn = _freeze(fn)
        if eng != "pe":
            self._flush_pe()
        waits = self._deps(eng, reads, writes)
        k = ("e", eng)
        if eng == "pe":
            v = self.count.get(k, 0) + 1
            entry = [waits, fn, None]
            self.ops[eng].append(entry)
            self.pe_run.append(entry)
            self._record((k, v), reads, writes)
            return
        v = self.count.get(k, 0) + 1
        self.count[k] = v
        self.ops[eng].append([waits, fn, (k, 1)])
        self._record((k, v), reads, writes)

    def dma(self, fn, reads=(), writes=(), eng="sp"):
        fn = _freeze(fn)
        self._flush_pe()
        waits = self._deps(eng, reads, writes)
        k = ("d", eng, self.dma_rr[eng] % self.n_dma)
        self.dma_rr[eng] += 1
        v = self.count.get(k, 0) + 16
        self.count[k] = v
        self.ops[eng].append([waits, fn, (k, 16)])
        self._record((k, v), reads, writes)

    def emit(self):
        self._flush_pe()
        nc = self.nc
        keys = sorted(self.count.keys(), key=str)
        with contextlib.ExitStack() as st:
            sems = {k: st.enter_context(nc.semaphore("s_" + "_".join(map(str, k)))) for k in keys}
            block = st.enter_context(nc.Block())
            final = [(k, self.count[k]) for k in keys]

            def run(engobj, lst, is_final):
                for waits, fn, incr in lst:
                    for wk, wv in waits:
                        engobj.wait_ge(sems[wk], wv)
                    ins_ = fn(engobj)
                    if incr is not None:
                        ins_.then_inc(sems[incr[0]], incr[1])
                if is_final:
                    for wk, wv in final:
                        engobj.wait_ge(sems[wk], wv)
            names = {"pe": "tensor", "act": "scalar", "dve": "vector", "pool": "gpsimd", "sp": "sync"}
            for e in ENGS:
                lst = self.ops[e]
                getattr(block, names[e])(lambda engobj, lst=lst, e=e: run(engobj, lst, e == "sp"))


def build(stage="full", off=()):
    nc = bass.Bass("TRN2", target_bir_lowering=False)
    S = Sched(nc)
    D = {}

    def din(name, shape, dt=F32):
        D[name] = nc.dram_tensor(name, list(shape), dt, kind="ExternalInput").ap()
        return D[name]
    xown = din("xown", [TOK, 1024]); xhalo = din("xhalo", [128, 1024]); xpre = din("xpre", [NPRE * 128, 1024])
    xmeta = din("xmeta", [128, 1024])
    din("ident", [128, 128]); din("mask_cur", [128, 128]); din("mask_prev", [128, 128]); din("mask_halo", [128, 128])
    din("w_in", [128, 8, 1280]); din("g1", [128, 8]); din("gq2", [128, 1]); din("gk2", [128, 1]); din("sinks", [64, 8])
    for nm in ("a_re", "a_im", "ldt"):
        din(nm + "_s", [128, 16]); din(nm + "_r", [128, 2048])
    din("bT_re", [128, 16, 128]); din("bT_im", [128, 16, 128]); din("bs_re", [128, 16, 32]); din("bs_im", [128, 16, 32])
    din("cb_re", [128, 16, 128]); din("cb_im", [128, 16, 128]); din("ssm_d", [128, 4]); din("glu_w", [128, 4, 512])
    din("glu_b", [128, 4]); din("ga", [64, 8]); din("gs", [128, 4]); din("woa", [64, 8, 1024]); din("wos", [128, 4, 1024])
    din("g2", [128, 8]); din("is0", [128, 1])
    if stage in ("peerfront", "full"):
        din("wq", [128, 8, 2048]); din("keysT", [128, 16, 128])
    if stage == "full":
        din("puT", [128, 128, 1024]); din("pv", [128, 128, 1024]); din("iota_i", [128, 128]); din("blkmask", [128, 8])
        UTs = nc.dram_tensor("UTs", [128, 128, 1024], BF16).ap(); Vs = nc.dram_tensor("Vs", [128, 128, 1024], BF16).ap()
    out = nc.dram_tensor("out", [TOK, 1024], F32, kind="ExternalOutput").ap()

    with contextlib.ExitStack() as top:
        def SB(st, name, shape, dt=F32):
            return st.enter_context(nc.sbuf_tensor("sb_" + name, list(shape), dt))

        def PS(st, name, shape, dt=F32):
            return st.enter_context(nc.psum_tensor("ps_" + name, list(shape), dt))

        def V(fn, r, w): S.op("dve", fn, r, w)
        def A(fn, r, w): S.op("act", fn, r, w)
        def G(fn, r, w): S.op("pool", fn, r, w)
        def M(fn, r, w): S.op("pe", fn, r, w)

        @contextlib.contextmanager
        def scope():
            with contextlib.ExitStack() as st_:
                yield st_
                S.barrier()

        def load(dst, src, key):
            S.dma(lambda e: e.dma_start(out=dst, in_=src), writes=[key])

        idf = SB(top, "idf", [128, 128]); idb = SB(top, "idb", [128, 128], BF16)
        load(idf[:], D["ident"], "idf")
        V(lambda e: e.tensor_copy(idb[:], idf[:]), ["idf"], ["idb"])
        ones = SB(top, "ones", [128, 64], BF16)
        V(lambda e: e.memset(ones[:], 1.0), [], ["ones"])
        h2T = SB(top, "h2T", [128, 8, TOK if "smallh2T" not in off else 128], BF16)

        mix = top.enter_context(contextlib.ExitStack())
        ptr = PS(mix, "ptr", [128, 1024], BF16)
        psA = PS(mix, "psA", [128, 1024])
        pb = [PS(mix, f"pb{i}", [128, 512]) for i in range(5)]

        win = SB(mix, "win", [128, 8, 1280], BF16); woa = SB(mix, "woa", [64, 8, 1024], BF16)
        wos = SB(mix, "wos", [128, 4, 1024], BF16); glu = SB(mix, "glu", [128, 4, 512], BF16)
        small = {}
        for nm, shp in (("g1", [128, 8]), ("gq2", [128, 1]), ("gk2", [128, 1]), ("sinks", [64, 8]), ("ssm_d", [128, 4]),
                        ("glu_b", [128, 4]), ("ga", [64, 8]), ("gs", [128, 4]), ("g2", [128, 8]), ("is0", [128, 1])):
            small[nm] = SB(mix, "c_" + nm, shp)
            load(small[nm][:], D[nm], "c_" + nm)
        with scope() as stg:
            s1 = SB(stg, "stg1", [128, 8, 1280])
            load(s1[:], D["w_in"], "stg1")
            for k in range(8):
                V(lambda e, k=k: e.tensor_scalar(win[:, k, :], s1[:, k, :], small["g1"][:, k:k + 1], None, ALU.mult),
                  ["stg1", "c_g1"], ["win"])
        with scope() as stg:
            s2 = SB(stg, "stg2", [64, 8, 1024]); s3 = SB(stg, "stg3", [128, 4, 1024]); s4 = SB(stg, "stg4", [128, 4, 512])
            load(s2[:], D["woa"], "stg2"); load(s3[:], D["wos"], "stg3"); load(s4[:], D["glu_w"], "stg4")
            for h in range(8):
                V(lambda e, h=h: e.tensor_scalar(woa[:, h, :], s2[:, h, :], small["ga"][:, h:h + 1], None, ALU.mult),
                  ["stg2", "c_ga"], ["woa"])
            for c in range(4):
                V(lambda e, c=c: e.tensor_scalar(wos[:, c, :], s3[:, c, :], small["gs"][:, c:c + 1], None, ALU.mult),
                  ["stg3", "c_gs"], ["wos"])
            V(lambda e: e.tensor_copy(glu[:], s4[:]), ["stg4"], ["glu"])
        cqk = SB(mix, "cqk", [128, 1]); esink = SB(mix, "esink", [64, 8])
        V(lambda e: e.tensor_tensor(cqk[:], small["gq2"][:], small["gk2"][:], ALU.mult), ["c_gq2", "c_gk2"], ["cqk"])
        V(lambda e: e.tensor_scalar(cqk[:], cqk[:], 0.125, None, ALU.mult), ["cqk"], ["cqk"])
        A(lambda e: e.activation(out=esink[:], in_=small["sinks"][:], func=AF.Exp), ["c_sinks"], ["esink"])
        mcur = SB(mix, "mcur", [128, 128], BF16); mprev = SB(mix, "mprev", [128, 128], BF16); mhalo = SB(mix, "mhalo", [128, 128], BF16)
        with scope() as stg:
            m1 = SB(stg, "m1", [128, 128]); m2 = SB(stg, "m2", [128, 128]); m3 = SB(stg, "m3", [128, 128])
            load(m1[:], D["mask_cur"], "m1"); load(m2[:], D["mask_prev"], "m2"); load(m3[:], D["mask_halo"], "m3")
            V(lambda e: e.tensor_copy(mcur[:], m1[:]), ["m1"], ["mcur"])
            V(lambda e: e.tensor_copy(mprev[:], m2[:]), ["m2"], ["mprev"])
            V(lambda e: e.tensor_copy(mhalo[:], m3[:]), ["m3"], ["mhalo"])

        TWO_PI = 2.0 * np.pi

        def ssm_scalars(st, tag, are, aim, ldt, shape, t=None):
            if t is None:
                t = {n: SB(st, f"{tag}_{n}", shape) for n in ("dt", "rho", "th", "ph", "cos", "sin", "abr", "abi", "den", "nr", "t1", "t2", "cfr", "cfi")}
                t["ri"] = SB(st, f"{tag}_ri", shape, mybir.dt.int32)
            k = lambda n: f"{tag}_{n}"
            A(lambda e: e.activation(out=t["dt"][:], in_=ldt[:], func=AF.Exp), [k("ldt")], [k("dt")])
            V(lambda e: e.tensor_tensor(t["rho"][:], are[:], t["dt"][:], ALU.mult), [k("are"), k("dt")], [k("rho")])
            A(lambda e: e.activation(out=t["rho"][:], in_=t["rho"][:], func=AF.Exp), [k("rho")], [k("rho")])
            V(lambda e: e.tensor_tensor(t["th"][:], aim[:], t["dt"][:], ALU.mult), [k("aim"), k("dt")], [k("th")])
            for nm, off in (("sin", 0.0), ("cos", np.pi / 2)):
                V(lambda e, off=off: e.tensor_scalar(t["t2"][:], t["th"][:], float(off), None, ALU.add), [k("th")], [k("t2")])
                V(lambda e: e.tensor_scalar(t["t1"][:], t["t2"][:], float(1.0 / TWO_PI), None, ALU.mult), [k("t2")], [k("t1")])
                V(lambda e: e.tensor_copy(t["ri"][:], t["t1"][:]), [k("t1")], [k("ri")])
                V(lambda e: e.tensor_copy(t["t1"][:], t["ri"][:]), [k("ri")], [k("t1")])
                V(lambda e: e.scalar_tensor_tensor(t["ph"][:], t["t1"][:], float(-TWO_PI), t["t2"][:], ALU.mult, ALU.add), [k("t1"), k("t2")], [k("ph")])
                V(lambda e: e.tensor_scalar(t["t1"][:], t["ph"][:], float(np.pi), float(-TWO_PI), ALU.is_gt, ALU.mult), [k("ph")], [k("t1")])
                V(lambda e: e.tensor_tensor(t["ph"][:], t["ph"][:], t["t1"][:], ALU.add), [k("ph"), k("t1")], [k("ph")])
                A(lambda e, nm=nm: e.activation(out=t[nm][:], in_=t["ph"][:], func=AF.Sin), [k("ph")], [k(nm)])
            V(lambda e: e.tensor_tensor(t["abr"][:], t["rho"][:], t["cos"][:], ALU.mult), [k("rho"), k("cos")], [k("abr")])
            V(lambda e: e.tensor_tensor(t["abi"][:], t["rho"][:], t["sin"][:], ALU.mult), [k("rho"), k("sin")], [k("abi")])
            V(lambda e: e.tensor_tensor(t["den"][:], are[:], are[:], ALU.mult), [k("are")], [k("den")])
            V(lambda e: e.tensor_tensor(t["t1"][:], aim[:], aim[:], ALU.mult), [k("aim")], [k("t1")])
            V(lambda e: e.tensor_tensor(t["den"][:], t["den"][:], t["t1"][:], ALU.add), [k("den"), k("t1")], [k("den")])
            V(lambda e: e.reciprocal(t["den"][:], t["den"][:]), [k("den")], [k("den")])
            V(lambda e: e.tensor_scalar(t["nr"][:], t["abr"][:], -1.0, None, ALU.add), [k("abr")], [k("nr")])
            V(lambda e: e.tensor_tensor(t["t1"][:], t["nr"][:], are[:], ALU.mult), [k("nr"), k("are")], [k("t1")])
            V(lambda e: e.tensor_tensor(t["t2"][:], t["abi"][:], aim[:], ALU.mult), [k("abi"), k("aim")], [k("t2")])
            V(lambda e: e.tensor_tensor(t["t1"][:], t["t1"][:], t["t2"][:], ALU.add), [k("t1"), k("t2")], [k("t1")])
            V(lambda e: e.tensor_tensor(t["cfr"][:], t["t1"][:], t["den"][:], ALU.mult), [k("t1"), k("den")], [k("cfr")])
            V(lambda e: e.tensor_tensor(t["t1"][:], t["abi"][:], are[:], ALU.mult), [k("abi"), k("are")], [k("t1")])
            V(lambda e: e.tensor_tensor(t["t2"][:], t["nr"][:], aim[:], ALU.mult), [k("nr"), k("aim")], [k("t2")])
            V(lambda e: e.tensor_tensor(t["t1"][:], t["t1"][:], t["t2"][:], ALU.subtract), [k("t1"), k("t2")], [k("t1")])
            V(lambda e: e.tensor_tensor(t["cfi"][:], t["t1"][:], t["den"][:], ALU.mult), [k("t1"), k("den")], [k("cfi")])
            return t

        def cmul(dst_r, dst_i, ar, ai, br, bi, t1, t2, rk, wk):
            V(lambda e: e.tensor_tensor(t1, ar, br, ALU.mult), rk, wk)
            V(lambda e: e.tensor_tensor(t2, ai, bi, ALU.mult), rk, wk)
            V(lambda e: e.tensor_tensor(dst_r, t1, t2, ALU.subtract), rk + wk, wk)
            V(lambda e: e.tensor_tensor(t1, ar, bi, ALU.mult), rk + wk, wk)
            V(lambda e: e.tensor_tensor(t2, ai, br, ALU.mult), rk + wk, wk)
            V(lambda e: e.tensor_tensor(dst_i, t1, t2, ALU.add), rk + wk, wk)

        ss_are = SB(mix, "ss_are", [128, 16]); ss_aim = SB(mix, "ss_aim", [128, 16]); ss_ldt = SB(mix, "ss_ldt", [128, 16])
        load(ss_are[:], D["a_re_s"], "ss_are"); load(ss_aim[:], D["a_im_s"], "ss_aim"); load(ss_ldt[:], D["ldt_s"], "ss_ldt")
        sc = ssm_scalars(mix, "ss", ss_are, ss_aim, ss_ldt, [128, 16])
        Er = SB(mix, "Er", [128, 16, 128]); Ei = SB(mix, "Ei", [128, 16, 128]); Rho0 = SB(mix, "Rho0", [128, 16, 128])
        WTr = SB(mix, "WTr", [128, 16, 128], BF16); WTi = SB(mix, "WTi", [128, 16, 128], BF16)
        A128r = SB(mix, "A128r", [128, 16]); A128i = SB(mix, "A128i", [128, 16])
        E127r = SB(mix, "E127r", [128, 16]); E127i = SB(mix, "E127i", [128, 16])
        bsr = SB(mix, "bsr", [128, 16, 32]); bsi = SB(mix, "bsi", [128, 16, 32])
        BTr = SB(mix, "BTr", [128, 16, 128], BF16); BTi = SB(mix, "BTi", [128, 16, 128], BF16)
        Cbr = SB(mix, "Cbr", [128, 16, 128], BF16); Cbi = SB(mix, "Cbi", [128, 16, 128], BF16)

        def bc3(ap2, n):
            return ap2.rearrange("p (a o) -> p a o", o=1).to_broadcast([128, 16, n])

        with scope() as stg:
            Hr = SB(stg, "Hr", [128, 16, 128]); Hi = SB(stg, "Hi", [128, 16, 128])
            T1 = SB(stg, "T1", [128, 16, 128]); T2 = SB(stg, "T2", [128, 16, 128])
            q1 = SB(stg, "q1", [128, 16]); q2 = SB(stg, "q2", [128, 16]); ir = SB(stg, "ir", [128, 16]); ii_ = SB(stg, "ii", [128, 16])
            pr = SB(stg, "pr", [128, 16]); pi_ = SB(stg, "pi", [128, 16])
            V(lambda e: e.memset(Er[:, :, 0:1], 1.0), [], ["Er"]); V(lambda e: e.memset(Ei[:, :, 0:1], 0.0), [], ["Ei"])
            V(lambda e: e.tensor_copy(Er[:, :, 1], sc["cos"][:]), ["ss_cos"], ["Er"])
            V(lambda e: e.tensor_copy(Ei[:, :, 1], sc["sin"][:]), ["ss_sin"], ["Ei"])
            V(lambda e: e.tensor_tensor(q1[:], sc["rho"][:], sc["rho"][:], ALU.mult), ["ss_rho"], ["q1"])
            V(lambda e: e.reciprocal(q1[:], q1[:]), ["q1"], ["q1"])
            V(lambda e: e.tensor_tensor(ir[:], sc["abr"][:], q1[:], ALU.mult), ["ss_abr", "q1"], ["ir"])
            V(lambda e: e.tensor_tensor(ii_[:], sc["abi"][:], q1[:], ALU.mult), ["ss_abi", "q1"], ["ii"])
            V(lambda e: e.tensor_scalar(ii_[:], ii_[:], -1.0, None, ALU.mult), ["ii"], ["ii"])
            V(lambda e: e.memset(Hr[:, :, 0:1], 1.0), [], ["Hr"]); V(lambda e: e.memset(Hi[:, :, 0:1], 0.0), [], ["Hi"])
            V(lambda e: e.tensor_copy(Hr[:, :, 1], ir[:]), ["ir"], ["Hr"]); V(lambda e: e.tensor_copy(Hi[:, :, 1], ii_[:]), ["ii"], ["Hi"])
            for (Xr, Xi, kr, ki) in ((Er, Ei, "Er", "Ei"), (Hr, Hi, "Hr", "Hi")):
                m = 2
                while m < 128:
                    h = m // 2
                    cmul(Xr[:, :, m], Xi[:, :, m], Xr[:, :, h], Xi[:, :, h], Xr[:, :, h], Xi[:, :, h], q1[:], q2[:], [kr, ki, "q1", "q2"], [kr, ki, "q1", "q2"])
                    cmul(Xr[:, :, m + 1:2 * m], Xi[:, :, m + 1:2 * m], Xr[:, :, 1:m], Xi[:, :, 1:m],
                         bc3(Xr[:, :, m], m - 1), bc3(Xi[:, :, m], m - 1), T1[:, :, 1:m], T2[:, :, 1:m], [kr, ki, "T1", "T2"], [kr, ki, "T1", "T2"])
                    m *= 2
            V(lambda e: e.tensor_copy(A128r[:], sc["abr"][:]), ["ss_abr"], ["A128"]); V(lambda e: e.tensor_copy(A128i[:], sc["abi"][:]), ["ss_abi"], ["A128"])
            for _ in range(7):
                cmul(pr[:], pi_[:], A128r[:], A128i[:], A128r[:], A128i[:], q1[:], q2[:], ["A128", "q1", "q2", "pp"], ["pp", "q1", "q2"])
                V(lambda e: e.tensor_copy(A128r[:], pr[:]), ["pp"], ["A128"]); V(lambda e: e.tensor_copy(A128i[:], pi_[:]), ["pp"], ["A128"])
            V(lambda e: e.tensor_copy(E127r[:], Er[:, :, 127]), ["Er"], ["E127"]); V(lambda e: e.tensor_copy(E127i[:], Ei[:, :, 127]), ["Ei"], ["E127"])
            cmul(pr[:], pi_[:], A128r[:], A128i[:], ir[:], ii_[:], q1[:], q2[:], ["A128", "ir", "ii", "q1", "q2", "pp"], ["pp", "q1", "q2"])
            cmul(T1[:], T2[:], Hr[:], Hi[:], bc3(pr[:], 128), bc3(pi_[:], 128), Er[:] if False else Rho0[:], SB(stg, "T3", [128, 16, 128])[:],
                 ["Hr", "Hi", "pp", "Rho0", "T3", "T1", "T2"], ["T1", "T2", "Rho0", "T3"])
            for (Ts, WT, kk) in ((T1, WTr, "WTr"), (T2, WTi, "WTi")):
                for g4 in range(4):
                    for j in range(4):
                        i = 4 * g4 + j
                        M(lambda e, i=i, j=j, Ts=Ts: e.transpose(pb[0][:, j * 128:(j + 1) * 128], Ts[:, i, :], idf[:]), ["T1", "T2", "idf"], ["pb0"])
                    V(lambda e, g4=g4, WT=WT: e.tensor_copy(WT[:, 4 * g4:4 * g4 + 4, :], pb[0][:].rearrange("p (a b) -> p a b", a=4)), ["pb0"], [kk])
            V(lambda e: e.tensor_copy(Rho0[:], bc3(sc["rho"][:], 128)), ["ss_rho", "T1", "T2"], ["Rho0"])
            V(lambda e: e.memset(Rho0[:, :, 0:1], 0.0), ["Rho0"], ["Rho0"])
            b0r = SB(stg, "b0r", [128, 16, 32]); b0i = SB(stg, "b0i", [128, 16, 32]); u1 = SB(stg, "u1", [128, 16, 32]); u2 = SB(stg, "u2", [128, 16, 32])
            load(b0r[:], D["bs_re"], "b0r"); load(b0i[:], D["bs_im"], "b0i")
            cmul(bsr[:], bsi[:], bc3(sc["cfr"][:], 32), bc3(sc["cfi"][:], 32), b0r[:], b0i[:], u1[:], u2[:], ["ss_cfr", "ss_cfi", "b0r", "b0i", "u1", "u2"], ["bs", "u1", "u2"])
        with scope() as stg:
            CW = 512
            r_are = SB(stg, "rr_are", [128, CW]); r_aim = SB(stg, "rr_aim", [128, CW]); r_ldt = SB(stg, "rr_ldt", [128, CW])
            rc = None
            BTr2 = BTr[:].rearrange("p a b -> p (a b)"); BTi2 = BTi[:].rearrange("p a b -> p (a b)")
            Cbr2 = Cbr[:].rearrange("p a b -> p (a b)"); Cbi2 = Cbi[:].rearrange("p a b -> p (a b)")
            for cc in range(2048 // CW):
                cs = slice(cc * CW, (cc + 1) * CW)
                load(r_are[:], D["a_re_r"][:, cs], "rr_are"); load(r_aim[:], D["a_im_r"][:, cs], "rr_aim"); load(r_ldt[:], D["ldt_r"][:, cs], "rr_ldt")
                rc = ssm_scalars(stg, "rr", r_are, r_aim, r_ldt, [128, CW], t=rc)
                b1r = rc["dt"]; b1i = rc["th"]
                load(b1r[:], D["bT_re"].rearrange("p a b -> p (a b)")[:, cs], "rr_dt"); load(b1i[:], D["bT_im"].rearrange("p a b -> p (a b)")[:, cs], "rr_th")
                cmul(rc["cos"][:], rc["sin"][:], rc["cfr"][:], rc["cfi"][:], b1r[:], b1i[:], rc["t1"][:], rc["t2"][:],
                     ["rr_cfr", "rr_cfi", "rr_dt", "rr_th", "rr_t1", "rr_t2", "rr_cos", "rr_sin"], ["rr_cos", "rr_sin", "rr_t1", "rr_t2"])
                V(lambda e, cs=cs: e.tensor_copy(BTr2[:, cs], rc["cos"][:]), ["rr_cos"], ["BT"])
                V(lambda e, cs=cs: e.tensor_copy(BTi2[:, cs], rc["sin"][:]), ["rr_sin"], ["BT"])
                load(rc["t1"][:], D["cb_re"].rearrange("p a b -> p (a b)")[:, cs], "rr_t1"); load(rc["t2"][:], D["cb_im"].rearrange("p a b -> p (a b)")[:, cs], "rr_t2")
                V(lambda e, cs=cs: e.tensor_copy(Cbr2[:, cs], rc["t1"][:]), ["rr_t1"], ["Cb"])
                V(lambda e, cs=cs: e.tensor_scalar(Cbi2[:, cs], rc["t2"][:], -1.0, None, ALU.mult), ["rr_t2"], ["Cb"])

        xt = [SB(mix, f"xt{i}", [128, 1024]) for i in range(2)]
        junk = SB(mix, "junk", [128, 1024], BF16); xnb = SB(mix, "xnb", [128, 1024], BF16); nT = SB(mix, "nT", [128, 8, 128], BF16)
        st1 = SB(mix, "st1", [128, 4])
        Sr = SB(mix, "Sr", [128, 16]); Si = SB(mix, "Si", [128, 16])
        V(lambda e: e.memset(Sr[:], 0.0), [], ["S"]); V(lambda e: e.memset(Si[:], 0.0), [], ["S"])
        c1 = SB(mix, "c1", [128, 16]); c2 = SB(mix, "c2", [128, 16]); c3 = SB(mix, "c3", [128, 16]); c4 = SB(mix, "c4", [128, 16])
        xi = [0]

        def front(src):
            sl = xi[0] % 2; xi[0] += 1
            kx = f"xt{sl}"
            S.dma(lambda e: e.dma_start(out=xt[sl][:], in_=src), writes=[kx])
            A(lambda e: e.activation(out=junk[:], in_=xt[sl][:], func=AF.Square, accum_out=st1[:, 0:1]), [kx], ["junk", "st1"])
            V(lambda e: e.tensor_scalar(st1[:, 1:2], st1[:, 0:1], 1.0 / 1024, EPS, ALU.mult, ALU.add), ["st1"], ["st1b"])
            A(lambda e: e.activation(out=st1[:, 3:4], in_=st1[:, 1:2], func=AF.Sqrt), ["st1b"], ["st1d"])
            V(lambda e: e.reciprocal(st1[:, 2:3], st1[:, 3:4]), ["st1d"], ["st1c"])
            V(lambda e: e.tensor_scalar(xnb[:], xt[sl][:], st1[:, 2:3], None, ALU.mult), [kx, "st1c"], ["xnb"])
            for k in range(8):
                M(lambda e, k=k: e.transpose(ptr[:, k * 128:(k + 1) * 128], xnb[:, k * 128:(k + 1) * 128], idb[:]), ["xnb", "idb"], ["ptr"])
            A(lambda e: e.copy(nT[:].rearrange("p a b -> p (a b)"), ptr[:]), ["ptr"], ["nT"])
            return sl

        ub = SB(mix, "ub", [128, 512], BF16)
        fr = SB(mix, "fr", [128, 16]); fi = SB(mix, "fi", [128, 16])
        w1 = SB(mix, "w1", [128, 16, 32]); w2 = SB(mix, "w2", [128, 16, 32])

        def state_step():
            cmul(c3[:], c4[:], A128r[:], A128i[:], Sr[:], Si[:], c1[:], c2[:], ["A128", "S", "c"], ["c"])
            V(lambda e: e.tensor_tensor(Sr[:], c3[:], fr[:], ALU.add), ["c", "f"], ["S"])
            V(lambda e: e.tensor_tensor(Si[:], c4[:], fi[:], ALU.add), ["c", "f"], ["S"])

        def prefix_tile(src):
            front(src)
            for k in range(8):
                M(lambda e, k=k: e.matmul(pb[0][:], lhsT=nT[:, k, :], rhs=win[:, k, 768:1280], start=(k == 0), stop=(k == 7)), ["nT", "win"], ["pb0"])
            A(lambda e: e.copy(ub[:], pb[0][:]), ["pb0"], ["ub"])
            for i in range(16):
                M(lambda e, i=i: e.matmul(pb[1][:, i * 32:(i + 1) * 32], lhsT=WTr[:, i, :], rhs=ub[:, i * 32:(i + 1) * 32], start=True, stop=True), ["ub", "WTr"], ["pb1"])
                M(lambda e, i=i: e.matmul(pb[2][:, i * 32:(i + 1) * 32], lhsT=WTi[:, i, :], rhs=ub[:, i * 32:(i + 1) * 32], start=True, stop=True), ["ub", "WTi"], ["pb2"])
            mr = pb[1][:].rearrange("p (a b) -> p a b", a=16); mi = pb[2][:].rearrange("p (a b) -> p a b", a=16)
            V(lambda e: e.tensor_tensor(w1[:], bsr[:], mr, ALU.mult), ["bs", "pb1"], ["w1"])
            V(lambda e: e.tensor_tensor(w2[:], bsi[:], mi, ALU.mult), ["bs", "pb2"], ["w2"])
            V(lambda e: e.tensor_tensor(w1[:], w1[:], w2[:], ALU.subtract), ["w1", "w2"], ["w1"])
            V(lambda e: e.tensor_reduce(fr[:], w1[:], AX.X, ALU.add), ["w1"], ["f"])
            V(lambda e: e.tensor_tensor(w1[:], bsr[:], mi, ALU.mult), ["bs", "pb2", "f"], ["w1"])
            V(lambda e: e.tensor_tensor(w2[:], bsi[:], mr, ALU.mult), ["bs", "pb1"], ["w2"])
            V(lambda e: e.tensor_tensor(w1[:], w1[:], w2[:], ALU.add), ["w1", "w2"], ["w1"])
            V(lambda e: e.tensor_reduce(fi[:], w1[:], AX.X, ALU.add), ["w1"], ["f"])
            state_step()

        chunks = []
        if stage == "full":
            for (src, dst, nm) in ((D["puT"], UTs, "UTs"), (D["pv"], Vs, "Vs")):
                for i in range(128):
                    chunks.append((src, dst, nm, i))
        with scope() as pst:
            NS = 3
            if chunks:
                cst = [SB(pst, f"cst{i}", [128, 1024]) for i in range(NS)]; cbf = [SB(pst, f"cbf{i}", [128, 1024], BF16) for i in range(NS)]
            cpos = [0]

            def emit_chunks(n):
                for _ in range(n):
                    if cpos[0] >= len(chunks):
                        return
                    src, dst, nm, i = chunks[cpos[0]]; sl = cpos[0] % NS; cpos[0] += 1
                    S.dma(lambda e: e.dma_start(out=cst[sl][:], in_=src[i]), writes=[f"cst{sl}"])
                    G(lambda e: e.tensor_copy(cbf[sl][:], cst[sl][:]), [f"cst{sl}"], [f"cbf{sl}"])
                    S.dma(lambda e: e.dma_start(out=dst[i], in_=cbf[sl][:]), reads=[f"cbf{sl}"], writes=[f"{nm}{i}"], eng="pool")
            for t in range(NPRE if "prefix" not in off else 0):
                prefix_tile(xpre[t * 128:(t + 1) * 128, :])
                emit_chunks(3)
            emit_chunks(len(chunks))

        if stage == "prefix":
            dbg = SB(mix, "dbg", [128, 1024])
            V(lambda e: e.memset(dbg[:], 0.0), [], ["dbg"])
            V(lambda e: e.tensor_copy(dbg[:, 0:16], Sr[:]), ["S"], ["dbg"]); V(lambda e: e.tensor_copy(dbg[:, 16:32], Si[:]), ["S"], ["dbg"])
            for j, (ap, key) in enumerate([(sc["cos"][:], "ss_cos"), (sc["sin"][:], "ss_sin"), (sc["abr"][:], "ss_abr"), (sc["abi"][:], "ss_abi"),
                                           (sc["cfr"][:], "ss_cfr"), (sc["cfi"][:], "ss_cfi"), (Er[:, :, 5], "Er"), (Ei[:, :, 5], "Ei"),
                                           (A128r[:], "A128"), (A128i[:], "A128"), (fr[:], "f"), (fi[:], "f"), (sc["rho"][:], "ss_rho"), (sc["th"][:], "ss_th")]):
                V(lambda e, j=j, ap=ap: e.tensor_copy(dbg[:, 32 + 16 * j:48 + 16 * j], ap), [key], ["dbg"])
            V(lambda e: e.tensor_copy(dbg[:, 512:1024], ub[:]), ["ub"], ["dbg"])
            S.dma(lambda e: e.dma_start(out=out[0:128, :], in_=dbg[:]), reads=["dbg"], writes=["out"])
            S.emit()
            return nc

        qkv_sb = SB(mix, "qkv_sb", [128, 768]); sqt = SB(mix, "sqt", [128, 640]); s10 = SB(mix, "s10", [128, 10]); s10b = SB(mix, "s10b", [128, 10])
        qkn = SB(mix, "qkn", [128, 640], BF16); qT = SB(mix, "qT", [128, 4, 128], BF16)
        kTs = [SB(mix, f"kT{i}", [128, 128], BF16) for i in range(3)]; vss = [SB(mix, f"vs{i}", [128, 128], BF16) for i in range(3)]
        kTm = SB(mix, "kTm", [128, 128], BF16); vm = SB(mix, "vm", [128, 128], BF16)
        pex = SB(mix, "pex", [128, 512], BF16); PT = [SB(mix, f"PT{i}", [128, 4, 128], BF16) for i in range(2)]; PTm = SB(mix, "PTm", [16, 512], BF16)
        den_sb = SB(mix, "den_sb", [64, 4, 128]); oT = SB(mix, "oT", [64, 8, 128], BF16); osq = SB(mix, "osq", [64, 4, 128], BF16)
        uTb = SB(mix, "uTb", [128, 4, 128], BF16)
        t1 = SB(mix, "t1", [128, 512]); t2 = SB(mix, "t2", [128, 512]); btr = SB(mix, "btr", [128, 512]); bti = SB(mix, "bti", [128, 512])
        str_ = SB(mix, "str", [128, 512]); sti = SB(mix, "sti", [128, 512]); t3 = SB(mix, "t3", [128, 512]); t4 = SB(mix, "t4", [128, 512])
        sre = SB(mix, "sre", [128, 512], BF16); sim = SB(mix, "sim", [128, 512], BF16)
        injr = SB(mix, "injr", [128, 16]); inji = SB(mix, "inji", [128, 16]); stlr = SB(mix, "stlr", [128, 16]); stli = SB(mix, "stli", [128, 16])
        ysb = SB(mix, "ysb", [128, 4, 128]); zb = SB(mix, "zb", [128, 4, 128], BF16); sg = SB(mix, "sg", [128, 4, 128], BF16)
        zz = SB(mix, "zz", [128, 4, 128], BF16); zsq = SB(mix, "zsq", [128, 4, 128], BF16)
        rs = SB(mix, "rs", [128, 4]); hm = SB(mix, "hm", [128, 1024]); h2b = SB(mix, "h2b", [128, 1024], BF16); st2 = SB(mix, "st2", [128, 4])

        def bcg(ap2, a, n):
            return ap2.rearrange("p (a o) -> p a o", o=1).to_broadcast([ap2.shape[0], a, n])

        def kv_part(kslot, vslot, with_q):
            c0 = 0 if with_q else 512
            g0 = c0 // 64
            if with_q:
                for k in range(8):
                    M(lambda e, k=k: e.matmul(psA[:, 0:512], lhsT=nT[:, k, :], rhs=win[:, k, 0:512], start=(k == 0), stop=(k == 7)), ["nT", "win"], ["psAq"])
                A(lambda e: e.copy(qkv_sb[:, 0:512], psA[:, 0:512]), ["psAq"], ["qkv_q"])
                if "q1" in off:
                    raise StopBuild()
            for k in range(8):
                M(lambda e, k=k: e.matmul(psA[:, 512:768], lhsT=nT[:, k, :], rhs=win[:, k, 512:768], start=(k == 0), stop=(k == 7)), ["nT", "win"], ["psAk"])
            A(lambda e: e.copy(qkv_sb[:, 512:768], psA[:, 512:768]), ["psAk"], ["qkv_k"])
            rk = ["qkv_q", "qkv_k"] if with_q else ["qkv_k"]
            V(lambda e: e.tensor_tensor(sqt[:, c0:640], qkv_sb[:, c0:640], qkv_sb[:, c0:640], ALU.mult), rk, ["sqt"])
            V(lambda e: e.tensor_reduce(s10[:, g0:10], sqt[:, c0:640].rearrange("p (a b) -> p a b", b=64), AX.X, ALU.add), ["sqt"], ["s10"])
            V(lambda e: e.tensor_scalar(s10[:, g0:10], s10[:, g0:10], 1.0 / 64, EPS, ALU.mult, ALU.add), ["s10"], ["s10"])
            A(lambda e: e.activation(out=s10b[:, g0:10], in_=s10[:, g0:10], func=AF.Sqrt), ["s10"], ["s10b"])
            V(lambda e: e.reciprocal(s10b[:, g0:10], s10b[:, g0:10]), ["s10b"], ["s10b"])
            V(lambda e: e.tensor_tensor(qkn[:, c0:640].rearrange("p (a b) -> p a b", b=64), qkv_sb[:, c0:640].rearrange("p (a b) -> p a b", b=64),
                                        bcg(s10b[:, g0:10], 10 - g0, 64), ALU.mult), rk + ["s10b"], ["qkn"])
            if with_q and "q2" in off:
                raise StopBuild()
            M(lambda e: e.transpose(ptr[:, 512:640], qkn[:, 512:640], idb[:]), ["qkn", "idb"], ["ptr"])
            A(lambda e: e.copy(kslot[0][:], ptr[:, 512:640]), ["ptr"], [kslot[1]])
            A(lambda e: e.copy(vslot[0][:], qkv_sb[:, 640:768]), ["qkv_k"], [vslot[1]])
            if with_q:
                for m in range(4):
                    M(lambda e, m=m: e.transpose(ptr[:, m * 128:(m + 1) * 128], qkn[:, m * 128:(m + 1) * 128], idb[:]), ["qkn", "idb"], ["ptr"])
                if "q3" in off:
                    raise StopBuild()
                V(lambda e: e.tensor_scalar(qT[:].rearrange("p a b -> p (a b)"), ptr[:, 0:512], cqk[:, 0:1], None, ALU.mult), ["ptr", "cqk"], ["qT"])

        def attention(prev, cur, pmask):
            first = [True]
            for g in range(2):
                ps_ = slice(64 * g, 64 * g + 64)
                for b, (kk, vv, msk, mkey) in enumerate(((prev[0], prev[1], pmask[0], pmask[1]), (cur[0], cur[1], mcur, "mcur"))):
                    M(lambda e, b=b, kk=kk: e.matmul(pb[b][:], lhsT=kk[0][ps_, :], rhs=qT[ps_, :, :].rearrange("p a b -> p (a b)"), start=True, stop=True), [kk[1], "qT"], [f"pb{b}"])
                    A(lambda e, b=b: e.activation(out=pex[:], in_=pb[b][:], func=AF.Exp), [f"pb{b}"], ["pex"])
                    V(lambda e, b=b, msk=msk: e.tensor_tensor(PT[b][:], pex[:].rearrange("p (a b) -> p a b", a=4),
                                                            msk[:].rearrange("p (o q) -> p o q", o=1).to_broadcast([128, 4, 128]), ALU.mult), ["pex", mkey], [f"PT{b}"])
                M(lambda e: e.matmul(pb[2][0:16, :], lhsT=kTm[ps_, 0:16], rhs=qT[ps_, :, :].rearrange("p a b -> p (a b)"), start=True, stop=True), ["kTm", "qT"], ["pb2"])
                A(lambda e: e.activation(out=PTm[:], in_=pb[2][0:16, :], func=AF.Exp), ["pb2"], ["PTm"])
                cs_ = slice(64 * g, 64 * g + 64)
                PT0 = PT[0][:].rearrange("p a b -> p (a b)"); PT1 = PT[1][:].rearrange("p a b -> p (a b)")
                M(lambda e: e.matmul(pb[3][0:64, :], lhsT=prev[1][0][:, cs_], rhs=PT0, start=True, stop=False), [prev[1][1], "PT0"], ["pb3"])
                M(lambda e: e.matmul(pb[3][0:64, :], lhsT=cur[1][0][:, cs_], rhs=PT1, start=False, stop=False), [cur[1][1], "PT1"], ["pb3"])
                M(lambda e: e.matmul(pb[3][0:64, :], lhsT=vm[0:16, cs_], rhs=PTm[:], start=False, stop=True), ["vm", "PTm"], ["pb3"])
                M(lambda e: e.matmul(pb[4][0:64, :], lhsT=ones[:, 0:64], rhs=PT0, start=True, stop=False), ["ones", "PT0"], ["pb4"])
                M(lambda e: e.matmul(pb[4][0:64, :], lhsT=ones[:, 0:64], rhs=PT1, start=False, stop=False), ["ones", "PT1"], ["pb4"])
                M(lambda e: e.matmul(pb[4][0:64, :], lhsT=ones[0:16, 0:64], rhs=PTm[:], start=False, stop=True), ["ones", "PTm"], ["pb4"])
                for j in range(4):
                    h = 4 * g + j
                    A(lambda e, j=j, h=h: e.activation(out=den_sb[:, j, :], in_=pb[4][0:64, j * 128:(j + 1) * 128], func=AF.Identity, bias=esink[:, h:h + 1]), ["pb4", "esink"], ["den"])
                V(lambda e: e.reciprocal(den_sb[:], den_sb[:]), ["den"], ["den"])
                V(lambda e, g=g: e.tensor_tensor(oT[:, 4 * g:4 * g + 4, :], pb[3][0:64, :].rearrange("p (a b) -> p a b", a=4), den_sb[:], ALU.mult), ["pb3", "den"], ["oT"])
                V(lambda e, g=g: e.tensor_tensor(osq[:], oT[:, 4 * g:4 * g + 4, :], oT[:, 4 * g:4 * g + 4, :], ALU.mult), ["oT"], ["osq"])
                for j in range(4):
                    M(lambda e, j=j, g=g: e.matmul(psA[:, 768:769], lhsT=osq[:, j, :], rhs=ones[0:64, 0:1], start=(g == 0 and j == 0), stop=(g == 1 and j == 3)), ["osq", "ones"], ["pssa"])

        def ssm_tile():
            for c in range(4):
                for k in range(8):
                    M(lambda e, c=c, k=k: e.matmul(pb[0][:, c * 128:(c + 1) * 128], lhsT=win[:, k, 768 + 128 * c:768 + 128 * (c + 1)], rhs=nT[:, k, :], start=(k == 0), stop=(k == 7)), ["nT", "win"], ["pb0"])
            A(lambda e: e.copy(uTb[:].rearrange("p a b -> p (a b)"), pb[0][:]), ["pb0"], ["uTb"])
            cmul(injr[:], inji[:], sc["abr"][:], sc["abi"][:], Sr[:], Si[:], c1[:], c2[:], ["ss_abr", "ss_abi", "S", "c"], ["inj", "c"])
            for c in range(4):
                isl = slice(4 * c, 4 * c + 4)
                Erc = Er[:, isl, :].rearrange("p a b -> p (a b)"); Eic = Ei[:, isl, :].rearrange("p a b -> p (a b)")
                Rc = Rho0[:, isl, :].rearrange("p a b -> p (a b)")
                for ii in range(4):
                    i = 4 * c + ii
                    M(lambda e, i=i, ii=ii, c=c: e.matmul(pb[1][:, ii * 128:(ii + 1) * 128], lhsT=BTr[:, i, :], rhs=uTb[:, c, :], start=True, stop=True), ["BT", "uTb"], ["pb1"])
                    M(lambda e, i=i, ii=ii, c=c: e.matmul(pb[2][:, ii * 128:(ii + 1) * 128], lhsT=BTi[:, i, :], rhs=uTb[:, c, :], start=True, stop=True), ["BT", "uTb"], ["pb2"])
                V(lambda e: e.tensor_tensor(t1[:], pb[1][:], Erc, ALU.mult), ["pb1", "Er"], ["t1"])
                V(lambda e: e.tensor_tensor(t2[:], pb[2][:], Eic, ALU.mult), ["pb2", "Ei"], ["t2"])
                V(lambda e: e.tensor_tensor(btr[:], t1[:], t2[:], ALU.add), ["t1", "t2"], ["btr"])
                V(lambda e: e.tensor_tensor(t1[:], pb[2][:], Erc, ALU.mult), ["pb2", "Er", "btr"], ["t1"])
                V(lambda e: e.tensor_tensor(t2[:], pb[1][:], Eic, ALU.mult), ["pb1", "Ei", "btr"], ["t2"])
                V(lambda e: e.tensor_tensor(bti[:], t1[:], t2[:], ALU.subtract), ["t1", "t2"], ["bti"])
                b3r = btr[:].rearrange("p (a b) -> p a b", a=4); b3i = bti[:].rearrange("p (a b) -> p a b", a=4)
                V(lambda e, isl=isl: e.tensor_tensor(b3r[:, :, 0], b3r[:, :, 0], injr[:, isl], ALU.add), ["btr", "inj"], ["btr"])
                V(lambda e, isl=isl: e.tensor_tensor(b3i[:, :, 0], b3i[:, :, 0], inji[:, isl], ALU.add), ["bti", "inj"], ["bti"])
                V(lambda e: e.tensor_tensor_scan(str_[:], Rc, btr[:], 0.0, ALU.mult, ALU.add), ["Rho0", "btr"], ["str"])
                V(lambda e: e.tensor_tensor_scan(sti[:], Rc, bti[:], 0.0, ALU.mult, ALU.add), ["Rho0", "bti"], ["sti"])
                s3r = str_[:].rearrange("p (a b) -> p a b", a=4); s3i = sti[:].rearrange("p (a b) -> p a b", a=4)
                V(lambda e, isl=isl: e.tensor_copy(stlr[:, isl], s3r[:, :, 127]), ["str"], ["stl"])
                V(lambda e, isl=isl: e.tensor_copy(stli[:, isl], s3i[:, :, 127]), ["sti"], ["stl"])
                G(lambda e: e.tensor_tensor(t3[:], str_[:], Erc, ALU.mult), ["str", "Er"], ["t3"])
                G(lambda e: e.tensor_tensor(t4[:], sti[:], Eic, ALU.mult), ["sti", "Ei"], ["t4"])
                G(lambda e: e.tensor_tensor(sre[:], t3[:], t4[:], ALU.subtract), ["t3", "t4"], ["sre"])
                G(lambda e: e.tensor_tensor(t3[:], str_[:], Eic, ALU.mult), ["str", "Ei", "sre"], ["t3"])
                G(lambda e: e.tensor_tensor(t4[:], sti[:], Erc, ALU.mult), ["sti", "Er", "sre"], ["t4"])
                G(lambda e: e.tensor_tensor(sim[:], t3[:], t4[:], ALU.add), ["t3", "t4"], ["sim"])
                for ii in range(4):
                    i = 4 * c + ii
                    M(lambda e, i=i, ii=ii, c=c: e.matmul(pb[3][:, c * 128:(c + 1) * 128], lhsT=Cbr[:, i, :], rhs=sre[:, ii * 128:(ii + 1) * 128], start=(ii == 0), stop=False), ["Cb", "sre"], ["pb3"])
                    M(lambda e, i=i, ii=ii, c=c: e.matmul(pb[3][:, c * 128:(c + 1) * 128], lhsT=Cbi[:, i, :], rhs=sim[:, ii * 128:(ii + 1) * 128], start=False, stop=(ii == 3)), ["Cb", "sim"], ["pb3"])
            cmul(Sr[:], Si[:], stlr[:], stli[:], E127r[:], E127i[:], c1[:], c2[:], ["stl", "E127", "c", "inj"], ["S", "c"])
            for c in range(4):
                V(lambda e, c=c: e.scalar_tensor_tensor(ysb[:, c, :], uTb[:, c, :], small["ssm_d"][:, c:c + 1], pb[3][:, c * 128:(c + 1) * 128], ALU.mult, ALU.add), ["uTb", "c_ssm_d", "pb3"], ["ysb"])
            A(lambda e: e.activation(out=zb[:], in_=ysb[:], func=AF.Gelu), ["ysb"], ["zb"])
            for cp in range(4):
                for c in range(4):
                    M(lambda e, c=c, cp=cp: e.matmul(pb[4][:, cp * 128:(cp + 1) * 128], lhsT=glu[:, c, cp * 128:(cp + 1) * 128], rhs=zb[:, c, :], start=(c == 0), stop=(c == 3)), ["glu", "zb"], ["pb4"])
            for cp in range(4):
                A(lambda e, cp=cp: e.activation(out=sg[:, cp, :], in_=pb[4][:, cp * 128:(cp + 1) * 128], func=AF.Sigmoid, bias=small["glu_b"][:, cp:cp + 1]), ["pb4", "c_glu_b"], ["sg"])
            V(lambda e: e.tensor_tensor(zz[:], zb[:], sg[:], ALU.mult), ["zb", "sg"], ["zz"])
            V(lambda e: e.tensor_tensor(zsq[:], zz[:], zz[:], ALU.mult), ["zz"], ["zsq"])
            for c in range(4):
                M(lambda e, c=c: e.matmul(psA[:, 769:770], lhsT=zsq[:, c, :], rhs=ones[:, 0:1], start=(c == 0), stop=(c == 3)), ["zsq", "ones"], ["psss"])

        def out_proj(sl, t):
            for half in range(2):
                hs = slice(half * 512, (half + 1) * 512)
                for h in range(8):
                    M(lambda e, h=h, half=half, hs=hs: e.matmul(pb[half][:], lhsT=oT[:, h, :], rhs=woa[:, h, hs], start=(h == 0), stop=(h == 7)), ["oT", "woa"], [f"pb{half}"])
                for c in range(4):
                    M(lambda e, c=c, half=half, hs=hs: e.matmul(pb[2 + half][:], lhsT=zz[:, c, :], rhs=wos[:, c, hs], start=(c == 0), stop=(c == 3)), ["zz", "wos"], [f"pb{2 + half}"])
            V(lambda e: e.tensor_scalar(rs[:, 0:2], psA[:, 768:770], 1.0 / 512, EPS, ALU.mult, ALU.add), ["pssa", "psss"], ["rs"])
            A(lambda e: e.activation(out=rs[:, 2:4], in_=rs[:, 0:2], func=AF.Sqrt), ["rs"], ["rsb"])
            V(lambda e: e.reciprocal(rs[:, 0:2], rs[:, 2:4]), ["rsb", "rs"], ["rs"])
            kx = f"xt{sl}"
            for half in range(2):
                hs = slice(half * 512, (half + 1) * 512)
                V(lambda e, half=half, hs=hs: e.scalar_tensor_tensor(hm[:, hs], pb[half][:], rs[:, 0:1], xt[sl][:, hs], ALU.mult, ALU.add), [f"pb{half}", "rs", kx], ["hm"])
                V(lambda e, half=half, hs=hs: e.scalar_tensor_tensor(hm[:, hs], pb[2 + half][:], rs[:, 1:2], hm[:, hs], ALU.mult, ALU.add), [f"pb{2 + half}", "rs", "hm"], ["hm"])
            S.dma(lambda e: e.dma_start(out=out[t * 128:(t + 1) * 128, :], in_=hm[:]), reads=["hm"], writes=[f"out{t}"])
            A(lambda e: e.activation(out=junk[:], in_=hm[:], func=AF.Square, accum_out=st2[:, 0:1]), ["hm"], ["junk", "st2"])
            V(lambda e: e.tensor_scalar(st2[:, 1:2], st2[:, 0:1], 1.0 / 1024, EPS, ALU.mult, ALU.add), ["st2"], ["st2b"])
            A(lambda e: e.activation(out=st2[:, 3:4], in_=st2[:, 1:2], func=AF.Sqrt), ["st2b"], ["st2d"])
            V(lambda e: e.reciprocal(st2[:, 2:3], st2[:, 3:4]), ["st2d"], ["st2c"])
            V(lambda e: e.tensor_scalar(h2b[:], hm[:], st2[:, 2:3], None, ALU.mult), ["hm", "st2c"], ["h2b"])
            for k in range(8):
                M(lambda e, k=k: e.transpose(ptr[:, k * 128:(k + 1) * 128], h2b[:, k * 128:(k + 1) * 128], idb[:]), ["h2b", "idb"], ["ptr"])
            V(lambda e: e.tensor_tensor(h2T[:, :, t * 128:(t + 1) * 128], ptr[:].rearrange("p (a b) -> p a b", a=8), bcg(small["g2"][:], 8, 128), ALU.mult), ["ptr", "c_g2"], ["h2T"])

        if "nomain" in off:
            S.dma(lambda e: e.dma_start(out=xt[0][:], in_=xown[0:128, :]), writes=["xt0"])
            S.dma(lambda e: e.dma_start(out=out[0:128, :], in_=xt[0][:]), reads=["xt0"], writes=["out0"])
            S.emit()
            return nc
        front(xmeta)
        kv_part((kTm, "kTm"), (vm, "vm"), False)
        if "stopmeta" in off:
            S.dma(lambda e: e.dma_start(out=out[0:128, :], in_=xt[0][:]), reads=["xt0", "kTm", "vm"], writes=["out0"])
            S.emit()
            return nc
        front(xhalo)
        kv_part((kTs[2], "kT2"), (vss[2], "vs2"), False)
        if "stophalo" in off:
            S.dma(lambda e: e.dma_start(out=out[0:128, :], in_=xt[1][:]), reads=["xt1", "kT2", "vs2"], writes=["out0"])
            S.emit()
            return nc
        n_own = 1 if (stage == "mix1" or "one" in off) else NT
        for t in range(n_own):
            sl = front(xown[t * 128:(t + 1) * 128, :])
            pslot, cslot = (t + 2) % 3, t % 3
            try:
                kv_part((kTs[cslot], f"kT{cslot}"), (vss[cslot], f"vs{cslot}"), True)
            except StopBuild:
                S.dma(lambda e: e.dma_start(out=out[0:128, :], in_=xt[sl][:]), reads=[f"xt{sl}", "qkv_q", "qkv_k", "qkn", "ptr"], writes=["out0"])
                S.emit()
                return nc
            if "attn" not in off:
                attention(((kTs[pslot], f"kT{pslot}"), (vss[pslot], f"vs{pslot}")), ((kTs[cslot], f"kT{cslot}"), (vss[cslot], f"vs{cslot}")),
                          (mhalo, "mhalo") if t == 0 else (mprev, "mprev"))
            if "dumpqk" in off:
                V(lambda e: e.tensor_copy(hm[:, 0:512], qT[:].rearrange("p a b -> p (a b)")), ["qT"], ["hm"])
                V(lambda e: e.tensor_copy(hm[:, 512:640], kTs[cslot][:]), [f"kT{cslot}"], ["hm"])
                V(lambda e: e.tensor_copy(hm[:, 640:768], vss[cslot][:]), [f"vs{cslot}"], ["hm"])
                V(lambda e: e.tensor_copy(hm[:, 768:896], kTm[:]), ["kTm"], ["hm"])
                V(lambda e: e.tensor_copy(hm[:, 896:1024], vm[:]), ["vm"], ["hm"])
                S.dma(lambda e: e.dma_start(out=out[0:128, :], in_=hm[:]), reads=["hm"], writes=["out0"])
                S.emit()
                return nc
            if "dumpv" in off:
                V(lambda e: e.tensor_copy(hm[0:64, 0:512], pb[3][0:64, :]), ["pb3"], ["hm"])
                V(lambda e: e.tensor_copy(hm[0:64, 512:1024], pb[4][0:64, :]), ["pb4", "hm"], ["hm"])
                S.dma(lambda e: e.dma_start(out=out[0:64, :], in_=hm[0:64, :]), reads=["hm"], writes=["out0"])
                S.emit()
                return nc
            if "dumpp" in off:
                V(lambda e: e.tensor_copy(hm[:, 0:512], PT[1][:].rearrange("p a b -> p (a b)")), ["PT1"], ["hm"])
                V(lambda e: e.tensor_copy(hm[0:64, 512:1024], den_sb[:].rearrange("p a b -> p (a b)")), ["den"], ["hm"])
                S.dma(lambda e: e.dma_start(out=out[0:128, :], in_=hm[:]), reads=["hm"], writes=["out0"])
                S.emit()
                return nc
            if "dumpo" in off:
                V(lambda e: e.tensor_copy(hm[0:64, :], oT[:].rearrange("p a b -> p (a b)")), ["oT"], ["hm"])
                S.dma(lambda e: e.dma_start(out=out[0:64, :], in_=hm[0:64, :]), reads=["hm"], writes=["out0"])
                S.emit()
                return nc
            if "ssm" not in off:
                ssm_tile()
            if "outp" not in off:
                out_proj(sl, t)
            else:
                S.dma(lambda e, t=t, sl=sl: e.dma_start(out=out[t * 128:(t + 1) * 128, :], in_=xt[sl][:]), reads=[f"xt{sl}"], writes=[f"out{t}"])

        if "dumph2" in off:
            V(lambda e: e.tensor_copy(hm[:], h2T[:, :, 0:128]), ["h2T", "hm"], ["hm"])
            S.dma(lambda e: e.dma_start(out=out[128:256, :], in_=hm[:]), reads=["hm"], writes=["out1"])
        if stage in ("mix", "mix1"):
            S.emit()
            return nc

        S.barrier()
        mix.close()
        pe_ = top.enter_context(contextlib.ExitStack())
        pA = PS(pe_, "pA", [128, 2048]); pQ = PS(pe_, "pQ", [128, 512])
        wq = SB(pe_, "wq", [128, 8, 2048], BF16); keysT = SB(pe_, "keysT", [128, 16, 128], BF16)
        with scope() as stg:
            wst = SB(stg, "wst", [128, 2, 2048])
            for kk4 in range(4):
                load(wst[:], D["wq"][:, 2 * kk4:2 * kk4 + 2, :], "wst")
                V(lambda e, kk4=kk4: e.tensor_copy(wq[:, 2 * kk4:2 * kk4 + 2, :], wst[:]), ["wst"], ["wq"])
            kst = SB(stg, "kst", [128, 16, 128])
            load(kst[:], D["keysT"], "kst")
            V(lambda e: e.tensor_copy(keysT[:], kst[:]), ["kst"], ["keysT"])
        qTp = SB(pe_, "qTp", [128, 16, 128], BF16); scs = SB(pe_, "scs", [128, 16, 128]); sc2 = SB(pe_, "sc2", [128, 16, 128])
        v16 = SB(pe_, "v16", [128, 16, 16]); cand = SB(pe_, "cand", [128, 8, 256]); cand2 = SB(pe_, "cand2", [128, 8, 256]); s16 = SB(pe_, "s16", [128, 8, 16])
        dbgp = SB(pe_, "dbgp", [128, 1024])

        def peer_front(t):
            ts_ = slice(t * 128, (t + 1) * 128)
            for g4 in range(4):
                for j in range(4):
                    cb = 4 * g4 + j
                    for k in range(8):
                        M(lambda e, cb=cb, j=j, k=k: e.matmul(pQ[:, j * 128:(j + 1) * 128], lhsT=wq[:, k, cb * 128:(cb + 1) * 128], rhs=h2T[:, k, ts_], start=(k == 0), stop=(k == 7)), ["wq", "h2T"], ["pQ"])
                A(lambda e, g4=g4: e.copy(qTp[:, 4 * g4:4 * g4 + 4, :].rearrange("p a b -> p (a b)"), pQ[:]), ["pQ"], ["qTp"])
            for idx in range(16):
                M(lambda e, idx=idx: e.matmul(pA[:, idx * 128:(idx + 1) * 128], lhsT=qTp[:, idx, :], rhs=keysT[:, idx, :], start=True, stop=True), ["qTp", "keysT"], ["pA"])
            for q4 in range(4):
                A(lambda e, q4=q4: e.copy(scs[:, 4 * q4:4 * q4 + 4, :].rearrange("p a b -> p (a b)"), pA[:, q4 * 512:(q4 + 1) * 512]), ["pA"], ["scs"])
            for idx in range(16):
                V(lambda e, idx=idx: e.max(out=v16[:, idx, 0:8], in_=scs[:, idx, :]), ["scs"], ["v16"])
                V(lambda e, idx=idx: e.match_replace(out=sc2[:, idx, :], in_to_replace=v16[:, idx, 0:8], in_values=scs[:, idx, :], imm_value=-1e30), ["scs", "v16"], ["sc2"])
                V(lambda e, idx=idx: e.max(out=v16[:, idx, 8:16], in_=sc2[:, idx, :]), ["sc2"], ["v16"])
            v4 = v16[:].rearrange("p (h two) a -> p h two a", two=2)
            V(lambda e: e.tensor_tensor(cand[:].rearrange("p h (a b) -> p h a b", a=16),
                                        v4[:, :, 0, :].rearrange("p h (a o) -> p h a o", o=1).to_broadcast([128, 8, 16, 16]),
                                        v4[:, :, 1, :].rearrange("p h (o b) -> p h o b", o=1).to_broadcast([128, 8, 16, 16]), ALU.add), ["v16"], ["cand"])
            for h in range(8):
                V(lambda e, h=h: e.max(out=s16[:, h, 0:8], in_=cand[:, h, :]), ["cand"], ["s16"])
                V(lambda e, h=h: e.match_replace(out=cand2[:, h, :], in_to_replace=s16[:, h, 0:8], in_values=cand[:, h, :], imm_value=-1e30), ["cand", "s16"], ["cand2"])
                V(lambda e, h=h: e.max(out=s16[:, h, 8:16], in_=cand2[:, h, :]), ["cand2"], ["s16"])

        if stage == "peerfront":
            peer_front(0)
            V(lambda e: e.memset(dbgp[:], 0.0), [], ["dbgp"])
            V(lambda e: e.tensor_copy(dbgp[:, 0:256], v16[:].rearrange("p a b -> p (a b)")), ["v16"], ["dbgp"])
            V(lambda e: e.tensor_copy(dbgp[:, 256:384], s16[:].rearrange("p a b -> p (a b)")), ["s16"], ["dbgp"])
            V(lambda e: e.tensor_copy(dbgp[:, 512:1024], scs[:, 0:4, :].rearrange("p a b -> p (a b)")), ["scs"], ["dbgp"])
            S.dma(lambda e: e.dma_start(out=out[256:384, :], in_=dbgp[:]), reads=["dbgp"], writes=["out2"])
            S.emit()
            return nc

        n_i = 128

        iota_i = SB(pe_, "iota_i", [128, 128]); bm = SB(pe_, "bm", [128, 8])
        load(iota_i[:], D["iota_i"], "iota_i"); load(bm[:], D["blkmask"], "bm")
        pG2 = PS(pe_, "pG2", [128, 512]); pZs = [PS(pe_, f"pZ{i}", [128, 512]) for i in range(2)]
        ixu = SB(pe_, "ixu", [128, 16, 16], U32); ixf = SB(pe_, "ixf", [128, 16, 16])
        i1c = SB(pe_, "i1c", [128, 128]); i2c = SB(pe_, "i2c", [128, 128]); i1T = SB(pe_, "i1T", [128, 128]); i2T = SB(pe_, "i2T", [128, 128])
        zs = SB(pe_, "zs", [128, 8]); wT = SB(pe_, "wT", [128, 128, 16], BF16)
        NB = 16
        P1b = SB(pe_, "P1b", [128, NB, 128], BF16); P2b = SB(pe_, "P2b", [128, NB, 128], BF16); Wb = SB(pe_, "Wb", [128, NB, 128], BF16)
        Tsb = SB(pe_, "Tsb", [128, 4, 128], BF16); Gt = SB(pe_, "Gt", [128, 128, 128], BF16)
        ND = 4
        ubuf = [SB(pe_, f"ubuf{i}", [128, 1024], BF16) for i in range(ND)]; vbuf = [SB(pe_, f"vbuf{i}", [128, 1024], BF16) for i in range(ND)]
        zg = [SB(pe_, f"zg{i}", [128, 128], BF16) for i in range(2)]; AT = [SB(pe_, f"AT{i}", [128, 128], BF16) for i in range(2)]
        hmt = SB(pe_, "hmt", [128, 1024]); res = SB(pe_, "res", [128, 1024])

        def b3(ap2, a, n):
            return ap2.rearrange("p (a o) -> p a o", o=1).to_broadcast([128, a, n])

        def gate_build(t):
            for idx in range(16):
                V(lambda e, idx=idx: e.max_index(out=ixu[:, idx, 0:8], in_max=v16[:, idx, 0:8], in_values=scs[:, idx, :]), ["v16", "scs"], ["ixu"])
                V(lambda e, idx=idx: e.max_index(out=ixu[:, idx, 8:16], in_max=v16[:, idx, 8:16], in_values=sc2[:, idx, :]), ["v16", "sc2"], ["ixu"])
            V(lambda e: e.tensor_copy(ixf[:], ixu[:]), ["ixu"], ["ixf"])
            ix4 = ixf[:].rearrange("p (h two) a -> p h two a", two=2)
            V(lambda e: e.tensor_copy(i1c[:].rearrange("p (h a) -> p h a", h=8), ix4[:, :, 0, :]), ["ixf"], ["i1c"])
            V(lambda e: e.tensor_copy(i2c[:].rearrange("p (h a) -> p h a", h=8), ix4[:, :, 1, :]), ["ixf"], ["i2c"])
            wx = scs[:].rearrange("p a b -> p (a b)").rearrange("p (h c) -> p h c", h=8)
            mk = sc2[:].rearrange("p a b -> p (a b)").rearrange("p (h c) -> p h c", h=8)
            V(lambda e: e.tensor_tensor(wx, cand[:], b3(s16[:, :, 0], 8, 256), ALU.subtract), ["cand", "s16", "ixu"], ["scs"])
            A(lambda e: e.activation(out=wx, in_=wx, func=AF.Exp), ["scs"], ["scs"])
            V(lambda e: e.tensor_tensor(mk, cand[:], b3(s16[:, :, 15], 8, 256), ALU.is_ge), ["cand", "s16", "ixu"], ["sc2"])
            V(lambda e: e.tensor_tensor(wx, wx, mk, ALU.mult), ["scs", "sc2"], ["scs"])
            V(lambda e: e.tensor_reduce(zs[:], wx, AX.X, ALU.add), ["scs"], ["zs"])
            V(lambda e: e.reciprocal(zs[:], zs[:]), ["zs"], ["zs"])
            V(lambda e: e.tensor_tensor(wx, wx, b3(zs[:], 8, 256), ALU.mult), ["scs", "zs"], ["scs"])
            wperm = sc2[:].rearrange("p a b -> p (a b)").rearrange("p (b h a) -> p b h a", b=16, h=8)
            V(lambda e: e.tensor_copy(wperm, scs[:].rearrange("p a b -> p (a b)").rearrange("p (h a b) -> p b h a", h=8, a=16)), ["scs", "sc2"], ["sc2"])
            w2d = sc2[:].rearrange("p a b -> p (a b)")
            for b in range(16):
                M(lambda e, b=b: e.transpose(pA[:, b * 128:(b + 1) * 128], w2d[:, b * 128:(b + 1) * 128], idf[:]), ["sc2", "idf"], ["pA"])
            V(lambda e: e.tensor_copy(wT[:].rearrange("p t b -> p b t"), pA[:].rearrange("p (b t) -> p b t", b=16)), ["pA"], ["wT"])
            M(lambda e: e.transpose(pQ[:, 0:128], i1c[:], idf[:]), ["i1c", "idf"], ["pQ"])
            M(lambda e: e.transpose(pQ[:, 128:256], i2c[:], idf[:]), ["i2c", "idf"], ["pQ"])
            V(lambda e: e.tensor_copy(i1T[:], pQ[:, 0:128]), ["pQ"], ["i1T"])
            V(lambda e: e.tensor_copy(i2T[:], pQ[:, 128:256]), ["pQ"], ["i2T"])
            for nb in range(128 // NB):
                tsl = slice(nb * NB, (nb + 1) * NB)
                io3 = iota_i[:].rearrange("p (o i) -> p o i", o=1).to_broadcast([128, NB, 128])
                V(lambda e, tsl=tsl: e.tensor_tensor(P1b[:], io3, b3(i1T[:, tsl], NB, 128), ALU.is_equal), ["iota_i", "i1T", "P1b"], ["P1b"])
                V(lambda e, tsl=tsl: e.tensor_tensor(P2b[:], io3, b3(i2T[:, tsl], NB, 128), ALU.is_equal), ["iota_i", "i2T", "P2b"], ["P2b"])
                V(lambda e, tsl=tsl: e.tensor_tensor(Wb[:].rearrange("p t (h b) -> p t h b", h=8),
                                                      wT[:, tsl, :].rearrange("p t (o b) -> p t o b", o=1).to_broadcast([128, NB, 8, 16]),
                                                      bm[:].rearrange("p (o h q) -> p o h q", o=1, q=1).to_broadcast([128, NB, 8, 16]), ALU.mult), ["wT", "bm", "Wb"], ["Wb"])
                for q4 in range(NB // 4):
                    for j in range(4):
                        tk = q4 * 4 + j
                        M(lambda e, tk=tk, j=j: e.matmul(pQ[:, j * 128:(j + 1) * 128], lhsT=Wb[:, tk, :], rhs=P1b[:, tk, :], start=True, stop=True), ["Wb", "P1b"], ["pQ"])
                    A(lambda e: e.copy(Tsb[:].rearrange("p a b -> p (a b)"), pQ[:]), ["pQ"], ["Tsb"])
                    for j in range(4):
                        tk = q4 * 4 + j
                        M(lambda e, tk=tk, j=j: e.matmul(pG2[:, j * 128:(j + 1) * 128], lhsT=P2b[:, tk, :], rhs=Tsb[:, j, :], start=True, stop=True), ["P2b", "Tsb"], ["pG2"])
                    t0 = nb * NB + q4 * 4
                    V(lambda e, t0=t0: e.tensor_copy(Gt[:, t0:t0 + 4, :].rearrange("p a b -> p (a b)"), pG2[:]), ["pG2"], ["Gt"])

        def dense_block(t):
            ts_ = slice(t * 128, (t + 1) * 128)
            def zstage(i):
                sl = i % 2; ds_ = i % ND
                S.dma(lambda e, i=i, ds_=ds_: e.dma_start(out=ubuf[ds_][:], in_=UTs[i]), reads=[f"UTs{i}"], writes=[f"ubuf{ds_}"])
                S.dma(lambda e, i=i, ds_=ds_: e.dma_start(out=vbuf[ds_][:], in_=Vs[i]), reads=[f"Vs{i}"], writes=[f"vbuf{ds_}"], eng="pool")
                for k in range(8):
                    M(lambda e, k=k, sl=sl, ds_=ds_: e.matmul(pZs[sl][:, 0:128], lhsT=ubuf[ds_][:, k * 128:(k + 1) * 128], rhs=h2T[:, k, ts_], start=(k == 0), stop=(k == 7)), [f"ubuf{ds_}", "h2T"], [f"pZ{sl}"])
                A(lambda e, sl=sl: e.activation(out=zg[sl][:], in_=pZs[sl][:, 0:128], func=AF.Gelu), [f"pZ{sl}"], [f"zg{sl}"])
                V(lambda e, sl=sl, i=i: e.tensor_tensor(AT[sl][:], zg[sl][:], Gt[:, :, i], ALU.mult), [f"zg{sl}", "Gt"], [f"AT{sl}"])

            def ostage(i):
                sl = i % 2; ds_ = i % ND
                for half in range(2):
                    M(lambda e, half=half, sl=sl, ds_=ds_, i=i: e.matmul(pA[:, half * 512:(half + 1) * 512], lhsT=AT[sl][:], rhs=vbuf[ds_][:, half * 512:(half + 1) * 512], start=(i == 0), stop=(i == n_i - 1)), [f"AT{sl}", f"vbuf{ds_}"], ["pA"])

            zstage(0)
            for i in range(n_i):
                if i + 1 < n_i:
                    zstage(i + 1)
                ostage(i)
            S.dma(lambda e: e.dma_start(out=hmt[:], in_=out[t * 128:(t + 1) * 128, :]), reads=[f"out{t}"], writes=["hmt"])
            for half in range(2):
                hs = slice(half * 512, (half + 1) * 512)
                V(lambda e, hs=hs: e.tensor_tensor(res[:, hs], hmt[:, hs], pA[:, hs], ALU.add), ["hmt", "pA"], ["res"])
            S.dma(lambda e: e.dma_start(out=out[t * 128:(t + 1) * 128, :], in_=res[:]), reads=["res"], writes=[f"out{t}"])

        if "keephm" in off:
            S.dma(lambda e: e.dma_start(out=hmt[:], in_=out[0:128, :]), reads=["out0"], writes=["hmt"])
            S.dma(lambda e: e.dma_start(out=out[128:256, :], in_=hmt[:]), reads=["hmt"], writes=["out1"])
        for t in range(n_own):
            peer_front(t)
            gate_build(t)
            dense_block(t)
        S.emit()
        return nc


def _host_inputs(inp):
    f = np.float32
    x = np.ascontiguousarray(inp["x"][0]); meta = inp["meta_tokens"]
    common = {}
    common["ident"] = np.eye(128, dtype=f)
    kq = np.arange(128)
    common["mask_cur"] = (kq[:, None] <= kq[None, :]).astype(f)
    common["xmeta"] = np.concatenate([meta, np.zeros((112, 1024), f)], 0)
    w_in = inp["w_in"][0]
    qperm = np.concatenate([np.arange(64) + 64 * h for h in (0, 4, 1, 5, 2, 6, 3, 7)])
    w_in = np.concatenate([w_in[:, :512][:, qperm], w_in[:, 512:]], 1)
    common["w_in"] = np.ascontiguousarray(w_in.reshape(8, 128, 1280).transpose(1, 0, 2))
    common["mask_prev"] = (kq[:, None] > kq[None, :]).astype(f)
    common["g1"] = np.ascontiguousarray(inp["norm1_g"][0].reshape(8, 128).T)
    common["gq2"] = np.tile(inp["q_norm_g"][0], 2).reshape(128, 1).astype(f)
    common["gk2"] = np.tile(inp["k_norm_g"][0], 2).reshape(128, 1).astype(f)
    common["sinks"] = np.ascontiguousarray(np.broadcast_to(inp["attn_sinks"][0][None, :], (64, 8))).astype(f)

    def state_layout(a):
        return np.ascontiguousarray(a.reshape(16, 2, 64).transpose(1, 2, 0).reshape(128, 16))

    def row_layout(a):
        return np.ascontiguousarray(np.broadcast_to(a.reshape(1, 2048), (128, 2048)))
    ldt64 = np.ascontiguousarray(np.broadcast_to(inp["ssm_log_dt"][0][:, None], (32, 64)))
    for nm, a in (("a_re", inp["ssm_a_re"][0]), ("a_im", inp["ssm_a_im"][0]), ("ldt", ldt64)):
        common[nm + "_s"] = state_layout(a); common[nm + "_r"] = row_layout(a)
    for nm, B in (("re", inp["ssm_b_re"][0]), ("im", inp["ssm_b_im"][0])):
        bT = np.zeros((128, 16, 128), f); bs = np.zeros((128, 16, 32), f)
        for g in range(32):
            i, hh = g // 2, g % 2
            bT[(g % 8) * 16:(g % 8) * 16 + 16, i, hh * 64:hh * 64 + 64] = B[g].T
            bs[hh * 64:hh * 64 + 64, i, hh * 16:hh * 16 + 16] = B[g]
        common["bT_" + nm] = bT; common["bs_" + nm] = bs
    for nm, C in (("re", inp["ssm_c_re"][0]), ("im", inp["ssm_c_im"][0])):
        cb = np.zeros((128, 16, 128), f)
        for g in range(32):
            i, hh = g // 2, g % 2
            cb[hh * 64:hh * 64 + 64, i, (g % 8) * 16:(g % 8) * 16 + 16] = C[g].T
        common["cb_" + nm] = cb
    common["ssm_d"] = np.ascontiguousarray(inp["ssm_d"][0].reshape(4, 128).T)
    common["glu_w"] = np.ascontiguousarray(inp["ssm_glu_w"][0].reshape(4, 128, 512).transpose(1, 0, 2))
    common["glu_b"] = np.ascontiguousarray(inp["ssm_glu_b"][0].reshape(4, 128).T)
    common["ga"] = np.ascontiguousarray(inp["attn_out_g"][0].reshape(8, 64).T)
    common["gs"] = np.ascontiguousarray(inp["ssm_out_g"][0].reshape(4, 128).T)
    common["woa"] = np.ascontiguousarray(inp["w_out"][0][:512].reshape(8, 64, 1024).transpose(1, 0, 2))
    common["wos"] = np.ascontiguousarray(inp["w_out"][0][512:].reshape(4, 128, 1024).transpose(1, 0, 2))
    common["g2"] = np.ascontiguousarray(inp["norm2_g"][0].reshape(8, 128).T)
    if inp.get("peer_w_query") is not None:
        common["wq"] = np.ascontiguousarray(inp["peer_w_query"][0].reshape(8, 128, 2048).transpose(1, 0, 2))
        sk = inp["peer_sub_keys"][0]
        common["keysT"] = np.ascontiguousarray(sk.reshape(16, 128, 128).transpose(2, 0, 1))
    if inp.get("peer_u") is not None:
        common["puT"] = np.ascontiguousarray(inp["peer_u"][0].reshape(128, 128, 8, 128).transpose(0, 3, 2, 1)).reshape(128, 128, 1024)
        common["pv"] = np.ascontiguousarray(inp["peer_v"][0].reshape(128, 128, 1024))
        common["iota_i"] = np.ascontiguousarray(np.broadcast_to(np.arange(128, dtype=f)[None, :], (128, 128)))
        common["blkmask"] = (np.arange(128)[:, None] // 16 == np.arange(8)[None, :]).astype(f)
    maps = []
    for r in range(NCORES):
        m = dict(common)
        m["xown"] = x[r * TOK:(r + 1) * TOK]
        m["xhalo"] = x[r * TOK - 128:r * TOK] if r > 0 else np.zeros((128, 1024), f)
        m["mask_halo"] = common["mask_prev"] if r > 0 else np.zeros((128, 128), f)
        pre = np.zeros((NPRE * 128, 1024), f)
        n = r * TOK
        pre[NPRE * 128 - n - 16:NPRE * 128 - n] = meta
        if n:
            pre[NPRE * 128 - n:] = x[:n]
        m["xpre"] = pre
        m["is0"] = np.full((128, 1), 1.0 if r == 0 else 0.0, f)
        maps.append(m)
    return maps


def kernel(**inputs):
    nc = build("full")
    maps = _host_inputs({k: np.asarray(v) for k, v in inputs.items()})
    res = run_bass_kernel_spmd(nc, maps, core_ids=list(range(NCORES)))
    return np.concatenate([r["out"] for r in res.results], 0)[None].astype(np.float32)
```

```python
import contextlib
import types
import numpy as np
import concourse.bass as bass
import concourse.mybir as mybir
from concourse.bass_utils import run_bass_kernel_spmd

F32 = mybir.dt.float32
BF16 = mybir.dt.bfloat16
U32 = mybir.dt.uint32
AF = mybir.ActivationFunctionType
ALU = mybir.AluOpType
AX = mybir.AxisListType

NCORES = 8
TOK = 2048
NT = 16
NPRE = 113
EPS = 1e-6
ENGS = ("pe", "act", "dve", "pool", "sp")


class StopBuild(Exception):
    pass


def _freeze(fn):
    if fn.__closure__ is None:
        return fn
    cells = []
    for c in fn.__closure__:
        try:
            cells.append(types.CellType(c.cell_contents))
        except ValueError:
            cells.append(c)
    return types.FunctionType(fn.__code__, fn.__globals__, fn.__name__, fn.__defaults__, tuple(cells))


class Sched:
    def __init__(self, nc, n_dma_streams=8):
        self.nc = nc
        self.ops = {e: [] for e in ENGS}
        self.count = {}
        self.last_w = {}
        self.readers = {}
        self.seen = {e: {} for e in ENGS}
        self.n_dma = n_dma_streams
        self.dma_rr = {e: 0 for e in ENGS}
        self.pending = {e: {} for e in ENGS}
        self.pe_run = []

    def barrier(self):
        self._flush_pe()
        for e in ENGS:
            for k, v in self.count.items():
                if self.pending[e].get(k, 0) < v:
                    self.pending[e][k] = v

    def _deps(self, eng, reads, writes):
        need = dict(self.pending[eng])
        self.pending[eng] = {}

        def add(ev):
            if ev is not None and need.get(ev[0], 0) < ev[1]:
                need[ev[0]] = ev[1]
        for b in reads:
            add(self.last_w.get(b))
        for b in writes:
            add(self.last_w.get(b))
            for ev in self.readers.get(b, ()):
                add(ev)
        waits = []
        for k, v in need.items():
            if eng == "pe" and k == ("e", "pe"):
                continue
            if self.seen[eng].get(k, 0) < v:
                self.seen[eng][k] = v
                waits.append((k, v))
        return waits

    def _record(self, ev, reads, writes):
        for b in reads:
            self.readers.setdefault(b, []).append(ev)
        for b in writes:
            self.last_w[b] = ev
            self.readers[b] = []

    def _flush_pe(self):
        if self.pe_run:
            k = ("e", "pe")
            self.count[k] = self.count.get(k, 0) + 1
            self.pe_run[-1][2] = (k, 1)
            self.pe_run = []

    def op(self, eng, fn, reads=(), writes=()):
        fn = _freeze(fn)
        if eng != "pe":
            self._flush_pe()
        waits = self._deps(eng, reads, writes)
        k = ("e", eng)
        if eng == "pe":
            v = self.count.get(k, 0) + 1
            entry = [waits, fn, None]
            self.ops[eng].append(entry)
            self.pe_run.append(entry)
            self._record((k, v), reads, writes)
            return
        v = self.count.get(k, 0) + 1
        self.count[k] = v
        self.ops[eng].append([waits, fn, (k, 1)])
        self._record((k, v), reads, writes)

    def dma(self, fn, reads=(), writes=(), eng="sp"):
        fn = _freeze(fn)
        self._flush_pe()
        waits = self._deps(eng, reads, writes)
        k = ("d", eng, self.dma_rr[eng] % self.n_dma)
        self.dma_rr[eng] += 1
        v = self.count.get(k, 0) + 16
        self.count[k] = v
        self.ops[eng].append([waits, fn, (k, 16)])
        self._record((k, v), reads, writes)

    def emit(self):
        self._flush_pe()
        nc = self.nc
        keys = sorted(self.count.keys(), key=str)
        with contextlib.ExitStack() as st:
            sems = {k: st.enter_context(nc.semaphore("s_" + "_".join(map(str, k)))) for k in keys}
            block = st.enter_context(nc.Block())
            final = [(k, self.count[k]) for k in keys]

            def run(engobj, lst, is_final):
                for waits, fn, incr in lst:
                    for wk, wv in waits:
                        engobj.wait_ge(sems[wk], wv)
                    ins_ = fn(engobj)
                    if incr is not None:
                        ins_.then_inc(sems[incr[0]], incr[1])
                if is_final:
                    for wk, wv in final:
                        engobj.wait_ge(sems[wk], wv)
            names = {"pe": "tensor", "act": "scalar", "dve": "vector", "pool": "gpsimd", "sp": "sync"}
            for e in ENGS:
                lst = self.ops[e]
                getattr(block, names[e])(lambda engobj, lst=lst, e=e: run(engobj, lst, e == "sp"))


def build(stage="full", off=()):
    nc = bass.Bass("TRN2", target_bir_lowering=False)
    S = Sched(nc)
    D = {}

    def din(name, shape, dt=F32):
        D[name] = nc.dram_tensor(name, list(shape), dt, kind="ExternalInput").ap()
        return D[name]
    xown = din("xown", [TOK, 1024]); xhalo = din("xhalo", [128, 1024]); xpre = din("xpre", [NPRE * 128, 1024])
    xmeta = din("xmeta", [128, 1024])
    din("ident", [128, 128]); din("mask_cur", [128, 128]); din("mask_prev", [128, 128]); din("mask_halo", [128, 128])
    din("w_in", [128, 8, 1280]); din("g1", [128, 8]); din("gq2", [128, 1]); din("gk2", [128, 1]); din("sinks", [64, 8])
    for nm in ("a_re", "a_im", "ldt"):
        din(nm + "_s", [128, 16]); din(nm + "_r", [128, 2048])
    din("bT_re", [128, 16, 128]); din("bT_im", [128, 16, 128]); din("bs_re", [128, 16, 32]); din("bs_im", [128, 16, 32])
    din("cb_re", [128, 16, 128]); din("cb_im", [128, 16, 128]); din("ssm_d", [128, 4]); din("glu_w", [128, 4, 512])
    din("glu_b", [128, 4]); din("ga", [64, 8]); din("gs", [128, 4]); din("woa", [64, 8, 1024]); din("wos", [128, 4, 1024])
    din("g2", [128, 8]); din("is0", [128, 1])
    if stage in ("peerfront", "full"):
        din("wq", [128, 8, 2048]); din("keysT", [128, 16, 128])
    if stage == "full":
        din("puT", [128, 128, 1024]); din("pv", [128, 128, 1024]); din("iota_i", [128, 128]); din("blkmask", [128, 8])
        UTs = nc.dram_tensor("UTs", [128, 128, 1024], BF16).ap(); Vs = nc.dram_tensor("Vs", [128, 128, 1024], BF16).ap()
    out = nc.dram_tensor("out", [TOK, 1024], F32, kind="ExternalOutput").ap()

    with contextlib.ExitStack() as top:
        def SB(st, name, shape, dt=F32):
            return st.enter_context(nc.sbuf_tensor("sb_" + name, list(shape), dt))

        def PS(st, name, shape, dt=F32):
            return st.enter_context(nc.psum_tensor("ps_" + name, list(shape), dt))

        def V(fn, r, w): S.op("dve", fn, r, w)
        def A(fn, r, w): S.op("act", fn, r, w)
        def G(fn, r, w): S.op("pool", fn, r, w)
        def M(fn, r, w): S.op("pe", fn, r, w)

        @contextlib.contextmanager
        def scope():
            with contextlib.ExitStack() as st_:
                yield st_
                S.barrier()

        def load(dst, src, key):
            S.dma(lambda e: e.dma_start(out=dst, in_=src), writes=[key])

        idf = SB(top, "idf", [128, 128]); idb = SB(top, "idb", [128, 128], BF16)
        load(idf[:], D["ident"], "idf")
        V(lambda e: e.tensor_copy(idb[:], idf[:]), ["idf"], ["idb"])
        ones = SB(top, "ones", [128, 64], BF16)
        V(lambda e: e.memset(ones[:], 1.0), [], ["ones"])
        h2T = SB(top, "h2T", [128, 8, TOK if "smallh2T" not in off else 128], BF16)

        mix = top.enter_context(contextlib.ExitStack())
        ptr = PS(mix, "ptr", [128, 1024], BF16)
        psA = PS(mix, "psA", [128, 1024])
        pb = [PS(mix, f"pb{i}", [128, 512]) for i in range(5)]

        win = SB(mix, "win", [128, 8, 1280], BF16); woa = SB(mix, "woa", [64, 8, 1024], BF16)
        wos = SB(mix, "wos", [128, 4, 1024], BF16); glu = SB(mix, "glu", [128, 4, 512], BF16)
        small = {}
        for nm, shp in (("g1", [128, 8]), ("gq2", [128, 1]), ("gk2", [128, 1]), ("sinks", [64, 8]), ("ssm_d", [128, 4]),
                        ("glu_b", [128, 4]), ("ga", [64, 8]), ("gs", [128, 4]), ("g2", [128, 8]), ("is0", [128, 1])):
            small[nm] = SB(mix, "c_" + nm, shp)
            load(small[nm][:], D[nm], "c_" + nm)
        with scope() as stg:
            s1 = SB(stg, "stg1", [128, 8, 1280])
            load(s1[:], D["w_in"], "stg1")
            for k in range(8):
                V(lambda e, k=k: e.tensor_scalar(win[:, k, :], s1[:, k, :], small["g1"][:, k:k + 1], None, ALU.mult),
                  ["stg1", "c_g1"], ["win"])
        with scope() as stg:
            s2 = SB(stg, "stg2", [64, 8, 1024]); s3 = SB(stg, "stg3", [128, 4, 1024]); s4 = SB(stg, "stg4", [128, 4, 512])
            load(s2[:], D["woa"], "stg2"); load(s3[:], D["wos"], "stg3"); load(s4[:], D["glu_w"], "stg4")
            for h in range(8):
                V(lambda e, h=h: e.tensor_scalar(woa[:, h, :], s2[:, h, :], small["ga"][:, h:h + 1], None, ALU.mult),
                  ["stg2", "c_ga"], ["woa"])
            for c in range(4):
                V(lambda e, c=c: e.tensor_scalar(wos[:, c, :], s3[:, c, :], small["gs"][:, c:c + 1], None, ALU.mult),
                  ["stg3", "c_gs"], ["wos"])
            V(lambda e: e.tensor_copy(glu[:], s4[:]), ["stg4"], ["glu"])
        cqk = SB(mix, "cqk", [128, 1]); esink = SB(mix, "esink", [64, 8])
        V(lambda e: e.tensor_tensor(cqk[:], small["gq2"][:], small["gk2"][:], ALU.mult), ["c_gq2", "c_gk2"], ["cqk"])
        V(lambda e: e.tensor_scalar(cqk[:], cqk[:], 0.125, None, ALU.mult), ["cqk"], ["cqk"])
        A(lambda e: e.activation(out=esink[:], in_=small["sinks"][:], func=AF.Exp), ["c_sinks"], ["esink"])
        mcur = SB(mix, "mcur", [128, 128], BF16); mprev = SB(mix, "mprev", [128, 128], BF16); mhalo = SB(mix, "mhalo", [128, 128], BF16)
        with scope() as stg:
            m1 = SB(stg, "m1", [128, 128]); m2 = SB(stg, "m2", [128, 128]); m3 = SB(stg, "m3", [128, 128])
            load(m1[:], D["mask_cur"], "m1"); load(m2[:], D["mask_prev"], "m2"); load(m3[:], D["mask_halo"], "m3")
            V(lambda e: e.tensor_copy(mcur[:], m1[:]), ["m1"], ["mcur"])
            V(lambda e: e.tensor_copy(mprev[:], m2[:]), ["m2"], ["mprev"])
            V(lambda e: e.tensor_copy(mhalo[:], m3[:]), ["m3"], ["mhalo"])

        TWO_PI = 2.0 * np.pi

        def ssm_scalars(st, tag, are, aim, ldt, shape, t=None):
            if t is None:
                t = {n: SB(st, f"{tag}_{n}", shape) for n in ("dt", "rho", "th", "ph", "cos", "sin", "abr", "abi", "den", "nr", "t1", "t2", "cfr", "cfi")}
                t["ri"] = SB(st, f"{tag}_ri", shape, mybir.dt.int32)
            k = lambda n: f"{tag}_{n}"
            A(lambda e: e.activation(out=t["dt"][:], in_=ldt[:], func=AF.Exp), [k("ldt")], [k("dt")])
            V(lambda e: e.tensor_tensor(t["rho"][:], are[:], t["dt"][:], ALU.mult), [k("are"), k("dt")], [k("rho")])
            A(lambda e: e.activation(out=t["rho"][:], in_=t["rho"][:], func=AF.Exp), [k("rho")], [k("rho")])
            V(lambda e: e.tensor_tensor(t["th"][:], aim[:], t["dt"][:], ALU.mult), [k("aim"), k("dt")], [k("th")])
            for nm, off in (("sin", 0.0), ("cos", np.pi / 2)):
                V(lambda e, off=off: e.tensor_scalar(t["t2"][:], t["th"][:], float(off), None, ALU.add), [k("th")], [k("t2")])
                V(lambda e: e.tensor_scalar(t["t1"][:], t["t2"][:], float(1.0 / TWO_PI), None, ALU.mult), [k("t2")], [k("t1")])
                V(lambda e: e.tensor_copy(t["ri"][:], t["t1"][:]), [k("t1")], [k("ri")])
                V(lambda e: e.tensor_copy(t["t1"][:], t["ri"][:]), [k("ri")], [k("t1")])
                V(lambda e: e.scalar_tensor_tensor(t["ph"][:], t["t1"][:], float(-TWO_PI), t["t2"][:], ALU.mult, ALU.add), [k("t1"), k("t2")], [k("ph")])
                V(lambda e: e.tensor_scalar(t["t1"][:], t["ph"][:], float(np.pi), float(-TWO_PI), ALU.is_gt, ALU.mult), [k("ph")], [k("t1")])
                V(lambda e: e.tensor_tensor(t["ph"][:], t["ph"][:], t["t1"][:], ALU.add), [k("ph"), k("t1")], [k("ph")])
                A(lambda e, nm=nm: e.activation(out=t[nm][:], in_=t["ph"][:], func=AF.Sin), [k("ph")], [k(nm)])
            V(lambda e: e.tensor_tensor(t["abr"][:], t["rho"][:], t["cos"][:], ALU.mult), [k("rho"), k("cos")], [k("abr")])
            V(lambda e: e.tensor_tensor(t["abi"][:], t["rho"][:], t["sin"][:], ALU.mult), [k("rho"), k("sin")], [k("abi")])
            V(lambda e: e.tensor_tensor(t["den"][:], are[:], are[:], ALU.mult), [k("are")], [k("den")])
            V(lambda e: e.tensor_tensor(t["t1"][:], aim[:], aim[:], ALU.mult), [k("aim")], [k("t1")])
            V(lambda e: e.tensor_tensor(t["den"][:], t["den"][:], t["t1"][:], ALU.add), [k("den"), k("t1")], [k("den")])
            V(lambda e: e.reciprocal(t["den"][:], t["den"][:]), [k("den")], [k("den")])
            V(lambda e: e.tensor_scalar(t["nr"][:], t["abr"][:], -1.0, None, ALU.add), [k("abr")], [k("nr")])
            V(lambda e: e.tensor_tensor(t["t1"][:], t["nr"][:], are[:], ALU.mult), [k("nr"), k("are")], [k("t1")])
            V(lambda e: e.tensor_tensor(t["t2"][:], t["abi"][:], aim[:], ALU.mult), [k("abi"), k("aim")], [k("t2")])
            V(lambda e: e.tensor_tensor(t["t1"][:], t["t1"][:], t["t2"][:], ALU.add), [k("t1"), k("t2")], [k("t1")])
            V(lambda e: e.tensor_tensor(t["cfr"][:], t["t1"][:], t["den"][:], ALU.mult), [k("t1"), k("den")], [k("cfr")])
            V(lambda e: e.tensor_tensor(t["t1"][:], t["abi"][:], are[:], ALU.mult), [k("abi"), k("are")], [k("t1")])
            V(lambda e: e.tensor_tensor(t["t2"][:], t["nr"][:], aim[:], ALU.mult), [k("nr"), k("aim")], [k("t2")])
            V(lambda e: e.tensor_tensor(t["t1"][:], t["t1"][:], t["t2"][:], ALU.subtract), [k("t1"), k("t2")], [k("t1")])
            V(lambda e: e.tensor_tensor(t["cfi"][:], t["t1"][:], t["den"][:], ALU.mult), [k("t1"), k("den")], [k("cfi")])
            return t

        def cmul(dst_r, dst_i, ar, ai, br, bi, t1, t2, rk, wk):
            V(lambda e: e.tensor_tensor(t1, ar, br, ALU.mult), rk, wk)
            V(lambda e: e.tensor_tensor(t2, ai, bi, ALU.mult), rk, wk)
            V(lambda e: e.tensor_tensor(dst_r, t1, t2, ALU.subtract), rk + wk, wk)
            V(lambda e: e.tensor_tensor(t1, ar, bi, ALU.mult), rk + wk, wk)
            V(lambda e: e.tensor_tensor(t2, ai, br, ALU.mult), rk + wk, wk)
            V(lambda e: e.tensor_tensor(dst_i, t1, t2, ALU.add), rk + wk, wk)

        ss_are = SB(mix, "ss_are", [128, 16]); ss_aim = SB(mix, "ss_aim", [128, 16]); ss_ldt = SB(mix, "ss_ldt", [128, 16])
        load(ss_are[:], D["a_re_s"], "ss_are"); load(ss_aim[:], D["a_im_s"], "ss_aim"); load(ss_ldt[:], D["ldt_s"], "ss_ldt")
        sc = ssm_scalars(mix, "ss", ss_are, ss_aim, ss_ldt, [128, 16])
        Er = SB(mix, "Er", [128, 16, 128]); Ei = SB(mix, "Ei", [128, 16, 128]); Rho0 = SB(mix, "Rho0", [128, 16, 128])
        WTr = SB(mix, "WTr", [128, 16, 128], BF16); WTi = SB(mix, "WTi", [128, 16, 128], BF16)
        A128r = SB(mix, "A128r", [128, 16]); A128i = SB(mix, "A128i", [128, 16])
        E127r = SB(mix, "E127r", [128, 16]); E127i = SB(mix, "E127i", [128, 16])
        bsr = SB(mix, "bsr", [128, 16, 32]); bsi = SB(mix, "bsi", [128, 16, 32])
        BTr = SB(mix, "BTr", [128, 16, 128], BF16); BTi = SB(mix, "BTi", [128, 16, 128], BF16)
        Cbr = SB(mix, "Cbr", [128, 16, 128], BF16); Cbi = SB(mix, "Cbi", [128, 16, 128], BF16)

        def bc3(ap2, n):
            return ap2.rearrange("p (a o) -> p a o", o=1).to_broadcast([128, 16, n])

        with scope() as stg:
            Hr = SB(stg, "Hr", [128, 16, 128]); Hi = SB(stg, "Hi", [128, 16, 128])
            T1 = SB(stg, "T1", [128, 16, 128]); T2 = SB(stg, "T2", [128, 16, 128])
            q1 = SB(stg, "q1", [128, 16]); q2 = SB(stg, "q2", [128, 16]); ir = SB(stg, "ir", [128, 16]); ii_ = SB(stg, "ii", [128, 16])
            pr = SB(stg, "pr", [128, 16]); pi_ = SB(stg, "pi", [128, 16])
            V(lambda e: e.memset(Er[:, :, 0:1], 1.0), [], ["Er"]); V(lambda e: e.memset(Ei[:, :, 0:1], 0.0), [], ["Ei"])
            V(lambda e: e.tensor_copy(Er[:, :, 1], sc["cos"][:]), ["ss_cos"], ["Er"])
            V(lambda e: e.tensor_copy(Ei[:, :, 1], sc["sin"][:]), ["ss_sin"], ["Ei"])
            V(lambda e: e.tensor_tensor(q1[:], sc["rho"][:], sc["rho"][:], ALU.mult), ["ss_rho"], ["q1"])
            V(lambda e: e.reciprocal(q1[:], q1[:]), ["q1"], ["q1"])
            V(lambda e: e.tensor_tensor(ir[:], sc["abr"][:], q1[:], ALU.mult), ["ss_abr", "q1"], ["ir"])
            V(lambda e: e.tensor_tensor(ii_[:], sc["abi"][:], q1[:], ALU.mult), ["ss_abi", "q1"], ["ii"])
            V(lambda e: e.tensor_scalar(ii_[:], ii_[:], -1.0, None, ALU.mult), ["ii"], ["ii"])
            V(lambda e: e.memset(Hr[:, :, 0:1], 1.0), [], ["Hr"]); V(lambda e: e.memset(Hi[:, :, 0:1], 0.0), [], ["Hi"])
            V(lambda e: e.tensor_copy(Hr[:, :, 1], ir[:]), ["ir"], ["Hr"]); V(lambda e: e.tensor_copy(Hi[:, :, 1], ii_[:]), ["ii"], ["Hi"])
            for (Xr, Xi, kr, ki) in ((Er, Ei, "Er", "Ei"), (Hr, Hi, "Hr", "Hi")):
                m = 2
                while m < 128:
                    h = m // 2
                    cmul(Xr[:, :, m], Xi[:, :, m], Xr[:, :, h], Xi[:, :, h], Xr[:, :, h], Xi[:, :, h], q1[:], q2[:], [kr, ki, "q1", "q2"], [kr, ki, "q1", "q2"])
                    cmul(Xr[:, :, m + 1:2 * m], Xi[:, :, m + 1:2 * m], Xr[:, :, 1:m], Xi[:, :, 1:m],
                         bc3(Xr[:, :, m], m - 1), bc3(Xi[:, :, m], m - 1), T1[:, :, 1:m], T2[:, :, 1:m], [kr, ki, "T1", "T2"], [kr, ki, "T1", "T2"])
                    m *= 2
            V(lambda e: e.tensor_copy(A128r[:], sc["abr"][:]), ["ss_abr"], ["A128"]); V(lambda e: e.tensor_copy(A128i[:], sc["abi"][:]), ["ss_abi"], ["A128"])
            for _ in range(7):
                cmul(pr[:], pi_[:], A128r[:], A128i[:], A128r[:], A128i[:], q1[:], q2[:], ["A128", "q1", "q2", "pp"], ["pp", "q1", "q2"])
                V(lambda e: e.tensor_copy(A128r[:], pr[:]), ["pp"], ["A128"]); V(lambda e: e.tensor_copy(A128i[:], pi_[:]), ["pp"], ["A128"])
            V(lambda e: e.tensor_copy(E127r[:], Er[:, :, 127]), ["Er"], ["E127"]); V(lambda e: e.tensor_copy(E127i[:], Ei[:, :, 127]), ["Ei"], ["E127"])
            cmul(pr[:], pi_[:], A128r[:], A128i[:], ir[:], ii_[:], q1[:], q2[:], ["A128", "ir", "ii", "q1", "q2", "pp"], ["pp", "q1", "q2"])
            cmul(T1[:], T2[:], Hr[:], Hi[:], bc3(pr[:], 128), bc3(pi_[:], 128), Er[:] if False else Rho0[:], SB(stg, "T3", [128, 16, 128])[:],
                 ["Hr", "Hi", "pp", "Rho0", "T3", "T1", "T2"], ["T1", "T2", "Rho0", "T3"])
            for (Ts, WT, kk) in ((T1, WTr, "WTr"), (T2, WTi, "WTi")):
                for g4 in range(4):
                    for j in range(4):
                        i = 4 * g4 + j
                        M(lambda e, i=i, j=j, Ts=Ts: e.transpose(pb[0][:, j * 128:(j + 1) * 128], Ts[:, i, :], idf[:]), ["T1", "T2", "idf"], ["pb0"])
                    V(lambda e, g4=g4, WT=WT: e.tensor_copy(WT[:, 4 * g4:4 * g4 + 4, :], pb[0][:].rearrange("p (a b) -> p a b", a=4)), ["pb0"], [kk])
            V(lambda e: e.tensor_copy(Rho0[:], bc3(sc["rho"][:], 128)), ["ss_rho", "T1", "T2"], ["Rho0"])
            V(lambda e: e.memset(Rho0[:, :, 0:1], 0.0), ["Rho0"], ["Rho0"])
            b0r = SB(stg, "b0r", [128, 16, 32]); b0i = SB(stg, "b0i", [128, 16, 32]); u1 = SB(stg, "u1", [128, 16, 32]); u2 = SB(stg, "u2", [128, 16, 32])
            load(b0r[:], D["bs_re"], "b0r"); load(b0i[:], D["bs_im"], "b0i")
            cmul(bsr[:], bsi[:], bc3(sc["cfr"][:], 32), bc3(sc["cfi"][:], 32), b0r[:], b0i[:], u1[:], u2[:], ["ss_cfr", "ss_cfi", "b0r", "b0i", "u1", "u2"], ["bs", "u1", "u2"])
        with scope() as stg:
            CW = 512
            r_are = SB(stg, "rr_are", [128, CW]); r_aim = SB(stg, "rr_aim", [128, CW]); r_ldt = SB(stg, "rr_ldt", [128, CW])
            rc = None
            BTr2 = BTr[:].rearrange("p a b -> p (a b)"); BTi2 = BTi[:].rearrange("p a b -> p (a b)")
            Cbr2 = Cbr[:].rearrange("p a b -> p (a b)"); Cbi2 = Cbi[:].rearrange("p a b -> p (a b)")
            for cc in range(2048 // CW):
                cs = slice(cc * CW, (cc + 1) * CW)
                load(r_are[:], D["a_re_r"][:, cs], "rr_are"); load(r_aim[:], D["a_im_r"][:, cs], "rr_aim"); load(r_ldt[:], D["ldt_r"][:, cs], "rr_ldt")
                rc = ssm_scalars(stg, "rr", r_are, r_aim, r_ldt, [128, CW], t=rc)
                b1r = rc["dt"]; b1i = rc["th"]
                load(b1r[:], D["bT_re"].rearrange("p a b -> p (a b)")[:, cs], "rr_dt"); load(b1i[:], D["bT_im"].rearrange("p a b -> p (a b)")[:, cs], "rr_th")
                cmul(rc["cos"][:], rc["sin"][:], rc["cfr"][:], rc["cfi"][:], b1r[:], b1i[:], rc["t1"][:], rc["t2"][:],
                     ["rr_cfr", "rr_cfi", "rr_dt", "rr_th", "rr_t1", "rr_t2", "rr_cos", "rr_sin"], ["rr_cos", "rr_sin", "rr_t1", "rr_t2"])
                V(lambda e, cs=cs: e.tensor_copy(BTr2[:, cs], rc["cos"][:]), ["rr_cos"], ["BT"])
                V(lambda e, cs=cs: e.tensor_copy(BTi2[:, cs], rc["sin"][:]), ["rr_sin"], ["BT"])
                load(rc["t1"][:], D["cb_re"].rearrange("p a b -> p (a b)")[:, cs], "rr_t1"); load(rc["t2"][:], D["cb_im"].rearrange("p a b -> p (a b)")[:, cs], "rr_t2")
                V(lambda e, cs=cs: e.tensor_copy(Cbr2[:, cs], rc["t1"][:]), ["rr_t1"], ["Cb"])
                V(lambda e, cs=cs: e.tensor_scalar(Cbi2[:, cs], rc["t2"][:], -1.0, None, ALU.mult), ["rr_t2"], ["Cb"])

        xt = [SB(mix, f"xt{i}", [128, 1024]) for i in range(2)]
        junk = SB(mix, "junk", [128, 1024], BF16); xnb = SB(mix, "xnb", [128, 1024], BF16); nT = SB(mix, "nT", [128, 8, 128], BF16)
        st1 = SB(mix, "st1", [128, 4])
        Sr = SB(mix, "Sr", [128, 16]); Si = SB(mix, "Si", [128, 16])
        V(lambda e: e.memset(Sr[:], 0.0), [], ["S"]); V(lambda e: e.memset(Si[:], 0.0), [], ["S"])
        c1 = SB(mix, "c1", [128, 16]); c2 = SB(mix, "c2", [128, 16]); c3 = SB(mix, "c3", [128, 16]); c4 = SB(mix, "c4", [128, 16])
        xi = [0]

        def front(src):
            sl = xi[0] % 2; xi[0] += 1
            kx = f"xt{sl}"
            S.dma(lambda e: e.dma_start(out=xt[sl][:], in_=src), writes=[kx])
            A(lambda e: e.activation(out=junk[:], in_=xt[sl][:], func=AF.Square, accum_out=st1[:, 0:1]), [kx], ["junk", "st1"])
            V(lambda e: e.tensor_scalar(st1[:, 1:2], st1[:, 0:1], 1.0 / 1024, EPS, ALU.mult, ALU.add), ["st1"], ["st1b"])
            A(lambda e: e.activation(out=st1[:, 3:4], in_=st1[:, 1:2], func=AF.Sqrt), ["st1b"], ["st1d"])
            V(lambda e: e.reciprocal(st1[:, 2:3], st1[:, 3:4]), ["st1d"], ["st1c"])
            V(lambda e: e.tensor_scalar(xnb[:], xt[sl][:], st1[:, 2:3], None, ALU.mult), [kx, "st1c"], ["xnb"])
            for k in range(8):
                M(lambda e, k=k: e.transpose(ptr[:, k * 128:(k + 1) * 128], xnb[:, k * 128:(k + 1) * 128], idb[:]), ["xnb", "idb"], ["ptr"])
            A(lambda e: e.copy(nT[:].rearrange("p a b -> p (a b)"), ptr[:]), ["ptr"], ["nT"])
            return sl

        ub = SB(mix, "ub", [128, 512], BF16)
        fr = SB(mix, "fr", [128, 16]); fi = SB(mix, "fi", [128, 16])
        w1 = SB(mix, "w1", [128, 16, 32]); w2 = SB(mix, "w2", [128, 16, 32])

        def state_step():
            cmul(c3[:], c4[:], A128r[:], A128i[:], Sr[:], Si[:], c1[:], c2[:], ["A128", "S", "c"], ["c"])
            V(lambda e: e.tensor_tensor(Sr[:], c3[:], fr[:], ALU.add), ["c", "f"], ["S"])
            V(lambda e: e.tensor_tensor(Si[:], c4[:], fi[:], ALU.add), ["c", "f"], ["S"])

        def prefix_tile(src):
            front(src)
            for k in range(8):
                M(lambda e, k=k: e.matmul(pb[0][:], lhsT=nT[:, k, :], rhs=win[:, k, 768:1280], start=(k == 0), stop=(k == 7)), ["nT", "win"], ["pb0"])
            A(lambda e: e.copy(ub[:], pb[0][:]), ["pb0"], ["ub"])
            for i in range(16):
                M(lambda e, i=i: e.matmul(pb[1][:, i * 32:(i + 1) * 32], lhsT=WTr[:, i, :], rhs=ub[:, i * 32:(i + 1) * 32], start=True, stop=True), ["ub", "WTr"], ["pb1"])
                M(lambda e, i=i: e.matmul(pb[2][:, i * 32:(i + 1) * 32], lhsT=WTi[:, i, :], rhs=ub[:, i * 32:(i + 1) * 32], start=True, stop=True), ["ub", "WTi"], ["pb2"])
            mr = pb[1][:].rearrange("p (a b) -> p a b", a=16); mi = pb[2][:].rearrange("p (a b) -> p a b", a=16)
            V(lambda e: e.tensor_tensor(w1[:], bsr[:], mr, ALU.mult), ["bs", "pb1"], ["w1"])
            V(lambda e: e.tensor_tensor(w2[:], bsi[:], mi, ALU.mult), ["bs", "pb2"], ["w2"])
            V(lambda e: e.tensor_tensor(w1[:], w1[:], w2[:], ALU.subtract), ["w1", "w2"], ["w1"])
            V(lambda e: e.tensor_reduce(fr[:], w1[:], AX.X, ALU.add), ["w1"], ["f"])
            V(lambda e: e.tensor_tensor(w1[:], bsr[:], mi, ALU.mult), ["bs", "pb2", "f"], ["w1"])
            V(lambda e: e.tensor_tensor(w2[:], bsi[:], mr, ALU.mult), ["bs", "pb1"], ["w2"])
            V(lambda e: e.tensor_tensor(w1[:], w1[:], w2[:], ALU.add), ["w1", "w2"], ["w1"])
            V(lambda e: e.tensor_reduce(fi[:], w1[:], AX.X, ALU.add), ["w1"], ["f"])
            state_step()

        chunks = []
        if stage == "full":
            for (src, dst, nm) in ((D["puT"], UTs, "UTs"), (D["pv"], Vs, "Vs")):
                for i in range(128):
                    chunks.append((src, dst, nm, i))
        with scope() as pst:
            NS = 3
            if chunks:
                cst = [SB(pst, f"cst{i}", [128, 1024]) for i in range(NS)]; cbf = [SB(pst, f"cbf{i}", [128, 1024], BF16) for i in range(NS)]
            cpos = [0]

            def emit_chunks(n):
                for _ in range(n):
                    if cpos[0] >= len(chunks):
                        return
                    src, dst, nm, i = chunks[cpos[0]]; sl = cpos[0] % NS; cpos[0] += 1
                    S.dma(lambda e: e.dma_start(out=cst[sl][:], in_=src[i]), writes=[f"cst{sl}"])
                    G(lambda e: e.tensor_copy(cbf[sl][:], cst[sl][:]), [f"cst{sl}"], [f"cbf{sl}"])
                    S.dma(lambda e: e.dma_start(out=dst[i], in_=cbf[sl][:]), reads=[f"cbf{sl}"], writes=[f"{nm}{i}"], eng="pool")
            for t in range(NPRE if "prefix" not in off else 0):
                prefix_tile(xpre[t * 128:(t + 1) * 128, :])
                emit_chunks(3)
            emit_chunks(len(chunks))

        if stage == "prefix":
            dbg = SB(mix, "dbg", [128, 1024])
            V(lambda e: e.memset(dbg[:], 0.0), [], ["dbg"])
            V(lambda e: e.tensor_copy(dbg[:, 0:16], Sr[:]), ["S"], ["dbg"]); V(lambda e: e.tensor_copy(dbg[:, 16:32], Si[:]), ["S"], ["dbg"])
            for j, (ap, key) in enumerate([(sc["cos"][:], "ss_cos"), (sc["sin"][:], "ss_sin"), (sc["abr"][:], "ss_abr"), (sc["abi"][:], "ss_abi"),
                                           (sc["cfr"][:], "ss_cfr"), (sc["cfi"][:], "ss_cfi"), (Er[:, :, 5], "Er"), (Ei[:, :, 5], "Ei"),
                                           (A128r[:], "A128"), (A128i[:], "A128"), (fr[:], "f"), (fi[:], "f"), (sc["rho"][:], "ss_rho"), (sc["th"][:], "ss_th")]):
                V(lambda e, j=j, ap=ap: e.tensor_copy(dbg[:, 32 + 16 * j:48 + 16 * j], ap), [key], ["dbg"])
            V(lambda e: e.tensor_copy(dbg[:, 512:1024], ub[:]), ["ub"], ["dbg"])
            S.dma(lambda e: e.dma_start(out=out[0:128, :], in_=dbg[:]), reads=["dbg"], writes=["out"])
            S.emit()
            return nc

        qkv_sb = SB(mix, "qkv_sb", [128, 768]); sqt = SB(mix, "sqt", [128, 640]); s10 = SB(mix, "s10", [128, 10]); s10b = SB(mix, "s10b", [128, 10])
        qkn = SB(mix, "qkn", [128, 640], BF16); qT = SB(mix, "qT", [128, 4, 128], BF16)
        kTs = [SB(mix, f"kT{i}", [128, 128], BF16) for i in range(3)]; vss = [SB(mix, f"vs{i}", [128, 128], BF16) for i in range(3)]
        kTm = SB(mix, "kTm", [128, 128], BF16); vm = SB(mix, "vm", [128, 128], BF16)
        pex = SB(mix, "pex", [128, 512], BF16); PT = [SB(mix, f"PT{i}", [128, 4, 128], BF16) for i in range(2)]; PTm = SB(mix, "PTm", [16, 512], BF16)
        den_sb = SB(mix, "den_sb", [64, 4, 128]); oT = SB(mix, "oT", [64, 8, 128], BF16); osq = SB(mix, "osq", [64, 4, 128], BF16)
        uTb = SB(mix, "uTb", [128, 4, 128], BF16)
        t1 = SB(mix, "t1", [128, 512]); t2 = SB(mix, "t2", [128, 512]); btr = SB(mix, "btr", [128, 512]); bti = SB(mix, "bti", [128, 512])
        str_ = SB(mix, "str", [128, 512]); sti = SB(mix, "sti", [128, 512]); t3 = SB(mix, "t3", [128, 512]); t4 = SB(mix, "t4", [128, 512])
        sre = SB(mix, "sre", [128, 512], BF16); sim = SB(mix, "sim", [128, 512], BF16)
        injr = SB(mix, "injr", [128, 16]); inji = SB(mix, "inji", [128, 16]); stlr = SB(mix, "stlr", [128, 16]); stli = SB(mix, "stli", [128, 16])
        ysb = SB(mix, "ysb", [128, 4, 128]); zb = SB(mix, "zb", [128, 4, 128], BF16); sg = SB(mix, "sg", [128, 4, 128], BF16)
        zz = SB(mix, "zz", [128, 4, 128], BF16); zsq = SB(mix, "zsq", [128, 4, 128], BF16)
        rs = SB(mix, "rs", [128, 4]); hm = SB(mix, "hm", [128, 1024]); h2b = SB(mix, "h2b", [128, 1024], BF16); st2 = SB(mix, "st2", [128, 4])

        def bcg(ap2, a, n):
            return ap2.rearrange("p (a o) -> p a o", o=1).to_broadcast([ap2.shape[0], a, n])

        def kv_part(kslot, vslot, with_q):
            c0 = 0 if with_q else 512
            g0 = c0 // 64
            if with_q:
                for k in range(8):
                    M(lambda e, k=k: e.matmul(psA[:, 0:512], lhsT=nT[:, k, :], rhs=win[:, k, 0:512], start=(k == 0), stop=(k == 7)), ["nT", "win"], ["psAq"])
                A(lambda e: e.copy(qkv_sb[:, 0:512], psA[:, 0:512]), ["psAq"], ["qkv_q"])
                if "q1" in off:
                    raise StopBuild()
            for k in range(8):
                M(lambda e, k=k: e.matmul(psA[:, 512:768], lhsT=nT[:, k, :], rhs=win[:, k, 512:768], start=(k == 0), stop=(k == 7)), ["nT", "win"], ["psAk"])
            A(lambda e: e.copy(qkv_sb[:, 512:768], psA[:, 512:768]), ["psAk"], ["qkv_k"])
            rk = ["qkv_q", "qkv_k"] if with_q else ["qkv_k"]
            V(lambda e: e.tensor_tensor(sqt[:, c0:640], qkv_sb[:, c0:640], qkv_sb[:, c0:640], ALU.mult), rk, ["sqt"])
            V(lambda e: e.tensor_reduce(s10[:, g0:10], sqt[:, c0:640].rearrange("p (a b) -> p a b", b=64), AX.X, ALU.add), ["sqt"], ["s10"])
            V(lambda e: e.tensor_scalar(s10[:, g0:10], s10[:, g0:10], 1.0 / 64, EPS, ALU.mult, ALU.add), ["s10"], ["s10"])
            A(lambda e: e.activation(out=s10b[:, g0:10], in_=s10[:, g0:10], func=AF.Sqrt), ["s10"], ["s10b"])
            V(lambda e: e.reciprocal(s10b[:, g0:10], s10b[:, g0:10]), ["s10b"], ["s10b"])
            V(lambda e: e.tensor_tensor(qkn[:, c0:640].rearrange("p (a b) -> p a b", b=64), qkv_sb[:, c0:640].rearrange("p (a b) -> p a b", b=64),
                                        bcg(s10b[:, g0:10], 10 - g0, 64), ALU.mult), rk + ["s10b"], ["qkn"])
            if with_q and "q2" in off:
                raise StopBuild()
            M(lambda e: e.transpose(ptr[:, 512:640], qkn[:, 512:640], idb[:]), ["qkn", "idb"], ["ptr"])
            A(lambda e: e.copy(kslot[0][:], ptr[:, 512:640]), ["ptr"], [kslot[1]])
            A(lambda e: e.copy(vslot[0][:], qkv_sb[:, 640:768]), ["qkv_k"], [vslot[1]])
            if with_q:
                for m in range(4):
                    M(lambda e, m=m: e.transpose(ptr[:, m * 128:(m + 1) * 128], qkn[:, m * 128:(m + 1) * 128], idb[:]), ["qkn", "idb"], ["ptr"])
                if "q3" in off:
                    raise StopBuild()
                V(lambda e: e.tensor_scalar(qT[:].rearrange("p a b -> p (a b)"), ptr[:, 0:512], cqk[:, 0:1], None, ALU.mult), ["ptr", "cqk"], ["qT"])

        def attention(prev, cur, pmask):
            first = [True]
            for g in range(2):
                ps_ = slice(64 * g, 64 * g + 64)
                for b, (kk, vv, msk, mkey) in enumerate(((prev[0], prev[1], pmask[0], pmask[1]), (cur[0], cur[1], mcur, "mcur"))):
                    M(lambda e, b=b, kk=kk: e.matmul(pb[b][:], lhsT=kk[0][ps_, :], rhs=qT[ps_, :, :].rearrange("p a b -> p (a b)"), start=True, stop=True), [kk[1], "qT"], [f"pb{b}"])
                    A(lambda e, b=b: e.activation(out=pex[:], in_=pb[b][:], func=AF.Exp), [f"pb{b}"], ["pex"])
                    V(lambda e, b=b, msk=msk: e.tensor_tensor(PT[b][:], pex[:].rearrange("p (a b) -> p a b", a=4),
                                                            msk[:].rearrange("p (o q) -> p o q", o=1).to_broadcast([128, 4, 128]), ALU.mult), ["pex", mkey], [f"PT{b}"])
                M(lambda e: e.matmul(pb[2][0:16, :], lhsT=kTm[ps_, 0:16], rhs=qT[ps_, :, :].rearrange("p a b -> p (a b)"), start=True, stop=True), ["kTm", "qT"], ["pb2"])
                A(lambda e: e.activation(out=PTm[:], in_=pb[2][0:16, :], func=AF.Exp), ["pb2"], ["PTm"])
                cs_ = slice(64 * g, 64 * g + 64)
                PT0 = PT[0][:].rearrange("p a b -> p (a b)"); PT1 = PT[1][:].rearrange("p a b -> p (a b)")
                M(lambda e: e.matmul(pb[3][0:64, :], lhsT=prev[1][0][:, cs_], rhs=PT0, start=True, stop=False), [prev[1][1], "PT0"], ["pb3"])
                M(lambda e: e.matmul(pb[3][0:64, :], lhsT=cur[1][0][:, cs_], rhs=PT1, start=False, stop=False), [cur[1][1], "PT1"], ["pb3"])
                M(lambda e: e.matmul(pb[3][0:64, :], lhsT=vm[0:16, cs_], rhs=PTm[:], start=False, stop=True), ["vm", "PTm"], ["pb3"])
                M(lambda e: e.matmul(pb[4][0:64, :], lhsT=ones[:, 0:64], rhs=PT0, start=True, stop=False), ["ones", "PT0"], ["pb4"])
                M(lambda e: e.matmul(pb[4][0:64, :], lhsT=ones[:, 0:64], rhs=PT1, start=False, stop=False), ["ones", "PT1"], ["pb4"])
                M(lambda e: e.matmul(pb[4][0:64, :], lhsT=ones[0:16, 0:64], rhs=PTm[:], start=False, stop=True), ["ones", "PTm"], ["pb4"])
                for j in range(4):
                    h = 4 * g + j
                    A(lambda e, j=j, h=h: e.activation(out=den_sb[:, j, :], in_=pb[4][0:64, j * 128:(j + 1) * 128], func=AF.Identity, bias=esink[:, h:h + 1]), ["pb4", "esink"], ["den"])
                V(lambda e: e.reciprocal(den_sb[:], den_sb[:]), ["den"], ["den"])
                V(lambda e, g=g: e.tensor_tensor(oT[:, 4 * g:4 * g + 4, :], pb[3][0:64, :].rearrange("p (a b) -> p a b", a=4), den_sb[:], ALU.mult), ["pb3", "den"], ["oT"])
                V(lambda e, g=g: e.tensor_tensor(osq[:], oT[:, 4 * g:4 * g + 4, :], oT[:, 4 * g:4 * g + 4, :], ALU.mult), ["oT"], ["osq"])
                for j in range(4):
                    M(lambda e, j=j, g=g: e.matmul(psA[:, 768:769], lhsT=osq[:, j, :], rhs=ones[0:64, 0:1], start=(g == 0 and j == 0), stop=(g == 1 and j == 3)), ["osq", "ones"], ["pssa"])

        def ssm_tile():
            for c in range(4):
                for k in range(8):
                    M(lambda e, c=c, k=k: e.matmul(pb[0][:, c * 128:(c + 1) * 128], lhsT=win[:, k, 768 + 128 * c:768 + 128 * (c + 1)], rhs=nT[:, k, :], start=(k == 0), stop=(k == 7)), ["nT", "win"], ["pb0"])
            A(lambda e: e.copy(uTb[:].rearrange("p a b -> p (a b)"), pb[0][:]), ["pb0"], ["uTb"])
            cmul(injr[:], inji[:], sc["abr"][:], sc["abi"][:], Sr[:], Si[:], c1[:], c2[:], ["ss_abr", "ss_abi", "S", "c"], ["inj", "c"])
            for c in range(4):
                isl = slice(4 * c, 4 * c + 4)
                Erc = Er[:, isl, :].rearrange("p a b -> p (a b)"); Eic = Ei[:, isl, :].rearrange("p a b -> p (a b)")
                Rc = Rho0[:, isl, :].rearrange("p a b -> p (a b)")
                for ii in range(4):
                    i = 4 * c + ii
                    M(lambda e, i=i, ii=ii, c=c: e.matmul(pb[1][:, ii * 128:(ii + 1) * 128], lhsT=BTr[:, i, :], rhs=uTb[:, c, :], start=True, stop=True), ["BT", "uTb"], ["pb1"])
                    M(lambda e, i=i, ii=ii, c=c: e.matmul(pb[2][:, ii * 128:(ii + 1) * 128], lhsT=BTi[:, i, :], rhs=uTb[:, c, :], start=True, stop=True), ["BT", "uTb"], ["pb2"])
                V(lambda e: e.tensor_tensor(t1[:], pb[1][:], Erc, ALU.mult), ["pb1", "Er"], ["t1"])
                V(lambda e: e.tensor_tensor(t2[:], pb[2][:], Eic, ALU.mult), ["pb2", "Ei"], ["t2"])
                V(lambda e: e.tensor_tensor(btr[:], t1[:], t2[:], ALU.add), ["t1", "t2"], ["btr"])
                V(lambda e: e.tensor_tensor(t1[:], pb[2][:], Erc, ALU.mult), ["pb2", "Er", "btr"], ["t1"])
                V(lambda e: e.tensor_tensor(t2[:], pb[1][:], Eic, ALU.mult), ["pb1", "Ei", "btr"], ["t2"])
                V(lambda e: e.tensor_tensor(bti[:], t1[:], t2[:], ALU.subtract), ["t1", "t2"], ["bti"])
                b3r = btr[:].rearrange("p (a b) -> p a b", a=4); b3i = bti[:].rearrange("p (a b) -> p a b", a=4)
                V(lambda e, isl=isl: e.tensor_tensor(b3r[:, :, 0], b3r[:, :, 0], injr[:, isl], ALU.add), ["btr", "inj"], ["btr"])
                V(lambda e, isl=isl: e.tensor_tensor(b3i[:, :, 0], b3i[:, :, 0], inji[:, isl], ALU.add), ["bti", "inj"], ["bti"])
                V(lambda e: e.tensor_tensor_scan(str_[:], Rc, btr[:], 0.0, ALU.mult, ALU.add), ["Rho0", "btr"], ["str"])
                V(lambda e: e.tensor_tensor_scan(sti[:], Rc, bti[:], 0.0, ALU.mult, ALU.add), ["Rho0", "bti"], ["sti"])
                s3r = str_[:].rearrange("p (a b) -> p a b", a=4); s3i = sti[:].rearrange("p (a b) -> p a b", a=4)
                V(lambda e, isl=isl: e.tensor_copy(stlr[:, isl], s3r[:, :, 127]), ["str"], ["stl"])
                V(lambda e, isl=isl: e.tensor_copy(stli[:, isl], s3i[:, :, 127]), ["sti"], ["stl"])
                G(lambda e: e.tensor_tensor(t3[:], str_[:], Erc, ALU.mult), ["str", "Er"], ["t3"])
                G(lambda e: e.tensor_tensor(t4[:], sti[:], Eic, ALU.mult), ["sti", "Ei"], ["t4"])
                G(lambda e: e.tensor_tensor(sre[:], t3[:], t4[:], ALU.subtract), ["t3", "t4"], ["sre"])
                G(lambda e: e.tensor_tensor(t3[:], str_[:], Eic, ALU.mult), ["str", "Ei", "sre"], ["t3"])
                G(lambda e: e.tensor_tensor(t4[:], sti[:], Erc, ALU.mult), ["sti", "Er", "sre"], ["t4"])
                G(lambda e: e.tensor_tensor(sim[:], t3[:], t4[:], ALU.add), ["t3", "t4"], ["sim"])
                for ii in range(4):
                    i = 4 * c + ii
                    M(lambda e, i=i, ii=ii, c=c: e.matmul(pb[3][:, c * 128:(c + 1) * 128], lhsT=Cbr[:, i, :], rhs=sre[:, ii * 128:(ii + 1) * 128], start=(ii == 0), stop=False), ["Cb", "sre"], ["pb3"])
                    M(lambda e, i=i, ii=ii, c=c: e.matmul(pb[3][:, c * 128:(c + 1) * 128], lhsT=Cbi[:, i, :], rhs=sim[:, ii * 128:(ii + 1) * 128], start=False, stop=(ii == 3)), ["Cb", "sim"], ["pb3"])
            cmul(Sr[:], Si[:], stlr[:], stli[:], E127r[:], E127i[:], c1[:], c2[:], ["stl", "E127", "c", "inj"], ["S", "c"])
            for c in range(4):
                V(lambda e, c=c: e.scalar_tensor_tensor(ysb[:, c, :], uTb[:, c, :], small["ssm_d"][:, c:c + 1], pb[3][:, c * 128:(c + 1) * 128], ALU.mult, ALU.add), ["uTb", "c_ssm_d", "pb3"], ["ysb"])
            A(lambda e: e.activation(out=zb[:], in_=ysb[:], func=AF.Gelu), ["ysb"], ["zb"])
            for cp in range(4):
                for c in range(4):
                    M(lambda e, c=c, cp=cp: e.matmul(pb[4][:, cp * 128:(cp + 1) * 128], lhsT=glu[:, c, cp * 128:(cp + 1) * 128], rhs=zb[:, c, :], start=(c == 0), stop=(c == 3)), ["glu", "zb"], ["pb4"])
            for cp in range(4):
                A(lambda e, cp=cp: e.activation(out=sg[:, cp, :], in_=pb[4][:, cp * 128:(cp + 1) * 128], func=AF.Sigmoid, bias=small["glu_b"][:, cp:cp + 1]), ["pb4", "c_glu_b"], ["sg"])
            V(lambda e: e.tensor_tensor(zz[:], zb[:], sg[:], ALU.mult), ["zb", "sg"], ["zz"])
            V(lambda e: e.tensor_tensor(zsq[:], zz[:], zz[:], ALU.mult), ["zz"], ["zsq"])
            for c in range(4):
                M(lambda e, c=c: e.matmul(psA[:, 769:770], lhsT=zsq[:, c, :], rhs=ones[:, 0:1], start=(c == 0), stop=(c == 3)), ["zsq", "ones"], ["psss"])

        def out_proj(sl, t):
            for half in range(2):
                hs = slice(half * 512, (half + 1) * 512)
                for h in range(8):
                    M(lambda e, h=h, half=half, hs=hs: e.matmul(pb[half][:], lhsT=oT[:, h, :], rhs=woa[:, h, hs], start=(h == 0), stop=(h == 7)), ["oT", "woa"], [f"pb{half}"])
                for c in range(4):
                    M(lambda e, c=c, half=half, hs=hs: e.matmul(pb[2 + half][:], lhsT=zz[:, c, :], rhs=wos[:, c, hs], start=(c == 0), stop=(c == 3)), ["zz", "wos"], [f"pb{2 + half}"])
            V(lambda e: e.tensor_scalar(rs[:, 0:2], psA[:, 768:770], 1.0 / 512, EPS, ALU.mult, ALU.add), ["pssa", "psss"], ["rs"])
            A(lambda e: e.activation(out=rs[:, 2:4], in_=rs[:, 0:2], func=AF.Sqrt), ["rs"], ["rsb"])
            V(lambda e: e.reciprocal(rs[:, 0:2], rs[:, 2:4]), ["rsb", "rs"], ["rs"])
            kx = f"xt{sl}"
            for half in range(2):
                hs = slice(half * 512, (half + 1) * 512)
                V(lambda e, half=half, hs=hs: e.scalar_tensor_tensor(hm[:, hs], pb[half][:], rs[:, 0:1], xt[sl][:, hs], ALU.mult, ALU.add), [f"pb{half}", "rs", kx], ["hm"])
                V(lambda e, half=half, hs=hs: e.scalar_tensor_tensor(hm[:, hs], pb[2 + half][:], rs[:, 1:2], hm[:, hs], ALU.mult, ALU.add), [f"pb{2 + half}", "rs", "hm"], ["hm"])
            S.dma(lambda e: e.dma_start(out=out[t * 128:(t + 1) * 128, :], in_=hm[:]), reads=["hm"], writes=[f"out{t}"])
            A(lambda e: e.activation(out=junk[:], in_=hm[:], func=AF.Square, accum_out=st2[:, 0:1]), ["hm"], ["junk", "st2"])
            V(lambda e: e.tensor_scalar(st2[:, 1:2], st2[:, 0:1], 1.0 / 1024, EPS, ALU.mult, ALU.add), ["st2"], ["st2b"])
            A(lambda e: e.activation(out=st2[:, 3:4], in_=st2[:, 1:2], func=AF.Sqrt), ["st2b"], ["st2d"])
            V(lambda e: e.reciprocal(st2[:, 2:3], st2[:, 3:4]), ["st2d"], ["st2c"])
            V(lambda e: e.tensor_scalar(h2b[:], hm[:], st2[:, 2:3], None, ALU.mult), ["hm", "st2c"], ["h2b"])
            for k in range(8):
                M(lambda e, k=k: e.transpose(ptr[:, k * 128:(k + 1) * 128], h2b[:, k * 128:(k + 1) * 128], idb[:]), ["h2b", "idb"], ["ptr"])
            V(lambda e: e.tensor_tensor(h2T[:, :, t * 128:(t + 1) * 128], ptr[:].rearrange("p (a b) -> p a b", a=8), bcg(small["g2"][:], 8, 128), ALU.mult), ["ptr", "c_g2"], ["h2T"])

        if "nomain" in off:
            S.dma(lambda e: e.dma_start(out=xt[0][:], in_=xown[0:128, :]), writes=["xt0"])
            S.dma(lambda e: e.dma_start(out=out[0:128, :], in_=xt[0][:]), reads=["xt0"], writes=["out0"])
            S.emit()
            return nc
        front(xmeta)
        kv_part((kTm, "kTm"), (vm, "vm"), False)
        if "stopmeta" in off:
            S.dma(lambda e: e.dma_start(out=out[0:128, :], in_=xt[0][:]), reads=["xt0", "kTm", "vm"], writes=["out0"])
            S.emit()
            return nc
        front(xhalo)
        kv_part((kTs[2], "kT2"), (vss[2], "vs2"), False)
        if "stophalo" in off:
            S.dma(lambda e: e.dma_start(out=out[0:128, :], in_=xt[1][:]), reads=["xt1", "kT2", "vs2"], writes=["out0"])
            S.emit()
            return nc
        n_own = 1 if (stage == "mix1" or "one" in off) else NT
        for t in range(n_own):
            sl = front(xown[t * 128:(t + 1) * 128, :])
            pslot, cslot = (t + 2) % 3, t % 3
            try:
                kv_part((kTs[cslot], f"kT{cslot}"), (vss[cslot], f"vs{cslot}"), True)
            except StopBuild:
                S.dma(lambda e: e.dma_start(out=out[0:128, :], in_=xt[sl][:]), reads=[f"xt{sl}", "qkv_q", "qkv_k", "qkn", "ptr"], writes=["out0"])
                S.emit()
                return nc
            if "attn" not in off:
                attention(((kTs[pslot], f"kT{pslot}"), (vss[pslot], f"vs{pslot}")), ((kTs[cslot], f"kT{cslot}"), (vss[cslot], f"vs{cslot}")),
                          (mhalo, "mhalo") if t == 0 else (mprev, "mprev"))
            if "dumpqk" in off:
                V(lambda e: e.tensor_copy(hm[:, 0:512], qT[:].rearrange("p a b -> p (a b)")), ["qT"], ["hm"])
                V(lambda e: e.tensor_copy(hm[:, 512:640], kTs[cslot][:]), [f"kT{cslot}"], ["hm"])
                V(lambda e: e.tensor_copy(hm[:, 640:768], vss[cslot][:]), [f"vs{cslot}"], ["hm"])
                V(lambda e: e.tensor_copy(hm[:, 768:896], kTm[:]), ["kTm"], ["hm"])
                V(lambda e: e.tensor_copy(hm[:, 896:1024], vm[:]), ["vm"], ["hm"])
                S.dma(lambda e: e.dma_start(out=out[0:128, :], in_=hm[:]), reads=["hm"], writes=["out0"])
                S.emit()
                return nc
            if "dumpv" in off:
                V(lambda e: e.tensor_copy(hm[0:64, 0:512], pb[3][0:64, :]), ["pb3"], ["hm"])
                V(lambda e: e.tensor_copy(hm[0:64, 512:1024], pb[4][0:64, :]), ["pb4", "hm"], ["hm"])
                S.dma(lambda e: e.dma_start(out=out[0:64, :], in_=hm[0:64, :]), reads=["hm"], writes=["out0"])
                S.emit()
                return nc
            if "dumpp" in off:
                V(lambda e: e.tensor_copy(hm[:, 0:512], PT[1][:].rearrange("p a b -> p (a b)")), ["PT1"], ["hm"])
                V(lambda e: e.tensor_copy(hm[0:64, 512:1024], den_sb[:].rearrange("p a b -> p (a b)")), ["den"], ["hm"])
                S.dma(lambda e: e.dma_start(out=out[0:128, :], in_=hm[:]), reads=["hm"], writes=["out0"])
                S.emit()
                return nc
            if "dumpo" in off:
                V(lambda e: e.tensor_copy(hm[0:64, :], oT[:].rearrange("p a b -> p (a b)")), ["oT"], ["hm"])
                S.dma(lambda e: e.dma_start(out=out[0:64, :], in_=hm[0:64, :]), reads=["hm"], writes=["out0"])
                S.emit()
                return nc
            if "ssm" not in off:
                ssm_tile()
            if "outp" not in off:
                out_proj(sl, t)
            else:
                S.dma(lambda e, t=t, sl=sl: e.dma_start(out=out[t * 128:(t + 1) * 128, :], in_=xt[sl][:]), reads=[f"xt{sl}"], writes=[f"out{t}"])

        if "dumph2" in off:
            V(lambda e: e.tensor_copy(hm[:], h2T[:, :, 0:128]), ["h2T", "hm"], ["hm"])
            S.dma(lambda e: e.dma_start(out=out[128:256, :], in_=hm[:]), reads=["hm"], writes=["out1"])
        if stage in ("mix", "mix1"):
            S.emit()
            return nc

        S.barrier()
        mix.close()
        pe_ = top.enter_context(contextlib.ExitStack())
        pA = PS(pe_, "pA", [128, 2048]); pQ = PS(pe_, "pQ", [128, 512])
        wq = SB(pe_, "wq", [128, 8, 2048], BF16); keysT = SB(pe_, "keysT", [128, 16, 128], BF16)
        with scope() as stg:
            wst = SB(stg, "wst", [128, 2, 2048])
            for kk4 in range(4):
                load(wst[:], D["wq"][:, 2 * kk4:2 * kk4 + 2, :], "wst")
                V(lambda e, kk4=kk4: e.tensor_copy(wq[:, 2 * kk4:2 * kk4 + 2, :], wst[:]), ["wst"], ["wq"])
            kst = SB(stg, "kst", [128, 16, 128])
            load(kst[:], D["keysT"], "kst")
            V(lambda e: e.tensor_copy(keysT[:], kst[:]), ["kst"], ["keysT"])
        qTp = SB(pe_, "qTp", [128, 16, 128], BF16); scs = SB(pe_, "scs", [128, 16, 128]); sc2 = SB(pe_, "sc2", [128, 16, 128])
        v16 = SB(pe_, "v16", [128, 16, 16]); cand = SB(pe_, "cand", [128, 8, 256]); cand2 = SB(pe_, "cand2", [128, 8, 256]); s16 = SB(pe_, "s16", [128, 8, 16])
        dbgp = SB(pe_, "dbgp", [128, 1024])

        def peer_front(t):
            ts_ = slice(t * 128, (t + 1) * 128)
            for g4 in range(4):
                for j in range(4):
                    cb = 4 * g4 + j
                    for k in range(8):
                        M(lambda e, cb=cb, j=j, k=k: e.matmul(pQ[:, j * 128:(j + 1) * 128], lhsT=wq[:, k, cb * 128:(cb + 1) * 128], rhs=h2T[:, k, ts_], start=(k == 0), stop=(k == 7)), ["wq", "h2T"], ["pQ"])
                A(lambda e, g4=g4: e.copy(qTp[:, 4 * g4:4 * g4 + 4, :].rearrange("p a b -> p (a b)"), pQ[:]), ["pQ"], ["qTp"])
            for idx in range(16):
                M(lambda e, idx=idx: e.matmul(pA[:, idx * 128:(idx + 1) * 128], lhsT=qTp[:, idx, :], rhs=keysT[:, idx, :], start=True, stop=True), ["qTp", "keysT"], ["pA"])
            for q4 in range(4):
                A(lambda e, q4=q4: e.copy(scs[:, 4 * q4:4 * q4 + 4, :].rearrange("p a b -> p (a b)"), pA[:, q4 * 512:(q4 + 1) * 512]), ["pA"], ["scs"])
            for idx in range(16):
                V(lambda e, idx=idx: e.max(out=v16[:, idx, 0:8], in_=scs[:, idx, :]), ["scs"], ["v16"])
                V(lambda e, idx=idx: e.match_replace(out=sc2[:, idx, :], in_to_replace=v16[:, idx, 0:8], in_values=scs[:, idx, :], imm_value=-1e30), ["scs", "v16"], ["sc2"])
                V(lambda e, idx=idx: e.max(out=v16[:, idx, 8:16], in_=sc2[:, idx, :]), ["sc2"], ["v16"])
            v4 = v16[:].rearrange("p (h two) a -> p h two a", two=2)
            V(lambda e: e.tensor_tensor(cand[:].rearrange("p h (a b) -> p h a b", a=16),
                                        v4[:, :, 0, :].rearrange("p h (a o) -> p h a o", o=1).to_broadcast([128, 8, 16, 16]),
                                        v4[:, :, 1, :].rearrange("p h (o b) -> p h o b", o=1).to_broadcast([128, 8, 16, 16]), ALU.add), ["v16"], ["cand"])
            for h in range(8):
                V(lambda e, h=h: e.max(out=s16[:, h, 0:8], in_=cand[:, h, :]), ["cand"], ["s16"])
                V(lambda e, h=h: e.match_replace(out=cand2[:, h, :], in_to_replace=s16[:, h, 0:8], in_values=cand[:, h, :], imm_value=-1e30), ["cand", "s16"], ["cand2"])
                V(lambda e, h=h: e.max(out=s16[:, h, 8:16], in_=cand2[:, h, :]), ["cand2"], ["s16"])

        if stage == "peerfront":
            peer_front(0)
            V(lambda e: e.memset(dbgp[:], 0.0), [], ["dbgp"])
            V(lambda e: e.tensor_copy(dbgp[:, 0:256], v16[:].rearrange("p a b -> p (a b)")), ["v16"], ["dbgp"])
            V(lambda e: e.tensor_copy(dbgp[:, 256:384], s16[:].rearrange("p a b -> p (a b)")), ["s16"], ["dbgp"])
            V(lambda e: e.tensor_copy(dbgp[:, 512:1024], scs[:, 0:4, :].rearrange("p a b -> p (a b)")), ["scs"], ["dbgp"])
            S.dma(lambda e: e.dma_start(out=out[256:384, :], in_=dbgp[:]), reads=["dbgp"], writes=["out2"])
            S.emit()
            return nc

        n_i = 128

        iota_i = SB(pe_, "iota_i", [128, 128]); bm = SB(pe_, "bm", [128, 8])
        load(iota_i[:], D["iota_i"], "iota_i"); load(bm[:], D["blkmask"], "bm")
        pG2 = PS(pe_, "pG2", [128, 512]); pZs = [PS(pe_, f"pZ{i}", [128, 512]) for i in range(2)]
        ixu = SB(pe_, "ixu", [128, 16, 16], U32); ixf = SB(pe_, "ixf", [128, 16, 16])
        i1c = SB(pe_, "i1c", [128, 128]); i2c = SB(pe_, "i2c", [128, 128]); i1T = SB(pe_, "i1T", [128, 128]); i2T = SB(pe_, "i2T", [128, 128])
        zs = SB(pe_, "zs", [128, 8]); wT = SB(pe_, "wT", [128, 128, 16], BF16)
        NB = 16
        P1b = [SB(pe_, f"P1b{i}", [128, NB, 128], BF16) for i in range(2)]; P2b = [SB(pe_, f"P2b{i}", [128, NB, 128], BF16) for i in range(2)]
        Wb = [SB(pe_, f"Wb{i}", [128, NB, 128], BF16) for i in range(2)]
        Tsb = [SB(pe_, f"Tsb{i}", [128, 4, 128], BF16) for i in range(2)]; Gt = SB(pe_, "Gt", [128, 128, 128], BF16)
        ND = 4
        ubuf = [SB(pe_, f"ubuf{i}", [128, 1024], BF16) for i in range(ND)]; vbuf = [SB(pe_, f"vbuf{i}", [128, 1024], BF16) for i in range(ND)]
        zg = [SB(pe_, f"zg{i}", [128, 128], BF16) for i in range(2)]; AT = [SB(pe_, f"AT{i}", [128, 128], BF16) for i in range(2)]
        hmt = SB(pe_, "hmt", [128, 1024]); res = SB(pe_, "res", [128, 1024])

        def b3(ap2, a, n):
            return ap2.rearrange("p (a o) -> p a o", o=1).to_broadcast([128, a, n])

        def gate_build(t):
            for idx in range(16):
                V(lambda e, idx=idx: e.max_index(out=ixu[:, idx, 0:8], in_max=v16[:, idx, 0:8], in_values=scs[:, idx, :]), ["v16", "scs"], ["ixu"])
                V(lambda e, idx=idx: e.max_index(out=ixu[:, idx, 8:16], in_max=v16[:, idx, 8:16], in_values=sc2[:, idx, :]), ["v16", "sc2"], ["ixu"])
            V(lambda e: e.tensor_copy(ixf[:], ixu[:]), ["ixu"], ["ixf"])
            ix4 = ixf[:].rearrange("p (h two) a -> p h two a", two=2)
            V(lambda e: e.tensor_copy(i1c[:].rearrange("p (h a) -> p h a", h=8), ix4[:, :, 0, :]), ["ixf"], ["i1c"])
            V(lambda e: e.tensor_copy(i2c[:].rearrange("p (h a) -> p h a", h=8), ix4[:, :, 1, :]), ["ixf"], ["i2c"])
            wx = scs[:].rearrange("p a b -> p (a b)").rearrange("p (h c) -> p h c", h=8)
            mk = sc2[:].rearrange("p a b -> p (a b)").rearrange("p (h c) -> p h c", h=8)
            V(lambda e: e.tensor_tensor(wx, cand[:], b3(s16[:, :, 0], 8, 256), ALU.subtract), ["cand", "s16", "ixu"], ["scs"])
            A(lambda e: e.activation(out=wx, in_=wx, func=AF.Exp), ["scs"], ["scs"])
            V(lambda e: e.tensor_tensor(mk, cand[:], b3(s16[:, :, 15], 8, 256), ALU.is_ge), ["cand", "s16", "ixu"], ["sc2"])
            V(lambda e: e.tensor_tensor(wx, wx, mk, ALU.mult), ["scs", "sc2"], ["scs"])
            V(lambda e: e.tensor_reduce(zs[:], wx, AX.X, ALU.add), ["scs"], ["zs"])
            V(lambda e: e.reciprocal(zs[:], zs[:]), ["zs"], ["zs"])
            V(lambda e: e.tensor_tensor(wx, wx, b3(zs[:], 8, 256), ALU.mult), ["scs", "zs"], ["scs"])
            wperm = sc2[:].rearrange("p a b -> p (a b)").rearrange("p (b h a) -> p b h a", b=16, h=8)
            V(lambda e: e.tensor_copy(wperm, scs[:].rearrange("p a b -> p (a b)").rearrange("p (h a b) -> p b h a", h=8, a=16)), ["scs", "sc2"], ["sc2"])
            w2d = sc2[:].rearrange("p a b -> p (a b)")
            for b in range(16):
                M(lambda e, b=b: e.transpose(pA[:, b * 128:(b + 1) * 128], w2d[:, b * 128:(b + 1) * 128], idf[:]), ["sc2", "idf"], ["pA"])
            V(lambda e: e.tensor_copy(wT[:].rearrange("p t b -> p b t"), pA[:].rearrange("p (b t) -> p b t", b=16)), ["pA"], ["wT"])
            M(lambda e: e.transpose(pQ[:, 0:128], i1c[:], idf[:]), ["i1c", "idf"], ["pQ"])
            M(lambda e: e.transpose(pQ[:, 128:256], i2c[:], idf[:]), ["i2c", "idf"], ["pQ"])
            V(lambda e: e.tensor_copy(i1T[:], pQ[:, 0:128]), ["pQ"], ["i1T"])
            V(lambda e: e.tensor_copy(i2T[:], pQ[:, 128:256]), ["pQ"], ["i2T"])
            io3 = iota_i[:].rearrange("p (o i) -> p o i", o=1).to_broadcast([128, NB, 128])
            GPB = NB // 4
            NG = 128 // 4

            def build(nb):
                p = nb % 2; tsl = slice(nb * NB, (nb + 1) * NB)
                V(lambda e: e.tensor_tensor(P1b[p][:], io3, b3(i1T[:, tsl], NB, 128), ALU.is_equal), ["iota_i", "i1T"], [f"P1b{p}"])
                V(lambda e: e.tensor_tensor(P2b[p][:], io3, b3(i2T[:, tsl], NB, 128), ALU.is_equal), ["iota_i", "i2T"], [f"P2b{p}"])
                V(lambda e: e.tensor_tensor(Wb[p][:].rearrange("p t (h b) -> p t h b", h=8),
                                            wT[:, tsl, :].rearrange("p t (o b) -> p t o b", o=1).to_broadcast([128, NB, 8, 16]),
                                            bm[:].rearrange("p (o h q) -> p o h q", o=1, q=1).to_broadcast([128, NB, 8, 16]), ALU.mult), ["wT", "bm"], [f"Wb{p}"])

            def s1(g):
                p = (g // GPB) % 2; q = g % 2
                for j in range(4):
                    tk = (g % GPB) * 4 + j
                    M(lambda e: e.matmul(pQ[:, j * 128:(j + 1) * 128], lhsT=Wb[p][:, tk, :], rhs=P1b[p][:, tk, :], start=True, stop=True), [f"Wb{p}", f"P1b{p}"], ["pQ"])
                A(lambda e: e.copy(Tsb[q][:].rearrange("p a b -> p (a b)"), pQ[:]), ["pQ"], [f"Tsb{q}"])

            def s2(g):
                p = (g // GPB) % 2; q = g % 2
                for j in range(4):
                    tk = (g % GPB) * 4 + j
                    M(lambda e: e.matmul(pG2[:, j * 128:(j + 1) * 128], lhsT=P2b[p][:, tk, :], rhs=Tsb[q][:, j, :], start=True, stop=True), [f"P2b{p}", f"Tsb{q}"], ["pG2"])
                t0 = g * 4
                A(lambda e: e.copy(Gt[:, t0:t0 + 4, :].rearrange("p a b -> p (a b)"), pG2[:]), ["pG2"], ["Gt"])

            build(0)
            s1(0)
            for g in range(NG):
                if g % GPB == 0 and g // GPB + 1 < 128 // NB:
                    build(g // GPB + 1)
                if g + 1 < NG:
                    s1(g + 1)
                s2(g)

        def dense_block(t):
            ts_ = slice(t * 128, (t + 1) * 128)
            def zstage(i):
                sl = i % 2; ds_ = i % ND
                S.dma(lambda e, i=i, ds_=ds_: e.dma_start(out=ubuf[ds_][:], in_=UTs[i]), reads=[f"UTs{i}"], writes=[f"ubuf{ds_}"])
                S.dma(lambda e, i=i, ds_=ds_: e.dma_start(out=vbuf[ds_][:], in_=Vs[i]), reads=[f"Vs{i}"], writes=[f"vbuf{ds_}"], eng="pool")
                for k in range(8):
                    M(lambda e, k=k, sl=sl, ds_=ds_: e.matmul(pZs[sl][:, 0:128], lhsT=ubuf[ds_][:, k * 128:(k + 1) * 128], rhs=h2T[:, k, ts_], start=(k == 0), stop=(k == 7)), [f"ubuf{ds_}", "h2T"], [f"pZ{sl}"])
                A(lambda e, sl=sl: e.activation(out=zg[sl][:], in_=pZs[sl][:, 0:128], func=AF.Gelu), [f"pZ{sl}"], [f"zg{sl}"])
                V(lambda e, sl=sl, i=i: e.tensor_tensor(AT[sl][:], zg[sl][:], Gt[:, :, i], ALU.mult), [f"zg{sl}", "Gt"], [f"AT{sl}"])

            def ostage(i):
                sl = i % 2; ds_ = i % ND
                for half in range(2):
                    M(lambda e, half=half, sl=sl, ds_=ds_, i=i: e.matmul(pA[:, half * 512:(half + 1) * 512], lhsT=AT[sl][:], rhs=vbuf[ds_][:, half * 512:(half + 1) * 512], start=(i == 0), stop=(i == n_i - 1)), [f"AT{sl}", f"vbuf{ds_}"], ["pA"])

            zstage(0)
            for i in range(n_i):
                if i + 1 < n_i:
                    zstage(i + 1)
                ostage(i)
            S.dma(lambda e: e.dma_start(out=hmt[:], in_=out[t * 128:(t + 1) * 128, :]), reads=[f"out{t}"], writes=["hmt"])
            for half in range(2):
                hs = slice(half * 512, (half + 1) * 512)
                V(lambda e, hs=hs: e.tensor_tensor(res[:, hs], hmt[:, hs], pA[:, hs], ALU.add), ["hmt", "pA"], ["res"])
            S.dma(lambda e: e.dma_start(out=out[t * 128:(t + 1) * 128, :], in_=res[:]), reads=["res"], writes=[f"out{t}"])

        if "keephm" in off:
            S.dma(lambda e: e.dma_start(out=hmt[:], in_=out[0:128, :]), reads=["out0"], writes=["hmt"])
            S.dma(lambda e: e.dma_start(out=out[128:256, :], in_=hmt[:]), reads=["hmt"], writes=["out1"])
        for t in range(n_own):
            peer_front(t)
            gate_build(t)
            dense_block(t)
        S.emit()
        return nc


def _host_inputs(inp):
    f = np.float32
    x = np.ascontiguousarray(inp["x"][0]); meta = inp["meta_tokens"]
    common = {}
    common["ident"] = np.eye(128, dtype=f)
    kq = np.arange(128)
    common["mask_cur"] = (kq[:, None] <= kq[None, :]).astype(f)
    common["xmeta"] = np.concatenate([meta, np.zeros((112, 1024), f)], 0)
    w_in = inp["w_in"][0]
    qperm = np.concatenate([np.arange(64) + 64 * h for h in (0, 4, 1, 5, 2, 6, 3, 7)])
    w_in = np.concatenate([w_in[:, :512][:, qperm], w_in[:, 512:]], 1)
    common["w_in"] = np.ascontiguousarray(w_in.reshape(8, 128, 1280).transpose(1, 0, 2))
    common["mask_prev"] = (kq[:, None] > kq[None, :]).astype(f)
    common["g1"] = np.ascontiguousarray(inp["norm1_g"][0].reshape(8, 128).T)
    common["gq2"] = np.tile(inp["q_norm_g"][0], 2).reshape(128, 1).astype(f)
    common["gk2"] = np.tile(inp["k_norm_g"][0], 2).reshape(128, 1).astype(f)
    common["sinks"] = np.ascontiguousarray(np.broadcast_to(inp["attn_sinks"][0][None, :], (64, 8))).astype(f)

    def state_layout(a):
        return np.ascontiguousarray(a.reshape(16, 2, 64).transpose(1, 2, 0).reshape(128, 16))

    def row_layout(a):
        return np.ascontiguousarray(np.broadcast_to(a.reshape(1, 2048), (128, 2048)))
    ldt64 = np.ascontiguousarray(np.broadcast_to(inp["ssm_log_dt"][0][:, None], (32, 64)))
    for nm, a in (("a_re", inp["ssm_a_re"][0]), ("a_im", inp["ssm_a_im"][0]), ("ldt", ldt64)):
        common[nm + "_s"] = state_layout(a); common[nm + "_r"] = row_layout(a)
    for nm, B in (("re", inp["ssm_b_re"][0]), ("im", inp["ssm_b_im"][0])):
        bT = np.zeros((128, 16, 128), f); bs = np.zeros((128, 16, 32), f)
        for g in range(32):
            i, hh = g // 2, g % 2
            bT[(g % 8) * 16:(g % 8) * 16 + 16, i, hh * 64:hh * 64 + 64] = B[g].T
            bs[hh * 64:hh * 64 + 64, i, hh * 16:hh * 16 + 16] = B[g]
        common["bT_" + nm] = bT; common["bs_" + nm] = bs
    for nm, C in (("re", inp["ssm_c_re"][0]), ("im", inp["ssm_c_im"][0])):
        cb = np.zeros((128, 16, 128), f)
        for g in range(32):
            i, hh = g // 2, g % 2
            cb[hh * 64:hh * 64 + 64, i, (g % 8) * 16:(g % 8) * 16 + 16] = C[g].T
        common["cb_" + nm] = cb
    common["ssm_d"] = np.ascontiguousarray(inp["ssm_d"][0].reshape(4, 128).T)
    common["glu_w"] = np.ascontiguousarray(inp["ssm_glu_w"][0].reshape(4, 128, 512).transpose(1, 0, 2))
    common["glu_b"] = np.ascontiguousarray(inp["ssm_glu_b"][0].reshape(4, 128).T)
    common["ga"] = np.ascontiguousarray(inp["attn_out_g"][0].reshape(8, 64).T)
    common["gs"] = np.ascontiguousarray(inp["ssm_out_g"][0].reshape(4, 128).T)
    common["woa"] = np.ascontiguousarray(inp["w_out"][0][:512].reshape(8, 64, 1024).transpose(1, 0, 2))
    common["wos"] = np.ascontiguousarray(inp["w_out"][0][512:].reshape(4, 128, 1024).transpose(1, 0, 2))
    common["g2"] = np.ascontiguousarray(inp["norm2_g"][0].reshape(8, 128).T)
    if inp.get("peer_w_query") is not None:
        common["wq"] = np.ascontiguousarray(inp["peer_w_query"][0].reshape(8, 128, 2048).transpose(1, 0, 2))
        sk = inp["peer_sub_keys"][0]
        common["keysT"] = np.ascontiguousarray(sk.reshape(16, 128, 128).transpose(2, 0, 1))
    if inp.get("peer_u") is not None:
        common["puT"] = np.ascontiguousarray(inp["peer_u"][0].reshape(128, 128, 8, 128).transpose(0, 3, 2, 1)).reshape(128, 128, 1024)
        common["pv"] = np.ascontiguousarray(inp["peer_v"][0].reshape(128, 128, 1024))
        common["iota_i"] = np.ascontiguousarray(np.broadcast_to(np.arange(128, dtype=f)[None, :], (128, 128)))
        common["blkmask"] = (np.arange(128)[:, None] // 16 == np.arange(8)[None, :]).astype(f)
    maps = []
    for r in range(NCORES):
        m = dict(common)
        m["xown"] = x[r * TOK:(r + 1) * TOK]
        m["xhalo"] = x[r * TOK - 128:r * TOK] if r > 0 else np.zeros((128, 1024), f)
        m["mask_halo"] = common["mask_prev"] if r > 0 else np.zeros((128, 128), f)
        pre = np.zeros((NPRE * 128, 1024), f)
        n = r * TOK
        pre[NPRE * 128 - n - 16:NPRE * 128 - n] = meta
        if n:
            pre[NPRE * 128 - n:] = x[:n]
        m["xpre"] = pre
        m["is0"] = np.full((128, 1), 1.0 if r == 0 else 0.0, f)
        maps.append(m)
    return maps


def kernel(**inputs):
    nc = build("full")
    maps = _host_inputs({k: np.asarray(v) for k, v in inputs.items()})
    res = run_bass_kernel_spmd(nc, maps, core_ids=list(range(NCORES)))
    return np.concatenate([r["out"] for r in res.results], 0)[None].astype(np.float32)
```

```python
import contextlib
import types
import numpy as np
import concourse.bass as bass
import concourse.mybir as mybir
from concourse.bass_utils import run_bass_kernel_spmd

F32 = mybir.dt.float32
BF16 = mybir.dt.bfloat16
U32 = mybir.dt.uint32
AF = mybir.ActivationFunctionType
ALU = mybir.AluOpType
AX = mybir.AxisListType

NCORES = 8
TOK = 2048
NT = 16
NPRE = 113
EPS = 1e-6
ENGS = ("pe", "act", "dve", "pool", "sp")


class StopBuild(Exception):
    pass


def _freeze(fn):
    if fn.__closure__ is None:
        return fn
    cells = []
    for c in fn.__closure__:
        try:
            cells.append(types.CellType(c.cell_contents))
        except ValueError:
            cells.append(c)
    return types.FunctionType(fn.__code__, fn.__globals__, fn.__name__, fn.__defaults__, tuple(cells))


class Sched:
    def __init__(self, nc, n_dma_streams=8):
        self.nc = nc
        self.ops = {e: [] for e in ENGS}
        self.count = {}
        self.last_w = {}
        self.readers = {}
        self.seen = {e: {} for e in ENGS}
        self.n_dma = n_dma_streams
        self.dma_rr = {e: 0 for e in ENGS}
        self.pending = {e: {} for e in ENGS}
        self.pe_run = []

    def barrier(self):
        self._flush_pe()
        for e in ENGS:
            for k, v in self.count.items():
                if self.pending[e].get(k, 0) < v:
                    self.pending[e][k] = v

    def _deps(self, eng, reads, writes):
        need = dict(self.pending[eng])
        self.pending[eng] = {}

        def add(ev):
            if ev is not None and need.get(ev[0], 0) < ev[1]:
                need[ev[0]] = ev[1]
        for b in reads:
            add(self.last_w.get(b))
        for b in writes:
            add(self.last_w.get(b))
            for ev in self.readers.get(b, ()):
                add(ev)
        waits = []
        for k, v in need.items():
            if eng == "pe" and k == ("e", "pe"):
                continue
            if self.seen[eng].get(k, 0) < v:
                self.seen[eng][k] = v
                waits.append((k, v))
        return waits

    def _record(self, ev, reads, writes):
        for b in reads:
            self.readers.setdefault(b, []).append(ev)
        for b in writes:
            self.last_w[b] = ev
            self.readers[b] = []

    def _flush_pe(self):
        if self.pe_run:
            k = ("e", "pe")
            self.count[k] = self.count.get(k, 0) + 1
            self.pe_run[-1][2] = (k, 1)
            self.pe_run = []

    def op(self, eng, fn, reads=(), writes=()):
        fn = _freeze(fn)
        if eng != "pe":
            self._flush_pe()
        waits = self._deps(eng, reads, writes)
        k = ("e", eng)
        if eng == "pe":
            v = self.count.get(k, 0) + 1
            entry = [waits, fn, None]
            self.ops[eng].append(entry)
            self.pe_run.append(entry)
            self._record((k, v), reads, writes)
            return
        v = self.count.get(k, 0) + 1
        self.count[k] = v
        self.ops[eng].append([waits, fn, (k, 1)])
        self._record((k, v), reads, writes)

    def dma(self, fn, reads=(), writes=(), eng="sp"):
        fn = _freeze(fn)
        self._flush_pe()
        waits = self._deps(eng, reads, writes)
        k = ("d", eng, self.dma_rr[eng] % self.n_dma)
        self.dma_rr[eng] += 1
        v = self.count.get(k, 0) + 16
        self.count[k] = v
        self.ops[eng].append([waits, fn, (k, 16)])
        self._record((k, v), reads, writes)

    def emit(self):
        self._flush_pe()
        nc = self.nc
        keys = sorted(self.count.keys(), key=str)
        with contextlib.ExitStack() as st:
            sems = {k: st.enter_context(nc.semaphore("s_" + "_".join(map(str, k)))) for k in keys}
            block = st.enter_context(nc.Block())
            final = [(k, self.count[k]) for k in keys]

            def run(engobj, lst, is_final):
                for waits, fn, incr in lst:
                    for wk, wv in waits:
                        engobj.wait_ge(sems[wk], wv)
                    ins_ = fn(engobj)
                    if incr is not None:
                        ins_.then_inc(sems[incr[0]], incr[1])
                if is_final:
                    for wk, wv in final:
                        engobj.wait_ge(sems[wk], wv)
            names = {"pe": "tensor", "act": "scalar", "dve": "vector", "pool": "gpsimd", "sp": "sync"}
            for e in ENGS:
                lst = self.ops[e]
                getattr(block, names[e])(lambda engobj, lst=lst, e=e: run(engobj, lst, e == "sp"))


def build(stage="full", off=()):
    nc = bass.Bass("TRN2", target_bir_lowering=False)
    S = Sched(nc)
    D = {}

    def din(name, shape, dt=F32):
        D[name] = nc.dram_tensor(name, list(shape), dt, kind="ExternalInput").ap()
        return D[name]
    xown = din("xown", [TOK, 1024]); xhalo = din("xhalo", [128, 1024]); xpre = din("xpre", [NPRE * 128, 1024])
    xmeta = din("xmeta", [128, 1024])
    din("ident", [128, 128]); din("mask_cur", [128, 128]); din("mask_prev", [128, 128]); din("mask_halo", [128, 128])
    din("w_in", [128, 8, 1280]); din("g1", [128, 8]); din("gq2", [128, 1]); din("gk2", [128, 1]); din("sinks", [64, 8])
    for nm in ("a_re", "a_im", "ldt"):
        din(nm + "_s", [128, 16]); din(nm + "_r", [128, 2048])
    din("bT_re", [128, 16, 128]); din("bT_im", [128, 16, 128]); din("bs_re", [128, 16, 32]); din("bs_im", [128, 16, 32])
    din("cb_re", [128, 16, 128]); din("cb_im", [128, 16, 128]); din("ssm_d", [128, 4]); din("glu_w", [128, 4, 512])
    din("glu_b", [128, 4]); din("ga", [64, 8]); din("gs", [128, 4]); din("woa", [64, 8, 1024]); din("wos", [128, 4, 1024])
    din("g2", [128, 8]); din("is0", [128, 1])
    if stage in ("peerfront", "full"):
        din("wq", [128, 8, 2048]); din("keysT", [128, 16, 128])
    if stage == "full":
        din("puT", [128, 128, 1024]); din("pv", [128, 128, 1024]); din("iota_i", [128, 128]); din("blkmask", [128, 8])
        UTs = nc.dram_tensor("UTs", [128, 128, 1024], BF16).ap(); Vs = nc.dram_tensor("Vs", [128, 128, 1024], BF16).ap()
    out = nc.dram_tensor("out", [TOK, 1024], F32, kind="ExternalOutput").ap()

    with contextlib.ExitStack() as top:
        def SB(st, name, shape, dt=F32):
            return st.enter_context(nc.sbuf_tensor("sb_" + name, list(shape), dt))

        def PS(st, name, shape, dt=F32):
            return st.enter_context(nc.psum_tensor("ps_" + name, list(shape), dt))

        def V(fn, r, w): S.op("dve", fn, r, w)
        def A(fn, r, w): S.op("act", fn, r, w)
        def G(fn, r, w): S.op("pool", fn, r, w)
        def M(fn, r, w): S.op("pe", fn, r, w)

        @contextlib.contextmanager
        def scope():
            with contextlib.ExitStack() as st_:
                yield st_
                S.barrier()

        def load(dst, src, key):
            S.dma(lambda e: e.dma_start(out=dst, in_=src), writes=[key])

        idf = SB(top, "idf", [128, 128]); idb = SB(top, "idb", [128, 128], BF16)
        load(idf[:], D["ident"], "idf")
        V(lambda e: e.tensor_copy(idb[:], idf[:]), ["idf"], ["idb"])
        ones = SB(top, "ones", [128, 64], BF16)
        V(lambda e: e.memset(ones[:], 1.0), [], ["ones"])
        h2T = SB(top, "h2T", [128, 8, TOK if "smallh2T" not in off else 128], BF16)

        mix = top.enter_context(contextlib.ExitStack())
        ptr = PS(mix, "ptr", [128, 1024], BF16)
        psA = PS(mix, "psA", [128, 1024])
        pb = [PS(mix, f"pb{i}", [128, 512]) for i in range(5)]

        win = SB(mix, "win", [128, 8, 1280], BF16); woa = SB(mix, "woa", [64, 8, 1024], BF16)
        wos = SB(mix, "wos", [128, 4, 1024], BF16); glu = SB(mix, "glu", [128, 4, 512], BF16)
        small = {}
        for nm, shp in (("g1", [128, 8]), ("gq2", [128, 1]), ("gk2", [128, 1]), ("sinks", [64, 8]), ("ssm_d", [128, 4]),
                        ("glu_b", [128, 4]), ("ga", [64, 8]), ("gs", [128, 4]), ("g2", [128, 8]), ("is0", [128, 1])):
            small[nm] = SB(mix, "c_" + nm, shp)
            load(small[nm][:], D[nm], "c_" + nm)
        with scope() as stg:
            s1 = SB(stg, "stg1", [128, 8, 1280])
            load(s1[:], D["w_in"], "stg1")
            for k in range(8):
                V(lambda e, k=k: e.tensor_scalar(win[:, k, :], s1[:, k, :], small["g1"][:, k:k + 1], None, ALU.mult),
                  ["stg1", "c_g1"], ["win"])
        with scope() as stg:
            s2 = SB(stg, "stg2", [64, 8, 1024]); s3 = SB(stg, "stg3", [128, 4, 1024]); s4 = SB(stg, "stg4", [128, 4, 512])
            load(s2[:], D["woa"], "stg2"); load(s3[:], D["wos"], "stg3"); load(s4[:], D["glu_w"], "stg4")
            for h in range(8):
                V(lambda e, h=h: e.tensor_scalar(woa[:, h, :], s2[:, h, :], small["ga"][:, h:h + 1], None, ALU.mult),
                  ["stg2", "c_ga"], ["woa"])
            for c in range(4):
                V(lambda e, c=c: e.tensor_scalar(wos[:, c, :], s3[:, c, :], small["gs"][:, c:c + 1], None, ALU.mult),
                  ["stg3", "c_gs"], ["wos"])
            V(lambda e: e.tensor_copy(glu[:], s4[:]), ["stg4"], ["glu"])
        cqk = SB(mix, "cqk", [128, 1]); esink = SB(mix, "esink", [64, 8])
        V(lambda e: e.tensor_tensor(cqk[:], small["gq2"][:], small["gk2"][:], ALU.mult), ["c_gq2", "c_gk2"], ["cqk"])
        V(lambda e: e.tensor_scalar(cqk[:], cqk[:], 0.125, None, ALU.mult), ["cqk"], ["cqk"])
        A(lambda e: e.activation(out=esink[:], in_=small["sinks"][:], func=AF.Exp), ["c_sinks"], ["esink"])
        mcur = SB(mix, "mcur", [128, 128], BF16); mprev = SB(mix, "mprev", [128, 128], BF16); mhalo = SB(mix, "mhalo", [128, 128], BF16)
        with scope() as stg:
            m1 = SB(stg, "m1", [128, 128]); m2 = SB(stg, "m2", [128, 128]); m3 = SB(stg, "m3", [128, 128])
            load(m1[:], D["mask_cur"], "m1"); load(m2[:], D["mask_prev"], "m2"); load(m3[:], D["mask_halo"], "m3")
            V(lambda e: e.tensor_copy(mcur[:], m1[:]), ["m1"], ["mcur"])
            V(lambda e: e.tensor_copy(mprev[:], m2[:]), ["m2"], ["mprev"])
            V(lambda e: e.tensor_copy(mhalo[:], m3[:]), ["m3"], ["mhalo"])

        TWO_PI = 2.0 * np.pi

        def ssm_scalars(st, tag, are, aim, ldt, shape, t=None):
            if t is None:
                t = {n: SB(st, f"{tag}_{n}", shape) for n in ("dt", "rho", "th", "ph", "cos", "sin", "abr", "abi", "den", "nr", "t1", "t2", "cfr", "cfi")}
                t["ri"] = SB(st, f"{tag}_ri", shape, mybir.dt.int32)
            k = lambda n: f"{tag}_{n}"
            A(lambda e: e.activation(out=t["dt"][:], in_=ldt[:], func=AF.Exp), [k("ldt")], [k("dt")])
            V(lambda e: e.tensor_tensor(t["rho"][:], are[:], t["dt"][:], ALU.mult), [k("are"), k("dt")], [k("rho")])
            A(lambda e: e.activation(out=t["rho"][:], in_=t["rho"][:], func=AF.Exp), [k("rho")], [k("rho")])
            V(lambda e: e.tensor_tensor(t["th"][:], aim[:], t["dt"][:], ALU.mult), [k("aim"), k("dt")], [k("th")])
            for nm, off in (("sin", 0.0), ("cos", np.pi / 2)):
                V(lambda e, off=off: e.tensor_scalar(t["t2"][:], t["th"][:], float(off), None, ALU.add), [k("th")], [k("t2")])
                V(lambda e: e.tensor_scalar(t["t1"][:], t["t2"][:], float(1.0 / TWO_PI), None, ALU.mult), [k("t2")], [k("t1")])
                V(lambda e: e.tensor_copy(t["ri"][:], t["t1"][:]), [k("t1")], [k("ri")])
                V(lambda e: e.tensor_copy(t["t1"][:], t["ri"][:]), [k("ri")], [k("t1")])
                V(lambda e: e.scalar_tensor_tensor(t["ph"][:], t["t1"][:], float(-TWO_PI), t["t2"][:], ALU.mult, ALU.add), [k("t1"), k("t2")], [k("ph")])
                V(lambda e: e.tensor_scalar(t["t1"][:], t["ph"][:], float(np.pi), float(-TWO_PI), ALU.is_gt, ALU.mult), [k("ph")], [k("t1")])
                V(lambda e: e.tensor_tensor(t["ph"][:], t["ph"][:], t["t1"][:], ALU.add), [k("ph"), k("t1")], [k("ph")])
                A(lambda e, nm=nm: e.activation(out=t[nm][:], in_=t["ph"][:], func=AF.Sin), [k("ph")], [k(nm)])
            V(lambda e: e.tensor_tensor(t["abr"][:], t["rho"][:], t["cos"][:], ALU.mult), [k("rho"), k("cos")], [k("abr")])
            V(lambda e: e.tensor_tensor(t["abi"][:], t["rho"][:], t["sin"][:], ALU.mult), [k("rho"), k("sin")], [k("abi")])
            V(lambda e: e.tensor_tensor(t["den"][:], are[:], are[:], ALU.mult), [k("are")], [k("den")])
            V(lambda e: e.tensor_tensor(t["t1"][:], aim[:], aim[:], ALU.mult), [k("aim")], [k("t1")])
            V(lambda e: e.tensor_tensor(t["den"][:], t["den"][:], t["t1"][:], ALU.add), [k("den"), k("t1")], [k("den")])
            V(lambda e: e.reciprocal(t["den"][:], t["den"][:]), [k("den")], [k("den")])
            V(lambda e: e.tensor_scalar(t["nr"][:], t["abr"][:], -1.0, None, ALU.add), [k("abr")], [k("nr")])
            V(lambda e: e.tensor_tensor(t["t1"][:], t["nr"][:], are[:], ALU.mult), [k("nr"), k("are")], [k("t1")])
            V(lambda e: e.tensor_tensor(t["t2"][:], t["abi"][:], aim[:], ALU.mult), [k("abi"), k("aim")], [k("t2")])
            V(lambda e: e.tensor_tensor(t["t1"][:], t["t1"][:], t["t2"][:], ALU.add), [k("t1"), k("t2")], [k("t1")])
            V(lambda e: e.tensor_tensor(t["cfr"][:], t["t1"][:], t["den"][:], ALU.mult), [k("t1"), k("den")], [k("cfr")])
            V(lambda e: e.tensor_tensor(t["t1"][:], t["abi"][:], are[:], ALU.mult), [k("abi"), k("are")], [k("t1")])
            V(lambda e: e.tensor_tensor(t["t2"][:], t["nr"][:], aim[:], ALU.mult), [k("nr"), k("aim")], [k("t2")])
            V(lambda e: e.tensor_tensor(t["t1"][:], t["t1"][:], t["t2"][:], ALU.subtract), [k("t1"), k("t2")], [k("t1")])
            V(lambda e: e.tensor_tensor(t["cfi"][:], t["t1"][:], t["den"][:], ALU.mult), [k("t1"), k("den")], [k("cfi")])
            return t

        def cmul(dst_r, dst_i, ar, ai, br, bi, t1, t2, rk, wk):
            V(lambda e: e.tensor_tensor(t1, ar, br, ALU.mult), rk, wk)
            V(lambda e: e.tensor_tensor(t2, ai, bi, ALU.mult), rk, wk)
            V(lambda e: e.tensor_tensor(dst_r, t1, t2, ALU.subtract), rk + wk, wk)
            V(lambda e: e.tensor_tensor(t1, ar, bi, ALU.mult), rk + wk, wk)
            V(lambda e: e.tensor_tensor(t2, ai, br, ALU.mult), rk + wk, wk)
            V(lambda e: e.tensor_tensor(dst_i, t1, t2, ALU.add), rk + wk, wk)

        ss_are = SB(mix, "ss_are", [128, 16]); ss_aim = SB(mix, "ss_aim", [128, 16]); ss_ldt = SB(mix, "ss_ldt", [128, 16])
        load(ss_are[:], D["a_re_s"], "ss_are"); load(ss_aim[:], D["a_im_s"], "ss_aim"); load(ss_ldt[:], D["ldt_s"], "ss_ldt")
        sc = ssm_scalars(mix, "ss", ss_are, ss_aim, ss_ldt, [128, 16])
        Er = SB(mix, "Er", [128, 16, 128]); Ei = SB(mix, "Ei", [128, 16, 128]); Rho0 = SB(mix, "Rho0", [128, 16, 128])
        WTr = SB(mix, "WTr", [128, 16, 128], BF16); WTi = SB(mix, "WTi", [128, 16, 128], BF16)
        A128r = SB(mix, "A128r", [128, 16]); A128i = SB(mix, "A128i", [128, 16])
        E127r = SB(mix, "E127r", [128, 16]); E127i = SB(mix, "E127i", [128, 16])
        bsr = SB(mix, "bsr", [128, 16, 32]); bsi = SB(mix, "bsi", [128, 16, 32])
        BTr = SB(mix, "BTr", [128, 16, 128], BF16); BTi = SB(mix, "BTi", [128, 16, 128], BF16)
        Cbr = SB(mix, "Cbr", [128, 16, 128], BF16); Cbi = SB(mix, "Cbi", [128, 16, 128], BF16)

        def bc3(ap2, n):
            return ap2.rearrange("p (a o) -> p a o", o=1).to_broadcast([128, 16, n])

        with scope() as stg:
            Hr = SB(stg, "Hr", [128, 16, 128]); Hi = SB(stg, "Hi", [128, 16, 128])
            T1 = SB(stg, "T1", [128, 16, 128]); T2 = SB(stg, "T2", [128, 16, 128])
            q1 = SB(stg, "q1", [128, 16]); q2 = SB(stg, "q2", [128, 16]); ir = SB(stg, "ir", [128, 16]); ii_ = SB(stg, "ii", [128, 16])
            pr = SB(stg, "pr", [128, 16]); pi_ = SB(stg, "pi", [128, 16])
            V(lambda e: e.memset(Er[:, :, 0:1], 1.0), [], ["Er"]); V(lambda e: e.memset(Ei[:, :, 0:1], 0.0), [], ["Ei"])
            V(lambda e: e.tensor_copy(Er[:, :, 1], sc["cos"][:]), ["ss_cos"], ["Er"])
            V(lambda e: e.tensor_copy(Ei[:, :, 1], sc["sin"][:]), ["ss_sin"], ["Ei"])
            V(lambda e: e.tensor_tensor(q1[:], sc["rho"][:], sc["rho"][:], ALU.mult), ["ss_rho"], ["q1"])
            V(lambda e: e.reciprocal(q1[:], q1[:]), ["q1"], ["q1"])
            V(lambda e: e.tensor_tensor(ir[:], sc["abr"][:], q1[:], ALU.mult), ["ss_abr", "q1"], ["ir"])
            V(lambda e: e.tensor_tensor(ii_[:], sc["abi"][:], q1[:], ALU.mult), ["ss_abi", "q1"], ["ii"])
            V(lambda e: e.tensor_scalar(ii_[:], ii_[:], -1.0, None, ALU.mult), ["ii"], ["ii"])
            V(lambda e: e.memset(Hr[:, :, 0:1], 1.0), [], ["Hr"]); V(lambda e: e.memset(Hi[:, :, 0:1], 0.0), [], ["Hi"])
            V(lambda e: e.tensor_copy(Hr[:, :, 1], ir[:]), ["ir"], ["Hr"]); V(lambda e: e.tensor_copy(Hi[:, :, 1], ii_[:]), ["ii"], ["Hi"])
            for (Xr, Xi, kr, ki) in ((Er, Ei, "Er", "Ei"), (Hr, Hi, "Hr", "Hi")):
                m = 2
                while m < 128:
                    h = m // 2
                    cmul(Xr[:, :, m], Xi[:, :, m], Xr[:, :, h], Xi[:, :, h], Xr[:, :, h], Xi[:, :, h], q1[:], q2[:], [kr, ki, "q1", "q2"], [kr, ki, "q1", "q2"])
                    cmul(Xr[:, :, m + 1:2 * m], Xi[:, :, m + 1:2 * m], Xr[:, :, 1:m], Xi[:, :, 1:m],
                         bc3(Xr[:, :, m], m - 1), bc3(Xi[:, :, m], m - 1), T1[:, :, 1:m], T2[:, :, 1:m], [kr, ki, "T1", "T2"], [kr, ki, "T1", "T2"])
                    m *= 2
            V(lambda e: e.tensor_copy(A128r[:], sc["abr"][:]), ["ss_abr"], ["A128"]); V(lambda e: e.tensor_copy(A128i[:], sc["abi"][:]), ["ss_abi"], ["A128"])
            for _ in range(7):
                cmul(pr[:], pi_[:], A128r[:], A128i[:], A128r[:], A128i[:], q1[:], q2[:], ["A128", "q1", "q2", "pp"], ["pp", "q1", "q2"])
                V(lambda e: e.tensor_copy(A128r[:], pr[:]), ["pp"], ["A128"]); V(lambda e: e.tensor_copy(A128i[:], pi_[:]), ["pp"], ["A128"])
            V(lambda e: e.tensor_copy(E127r[:], Er[:, :, 127]), ["Er"], ["E127"]); V(lambda e: e.tensor_copy(E127i[:], Ei[:, :, 127]), ["Ei"], ["E127"])
            cmul(pr[:], pi_[:], A128r[:], A128i[:], ir[:], ii_[:], q1[:], q2[:], ["A128", "ir", "ii", "q1", "q2", "pp"], ["pp", "q1", "q2"])
            cmul(T1[:], T2[:], Hr[:], Hi[:], bc3(pr[:], 128), bc3(pi_[:], 128), Er[:] if False else Rho0[:], SB(stg, "T3", [128, 16, 128])[:],
                 ["Hr", "Hi", "pp", "Rho0", "T3", "T1", "T2"], ["T1", "T2", "Rho0", "T3"])
            for (Ts, WT, kk) in ((T1, WTr, "WTr"), (T2, WTi, "WTi")):
                for g4 in range(4):
                    for j in range(4):
                        i = 4 * g4 + j
                        M(lambda e, i=i, j=j, Ts=Ts: e.transpose(pb[0][:, j * 128:(j + 1) * 128], Ts[:, i, :], idf[:]), ["T1", "T2", "idf"], ["pb0"])
                    V(lambda e, g4=g4, WT=WT: e.tensor_copy(WT[:, 4 * g4:4 * g4 + 4, :], pb[0][:].rearrange("p (a b) -> p a b", a=4)), ["pb0"], [kk])
            V(lambda e: e.tensor_copy(Rho0[:], bc3(sc["rho"][:], 128)), ["ss_rho", "T1", "T2"], ["Rho0"])
            V(lambda e: e.memset(Rho0[:, :, 0:1], 0.0), ["Rho0"], ["Rho0"])
            b0r = SB(stg, "b0r", [128, 16, 32]); b0i = SB(stg, "b0i", [128, 16, 32]); u1 = SB(stg, "u1", [128, 16, 32]); u2 = SB(stg, "u2", [128, 16, 32])
            load(b0r[:], D["bs_re"], "b0r"); load(b0i[:], D["bs_im"], "b0i")
            cmul(bsr[:], bsi[:], bc3(sc["cfr"][:], 32), bc3(sc["cfi"][:], 32), b0r[:], b0i[:], u1[:], u2[:], ["ss_cfr", "ss_cfi", "b0r", "b0i", "u1", "u2"], ["bs", "u1", "u2"])
        with scope() as stg:
            CW = 512
            r_are = SB(stg, "rr_are", [128, CW]); r_aim = SB(stg, "rr_aim", [128, CW]); r_ldt = SB(stg, "rr_ldt", [128, CW])
            rc = None
            BTr2 = BTr[:].rearrange("p a b -> p (a b)"); BTi2 = BTi[:].rearrange("p a b -> p (a b)")
            Cbr2 = Cbr[:].rearrange("p a b -> p (a b)"); Cbi2 = Cbi[:].rearrange("p a b -> p (a b)")
            for cc in range(2048 // CW):
                cs = slice(cc * CW, (cc + 1) * CW)
                load(r_are[:], D["a_re_r"][:, cs], "rr_are"); load(r_aim[:], D["a_im_r"][:, cs], "rr_aim"); load(r_ldt[:], D["ldt_r"][:, cs], "rr_ldt")
                rc = ssm_scalars(stg, "rr", r_are, r_aim, r_ldt, [128, CW], t=rc)
                b1r = rc["dt"]; b1i = rc["th"]
                load(b1r[:], D["bT_re"].rearrange("p a b -> p (a b)")[:, cs], "rr_dt"); load(b1i[:], D["bT_im"].rearrange("p a b -> p (a b)")[:, cs], "rr_th")
                cmul(rc["cos"][:], rc["sin"][:], rc["cfr"][:], rc["cfi"][:], b1r[:], b1i[:], rc["t1"][:], rc["t2"][:],
                     ["rr_cfr", "rr_cfi", "rr_dt", "rr_th", "rr_t1", "rr_t2", "rr_cos", "rr_sin"], ["rr_cos", "rr_sin", "rr_t1", "rr_t2"])
                V(lambda e, cs=cs: e.tensor_copy(BTr2[:, cs], rc["cos"][:]), ["rr_cos"], ["BT"])
                V(lambda e, cs=cs: e.tensor_copy(BTi2[:, cs], rc["sin"][:]), ["rr_sin"], ["BT"])
                load(rc["t1"][:], D["cb_re"].rearrange("p a b -> p (a b)")[:, cs], "rr_t1"); load(rc["t2"][:], D["cb_im"].rearrange("p a b -> p (a b)")[:, cs], "rr_t2")
                V(lambda e, cs=cs: e.tensor_copy(Cbr2[:, cs], rc["t1"][:]), ["rr_t1"], ["Cb"])
                V(lambda e, cs=cs: e.tensor_scalar(Cbi2[:, cs], rc["t2"][:], -1.0, None, ALU.mult), ["rr_t2"], ["Cb"])

        xt = [SB(mix, f"xt{i}", [128, 1024]) for i in range(2)]
        junk = SB(mix, "junk", [128, 1024], BF16); xnb = SB(mix, "xnb", [128, 1024], BF16); nT = SB(mix, "nT", [128, 8, 128], BF16)
        st1 = SB(mix, "st1", [128, 4])
        Sr = SB(mix, "Sr", [128, 16]); Si = SB(mix, "Si", [128, 16])
        V(lambda e: e.memset(Sr[:], 0.0), [], ["S"]); V(lambda e: e.memset(Si[:], 0.0), [], ["S"])
        c1 = SB(mix, "c1", [128, 16]); c2 = SB(mix, "c2", [128, 16]); c3 = SB(mix, "c3", [128, 16]); c4 = SB(mix, "c4", [128, 16])
        xi = [0]

        def front_pre(src):
            sl = xi[0] % 2; xi[0] += 1
            kx = f"xt{sl}"
            S.dma(lambda e: e.dma_start(out=xt[sl][:], in_=src), writes=[kx])
            A(lambda e: e.activation(out=junk[:], in_=xt[sl][:], func=AF.Square, accum_out=st1[:, 0:1]), [kx], ["junk", "st1"])
            V(lambda e: e.tensor_scalar(st1[:, 1:2], st1[:, 0:1], 1.0 / 1024, EPS, ALU.mult, ALU.add), ["st1"], ["st1b"])
            A(lambda e: e.activation(out=st1[:, 3:4], in_=st1[:, 1:2], func=AF.Sqrt), ["st1b"], ["st1d"])
            V(lambda e: e.reciprocal(st1[:, 2:3], st1[:, 3:4]), ["st1d"], ["st1c"])
            V(lambda e: e.tensor_scalar(xnb[:], xt[sl][:], st1[:, 2:3], None, ALU.mult), [kx, "st1c"], ["xnb"])
            return sl

        def front_tr():
            for k in range(8):
                M(lambda e, k=k: e.transpose(ptr[:, k * 128:(k + 1) * 128], xnb[:, k * 128:(k + 1) * 128], idb[:]), ["xnb", "idb"], ["ptr"])
            A(lambda e: e.copy(nT[:].rearrange("p a b -> p (a b)"), ptr[:]), ["ptr"], ["nT"])

        def front(src):
            sl = front_pre(src)
            front_tr()
            return sl

        ub = SB(mix, "ub", [128, 512], BF16)
        fr = SB(mix, "fr", [128, 16]); fi = SB(mix, "fi", [128, 16])
        w1 = SB(mix, "w1", [128, 16, 32]); w2 = SB(mix, "w2", [128, 16, 32])

        def state_step():
            cmul(c3[:], c4[:], A128r[:], A128i[:], Sr[:], Si[:], c1[:], c2[:], ["A128", "S", "c"], ["c"])
            V(lambda e: e.tensor_tensor(Sr[:], c3[:], fr[:], ALU.add), ["c", "f"], ["S"])
            V(lambda e: e.tensor_tensor(Si[:], c4[:], fi[:], ALU.add), ["c", "f"], ["S"])

        def prefix_u():
            for k in range(8):
                M(lambda e, k=k: e.matmul(pb[0][:], lhsT=nT[:, k, :], rhs=win[:, k, 768:1280], start=(k == 0), stop=(k == 7)), ["nT", "win"], ["pb0"])

        def prefix_m():
            A(lambda e: e.copy(ub[:], pb[0][:]), ["pb0"], ["ub"])
            for i in range(16):
                M(lambda e, i=i: e.matmul(pb[1][:, i * 32:(i + 1) * 32], lhsT=WTr[:, i, :], rhs=ub[:, i * 32:(i + 1) * 32], start=True, stop=True), ["ub", "WTr"], ["pb1"])
                M(lambda e, i=i: e.matmul(pb[2][:, i * 32:(i + 1) * 32], lhsT=WTi[:, i, :], rhs=ub[:, i * 32:(i + 1) * 32], start=True, stop=True), ["ub", "WTi"], ["pb2"])

        def prefix_f():
            mr = pb[1][:].rearrange("p (a b) -> p a b", a=16); mi = pb[2][:].rearrange("p (a b) -> p a b", a=16)
            V(lambda e: e.tensor_tensor(w1[:], bsr[:], mr, ALU.mult), ["bs", "pb1"], ["w1"])
            V(lambda e: e.tensor_tensor(w2[:], bsi[:], mi, ALU.mult), ["bs", "pb2"], ["w2"])
            V(lambda e: e.tensor_tensor(w1[:], w1[:], w2[:], ALU.subtract), ["w1", "w2"], ["w1"])
            V(lambda e: e.tensor_reduce(fr[:], w1[:], AX.X, ALU.add), ["w1"], ["f"])
            V(lambda e: e.tensor_tensor(w1[:], bsr[:], mi, ALU.mult), ["bs", "pb2", "f"], ["w1"])
            V(lambda e: e.tensor_tensor(w2[:], bsi[:], mr, ALU.mult), ["bs", "pb1"], ["w2"])
            V(lambda e: e.tensor_tensor(w1[:], w1[:], w2[:], ALU.add), ["w1", "w2"], ["w1"])
            V(lambda e: e.tensor_reduce(fi[:], w1[:], AX.X, ALU.add), ["w1"], ["f"])
            state_step()

        chunks = []
        if stage == "full":
            for (src, dst, nm) in ((D["puT"], UTs, "UTs"), (D["pv"], Vs, "Vs")):
                for i in range(128):
                    chunks.append((src, dst, nm, i))
        with scope() as pst:
            NS = 3
            if chunks:
                cst = [SB(pst, f"cst{i}", [128, 1024]) for i in range(NS)]; cbf = [SB(pst, f"cbf{i}", [128, 1024], BF16) for i in range(NS)]
            cpos = [0]

            def emit_chunks(n):
                for _ in range(n):
                    if cpos[0] >= len(chunks):
                        return
                    src, dst, nm, i = chunks[cpos[0]]; sl = cpos[0] % NS; cpos[0] += 1
                    S.dma(lambda e: e.dma_start(out=cst[sl][:], in_=src[i]), writes=[f"cst{sl}"])
                    G(lambda e: e.tensor_copy(cbf[sl][:], cst[sl][:]), [f"cst{sl}"], [f"cbf{sl}"])
                    S.dma(lambda e: e.dma_start(out=dst[i], in_=cbf[sl][:]), reads=[f"cbf{sl}"], writes=[f"{nm}{i}"], eng="pool")
            npre = NPRE if "prefix" not in off else 0
            if npre:
                front(xpre[0:128, :])
            for t in range(npre):
                prefix_u()
                if t + 1 < npre:
                    front_pre(xpre[(t + 1) * 128:(t + 2) * 128, :])
                prefix_m()
                if t + 1 < npre:
                    front_tr()
                prefix_f()
                emit_chunks(3)
            emit_chunks(len(chunks))

        if stage == "prefix":
            dbg = SB(mix, "dbg", [128, 1024])
            V(lambda e: e.memset(dbg[:], 0.0), [], ["dbg"])
            V(lambda e: e.tensor_copy(dbg[:, 0:16], Sr[:]), ["S"], ["dbg"]); V(lambda e: e.tensor_copy(dbg[:, 16:32], Si[:]), ["S"], ["dbg"])
            for j, (ap, key) in enumerate([(sc["cos"][:], "ss_cos"), (sc["sin"][:], "ss_sin"), (sc["abr"][:], "ss_abr"), (sc["abi"][:], "ss_abi"),
                                           (sc["cfr"][:], "ss_cfr"), (sc["cfi"][:], "ss_cfi"), (Er[:, :, 5], "Er"), (Ei[:, :, 5], "Ei"),
                                           (A128r[:], "A128"), (A128i[:], "A128"), (fr[:], "f"), (fi[:], "f"), (sc["rho"][:], "ss_rho"), (sc["th"][:], "ss_th")]):
                V(lambda e, j=j, ap=ap: e.tensor_copy(dbg[:, 32 + 16 * j:48 + 16 * j], ap), [key], ["dbg"])
            V(lambda e: e.tensor_copy(dbg[:, 512:1024], ub[:]), ["ub"], ["dbg"])
            S.dma(lambda e: e.dma_start(out=out[0:128, :], in_=dbg[:]), reads=["dbg"], writes=["out"])
            S.emit()
            return nc

        qkv_sb = SB(mix, "qkv_sb", [128, 768]); sqt = SB(mix, "sqt", [128, 640]); s10 = SB(mix, "s10", [128, 10]); s10b = SB(mix, "s10b", [128, 10])
        qkn = SB(mix, "qkn", [128, 640], BF16); qT = SB(mix, "qT", [128, 4, 128], BF16)
        kTs = [SB(mix, f"kT{i}", [128, 128], BF16) for i in range(3)]; vss = [SB(mix, f"vs{i}", [128, 128], BF16) for i in range(3)]
        kTm = SB(mix, "kTm", [128, 128], BF16); vm = SB(mix, "vm", [128, 128], BF16)
        pex = SB(mix, "pex", [128, 512], BF16); PT = [SB(mix, f"PT{i}", [128, 4, 128], BF16) for i in range(2)]; PTm = SB(mix, "PTm", [16, 512], BF16)
        den_sb = SB(mix, "den_sb", [64, 4, 128]); oT = SB(mix, "oT", [64, 8, 128], BF16); osq = SB(mix, "osq", [64, 4, 128], BF16)
        uTb = SB(mix, "uTb", [128, 4, 128], BF16)
        t1 = SB(mix, "t1", [128, 512]); t2 = SB(mix, "t2", [128, 512]); btr = SB(mix, "btr", [128, 512]); bti = SB(mix, "bti", [128, 512])
        str_ = SB(mix, "str", [128, 512]); sti = SB(mix, "sti", [128, 512]); t3 = SB(mix, "t3", [128, 512]); t4 = SB(mix, "t4", [128, 512])
        sre = SB(mix, "sre", [128, 512], BF16); sim = SB(mix, "sim", [128, 512], BF16)
        injr = SB(mix, "injr", [128, 16]); inji = SB(mix, "inji", [128, 16]); stlr = SB(mix, "stlr", [128, 16]); stli = SB(mix, "stli", [128, 16])
        ysb = SB(mix, "ysb", [128, 4, 128]); zb = SB(mix, "zb", [128, 4, 128], BF16); sg = SB(mix, "sg", [128, 4, 128], BF16)
        zz = SB(mix, "zz", [128, 4, 128], BF16); zsq = SB(mix, "zsq", [128, 4, 128], BF16)
        rs = SB(mix, "rs", [128, 4]); hm = SB(mix, "hm", [128, 1024]); h2b = SB(mix, "h2b", [128, 1024], BF16); st2 = SB(mix, "st2", [128, 4])

        def bcg(ap2, a, n):
            return ap2.rearrange("p (a o) -> p a o", o=1).to_broadcast([ap2.shape[0], a, n])

        def kv_part(kslot, vslot, with_q):
            c0 = 0 if with_q else 512
            g0 = c0 // 64
            if with_q:
                for k in range(8):
                    M(lambda e, k=k: e.matmul(psA[:, 0:512], lhsT=nT[:, k, :], rhs=win[:, k, 0:512], start=(k == 0), stop=(k == 7)), ["nT", "win"], ["psAq"])
                A(lambda e: e.copy(qkv_sb[:, 0:512], psA[:, 0:512]), ["psAq"], ["qkv_q"])
                if "q1" in off:
                    raise StopBuild()
            for k in range(8):
                M(lambda e, k=k: e.matmul(psA[:, 512:768], lhsT=nT[:, k, :], rhs=win[:, k, 512:768], start=(k == 0), stop=(k == 7)), ["nT", "win"], ["psAk"])
            A(lambda e: e.copy(qkv_sb[:, 512:768], psA[:, 512:768]), ["psAk"], ["qkv_k"])
            rk = ["qkv_q", "qkv_k"] if with_q else ["qkv_k"]
            V(lambda e: e.tensor_tensor(sqt[:, c0:640], qkv_sb[:, c0:640], qkv_sb[:, c0:640], ALU.mult), rk, ["sqt"])
            V(lambda e: e.tensor_reduce(s10[:, g0:10], sqt[:, c0:640].rearrange("p (a b) -> p a b", b=64), AX.X, ALU.add), ["sqt"], ["s10"])
            V(lambda e: e.tensor_scalar(s10[:, g0:10], s10[:, g0:10], 1.0 / 64, EPS, ALU.mult, ALU.add), ["s10"], ["s10"])
            A(lambda e: e.activation(out=s10b[:, g0:10], in_=s10[:, g0:10], func=AF.Sqrt), ["s10"], ["s10b"])
            V(lambda e: e.reciprocal(s10b[:, g0:10], s10b[:, g0:10]), ["s10b"], ["s10b"])
            V(lambda e: e.tensor_tensor(qkn[:, c0:640].rearrange("p (a b) -> p a b", b=64), qkv_sb[:, c0:640].rearrange("p (a b) -> p a b", b=64),
                                        bcg(s10b[:, g0:10], 10 - g0, 64), ALU.mult), rk + ["s10b"], ["qkn"])
            if with_q and "q2" in off:
                raise StopBuild()
            M(lambda e: e.transpose(ptr[:, 512:640], qkn[:, 512:640], idb[:]), ["qkn", "idb"], ["ptr"])
            A(lambda e: e.copy(kslot[0][:], ptr[:, 512:640]), ["ptr"], [kslot[1]])
            A(lambda e: e.copy(vslot[0][:], qkv_sb[:, 640:768]), ["qkv_k"], [vslot[1]])
            if with_q:
                for m in range(4):
                    M(lambda e, m=m: e.transpose(ptr[:, m * 128:(m + 1) * 128], qkn[:, m * 128:(m + 1) * 128], idb[:]), ["qkn", "idb"], ["ptr"])
                if "q3" in off:
                    raise StopBuild()
                V(lambda e: e.tensor_scalar(qT[:].rearrange("p a b -> p (a b)"), ptr[:, 0:512], cqk[:, 0:1], None, ALU.mult), ["ptr", "cqk"], ["qT"])

        def attention(prev, cur, pmask):
            first = [True]
            for g in range(2):
                ps_ = slice(64 * g, 64 * g + 64)
                for b, (kk, vv, msk, mkey) in enumerate(((prev[0], prev[1], pmask[0], pmask[1]), (cur[0], cur[1], mcur, "mcur"))):
                    M(lambda e, b=b, kk=kk: e.matmul(pb[b][:], lhsT=kk[0][ps_, :], rhs=qT[ps_, :, :].rearrange("p a b -> p (a b)"), start=True, stop=True), [kk[1], "qT"], [f"pb{b}"])
                    A(lambda e, b=b: e.activation(out=pex[:], in_=pb[b][:], func=AF.Exp), [f"pb{b}"], ["pex"])
                    V(lambda e, b=b, msk=msk: e.tensor_tensor(PT[b][:], pex[:].rearrange("p (a b) -> p a b", a=4),
                                                            msk[:].rearrange("p (o q) -> p o q", o=1).to_broadcast([128, 4, 128]), ALU.mult), ["pex", mkey], [f"PT{b}"])
                M(lambda e: e.matmul(pb[2][0:16, :], lhsT=kTm[ps_, 0:16], rhs=qT[ps_, :, :].rearrange("p a b -> p (a b)"), start=True, stop=True), ["kTm", "qT"], ["pb2"])
                A(lambda e: e.activation(out=PTm[:], in_=pb[2][0:16, :], func=AF.Exp), ["pb2"], ["PTm"])
                cs_ = slice(64 * g, 64 * g + 64)
                PT0 = PT[0][:].rearrange("p a b -> p (a b)"); PT1 = PT[1][:].rearrange("p a b -> p (a b)")
                M(lambda e: e.matmul(pb[3][0:64, :], lhsT=prev[1][0][:, cs_], rhs=PT0, start=True, stop=False), [prev[1][1], "PT0"], ["pb3"])
                M(lambda e: e.matmul(pb[3][0:64, :], lhsT=cur[1][0][:, cs_], rhs=PT1, start=False, stop=False), [cur[1][1], "PT1"], ["pb3"])
                M(lambda e: e.matmul(pb[3][0:64, :], lhsT=vm[0:16, cs_], rhs=PTm[:], start=False, stop=True), ["vm", "PTm"], ["pb3"])
                M(lambda e: e.matmul(pb[4][0:64, :], lhsT=ones[:, 0:64], rhs=PT0, start=True, stop=False), ["ones", "PT0"], ["pb4"])
                M(lambda e: e.matmul(pb[4][0:64, :], lhsT=ones[:, 0:64], rhs=PT1, start=False, stop=False), ["ones", "PT1"], ["pb4"])
                M(lambda e: e.matmul(pb[4][0:64, :], lhsT=ones[0:16, 0:64], rhs=PTm[:], start=False, stop=True), ["ones", "PTm"], ["pb4"])
                for j in range(4):
                    h = 4 * g + j
                    A(lambda e, j=j, h=h: e.activation(out=den_sb[:, j, :], in_=pb[4][0:64, j * 128:(j + 1) * 128], func=AF.Identity, bias=esink[:, h:h + 1]), ["pb4", "esink"], ["den"])
                V(lambda e: e.reciprocal(den_sb[:], den_sb[:]), ["den"], ["den"])
                V(lambda e, g=g: e.tensor_tensor(oT[:, 4 * g:4 * g + 4, :], pb[3][0:64, :].rearrange("p (a b) -> p a b", a=4), den_sb[:], ALU.mult), ["pb3", "den"], ["oT"])
                V(lambda e, g=g: e.tensor_tensor(osq[:], oT[:, 4 * g:4 * g + 4, :], oT[:, 4 * g:4 * g + 4, :], ALU.mult), ["oT"], ["osq"])
                for j in range(4):
                    M(lambda e, j=j, g=g: e.matmul(psA[:, 768:769], lhsT=osq[:, j, :], rhs=ones[0:64, 0:1], start=(g == 0 and j == 0), stop=(g == 1 and j == 3)), ["osq", "ones"], ["pssa"])

        def ssm_tile():
            for c in range(4):
                for k in range(8):
                    M(lambda e, c=c, k=k: e.matmul(pb[0][:, c * 128:(c + 1) * 128], lhsT=win[:, k, 768 + 128 * c:768 + 128 * (c + 1)], rhs=nT[:, k, :], start=(k == 0), stop=(k == 7)), ["nT", "win"], ["pb0"])
            A(lambda e: e.copy(uTb[:].rearrange("p a b -> p (a b)"), pb[0][:]), ["pb0"], ["uTb"])
            cmul(injr[:], inji[:], sc["abr"][:], sc["abi"][:], Sr[:], Si[:], c1[:], c2[:], ["ss_abr", "ss_abi", "S", "c"], ["inj", "c"])
            for c in range(4):
                isl = slice(4 * c, 4 * c + 4)
                Erc = Er[:, isl, :].rearrange("p a b -> p (a b)"); Eic = Ei[:, isl, :].rearrange("p a b -> p (a b)")
                Rc = Rho0[:, isl, :].rearrange("p a b -> p (a b)")
                for ii in range(4):
                    i = 4 * c + ii
                    M(lambda e, i=i, ii=ii, c=c: e.matmul(pb[1][:, ii * 128:(ii + 1) * 128], lhsT=BTr[:, i, :], rhs=uTb[:, c, :], start=True, stop=True), ["BT", "uTb"], ["pb1"])
                    M(lambda e, i=i, ii=ii, c=c: e.matmul(pb[2][:, ii * 128:(ii + 1) * 128], lhsT=BTi[:, i, :], rhs=uTb[:, c, :], start=True, stop=True), ["BT", "uTb"], ["pb2"])
                V(lambda e: e.tensor_tensor(t1[:], pb[1][:], Erc, ALU.mult), ["pb1", "Er"], ["t1"])
                V(lambda e: e.tensor_tensor(t2[:], pb[2][:], Eic, ALU.mult), ["pb2", "Ei"], ["t2"])
                V(lambda e: e.tensor_tensor(btr[:], t1[:], t2[:], ALU.add), ["t1", "t2"], ["btr"])
                V(lambda e: e.tensor_tensor(t1[:], pb[2][:], Erc, ALU.mult), ["pb2", "Er", "btr"], ["t1"])
                V(lambda e: e.tensor_tensor(t2[:], pb[1][:], Eic, ALU.mult), ["pb1", "Ei", "btr"], ["t2"])
                V(lambda e: e.tensor_tensor(bti[:], t1[:], t2[:], ALU.subtract), ["t1", "t2"], ["bti"])
                b3r = btr[:].rearrange("p (a b) -> p a b", a=4); b3i = bti[:].rearrange("p (a b) -> p a b", a=4)
                V(lambda e, isl=isl: e.tensor_tensor(b3r[:, :, 0], b3r[:, :, 0], injr[:, isl], ALU.add), ["btr", "inj"], ["btr"])
                V(lambda e, isl=isl: e.tensor_tensor(b3i[:, :, 0], b3i[:, :, 0], inji[:, isl], ALU.add), ["bti", "inj"], ["bti"])
                V(lambda e: e.tensor_tensor_scan(str_[:], Rc, btr[:], 0.0, ALU.mult, ALU.add), ["Rho0", "btr"], ["str"])
                V(lambda e: e.tensor_tensor_scan(sti[:], Rc, bti[:], 0.0, ALU.mult, ALU.add), ["Rho0", "bti"], ["sti"])
                s3r = str_[:].rearrange("p (a b) -> p a b", a=4); s3i = sti[:].rearrange("p (a b) -> p a b", a=4)
                V(lambda e, isl=isl: e.tensor_copy(stlr[:, isl], s3r[:, :, 127]), ["str"], ["stl"])
                V(lambda e, isl=isl: e.tensor_copy(stli[:, isl], s3i[:, :, 127]), ["sti"], ["stl"])
                G(lambda e: e.tensor_tensor(t3[:], str_[:], Erc, ALU.mult), ["str", "Er"], ["t3"])
                G(lambda e: e.tensor_tensor(t4[:], sti[:], Eic, ALU.mult), ["sti", "Ei"], ["t4"])
                G(lambda e: e.tensor_tensor(sre[:], t3[:], t4[:], ALU.subtract), ["t3", "t4"], ["sre"])
                G(lambda e: e.tensor_tensor(t3[:], str_[:], Eic, ALU.mult), ["str", "Ei", "sre"], ["t3"])
                G(lambda e: e.tensor_tensor(t4[:], sti[:], Erc, ALU.mult), ["sti", "Er", "sre"], ["t4"])
                G(lambda e: e.tensor_tensor(sim[:], t3[:], t4[:], ALU.add), ["t3", "t4"], ["sim"])
                for ii in range(4):
                    i = 4 * c + ii
                    M(lambda e, i=i, ii=ii, c=c: e.matmul(pb[3][:, c * 128:(c + 1) * 128], lhsT=Cbr[:, i, :], rhs=sre[:, ii * 128:(ii + 1) * 128], start=(ii == 0), stop=False), ["Cb", "sre"], ["pb3"])
                    M(lambda e, i=i, ii=ii, c=c: e.matmul(pb[3][:, c * 128:(c + 1) * 128], lhsT=Cbi[:, i, :], rhs=sim[:, ii * 128:(ii + 1) * 128], start=False, stop=(ii == 3)), ["Cb", "sim"], ["pb3"])
            cmul(Sr[:], Si[:], stlr[:], stli[:], E127r[:], E127i[:], c1[:], c2[:], ["stl", "E127", "c", "inj"], ["S", "c"])
            for c in range(4):
                V(lambda e, c=c: e.scalar_tensor_tensor(ysb[:, c, :], uTb[:, c, :], small["ssm_d"][:, c:c + 1], pb[3][:, c * 128:(c + 1) * 128], ALU.mult, ALU.add), ["uTb", "c_ssm_d", "pb3"], ["ysb"])
            A(lambda e: e.activation(out=zb[:], in_=ysb[:], func=AF.Gelu), ["ysb"], ["zb"])
            for cp in range(4):
                for c in range(4):
                    M(lambda e, c=c, cp=cp: e.matmul(pb[4][:, cp * 128:(cp + 1) * 128], lhsT=glu[:, c, cp * 128:(cp + 1) * 128], rhs=zb[:, c, :], start=(c == 0), stop=(c == 3)), ["glu", "zb"], ["pb4"])
            for cp in range(4):
                A(lambda e, cp=cp: e.activation(out=sg[:, cp, :], in_=pb[4][:, cp * 128:(cp + 1) * 128], func=AF.Sigmoid, bias=small["glu_b"][:, cp:cp + 1]), ["pb4", "c_glu_b"], ["sg"])
            V(lambda e: e.tensor_tensor(zz[:], zb[:], sg[:], ALU.mult), ["zb", "sg"], ["zz"])
            V(lambda e: e.tensor_tensor(zsq[:], zz[:], zz[:], ALU.mult), ["zz"], ["zsq"])
            for c in range(4):
                M(lambda e, c=c: e.matmul(psA[:, 769:770], lhsT=zsq[:, c, :], rhs=ones[:, 0:1], start=(c == 0), stop=(c == 3)), ["zsq", "ones"], ["psss"])

        def out_proj(sl, t):
            for half in range(2):
                hs = slice(half * 512, (half + 1) * 512)
                for h in range(8):
                    M(lambda e, h=h, half=half, hs=hs: e.matmul(pb[half][:], lhsT=oT[:, h, :], rhs=woa[:, h, hs], start=(h == 0), stop=(h == 7)), ["oT", "woa"], [f"pb{half}"])
                for c in range(4):
                    M(lambda e, c=c, half=half, hs=hs: e.matmul(pb[2 + half][:], lhsT=zz[:, c, :], rhs=wos[:, c, hs], start=(c == 0), stop=(c == 3)), ["zz", "wos"], [f"pb{2 + half}"])
            V(lambda e: e.tensor_scalar(rs[:, 0:2], psA[:, 768:770], 1.0 / 512, EPS, ALU.mult, ALU.add), ["pssa", "psss"], ["rs"])
            A(lambda e: e.activation(out=rs[:, 2:4], in_=rs[:, 0:2], func=AF.Sqrt), ["rs"], ["rsb"])
            V(lambda e: e.reciprocal(rs[:, 0:2], rs[:, 2:4]), ["rsb", "rs"], ["rs"])
            kx = f"xt{sl}"
            for half in range(2):
                hs = slice(half * 512, (half + 1) * 512)
                V(lambda e, half=half, hs=hs: e.scalar_tensor_tensor(hm[:, hs], pb[half][:], rs[:, 0:1], xt[sl][:, hs], ALU.mult, ALU.add), [f"pb{half}", "rs", kx], ["hm"])
                V(lambda e, half=half, hs=hs: e.scalar_tensor_tensor(hm[:, hs], pb[2 + half][:], rs[:, 1:2], hm[:, hs], ALU.mult, ALU.add), [f"pb{2 + half}", "rs", "hm"], ["hm"])
            S.dma(lambda e: e.dma_start(out=out[t * 128:(t + 1) * 128, :], in_=hm[:]), reads=["hm"], writes=[f"out{t}"])
            A(lambda e: e.activation(out=junk[:], in_=hm[:], func=AF.Square, accum_out=st2[:, 0:1]), ["hm"], ["junk", "st2"])
            V(lambda e: e.tensor_scalar(st2[:, 1:2], st2[:, 0:1], 1.0 / 1024, EPS, ALU.mult, ALU.add), ["st2"], ["st2b"])
            A(lambda e: e.activation(out=st2[:, 3:4], in_=st2[:, 1:2], func=AF.Sqrt), ["st2b"], ["st2d"])
            V(lambda e: e.reciprocal(st2[:, 2:3], st2[:, 3:4]), ["st2d"], ["st2c"])
            V(lambda e: e.tensor_scalar(h2b[:], hm[:], st2[:, 2:3], None, ALU.mult), ["hm", "st2c"], ["h2b"])
            for k in range(8):
                M(lambda e, k=k: e.transpose(ptr[:, k * 128:(k + 1) * 128], h2b[:, k * 128:(k + 1) * 128], idb[:]), ["h2b", "idb"], ["ptr"])
            V(lambda e: e.tensor_tensor(h2T[:, :, t * 128:(t + 1) * 128], ptr[:].rearrange("p (a b) -> p a b", a=8), bcg(small["g2"][:], 8, 128), ALU.mult), ["ptr", "c_g2"], ["h2T"])

        if "nomain" in off:
            S.dma(lambda e: e.dma_start(out=xt[0][:], in_=xown[0:128, :]), writes=["xt0"])
            S.dma(lambda e: e.dma_start(out=out[0:128, :], in_=xt[0][:]), reads=["xt0"], writes=["out0"])
            S.emit()
            return nc
        front(xmeta)
        kv_part((kTm, "kTm"), (vm, "vm"), False)
        if "stopmeta" in off:
            S.dma(lambda e: e.dma_start(out=out[0:128, :], in_=xt[0][:]), reads=["xt0", "kTm", "vm"], writes=["out0"])
            S.emit()
            return nc
        front(xhalo)
        kv_part((kTs[2], "kT2"), (vss[2], "vs2"), False)
        if "stophalo" in off:
            S.dma(lambda e: e.dma_start(out=out[0:128, :], in_=xt[1][:]), reads=["xt1", "kT2", "vs2"], writes=["out0"])
            S.emit()
            return nc
        n_own = 1 if (stage == "mix1" or "one" in off) else NT
        for t in range(n_own):
            sl = front(xown[t * 128:(t + 1) * 128, :])
            pslot, cslot = (t + 2) % 3, t % 3
            try:
                kv_part((kTs[cslot], f"kT{cslot}"), (vss[cslot], f"vs{cslot}"), True)
            except StopBuild:
                S.dma(lambda e: e.dma_start(out=out[0:128, :], in_=xt[sl][:]), reads=[f"xt{sl}", "qkv_q", "qkv_k", "qkn", "ptr"], writes=["out0"])
                S.emit()
                return nc
            if "attn" not in off:
                attention(((kTs[pslot], f"kT{pslot}"), (vss[pslot], f"vs{pslot}")), ((kTs[cslot], f"kT{cslot}"), (vss[cslot], f"vs{cslot}")),
                          (mhalo, "mhalo") if t == 0 else (mprev, "mprev"))
            if "dumpqk" in off:
                V(lambda e: e.tensor_copy(hm[:, 0:512], qT[:].rearrange("p a b -> p (a b)")), ["qT"], ["hm"])
                V(lambda e: e.tensor_copy(hm[:, 512:640], kTs[cslot][:]), [f"kT{cslot}"], ["hm"])
                V(lambda e: e.tensor_copy(hm[:, 640:768], vss[cslot][:]), [f"vs{cslot}"], ["hm"])
                V(lambda e: e.tensor_copy(hm[:, 768:896], kTm[:]), ["kTm"], ["hm"])
                V(lambda e: e.tensor_copy(hm[:, 896:1024], vm[:]), ["vm"], ["hm"])
                S.dma(lambda e: e.dma_start(out=out[0:128, :], in_=hm[:]), reads=["hm"], writes=["out0"])
                S.emit()
                return nc
            if "dumpv" in off:
                V(lambda e: e.tensor_copy(hm[0:64, 0:512], pb[3][0:64, :]), ["pb3"], ["hm"])
                V(lambda e: e.tensor_copy(hm[0:64, 512:1024], pb[4][0:64, :]), ["pb4", "hm"], ["hm"])
                S.dma(lambda e: e.dma_start(out=out[0:64, :], in_=hm[0:64, :]), reads=["hm"], writes=["out0"])
                S.emit()
                return nc
            if "dumpp" in off:
                V(lambda e: e.tensor_copy(hm[:, 0:512], PT[1][:].rearrange("p a b -> p (a b)")), ["PT1"], ["hm"])
                V(lambda e: e.tensor_copy(hm[0:64, 512:1024], den_sb[:].rearrange("p a b -> p (a b)")), ["den"], ["hm"])
                S.dma(lambda e: e.dma_start(out=out[0:128, :], in_=hm[:]), reads=["hm"], writes=["out0"])
                S.emit()
                return nc
            if "dumpo" in off:
                V(lambda e: e.tensor_copy(hm[0:64, :], oT[:].rearrange("p a b -> p (a b)")), ["oT"], ["hm"])
                S.dma(lambda e: e.dma_start(out=out[0:64, :], in_=hm[0:64, :]), reads=["hm"], writes=["out0"])
                S.emit()
                return nc
            if "ssm" not in off:
                ssm_tile()
            if "outp" not in off:
                out_proj(sl, t)
            else:
                S.dma(lambda e, t=t, sl=sl: e.dma_start(out=out[t * 128:(t + 1) * 128, :], in_=xt[sl][:]), reads=[f"xt{sl}"], writes=[f"out{t}"])

        if "dumph2" in off:
            V(lambda e: e.tensor_copy(hm[:], h2T[:, :, 0:128]), ["h2T", "hm"], ["hm"])
            S.dma(lambda e: e.dma_start(out=out[128:256, :], in_=hm[:]), reads=["hm"], writes=["out1"])
        if stage in ("mix", "mix1"):
            S.emit()
            return nc

        S.barrier()
        mix.close()
        pe_ = top.enter_context(contextlib.ExitStack())
        pA = PS(pe_, "pA", [128, 2048]); pQ = PS(pe_, "pQ", [128, 512])
        wq = SB(pe_, "wq", [128, 8, 2048], BF16); keysT = SB(pe_, "keysT", [128, 16, 128], BF16)
        with scope() as stg:
            wst = SB(stg, "wst", [128, 2, 2048])
            for kk4 in range(4):
                load(wst[:], D["wq"][:, 2 * kk4:2 * kk4 + 2, :], "wst")
                V(lambda e, kk4=kk4: e.tensor_copy(wq[:, 2 * kk4:2 * kk4 + 2, :], wst[:]), ["wst"], ["wq"])
            kst = SB(stg, "kst", [128, 16, 128])
            load(kst[:], D["keysT"], "kst")
            V(lambda e: e.tensor_copy(keysT[:], kst[:]), ["kst"], ["keysT"])
        qTp = SB(pe_, "qTp", [128, 16, 128], BF16); scs = SB(pe_, "scs", [128, 16, 128]); sc2 = SB(pe_, "sc2", [128, 16, 128])
        v16 = SB(pe_, "v16", [128, 16, 16]); cand = SB(pe_, "cand", [128, 8, 256]); cand2 = SB(pe_, "cand2", [128, 8, 256]); s16 = SB(pe_, "s16", [128, 8, 16])
        dbgp = SB(pe_, "dbgp", [128, 1024])

        def peer_front(t):
            ts_ = slice(t * 128, (t + 1) * 128)
            for g4 in range(4):
                for j in range(4):
                    cb = 4 * g4 + j
                    for k in range(8):
                        M(lambda e, cb=cb, j=j, k=k: e.matmul(pQ[:, j * 128:(j + 1) * 128], lhsT=wq[:, k, cb * 128:(cb + 1) * 128], rhs=h2T[:, k, ts_], start=(k == 0), stop=(k == 7)), ["wq", "h2T"], ["pQ"])
                A(lambda e, g4=g4: e.copy(qTp[:, 4 * g4:4 * g4 + 4, :].rearrange("p a b -> p (a b)"), pQ[:]), ["pQ"], ["qTp"])
            for idx in range(16):
                M(lambda e, idx=idx: e.matmul(pA[:, idx * 128:(idx + 1) * 128], lhsT=qTp[:, idx, :], rhs=keysT[:, idx, :], start=True, stop=True), ["qTp", "keysT"], ["pA"])
            for q4 in range(4):
                A(lambda e, q4=q4: e.copy(scs[:, 4 * q4:4 * q4 + 4, :].rearrange("p a b -> p (a b)"), pA[:, q4 * 512:(q4 + 1) * 512]), ["pA"], ["scs"])
            for idx in range(16):
                V(lambda e, idx=idx: e.max(out=v16[:, idx, 0:8], in_=scs[:, idx, :]), ["scs"], ["v16"])
                V(lambda e, idx=idx: e.match_replace(out=sc2[:, idx, :], in_to_replace=v16[:, idx, 0:8], in_values=scs[:, idx, :], imm_value=-1e30), ["scs", "v16"], ["sc2"])
                V(lambda e, idx=idx: e.max(out=v16[:, idx, 8:16], in_=sc2[:, idx, :]), ["sc2"], ["v16"])
            v4 = v16[:].rearrange("p (h two) a -> p h two a", two=2)
            V(lambda e: e.tensor_tensor(cand[:].rearrange("p h (a b) -> p h a b", a=16),
                                        v4[:, :, 0, :].rearrange("p h (a o) -> p h a o", o=1).to_broadcast([128, 8, 16, 16]),
                                        v4[:, :, 1, :].rearrange("p h (o b) -> p h o b", o=1).to_broadcast([128, 8, 16, 16]), ALU.add), ["v16"], ["cand"])
            for h in range(8):
                V(lambda e, h=h: e.max(out=s16[:, h, 0:8], in_=cand[:, h, :]), ["cand"], ["s16"])
                V(lambda e, h=h: e.match_replace(out=cand2[:, h, :], in_to_replace=s16[:, h, 0:8], in_values=cand[:, h, :], imm_value=-1e30), ["cand", "s16"], ["cand2"])
                V(lambda e, h=h: e.max(out=s16[:, h, 8:16], in_=cand2[:, h, :]), ["cand2"], ["s16"])

        if stage == "peerfront":
            peer_front(0)
            V(lambda e: e.memset(dbgp[:], 0.0), [], ["dbgp"])
            V(lambda e: e.tensor_copy(dbgp[:, 0:256], v16[:].rearrange("p a b -> p (a b)")), ["v16"], ["dbgp"])
            V(lambda e: e.tensor_copy(dbgp[:, 256:384], s16[:].rearrange("p a b -> p (a b)")), ["s16"], ["dbgp"])
            V(lambda e: e.tensor_copy(dbgp[:, 512:1024], scs[:, 0:4, :].rearrange("p a b -> p (a b)")), ["scs"], ["dbgp"])
            S.dma(lambda e: e.dma_start(out=out[256:384, :], in_=dbgp[:]), reads=["dbgp"], writes=["out2"])
            S.emit()
            return nc

        n_i = 128

        iota_i = SB(pe_, "iota_i", [128, 128]); bm = SB(pe_, "bm", [128, 8])
        load(iota_i[:], D["iota_i"], "iota_i"); load(bm[:], D["blkmask"], "bm")
        pG2 = PS(pe_, "pG2", [128, 512]); pZs = [PS(pe_, f"pZ{i}", [128, 512]) for i in range(2)]
        ixu = SB(pe_, "ixu", [128, 16, 16], U32); ixf = SB(pe_, "ixf", [128, 16, 16])
        i1c = SB(pe_, "i1c", [128, 128]); i2c = SB(pe_, "i2c", [128, 128]); i1T = SB(pe_, "i1T", [128, 128]); i2T = SB(pe_, "i2T", [128, 128])
        zs = SB(pe_, "zs", [128, 8]); wT = SB(pe_, "wT", [128, 128, 16], BF16)
        NB = 16
        P1b = [SB(pe_, f"P1b{i}", [128, NB, 128], BF16) for i in range(2)]; P2b = [SB(pe_, f"P2b{i}", [128, NB, 128], BF16) for i in range(2)]
        Wb = [SB(pe_, f"Wb{i}", [128, NB, 128], BF16) for i in range(2)]
        Tsb = [SB(pe_, f"Tsb{i}", [128, 4, 128], BF16) for i in range(2)]; Gt = SB(pe_, "Gt", [128, 128, 128], BF16)
        ND = 4
        ubuf = [SB(pe_, f"ubuf{i}", [128, 1024], BF16) for i in range(ND)]; vbuf = [SB(pe_, f"vbuf{i}", [128, 1024], BF16) for i in range(ND)]
        zg = [SB(pe_, f"zg{i}", [128, 128], BF16) for i in range(2)]; AT = [SB(pe_, f"AT{i}", [128, 128], BF16) for i in range(2)]
        hmt = SB(pe_, "hmt", [128, 1024]); res = SB(pe_, "res", [128, 1024])

        def b3(ap2, a, n):
            return ap2.rearrange("p (a o) -> p a o", o=1).to_broadcast([128, a, n])

        def gate_build(t):
            for idx in range(16):
                V(lambda e, idx=idx: e.max_index(out=ixu[:, idx, 0:8], in_max=v16[:, idx, 0:8], in_values=scs[:, idx, :]), ["v16", "scs"], ["ixu"])
                V(lambda e, idx=idx: e.max_index(out=ixu[:, idx, 8:16], in_max=v16[:, idx, 8:16], in_values=sc2[:, idx, :]), ["v16", "sc2"], ["ixu"])
            V(lambda e: e.tensor_copy(ixf[:], ixu[:]), ["ixu"], ["ixf"])
            ix4 = ixf[:].rearrange("p (h two) a -> p h two a", two=2)
            V(lambda e: e.tensor_copy(i1c[:].rearrange("p (h a) -> p h a", h=8), ix4[:, :, 0, :]), ["ixf"], ["i1c"])
            V(lambda e: e.tensor_copy(i2c[:].rearrange("p (h a) -> p h a", h=8), ix4[:, :, 1, :]), ["ixf"], ["i2c"])
            wx = scs[:].rearrange("p a b -> p (a b)").rearrange("p (h c) -> p h c", h=8)
            mk = sc2[:].rearrange("p a b -> p (a b)").rearrange("p (h c) -> p h c", h=8)
            V(lambda e: e.tensor_tensor(wx, cand[:], b3(s16[:, :, 0], 8, 256), ALU.subtract), ["cand", "s16", "ixu"], ["scs"])
            A(lambda e: e.activation(out=wx, in_=wx, func=AF.Exp), ["scs"], ["scs"])
            V(lambda e: e.tensor_tensor(mk, cand[:], b3(s16[:, :, 15], 8, 256), ALU.is_ge), ["cand", "s16", "ixu"], ["sc2"])
            V(lambda e: e.tensor_tensor(wx, wx, mk, ALU.mult), ["scs", "sc2"], ["scs"])
            V(lambda e: e.tensor_reduce(zs[:], wx, AX.X, ALU.add), ["scs"], ["zs"])
            V(lambda e: e.reciprocal(zs[:], zs[:]), ["zs"], ["zs"])
            V(lambda e: e.tensor_tensor(wx, wx, b3(zs[:], 8, 256), ALU.mult), ["scs", "zs"], ["scs"])
            wperm = sc2[:].rearrange("p a b -> p (a b)").rearrange("p (b h a) -> p b h a", b=16, h=8)
            V(lambda e: e.tensor_copy(wperm, scs[:].rearrange("p a b -> p (a b)").rearrange("p (h a b) -> p b h a", h=8, a=16)), ["scs", "sc2"], ["sc2"])
            w2d = sc2[:].rearrange("p a b -> p (a b)")
            for b in range(16):
                M(lambda e, b=b: e.transpose(pA[:, b * 128:(b + 1) * 128], w2d[:, b * 128:(b + 1) * 128], idf[:]), ["sc2", "idf"], ["pA"])
            V(lambda e: e.tensor_copy(wT[:].rearrange("p t b -> p b t"), pA[:].rearrange("p (b t) -> p b t", b=16)), ["pA"], ["wT"])
            M(lambda e: e.transpose(pQ[:, 0:128], i1c[:], idf[:]), ["i1c", "idf"], ["pQ"])
            M(lambda e: e.transpose(pQ[:, 128:256], i2c[:], idf[:]), ["i2c", "idf"], ["pQ"])
            V(lambda e: e.tensor_copy(i1T[:], pQ[:, 0:128]), ["pQ"], ["i1T"])
            V(lambda e: e.tensor_copy(i2T[:], pQ[:, 128:256]), ["pQ"], ["i2T"])
            io3 = iota_i[:].rearrange("p (o i) -> p o i", o=1).to_broadcast([128, NB, 128])
            GPB = NB // 4
            NG = 128 // 4

            def build(nb):
                p = nb % 2; tsl = slice(nb * NB, (nb + 1) * NB)
                V(lambda e: e.tensor_tensor(P1b[p][:], io3, b3(i1T[:, tsl], NB, 128), ALU.is_equal), ["iota_i", "i1T"], [f"P1b{p}"])
                V(lambda e: e.tensor_tensor(P2b[p][:], io3, b3(i2T[:, tsl], NB, 128), ALU.is_equal), ["iota_i", "i2T"], [f"P2b{p}"])
                V(lambda e: e.tensor_tensor(Wb[p][:].rearrange("p t (h b) -> p t h b", h=8),
                                            wT[:, tsl, :].rearrange("p t (o b) -> p t o b", o=1).to_broadcast([128, NB, 8, 16]),
                                            bm[:].rearrange("p (o h q) -> p o h q", o=1, q=1).to_broadcast([128, NB, 8, 16]), ALU.mult), ["wT", "bm"], [f"Wb{p}"])

            def s1(g):
                p = (g // GPB) % 2; q = g % 2
                for j in range(4):
                    tk = (g % GPB) * 4 + j
                    M(lambda e: e.matmul(pQ[:, j * 128:(j + 1) * 128], lhsT=Wb[p][:, tk, :], rhs=P1b[p][:, tk, :], start=True, stop=True), [f"Wb{p}", f"P1b{p}"], ["pQ"])
                A(lambda e: e.copy(Tsb[q][:].rearrange("p a b -> p (a b)"), pQ[:]), ["pQ"], [f"Tsb{q}"])

            def s2(g):
                p = (g // GPB) % 2; q = g % 2
                for j in range(4):
                    tk = (g % GPB) * 4 + j
                    M(lambda e: e.matmul(pG2[:, j * 128:(j + 1) * 128], lhsT=P2b[p][:, tk, :], rhs=Tsb[q][:, j, :], start=True, stop=True), [f"P2b{p}", f"Tsb{q}"], ["pG2"])
                t0 = g * 4
                A(lambda e: e.copy(Gt[:, t0:t0 + 4, :].rearrange("p a b -> p (a b)"), pG2[:]), ["pG2"], ["Gt"])

            build(0)
            s1(0)
            for g in range(NG):
                if g % GPB == 0 and g // GPB + 1 < 128 // NB:
                    build(g // GPB + 1)
                if g + 1 < NG:
                    s1(g + 1)
                s2(g)

        def dense_block(t):
            ts_ = slice(t * 128, (t + 1) * 128)
            def zstage(i):
                sl = i % 2; ds_ = i % ND
                S.dma(lambda e, i=i, ds_=ds_: e.dma_start(out=ubuf[ds_][:], in_=UTs[i]), reads=[f"UTs{i}"], writes=[f"ubuf{ds_}"])
                S.dma(lambda e, i=i, ds_=ds_: e.dma_start(out=vbuf[ds_][:], in_=Vs[i]), reads=[f"Vs{i}"], writes=[f"vbuf{ds_}"], eng="pool")
                for k in range(8):
                    M(lambda e, k=k, sl=sl, ds_=ds_: e.matmul(pZs[sl][:, 0:128], lhsT=ubuf[ds_][:, k * 128:(k + 1) * 128], rhs=h2T[:, k, ts_], start=(k == 0), stop=(k == 7)), [f"ubuf{ds_}", "h2T"], [f"pZ{sl}"])
                A(lambda e, sl=sl: e.activation(out=zg[sl][:], in_=pZs[sl][:, 0:128], func=AF.Gelu), [f"pZ{sl}"], [f"zg{sl}"])
                V(lambda e, sl=sl, i=i: e.tensor_tensor(AT[sl][:], zg[sl][:], Gt[:, :, i], ALU.mult), [f"zg{sl}", "Gt"], [f"AT{sl}"])

            def ostage(i):
                sl = i % 2; ds_ = i % ND
                for half in range(2):
                    M(lambda e, half=half, sl=sl, ds_=ds_, i=i: e.matmul(pA[:, half * 512:(half + 1) * 512], lhsT=AT[sl][:], rhs=vbuf[ds_][:, half * 512:(half + 1) * 512], start=(i == 0), stop=(i == n_i - 1)), [f"AT{sl}", f"vbuf{ds_}"], ["pA"])

            zstage(0)
            for i in range(n_i):
                if i + 1 < n_i:
                    zstage(i + 1)
                ostage(i)
            S.dma(lambda e: e.dma_start(out=hmt[:], in_=out[t * 128:(t + 1) * 128, :]), reads=[f"out{t}"], writes=["hmt"])
            for half in range(2):
                hs = slice(half * 512, (half + 1) * 512)
                V(lambda e, hs=hs: e.tensor_tensor(res[:, hs], hmt[:, hs], pA[:, hs], ALU.add), ["hmt", "pA"], ["res"])
            S.dma(lambda e: e.dma_start(out=out[t * 128:(t + 1) * 128, :], in_=res[:]), reads=["res"], writes=[f"out{t}"])

        if "keephm" in off:
            S.dma(lambda e: e.dma_start(out=hmt[:], in_=out[0:128, :]), reads=["out0"], writes=["hmt"])
            S.dma(lambda e: e.dma_start(out=out[128:256, :], in_=hmt[:]), reads=["hmt"], writes=["out1"])
        for t in range(n_own):
            peer_front(t)
            gate_build(t)
            dense_block(t)
        S.emit()
        return nc


def _host_inputs(inp):
    f = np.float32
    x = np.ascontiguousarray(inp["x"][0]); meta = inp["meta_tokens"]
    common = {}
    common["ident"] = np.eye(128, dtype=f)
    kq = np.arange(128)
    common["mask_cur"] = (kq[:, None] <= kq[None, :]).astype(f)
    common["xmeta"] = np.concatenate([meta, np.zeros((112, 1024), f)], 0)
    w_in = inp["w_in"][0]
    qperm = np.concatenate([np.arange(64) + 64 * h for h in (0, 4, 1, 5, 2, 6, 3, 7)])
    w_in = np.concatenate([w_in[:, :512][:, qperm], w_in[:, 512:]], 1)
    common["w_in"] = np.ascontiguousarray(w_in.reshape(8, 128, 1280).transpose(1, 0, 2))
    common["mask_prev"] = (kq[:, None] > kq[None, :]).astype(f)
    common["g1"] = np.ascontiguousarray(inp["norm1_g"][0].reshape(8, 128).T)
    common["gq2"] = np.tile(inp["q_norm_g"][0], 2).reshape(128, 1).astype(f)
    common["gk2"] = np.tile(inp["k_norm_g"][0], 2).reshape(128, 1).astype(f)
    common["sinks"] = np.ascontiguousarray(np.broadcast_to(inp["attn_sinks"][0][None, :], (64, 8))).astype(f)

    def state_layout(a):
        return np.ascontiguousarray(a.reshape(16, 2, 64).transpose(1, 2, 0).reshape(128, 16))

    def row_layout(a):
        return np.ascontiguousarray(np.broadcast_to(a.reshape(1, 2048), (128, 2048)))
    ldt64 = np.ascontiguousarray(np.broadcast_to(inp["ssm_log_dt"][0][:, None], (32, 64)))
    for nm, a in (("a_re", inp["ssm_a_re"][0]), ("a_im", inp["ssm_a_im"][0]), ("ldt", ldt64)):
        common[nm + "_s"] = state_layout(a); common[nm + "_r"] = row_layout(a)
    for nm, B in (("re", inp["ssm_b_re"][0]), ("im", inp["ssm_b_im"][0])):
        bT = np.zeros((128, 16, 128), f); bs = np.zeros((128, 16, 32), f)
        for g in range(32):
            i, hh = g // 2, g % 2
            bT[(g % 8) * 16:(g % 8) * 16 + 16, i, hh * 64:hh * 64 + 64] = B[g].T
            bs[hh * 64:hh * 64 + 64, i, hh * 16:hh * 16 + 16] = B[g]
        common["bT_" + nm] = bT; common["bs_" + nm] = bs
    for nm, C in (("re", inp["ssm_c_re"][0]), ("im", inp["ssm_c_im"][0])):
        cb = np.zeros((128, 16, 128), f)
        for g in range(32):
            i, hh = g // 2, g % 2
            cb[hh * 64:hh * 64 + 64, i, (g % 8) * 16:(g % 8) * 16 + 16] = C[g].T
        common["cb_" + nm] = cb
    common["ssm_d"] = np.ascontiguousarray(inp["ssm_d"][0].reshape(4, 128).T)
    common["glu_w"] = np.ascontiguousarray(inp["ssm_glu_w"][0].reshape(4, 128, 512).transpose(1, 0, 2))
    common["glu_b"] = np.ascontiguousarray(inp["ssm_glu_b"][0].reshape(4, 128).T)
    common["ga"] = np.ascontiguousarray(inp["attn_out_g"][0].reshape(8, 64).T)
    common["gs"] = np.ascontiguousarray(inp["ssm_out_g"][0].reshape(4, 128).T)
    common["woa"] = np.ascontiguousarray(inp["w_out"][0][:512].reshape(8, 64, 1024).transpose(1, 0, 2))
    common["wos"] = np.ascontiguousarray(inp["w_out"][0][512:].reshape(4, 128, 1024).transpose(1, 0, 2))
    common["g2"] = np.ascontiguousarray(inp["norm2_g"][0].reshape(8, 128).T)
    if inp.get("peer_w_query") is not None:
        common["wq"] = np.ascontiguousarray(inp["peer_w_query"][0].reshape(8, 128, 2048).transpose(1, 0, 2))
        sk = inp["peer_sub_keys"][0]
        common["keysT"] = np.ascontiguousarray(sk.reshape(16, 128, 128).transpose(2, 0, 1))
    if inp.get("peer_u") is not None:
        common["puT"] = np.ascontiguousarray(inp["peer_u"][0].reshape(128, 128, 8, 128).transpose(0, 3, 2, 1)).reshape(128, 128, 1024)
        common["pv"] = np.ascontiguousarray(inp["peer_v"][0].reshape(128, 128, 1024))
        common["iota_i"] = np.ascontiguousarray(np.broadcast_to(np.arange(128, dtype=f)[None, :], (128, 128)))
        common["blkmask"] = (np.arange(128)[:, None] // 16 == np.arange(8)[None, :]).astype(f)
    maps = []
    for r in range(NCORES):
        m = dict(common)
        m["xown"] = x[r * TOK:(r + 1) * TOK]
        m["xhalo"] = x[r * TOK - 128:r * TOK] if r > 0 else np.zeros((128, 1024), f)
        m["mask_halo"] = common["mask_prev"] if r > 0 else np.zeros((128, 128), f)
        pre = np.zeros((NPRE * 128, 1024), f)
        n = r * TOK
        pre[NPRE * 128 - n - 16:NPRE * 128 - n] = meta
        if n:
            pre[NPRE * 128 - n:] = x[:n]
        m["xpre"] = pre
        m["is0"] = np.full((128, 1), 1.0 if r == 0 else 0.0, f)
        maps.append(m)
    return maps


def kernel(**inputs):
    nc = build("full")
    maps = _host_inputs({k: np.asarray(v) for k, v in inputs.items()})
    res = run_bass_kernel_spmd(nc, maps, core_ids=list(range(NCORES)))
    return np.concatenate([r["out"] for r in res.results], 0)[None].astype(np.float32)
```
